# Optimizing a Trainium2 kernel written in Bass

```python
import math
import jax
import jax.numpy as jnp
from jax import lax
import numpy as np

D_MODEL = 1024
BATCH = 4
SEQ = 8192
DEPTH = 2

N_MIXERS = 2
M_EXPAND = 2
M_D_INNER = M_EXPAND * D_MODEL
M_HEAD_DIM = 64
M_HEADS = M_D_INNER // M_HEAD_DIM
M_GROUPS = 8
M_HPG = M_HEADS // M_GROUPS
M_STATE = 128
M_CONV = 4
M_CHUNK = 128
M_CONV_DIM = M_D_INNER + 2 * M_GROUPS * M_STATE
M_IN_DIM = M_D_INNER + M_CONV_DIM + M_HEADS
M_NORM_GROUP = M_D_INNER // M_GROUPS
A_HEADS = D_MODEL // 128
A_HEAD_DIM = D_MODEL // A_HEADS // 2
A_V_DIM = 2 * A_HEAD_DIM
A_QK_DIM = A_HEADS * 2 * A_HEAD_DIM
A_QBLOCK = 128
FFN_DIM = ((8 * D_MODEL // 3 + 255) // 256) * 256
N_EXPERTS = 8
TOP_K = 2
EXPERT_DIM = 7 * D_MODEL // 2
MOE_BLOCK = 256
EPS = 1e-6

kernel_name = "hybrid_ssd_diffattn_moe_adaln"


def rms_norm(x):
    xf = x.astype(jnp.float32)
    y = xf * lax.rsqrt(jnp.mean(xf * xf, axis=-1, keepdims=True) + EPS)
    return y.astype(x.dtype)


def causal_segsum(a_cs):
    t = a_cs.shape[-1]
    mask = jnp.tril(jnp.ones((t, t), dtype=bool))
    diff = a_cs[..., :, None] - a_cs[..., None, :]
    return jnp.where(mask, diff, -jnp.inf)


def ssd_chunked(xdt, dta, bm, cm):
    b, l, g, r, p = xdt.shape
    n = bm.shape[-1]
    nc = l // M_CHUNK
    xc = xdt.reshape(b, nc, M_CHUNK, g, r, p)
    bc = bm.reshape(b, nc, M_CHUNK, g, n)
    cc = cm.reshape(b, nc, M_CHUNK, g, n)
    ac = dta.reshape(b, nc, M_CHUNK, g, r).transpose(0, 1, 3, 4, 2)
    acs = jnp.cumsum(ac, axis=-1)
    decay_in = jnp.exp(causal_segsum(acs))
    cb = jnp.einsum('bcqgn,bcsgn->bcgqs', cc, bc)
    y_diag = jnp.einsum('bcgrqs,bcsgrp->bcqgrp', cb[:, :, :, None] * decay_in, xc)
    decay_states = jnp.exp(acs[..., -1:] - acs)
    states = jnp.einsum('bcqgn,bcgrq,bcqgrp->bcgrpn', bc, decay_states, xc)
    chunk_a = jnp.pad(acs[..., -1].transpose(0, 2, 3, 1), ((0, 0), (0, 0), (0, 0), (1, 0)))
    decay_chunk = jnp.exp(causal_segsum(jnp.cumsum(chunk_a, axis=-1)))
    states = jnp.concatenate([jnp.zeros_like(states[:, :1]), states], axis=1)
    new_states = jnp.einsum('bgrzc,bcgrpn->bzgrpn', decay_chunk, states)
    prev_states = new_states[:, :-1]
    y_off = jnp.einsum('bcqgn,bcgrpn,bcgrq->bcqgrp', cc, prev_states, jnp.exp(acs))
    return (y_diag + y_off).reshape(b, l, g, r, p).astype(xdt.dtype)


def mamba2_mixer(h, w_in, conv_w, conv_b, dt_bias, a_log, d_skip, norm_g, w_out):
    b, s, _ = h.shape
    zxbcdt = h @ w_in
    z, xbc, dt_raw = jnp.split(zxbcdt, [M_D_INNER, M_D_INNER + M_CONV_DIM], axis=-1)
    xbc = lax.conv_general_dilated(
        xbc, conv_w[:, None, :], window_strides=(1,), padding=[(M_CONV - 1, 0)],
        dimension_numbers=('NWC', 'WIO', 'NWC'), feature_group_count=M_CONV_DIM) + conv_b
    xbc = jax.nn.silu(xbc)
    xs, bm, cm = jnp.split(xbc, [M_D_INNER, M_D_INNER + M_GROUPS * M_STATE], axis=-1)
    xs = xs.reshape(b, s, M_GROUPS, M_HPG, M_HEAD_DIM)
    bm = bm.reshape(b, s, M_GROUPS, M_STATE)
    cm = cm.reshape(b, s, M_GROUPS, M_STATE)
    dt = jax.nn.softplus((dt_raw + dt_bias).astype(jnp.float32)).reshape(b, s, M_GROUPS, M_HPG)
    a = -jnp.exp(a_log.astype(jnp.float32)).reshape(M_GROUPS, M_HPG)
    y = ssd_chunked(xs * dt[..., None].astype(xs.dtype), dt * a, bm, cm)
    y = y + xs * d_skip.reshape(M_GROUPS, M_HPG, 1)
    y = y.reshape(b, s, M_D_INNER) * jax.nn.silu(z)
    y = rms_norm(y.reshape(b, s, M_GROUPS, M_NORM_GROUP)).reshape(b, s, M_D_INNER) * norm_g
    return y @ w_out


def diff_attention(h, w_qkv, lam_q1, lam_k1, lam_q2, lam_k2, subln_g, w_o, lambda_init):
    b, s, _ = h.shape
    qkv = h @ w_qkv
    q, k, v = jnp.split(qkv, [A_QK_DIM, 2 * A_QK_DIM], axis=-1)
    q = q.reshape(b, s, A_HEADS, 2, A_HEAD_DIM)
    k = k.reshape(b, s, A_HEADS, 2, A_HEAD_DIM)
    v = v.reshape(b, s, A_HEADS, A_V_DIM)
    lam = (jnp.exp(jnp.sum(lam_q1.astype(jnp.float32) * lam_k1.astype(jnp.float32)))
           - jnp.exp(jnp.sum(lam_q2.astype(jnp.float32) * lam_k2.astype(jnp.float32)))
           + lambda_init)
    scale = A_HEAD_DIM ** -0.5
    nb = s // A_QBLOCK
    qb = q.reshape(b, nb, A_QBLOCK, A_HEADS, 2, A_HEAD_DIM).transpose(1, 0, 2, 3, 4, 5)
    k_pos = jnp.arange(s, dtype=jnp.int32)

    def query_block(args):
        qi, bi = args
        sc = jnp.einsum('bqhjd,bkhjd->bhjqk', qi, k).astype(jnp.float32) * scale
        q_pos = bi * A_QBLOCK + jnp.arange(A_QBLOCK, dtype=jnp.int32)
        sc = jnp.where(k_pos[None, :] <= q_pos[:, None], sc, -jnp.inf)
        pr = jax.nn.softmax(sc, axis=-1)
        att = pr[:, :, 0] - lam * pr[:, :, 1]
        return jnp.einsum('bhqk,bkhe->bqhe', att.astype(v.dtype), v)

    o = lax.map(query_block, (qb, jnp.arange(nb, dtype=jnp.int32)))
    o = o.transpose(1, 0, 2, 3, 4).reshape(b, s, A_HEADS, A_V_DIM)
    o = rms_norm(o) * subln_g * (1.0 - lambda_init)
    return o.reshape(b, s, A_HEADS * A_V_DIM) @ w_o


def swiglu(h, w_gate, w_up, w_down):
    return (jax.nn.silu(h @ w_gate) * (h @ w_up)) @ w_down


def moe_swiglu(h, w_router, w_gate, w_up, w_down):
    b, s, d = h.shape
    t = b * s
    xt = h.reshape(t, d)
    logits = (xt @ w_router).astype(jnp.float32)
    top_val, top_idx = lax.top_k(logits, TOP_K)
    gates = jax.nn.softmax(top_val, axis=-1)
    e_flat = top_idx.reshape(-1).astype(jnp.int32)
    t_flat = jnp.repeat(jnp.arange(t, dtype=jnp.int32), TOP_K)
    g_flat = gates.reshape(-1)
    order = jnp.argsort(e_flat)
    e_s, t_s, g_s = e_flat[order], t_flat[order], g_flat[order]
    counts = jnp.bincount(e_flat, length=N_EXPERTS).astype(jnp.int32)
    starts = jnp.cumsum(counts) - counts
    padded = (counts + MOE_BLOCK - 1) // MOE_BLOCK * MOE_BLOCK
    pends = jnp.cumsum(padded)
    pstarts = pends - padded
    n_assign = TOP_K * t
    dest = pstarts[e_s] + (jnp.arange(n_assign, dtype=jnp.int32) - starts[e_s])
    n_rows = n_assign + N_EXPERTS * MOE_BLOCK
    n_blocks = n_rows // MOE_BLOCK
    row_tok = jnp.full((n_rows,), t, dtype=jnp.int32).at[dest].set(t_s)
    row_gate = jnp.zeros((n_rows,), jnp.float32).at[dest].set(g_s)
    block_start = jnp.arange(n_blocks, dtype=jnp.int32) * MOE_BLOCK
    block_exp = jnp.minimum(jnp.searchsorted(pends, block_start, side='right'), N_EXPERTS - 1)
    x_pad = jnp.concatenate([xt, jnp.zeros((1, d), xt.dtype)], axis=0)

    def expert_block(args):
        tok, e = args
        xb = x_pad[tok]
        return (jax.nn.silu(xb @ w_gate[e]) * (xb @ w_up[e])) @ w_down[e]

    y = lax.map(expert_block, (row_tok.reshape(n_blocks, MOE_BLOCK), block_exp))
    y = y.reshape(n_rows, d) * row_gate[:, None].astype(y.dtype)
    out = jnp.zeros((t + 1, d), y.dtype).at[row_tok].add(y)[:t]
    return out.reshape(b, s, d)


def adaln_params(c, ada_w, ada_b):
    mod = jax.nn.silu(c) @ ada_w + ada_b
    return [m[:, None, :] for m in jnp.split(mod, 6, axis=-1)]


def _w(key, shape, fan_in):
    return jax.random.normal(key, shape, jnp.float32) * (fan_in ** -0.5)


def setup_inputs(seed: int = 0) -> dict:
    key = jax.random.key(seed)
    ks = jax.random.split(key, 32)
    f32 = jnp.float32
    u = jax.random.uniform(ks[8], (M_HEADS,), f32)
    dt0 = jnp.exp(u * (math.log(0.1) - math.log(0.001)) + math.log(0.001))
    return {
        'x': jax.random.normal(ks[0], (BATCH, SEQ, D_MODEL), f32),
        'c': jax.random.normal(ks[1], (BATCH, D_MODEL), f32),
        'ada_w0': _w(ks[2], (D_MODEL, 6 * D_MODEL), D_MODEL),
        'ada_b0': 0.02 * jax.random.normal(ks[3], (6 * D_MODEL,), f32),
        'm_w_in': _w(ks[4], (D_MODEL, M_IN_DIM), D_MODEL),
        'm_conv_w': _w(ks[5], (M_CONV, M_CONV_DIM), M_CONV),
        'm_conv_b': 0.02 * jax.random.normal(ks[6], (M_CONV_DIM,), f32),
        'm_dt_bias': dt0 + jnp.log(-jnp.expm1(-dt0)),
        'm_a_log': jnp.log(jax.random.uniform(ks[9], (M_HEADS,), f32, 1.0, 16.0)),
        'm_d_skip': 1.0 + 0.1 * jax.random.normal(ks[10], (M_HEADS,), f32),
        'm_norm_g': 1.0 + 0.1 * jax.random.normal(ks[11], (M_D_INNER,), f32),
        'm_w_out': _w(ks[12], (M_D_INNER, D_MODEL), M_D_INNER),
        'ffn_w_gate': _w(ks[13], (D_MODEL, FFN_DIM), D_MODEL),
        'ffn_w_up': _w(ks[14], (D_MODEL, FFN_DIM), D_MODEL),
        'ffn_w_down': _w(ks[15], (FFN_DIM, D_MODEL), FFN_DIM),
        'ada_w1': _w(ks[16], (D_MODEL, 6 * D_MODEL), D_MODEL),
        'ada_b1': 0.02 * jax.random.normal(ks[17], (6 * D_MODEL,), f32),
        'a_w_qkv': _w(ks[18], (D_MODEL, 2 * A_QK_DIM + A_HEADS * A_V_DIM), D_MODEL),
        'a_lam_q1': 0.1 * jax.random.normal(ks[19], (A_HEAD_DIM,), f32),
        'a_lam_k1': 0.1 * jax.random.normal(ks[20], (A_HEAD_DIM,), f32),
        'a_lam_q2': 0.1 * jax.random.normal(ks[21], (A_HEAD_DIM,), f32),
        'a_lam_k2': 0.1 * jax.random.normal(ks[22], (A_HEAD_DIM,), f32),
        'a_subln_g': 1.0 + 0.1 * jax.random.normal(ks[23], (A_V_DIM,), f32),
        'a_w_o': _w(ks[24], (A_HEADS * A_V_DIM, D_MODEL), A_HEADS * A_V_DIM),
        'moe_w_router': _w(ks[25], (D_MODEL, N_EXPERTS), D_MODEL),
        'moe_w_gate': _w(ks[26], (N_EXPERTS, D_MODEL, EXPERT_DIM), D_MODEL),
        'moe_w_up': _w(ks[27], (N_EXPERTS, D_MODEL, EXPERT_DIM), D_MODEL),
        'moe_w_down': _w(ks[28], (N_EXPERTS, EXPERT_DIM, D_MODEL), EXPERT_DIM),
        'final_g': 1.0 + 0.1 * jax.random.normal(ks[29], (D_MODEL,), f32),
    }


def reference(x, c, ada_w0, ada_b0, m_w_in, m_conv_w, m_conv_b, m_dt_bias, m_a_log, m_d_skip,
              m_norm_g, m_w_out, ffn_w_gate, ffn_w_up, ffn_w_down, ada_w1, ada_b1, a_w_qkv,
              a_lam_q1, a_lam_k1, a_lam_q2, a_lam_k2, a_subln_g, a_w_o, moe_w_router,
              moe_w_gate, moe_w_up, moe_w_down, final_g):
    ada_w = (ada_w0, ada_w1)
    ada_b = (ada_b0, ada_b1)
    for i in range(DEPTH):
        sh1, sc1, g1, sh2, sc2, g2 = adaln_params(c, ada_w[i], ada_b[i])
        h = rms_norm(x) * (1.0 + sc1) + sh1
        if i % N_MIXERS == 0:
            mix = mamba2_mixer(h, m_w_in, m_conv_w, m_conv_b, m_dt_bias, m_a_log, m_d_skip,
                               m_norm_g, m_w_out)
        else:
            lambda_init = 0.8 - 0.6 * math.exp(-0.3 * i)
            mix = diff_attention(h, a_w_qkv, a_lam_q1, a_lam_k1, a_lam_q2, a_lam_k2, a_subln_g,
                                 a_w_o, lambda_init)
        x = x + g1 * mix
        h = rms_norm(x) * (1.0 + sc2) + sh2
        if i % 2 == 0:
            ffn = swiglu(h, ffn_w_gate, ffn_w_up, ffn_w_down)
        else:
            ffn = moe_swiglu(h, moe_w_router, moe_w_gate, moe_w_up, moe_w_down)
        x = x + g2 * ffn
    return rms_norm(x) * final_g
```

```python
from contextlib import ExitStack
import numpy as np
import ml_dtypes
import concourse.bass as bass
import concourse.mybir as mybir
from concourse.bass_utils import run_bass_kernel_spmd

F32 = mybir.dt.float32
BF16 = mybir.dt.bfloat16
AF = mybir.ActivationFunctionType
ALU = mybir.AluOpType
AX = mybir.AxisListType

D = 1024
KC = 8
EPS = 1e-6
FFN_DIM = 2816
N_EXP = 8
EXP_DIM = 3584

ENGS = ["pe", "act", "dve", "pool", "sp"]


def _split(key):
    if isinstance(key, tuple):
        return key[0], key[1]
    return key, None


class Prog:
    def __init__(self, nc, stack):
        self.nc = nc
        self.stack = stack
        self.ops = {e: [] for e in ENGS}
        self.res = {}
        self.dma_cnt = []
        self.key2phys = {}
        self.free_phys = {True: [], False: []}
        self.phys_sw = []
        self.base = {}

    @staticmethod
    def _merge(d, ev):
        k = (ev[0], ev[1])
        if d.get(k, -1) < ev[2]:
            d[k] = ev[2]

    def _collect(self, deps, reads, writes):
        for key in reads:
            name, idx = _split(key)
            ent = self.res.get(name)
            if ent is None:
                continue
            for (i, ev) in ent["w"]:
                if i is None or idx is None or i == idx:
                    self._merge(deps, ev)
        for key in writes:
            name, idx = _split(key)
            ent = self.res.get(name)
            if ent is None:
                continue
            for (i, ev) in ent["w"] + ent["r"]:
                if i is None or idx is None or i == idx:
                    self._merge(deps, ev)

    def _update(self, ev, reads, writes):
        for key in reads:
            name, idx = _split(key)
            ent = self.res.setdefault(name, {"w": [], "r": []})
            ent["r"] = [(i, e) for (i, e) in ent["r"]
                        if not (i == idx and e[0] == ev[0] and e[1] == ev[1])]
            ent["r"].append((idx, ev))
        for key in writes:
            name, idx = _split(key)
            ent = self.res.setdefault(name, {"w": [], "r": []})
            if idx is None:
                ent["w"] = [(None, ev)]
                ent["r"] = []
            else:
                ent["w"] = [(i, e) for (i, e) in ent["w"] if i != idx]
                ent["w"].append((idx, ev))
                ent["r"] = [(i, e) for (i, e) in ent["r"] if i != idx]

    def op(self, eng, fn, r=(), w=(), dma=None):
        deps = dict(self.base)
        self._collect(deps, r, w)
        seq = len(self.ops[eng])
        if dma is not None:
            ph = self.key2phys.get(dma)
            if ph is None:
                sw = (eng == "pool")
                if self.free_phys[sw]:
                    ph = self.free_phys[sw].pop()
                else:
                    ph = len(self.dma_cnt)
                    self.dma_cnt.append(0)
                    self.phys_sw.append(sw)
                self.key2phys[dma] = ph
            self.dma_cnt[ph] += 1
            ev = ("d", ph, self.dma_cnt[ph])
        else:
            ev = ("c", eng, seq)
        o = {"fn": fn, "deps": deps, "ev": ev, "signal": False}
        self.ops[eng].append(o)
        self._update(ev, r, w)
        return o

    def barrier(self):
        fr = {}
        for e in ENGS:
            for s in range(len(self.ops[e]) - 1, -1, -1):
                if self.ops[e][s]["ev"][0] == "c":
                    fr[("c", e)] = s
                    break
        for k, c in enumerate(self.dma_cnt):
            fr[("d", k)] = c
        self.base = fr
        self.res = {}
        self.key2phys = {}
        self.free_phys = {True: [i for i in range(len(self.dma_cnt)) if self.phys_sw[i]],
                          False: [i for i in range(len(self.dma_cnt)) if not self.phys_sw[i]]}

    def emit(self, final_wait_keys=()):
        nc = self.nc
        for e in ENGS:
            for o in self.ops[e]:
                for (kind, src), val in o["deps"].items():
                    if kind == "c" and not (src == "pe" and e == "pe"):
                        self.ops[src][val]["signal"] = True
        for e in ENGS:
            cnt = 0
            for o in self.ops[e]:
                if o["signal"]:
                    cnt += 1
                o["sigval"] = cnt
        sems = {}
        for e in ENGS:
            sems[("c", e)] = self.stack.enter_context(nc.semaphore("s_" + e))
        for k in range(len(self.dma_cnt)):
            sems[("d", k)] = self.stack.enter_context(nc.semaphore("d_" + str(k)))
        block = self.stack.enter_context(nc.Block())
        prog = self

        def run(eng_name, handle):
            waited = {}
            for o in prog.ops[eng_name]:
                for (kind, src), val in o["deps"].items():
                    if kind == "c":
                        if src == "pe" and eng_name == "pe":
                            continue
                        target = prog.ops[src][val]["sigval"]
                    else:
                        target = 16 * val
                    if waited.get((kind, src), 0) >= target:
                        continue
                    waited[(kind, src)] = target
                    handle.wait_ge(sems[(kind, src)], target)
                ins = o["fn"](handle)
                if o["ev"][0] == "d":
                    ins.then_inc(sems[("d", o["ev"][1])], 16)
                elif o["signal"]:
                    ins.then_inc(sems[("c", eng_name)], 1)
            if eng_name == "sp":
                for k in final_wait_keys:
                    handle.wait_ge(sems[("d", k)], 16 * prog.dma_cnt[k])

        @block.tensor
        def _(h):
            run("pe", h)

        @block.scalar
        def _(h):
            run("act", h)

        @block.vector
        def _(h):
            run("dve", h)

        @block.gpsimd
        def _(h):
            run("pool", h)

        @block.sync
        def _(h):
            run("sp", h)


class Ctx:
    pass


def sb(nc, st, name, shape, dt):
    return st.enter_context(nc.sbuf_tensor("sb_" + name, list(shape), dt))


def stage_consts(C):
    nc, P = C.nc, C.P
    st = C.top
    C.ident_bf = sb(nc, st, "ident_bf", [128, 128], BF16)
    C.ident_f = sb(nc, st, "ident_f", [128, 128], F32)
    C.ones_bf = sb(nc, st, "ones_bf", [128, 128], BF16)
    C.ones_f = sb(nc, st, "ones_f", [128, 128], F32)
    P.op("pool", lambda e: e.dma_start(out=C.ident_bf[:], in_=C.d["ident"][:, :]), w=["ident_bf"], dma="c0")
    P.op("sp", lambda e: e.dma_start(out=C.ident_f[:], in_=C.d["ident"][:, :]), w=["ident_f"], dma="c1")
    P.op("dve", lambda e: e.memset(C.ones_bf[:], 1.0), w=["ones_bf"])
    P.op("dve", lambda e: e.memset(C.ones_f[:], 1.0), w=["ones_f"])
    C.mod = [sb(nc, st, "mod%d" % l, [128, 48], F32) for l in range(2)]
    ada_w_aps = [C.d["ada_w0"], C.d["ada_w1"]]
    ada_b_aps = [C.d["ada_bT0"], C.d["ada_bT1"]]
    with ExitStack() as ls:
        cT = sb(nc, ls, "cT", [128, 8], F32)
        s2 = sb(nc, ls, "s2", [128, 8, 2], F32)
        wb = [sb(nc, ls, "adaw%d" % i, [128, 8, 512], F32) for i in range(2)]
        bT = sb(nc, ls, "adab", [128, 48], F32)
        ps = ls.enter_context(nc.psum_tensor("ps_mod", [128, 48, 2], F32))
        P.op("sp", lambda e: e.dma_start(out=cT[:], in_=C.d["cT"][:, :]), w=["cT"], dma="c2")
        P.op("act", lambda e: e.activation(out=s2[:, :, 0], in_=cT[:], func=AF.Silu), r=["cT"], w=["s2"])
        P.op("act", lambda e: e.activation(out=s2[:, :, 1], in_=cT[:], func=AF.Silu), r=["cT"], w=["s2"])
        for l in range(2):
            mod = C.mod[l]
            aw = ada_w_aps[l].rearrange("(k p) f -> p k f", p=128)
            P.op("sp", lambda e, l=l: e.dma_start(out=bT[:], in_=ada_b_aps[l][:, :]), w=["adab"], dma="c3")
            for cb in range(12):
                buf = wb[cb % 2]
                bk = "adaw%d" % (cb % 2)
                P.op("sp", lambda e, buf=buf, aw=aw, cb=cb: e.dma_start(out=buf[:], in_=aw[:, :, cb * 512:(cb + 1) * 512]),
                     w=[bk], dma="aw%d" % (cb % 2))
                for fi in range(4):
                    f = cb * 4 + fi
                    for k in range(8):
                        P.op("pe", lambda e, buf=buf, f=f, fi=fi, k=k: e.matmul(
                            ps[:, f, :], lhsT=buf[:, k, fi * 128:(fi + 1) * 128], rhs=s2[:, k, :],
                            start=(k == 0), stop=(k == 7)), r=[bk, "s2"], w=["ps_mod"])
            P.op("dve", lambda e, mod=mod: e.tensor_tensor(out=mod[:], in0=ps[:, :, 0], in1=bT[:], op=ALU.add),
                 r=["ps_mod", "adab"], w=["mod%d" % l])
            for j in (1, 4):
                P.op("dve", lambda e, mod=mod, j=j: e.tensor_scalar(
                    out=mod[:, j * 8:(j + 1) * 8], in0=mod[:, j * 8:(j + 1) * 8], scalar1=1.0, scalar2=None,
                    op0=ALU.add), r=["mod%d" % l], w=["mod%d" % l])
        P.barrier()


def norm_mod(C, xsrc, hT, rs_ps, sq, tmp, sqv, mod, joff, nm, x_key, h_key, h32=None):
    nc, P = C.nc, C.P
    sh0, sc0 = joff * 8, (joff + 1) * 8
    P.op("act", lambda e: e.activation(out=sq[:], in_=xsrc, func=AF.Square), r=[x_key], w=[nm + "sq"])
    for k in range(8):
        P.op("pe", lambda e, k=k: e.matmul(rs_ps[:], lhsT=C.ones_bf[:], rhs=sq[:, k, :], start=(k == 0), stop=(k == 7)),
             r=[nm + "sq", "ones_bf"], w=[nm + "rs_ps"])
    P.op("act", lambda e: e.activation(out=sqv[:], in_=rs_ps[:], func=AF.Sqrt, bias=C.eps_t[:, 0:1], scale=1.0 / D),
         r=[nm + "rs_ps"], w=[nm + "sqv"])
    P.op("dve", lambda e: e.reciprocal(out=sqv[:], in_=sqv[:]), r=[nm + "sqv"], w=[nm + "sqv"])
    for k in range(8):
        t = tmp[k % 2]
        tk = nm + "tmp%d" % (k % 2)
        P.op("dve", lambda e, k=k, t=t: e.scalar_tensor_tensor(
            out=t[:], in0=xsrc[:, k, :], scalar=mod[:, sc0 + k:sc0 + k + 1], in1=sqv[:], op0=ALU.mult, op1=ALU.mult),
            r=[x_key, nm + "sqv"], w=[tk])
        P.op("act", lambda e, k=k, t=t: e.activation(out=hT[:, k, :], in_=t[:], func=AF.Identity,
                                                    bias=mod[:, sh0 + k:sh0 + k + 1], scale=1.0),
             r=[tk], w=[(h_key, k)])
        if h32 is not None:
            P.op("pool", lambda e, k=k, t=t: e.tensor_scalar(
                out=h32[:, k, :], in0=t[:], scalar1=mod[:, sh0 + k:sh0 + k + 1], scalar2=None, op0=ALU.add),
                r=[tk], w=[("h32", k)])


def stage_ffn(C, x_in, x_out, T, mod, wg, wu, wd, HID, nm, n_exp=1, wr=None):
    nc, P = C.nc, C.P
    HG = 4 if (HID // 128) % 4 == 0 else 2
    NT = 4 if T >= 2048 else T // 512
    n_super = T // (NT * 512)
    units_per_exp = HID // (HG * 128)
    assert HID % (HG * 128) == 0
    xin_v = x_in.rearrange("(k p) t -> p k t", p=128)
    xout_v = x_out.rearrange("(k p) t -> p k t", p=128)
    g0 = 5 * 8
    with ExitStack() as ls:
        acc = sb(nc, ls, nm + "acc", [128, NT, 8, 512], F32)
        hT = sb(nc, ls, nm + "hT", [128, NT, 8, 512], BF16)
        sq = sb(nc, ls, nm + "sq", [128, 8, 512], BF16)
        tmp = [sb(nc, ls, nm + "tmp%d" % i, [128, 512], F32) for i in range(2)]
        sqv = sb(nc, ls, nm + "sqv", [128, 512], F32)
        wgb = [sb(nc, ls, nm + "wg%d" % i, [128, 8, HG * 128], BF16) for i in range(2)]
        wub = [sb(nc, ls, nm + "wu%d" % i, [128, 8, HG * 128], BF16) for i in range(2)]
        wdb = [sb(nc, ls, nm + "wd%d" % i, [128, HG, 1024], BF16) for i in range(2)]
        aT = [sb(nc, ls, nm + "aT%d" % i, [128, HG, 512], BF16) for i in range(2)]
        sg = [sb(nc, ls, nm + "sg%d" % i, [128, 512], F32) for i in range(2)]
        ps_g = [ls.enter_context(nc.psum_tensor(nm + "psg%d" % i, [128, 512], F32)) for i in range(2)]
        ps_u = [ls.enter_context(nc.psum_tensor(nm + "psu%d" % i, [128, 512], F32)) for i in range(2)]
        ps_d = [ls.enter_context(nc.psum_tensor(nm + "psd%d" % i, [128, 512], F32)) for i in range(2)]
        ps_x = ls.enter_context(nc.psum_tensor(nm + "psx", [128, 512], F32))
        if n_exp > 1:
            h32 = sb(nc, ls, nm + "h32", [128, 8, 512], F32)
            wrt = sb(nc, ls, nm + "wr", [128, 8, 8], F32)
            GT = sb(nc, ls, nm + "GT", [8, NT * 512], F32)
            lg = sb(nc, ls, nm + "lg", [128, 8], F32)
            mx = sb(nc, ls, nm + "mx", [128, 8], F32)
            gw = sb(nc, ls, nm + "gw", [128, 4], F32)
            Gm = sb(nc, ls, nm + "Gm", [128, 8], F32)
            Gm2 = sb(nc, ls, nm + "Gm2", [128, 8], F32)
            sel = sb(nc, ls, nm + "sel", [8, 8, 128], F32)
            ps_gb = ls.enter_context(nc.psum_tensor(nm + "psgb", [128, 512], F32))
            P.op("sp", lambda e: e.dma_start(out=wrt[:], in_=wr.rearrange("(k p) e -> p k e", p=128)), w=["wr"], dma=nm + "wr")
            P.op("sp", lambda e: e.dma_start(out=sel[:], in_=C.d["sel"][:, :, :]), w=["sel"], dma=nm + "sel")
        pend_down = [None]
        for s_i in range(n_super):
            for ti in range(NT):
                t0 = (s_i * NT + ti) * 512
                P.op("sp", lambda e, ti=ti, t0=t0: e.dma_start(out=acc[:, ti], in_=xin_v[:, :, t0:t0 + 512]),
                     w=[("acc", ti)], dma=nm + "x%d" % ti)
                norm_mod(C, acc[:, ti], hT[:, ti], ps_x, sq, tmp, sqv, mod, 3, nm, ("acc", ti), "hT%d" % ti,
                         h32=(h32 if n_exp > 1 else None))
                if n_exp > 1:
                    for c4 in range(4):
                        for k in range(8):
                            P.op("pe", lambda e, c4=c4, k=k: e.matmul(
                                ps_gb[:, 0:8], lhsT=h32[:, k, c4 * 128:(c4 + 1) * 128], rhs=wrt[:, k, :],
                                start=(k == 0), stop=(k == 7)), r=[("h32", k), "wr"], w=["psgb"])
                        P.op("dve", lambda e: e.tensor_copy(out=lg[:], in_=ps_gb[:, 0:8]), r=["psgb"], w=["lg"])
                        P.op("dve", lambda e: e.max(out=mx[:], in_=lg[:]), r=["lg"], w=["mx"])
                        P.op("dve", lambda e: e.tensor_tensor(out=gw[:, 0:1], in0=mx[:, 0:1], in1=mx[:, 1:2], op=ALU.subtract),
                             r=["mx"], w=["gw"])
                        P.op("act", lambda e: e.activation(out=gw[:, 1:2], in_=gw[:, 0:1], func=AF.Sigmoid), r=["gw"], w=["gw"])
                        P.op("dve", lambda e: e.tensor_scalar(out=gw[:, 2:3], in0=gw[:, 1:2], scalar1=-1.0, scalar2=1.0,
                                                               op0=ALU.mult, op1=ALU.add), r=["gw"], w=["gw"])
                        P.op("dve", lambda e: e.tensor_scalar(out=Gm[:], in0=lg[:], scalar1=mx[:, 0:1], scalar2=gw[:, 1:2],
                                                               op0=ALU.is_equal, op1=ALU.mult), r=["lg", "mx", "gw"], w=["Gm"])
                        P.op("dve", lambda e: e.tensor_scalar(out=Gm2[:], in0=lg[:], scalar1=mx[:, 1:2], scalar2=gw[:, 2:3],
                                                               op0=ALU.is_equal, op1=ALU.mult), r=["lg", "mx", "gw"], w=["Gm2"])
                        P.op("dve", lambda e: e.tensor_tensor(out=Gm[:], in0=Gm[:], in1=Gm2[:], op=ALU.add),
                             r=["Gm", "Gm2"], w=["Gm"])
                        P.op("pe", lambda e: e.transpose(out=ps_gb[0:8, 128:256], in_=Gm[:], identity=C.ident_f[:]),
                             r=["Gm", "ident_f"], w=["psgb"])
                        P.op("dve", lambda e, ti=ti, c4=c4: e.tensor_copy(
                            out=GT[:, ti * 512 + c4 * 128: ti * 512 + (c4 + 1) * 128], in_=ps_gb[0:8, 128:256]),
                            r=["psgb"], w=[("GT", ti)])
            n_units = n_exp * units_per_exp
            for u in range(n_units):
                ex, uu = divmod(u, units_per_exp)
                b = u % 2
                h0 = uu * HG * 128
                wgs = (wg[ex] if n_exp > 1 else wg).rearrange("(k p) h -> p k h", p=128)
                wus = (wu[ex] if n_exp > 1 else wu).rearrange("(k p) h -> p k h", p=128)
                wds = (wd[ex] if n_exp > 1 else wd).rearrange("(j p) f -> p j f", p=128)
                P.op("pool", lambda e, b=b, wgs=wgs, h0=h0: e.dma_start(out=wgb[b][:], in_=wgs[:, :, h0:h0 + HG * 128]),
                     w=["wg%d" % b], dma=nm + "wg%d" % b)
                P.op("pool", lambda e, b=b, wus=wus, h0=h0: e.dma_start(out=wub[b][:], in_=wus[:, :, h0:h0 + HG * 128]),
                     w=["wu%d" % b], dma=nm + "wu%d" % b)
                P.op("pool", lambda e, b=b, wds=wds, uu=uu: e.dma_start(out=wdb[b][:], in_=wds[:, uu * HG:(uu + 1) * HG, :]),
                     w=["wd%d" % b], dma=nm + "wd%d" % b)
                for ti in range(NT):
                    ab = (u * NT + ti) % 2
                    if n_exp > 1:
                        for pp in range(1):
                            P.op("pe", lambda e, ex=ex, ti=ti: e.matmul(
                                ps_gb[:], lhsT=sel[:, ex, :], rhs=GT[:, ti * 512:(ti + 1) * 512], start=True, stop=True),
                                r=["sel", ("GT", ti)], w=["psgb"])
                    for j in range(HG):
                        pb = j % 2
                        for k in range(8):
                            P.op("pe", lambda e, b=b, pb=pb, j=j, k=k, ti=ti: e.matmul(
                                ps_g[pb][:], lhsT=wgb[b][:, k, j * 128:(j + 1) * 128], rhs=hT[:, ti, k, :],
                                start=(k == 0), stop=(k == 7)), r=["wg%d" % b, "hT%d" % ti], w=["psg%d" % pb])
                        for k in range(8):
                            P.op("pe", lambda e, b=b, pb=pb, j=j, k=k, ti=ti: e.matmul(
                                ps_u[pb][:], lhsT=wub[b][:, k, j * 128:(j + 1) * 128], rhs=hT[:, ti, k, :],
                                start=(k == 0), stop=(k == 7)), r=["wu%d" % b, "hT%d" % ti], w=["psu%d" % pb])
                        P.op("act", lambda e, pb=pb: e.activation(out=sg[pb][:], in_=ps_g[pb][:], func=AF.Silu),
                             r=["psg%d" % pb], w=["sg%d" % pb])
                        if n_exp > 1:
                            P.op("dve", lambda e, pb=pb: e.tensor_tensor(out=sg[pb][:], in0=sg[pb][:], in1=ps_u[pb][:], op=ALU.mult),
                                 r=["sg%d" % pb, "psu%d" % pb], w=["sg%d" % pb])
                            P.op("dve", lambda e, pb=pb, ab=ab, j=j: e.tensor_tensor(
                                out=aT[ab][:, j, :], in0=sg[pb][:], in1=ps_gb[:], op=ALU.mult),
                                r=["sg%d" % pb, "psgb"], w=[("aT%d" % ab, j)])
                        else:
                            P.op("dve", lambda e, pb=pb, ab=ab, j=j: e.tensor_tensor(
                                out=aT[ab][:, j, :], in0=sg[pb][:], in1=ps_u[pb][:], op=ALU.mult),
                                r=["sg%d" % pb, "psu%d" % pb], w=[("aT%d" % ab, j)])
                    def _down(b=b, ab=ab, ti=ti):
                        for f in range(8):
                            db = f % 2
                            for j in range(HG):
                                P.op("pe", lambda e, b=b, db=db, j=j, f=f, ab=ab: e.matmul(
                                    ps_d[db][:], lhsT=wdb[b][:, j, f * 128:(f + 1) * 128], rhs=aT[ab][:, j, :],
                                    start=(j == 0), stop=(j == HG - 1)), r=["wd%d" % b, ("aT%d" % ab, j)], w=["psd%d" % db])
                            P.op("dve", lambda e, db=db, f=f, ti=ti: e.scalar_tensor_tensor(
                                out=acc[:, ti, f, :], in0=ps_d[db][:], scalar=mod[:, g0 + f:g0 + f + 1], in1=acc[:, ti, f, :],
                                op0=ALU.mult, op1=ALU.add), r=["psd%d" % db, ("acc", ti)], w=[("acc", ti)])
                    if pend_down[0] is not None:
                        pend_down[0]()
                    pend_down[0] = _down
            if pend_down[0] is not None:
                pend_down[0]()
                pend_down[0] = None
            for ti in range(NT):
                t0 = (s_i * NT + ti) * 512
                P.op("sp", lambda e, ti=ti, t0=t0: e.dma_start(out=xout_v[:, :, t0:t0 + 512], in_=acc[:, ti]),
                     r=[("acc", ti)], w=[nm + "xout"], dma=nm + "xo%d" % ti)
        P.barrier()


LAMBDA_INIT = 0.8 - 0.6 * float(np.exp(-0.3 * 1))


def stage_qkv(C, x_in, S, mod):
    nc, P, d = C.nc, C.P, C.d
    xin_v = x_in.rearrange("(k p) t -> p k t", p=128)
    qv = d["QT"].rearrange("(c p) t -> p c t", p=128)
    kv = d["KT"].rearrange("(c p) t -> p c t", p=128)
    vv = d["V"].rearrange("(n p) e -> p n e", p=128)
    wq = d["a_w_qkv"].rearrange("(k p) f -> p k f", p=128)
    with ExitStack() as ls:
        w = sb(nc, ls, "qw", [128, 8, 3072], BF16)
        xt = [sb(nc, ls, "qxt%d" % i, [128, 8, 512], F32) for i in range(2)]
        hT2 = [sb(nc, ls, "qhT%d" % i, [128, 8, 512], BF16) for i in range(2)]
        sq = sb(nc, ls, "qsq", [128, 8, 512], BF16)
        tmp = [sb(nc, ls, "qtmp%d" % i, [128, 512], F32) for i in range(2)]
        sqv = sb(nc, ls, "qsqv", [128, 512], F32)
        qt = [sb(nc, ls, "qqt%d" % i, [128, 8, 512], BF16) for i in range(2)]
        kt = [sb(nc, ls, "qkt%d" % i, [128, 8, 512], BF16) for i in range(2)]
        vt = [sb(nc, ls, "qvt%d" % i, [128, 4, 1024], BF16) for i in range(2)]
        ps = [ls.enter_context(nc.psum_tensor("qps%d" % i, [128, 512], F32)) for i in range(4)]
        ps_x = ls.enter_context(nc.psum_tensor("qpsx", [128, 512], F32))
        for i in range(3):
            P.op("pool", lambda e, i=i: e.dma_start(out=w[:, :, i * 1024:(i + 1) * 1024], in_=wq[:, :, i * 1024:(i + 1) * 1024]),
                 w=[("qw", i)], dma="qw%d" % i)
        pi = 0
        NT_ = S // 512

        def emit_norm(t):
            b = t % 2
            P.op("sp", lambda e, b=b, t=t: e.dma_start(out=xt[b][:], in_=xin_v[:, :, t * 512:(t + 1) * 512]),
                 w=["qxt%d" % b], dma="qx%d" % b)
            norm_mod(C, xt[b][:], hT2[b], ps_x, sq, tmp, sqv, mod, 0, "q", "qxt%d" % b, "qhT%d" % b)

        emit_norm(0)
        for t in range(NT_):
            b = t % 2
            hT = hT2[b]
            hk = "qhT%d" % b
            if t + 1 < NT_:
                emit_norm(t + 1)
            for (dst, dkey, coff, scale) in ((qt[b], "qqt%d" % b, 0, 0.125), (kt[b], "qkt%d" % b, 1024, 1.0)):
                for c in range(8):
                    pb = pi % 4
                    pi += 1
                    for k in range(8):
                        P.op("pe", lambda e, pb=pb, k=k, c=c, coff=coff, hT=hT: e.matmul(
                            ps[pb][:], lhsT=w[:, k, coff + c * 128: coff + (c + 1) * 128], rhs=hT[:, k, :],
                            start=(k == 0), stop=(k == 7)), r=["qw", hk], w=["qps%d" % pb])
                    P.op("act", lambda e, pb=pb, dst=dst, c=c, scale=scale: e.activation(
                        out=dst[:, c, :], in_=ps[pb][:], func=AF.Copy, scale=scale), r=["qps%d" % pb], w=[(dkey, c)])
            for tc in range(4):
                for cg in range(2):
                    pb = pi % 4
                    pi += 1
                    for k in range(8):
                        P.op("pe", lambda e, pb=pb, k=k, tc=tc, cg=cg, hT=hT: e.matmul(
                            ps[pb][:], lhsT=hT[:, k, tc * 128:(tc + 1) * 128], rhs=w[:, k, 2048 + cg * 512: 2048 + (cg + 1) * 512],
                            start=(k == 0), stop=(k == 7)), r=["qw", hk], w=["qps%d" % pb])
                    P.op("dve", lambda e, pb=pb, b=b, tc=tc, cg=cg: e.tensor_copy(
                        out=vt[b][:, tc, cg * 512:(cg + 1) * 512], in_=ps[pb][:]), r=["qps%d" % pb], w=[("qvt%d" % b, tc)])
            P.op("sp", lambda e, b=b, t=t: e.dma_start(out=qv[:, :, t * 512:(t + 1) * 512], in_=qt[b][:]),
                 r=["qqt%d" % b], w=["QT"], dma="qqo%d" % b)
            P.op("sp", lambda e, b=b, t=t: e.dma_start(out=kv[:, :, t * 512:(t + 1) * 512], in_=kt[b][:]),
                 r=["qkt%d" % b], w=["KT"], dma="qko%d" % b)
            P.op("sp", lambda e, b=b, t=t: e.dma_start(out=vv[:, t * 4:(t + 1) * 4, :], in_=vt[b][:]),
                 r=["qvt%d" % b], w=["V"], dma="qvo%d" % b)
        P.barrier()


def own_tiles(n_slots):
    A = [2 * i if i % 2 == 0 else 2 * i + 1 for i in range(n_slots)]
    B = [2 * i + 1 if i % 2 == 0 else 2 * i for i in range(n_slots)]
    return A, B


def stage_attn(C, x_in, x_out, S, mod):
    nc, P, d = C.nc, C.P, C.d
    n_slots = S // 1024
    TA, TB = own_tiles(n_slots)
    xin_v = x_in.rearrange("(k p) t -> p k t", p=128)
    xout_v = x_out.rearrange("(k p) t -> p k t", p=128)
    qv = d["QT"].rearrange("(c p) t -> p c t", p=128)
    g0 = 2 * 8
    with ExitStack() as ls:
        wo = sb(nc, ls, "awo", [128, 8, 1024], BF16)
        masks = sb(nc, ls, "amask", [128, 4, 4, 512], BF16)
        rsel = sb(nc, ls, "arsel", [128, 2], F32)
        lamv = sb(nc, ls, "alamv", [128, 4, 64], F32)
        lsc = sb(nc, ls, "alsc", [128, 8], F32)
        gsub = sb(nc, ls, "agsub", [128, 1], F32)
        qa = sb(nc, ls, "aqa", [128, 8, 512], BF16)
        qb = sb(nc, ls, "aqb", [128, 8, 512], BF16)
        q = sb(nc, ls, "aq", [128, 8, 512], BF16)
        xa = sb(nc, ls, "axa", [128, 8, 512], F32)
        xb = sb(nc, ls, "axb", [128, 8, 512], F32)
        ktb = [sb(nc, ls, "akt%d" % i, [128, 512], BF16) for i in range(2)]
        vtb = [sb(nc, ls, "avt%d" % i, [128, 4, 128], BF16) for i in range(2)]
        pT = [[sb(nc, ls, "apT%d%d" % (i, j), [128, 512], BF16) for j in range(2)] for i in range(2)]
        oT = sb(nc, ls, "aoT", [128, 8, 512], BF16)
        t32 = [sb(nc, ls, "at32%d" % i, [128, 512], F32) for i in range(4)]
        sqb = sb(nc, ls, "asqb", [128, 512], BF16)
        ps_s = [[ls.enter_context(nc.psum_tensor("aps%d%d" % (i, j), [128, 512], F32)) for j in range(2)] for i in range(2)]
        ps_o = [ls.enter_context(nc.psum_tensor("apo%d" % j, [128, 512], F32)) for j in range(2)]
        ps_l = [ls.enter_context(nc.psum_tensor("apl%d" % j, [128, 512], F32)) for j in range(2)]
        P.op("pool", lambda e: e.dma_start(out=wo[:], in_=d["a_w_o"].rearrange("(k p) f -> p k f", p=128)), w=["awo"], dma="awo")
        for i in range(4):
            P.op("pool", lambda e, i=i: e.dma_start(out=masks[:, i], in_=d["masks"][i]), w=[("amask", i)], dma="amask%d" % i)
        P.op("sp", lambda e: e.dma_start(out=rsel[:], in_=d["rolesel"][:, :]), w=["arsel"], dma="arsel")
        P.op("sp", lambda e: e.dma_start(out=lamv[:], in_=d["lamv"][:, :, :]), w=["alamv"], dma="alamv")
        P.op("sp", lambda e: e.dma_start(out=gsub[:], in_=d["sublnT"][:, :]), w=["agsub"], dma="agsub")
        for i in range(2):
            P.op("dve", lambda e, i=i: e.tensor_tensor(out=lamv[:, 2 * i, :], in0=lamv[:, 2 * i, :], in1=lamv[:, 2 * i + 1, :], op=ALU.mult),
                 r=["alamv"], w=["alamv"])
            P.op("dve", lambda e, i=i: e.tensor_reduce(out=lsc[:, i:i + 1], in_=lamv[:, 2 * i, :], axis=AX.X, op=ALU.add),
                 r=["alamv"], w=["alsc"])
            P.op("act", lambda e, i=i: e.activation(out=lsc[:, 2 + i:3 + i], in_=lsc[:, i:i + 1], func=AF.Exp), r=["alsc"], w=["alsc"])
        P.op("dve", lambda e: e.tensor_tensor(out=lsc[:, 4:5], in0=lsc[:, 3:4], in1=lsc[:, 2:3], op=ALU.subtract), r=["alsc"], w=["alsc"])
        P.op("dve", lambda e: e.tensor_scalar(out=lsc[:, 4:5], in0=lsc[:, 4:5], scalar1=-LAMBDA_INIT, scalar2=None, op0=ALU.add),
             r=["alsc"], w=["alsc"])
        P.op("dve", lambda e: e.tensor_scalar(out=gsub[:], in0=gsub[:], scalar1=1.0 - LAMBDA_INIT, scalar2=None, op0=ALU.mult),
             r=["agsub"], w=["agsub"])
        step = 0
        for si in range(n_slots):
            ta, tb = TA[si], TB[si]
            P.op("sp", lambda e, ta=ta: e.dma_start(out=qa[:], in_=qv[:, :, ta * 512:(ta + 1) * 512]), w=["aqa"], dma="aqa")
            P.op("sp", lambda e, tb=tb: e.dma_start(out=qb[:], in_=qv[:, :, tb * 512:(tb + 1) * 512]), w=["aqb"], dma="aqb")
            P.op("sp", lambda e, ta=ta: e.dma_start(out=xa[:], in_=xin_v[:, :, ta * 512:(ta + 1) * 512]), w=["axa"], dma="axa")
            P.op("sp", lambda e, tb=tb: e.dma_start(out=xb[:], in_=xin_v[:, :, tb * 512:(tb + 1) * 512]), w=["axb"], dma="axb")
            P.op("dve", lambda e: e.tensor_scalar(out=qa[:], in0=qa[:], scalar1=rsel[:, 0:1], scalar2=None, op0=ALU.mult),
                 r=["aqa", "arsel"], w=["aqa"])
            P.op("dve", lambda e: e.scalar_tensor_tensor(out=q[:], in0=qb[:], scalar=rsel[:, 1:2], in1=qa[:], op0=ALU.mult, op1=ALU.add),
                 r=["aqa", "aqb", "arsel"], w=["aq"])
            P.op("pool", lambda e: e.tensor_scalar(out=xa[:], in0=xa[:], scalar1=rsel[:, 0:1], scalar2=None, op0=ALU.mult),
                 r=["axa", "arsel"], w=["axa"])
            P.op("dve", lambda e: e.scalar_tensor_tensor(out=xa[:], in0=xb[:], scalar=rsel[:, 1:2], in1=xa[:], op0=ALU.mult, op1=ALU.add),
                 r=["axa", "axb", "arsel"], w=["axa"])
            n_units = 2 * si + 2
            par = si % 2
            for h in range(8):
                steps = [(u, kb) for u in range(n_units) for kb in range(4)]
                lbs = {}

                def emit_qk(si_, h=h, steps=steps, lbs=lbs, n_units=n_units, par=par):
                    nonlocal step
                    u, kb = steps[si_]
                    if kb == 0:
                        lb = step % 2
                        step += 1
                        lbs[u] = lb
                        P.op("sp", lambda e, lb=lb, h=h, u=u: e.dma_start(
                            out=ktb[lb][:], in_=d["KT"][h * 128:(h + 1) * 128, u * 512:(u + 1) * 512]), r=["KT"], w=["akt%d" % lb], dma="akt%d" % lb)
                        P.op("sp", lambda e, lb=lb, h=h, u=u: e.dma_start(
                            out=vtb[lb][:], in_=d["V"].rearrange("(n p) e -> p n e", p=128)[:, u * 4:(u + 1) * 4, h * 128:(h + 1) * 128]),
                            r=["V"], w=["avt%d" % lb], dma="avt%d" % lb)
                    lb = lbs[u]
                    mk = None
                    if u == n_units - 2:
                        mk = 2 * par
                    elif u == n_units - 1:
                        mk = 2 * par + 1
                    sbuf_i = si_ % 2
                    for j in range(2):
                        P.op("pe", lambda e, sbuf_i=sbuf_i, j=j, lb=lb, kb=kb, h=h: e.matmul(
                            ps_s[sbuf_i][j][:], lhsT=ktb[lb][j * 64:(j + 1) * 64, kb * 128:(kb + 1) * 128],
                            rhs=q[j * 64:(j + 1) * 64, h, :], start=True, stop=True),
                            r=["akt%d" % lb, "aq"], w=["aps%d%d" % (sbuf_i, j)])
                        P.op("act", lambda e, sbuf_i=sbuf_i, j=j: e.activation(
                            out=pT[sbuf_i][j][:], in_=ps_s[sbuf_i][j][:], func=AF.Exp),
                            r=["aps%d%d" % (sbuf_i, j)], w=["apT%d%d" % (sbuf_i, j)])
                        if mk is not None:
                            P.op("dve" if j == 0 else "pool", lambda e, sbuf_i=sbuf_i, j=j, mk=mk, kb=kb: e.tensor_tensor(
                                out=pT[sbuf_i][j][:], in0=pT[sbuf_i][j][:], in1=masks[:, mk, kb, :], op=ALU.mult),
                                r=["apT%d%d" % (sbuf_i, j), "amask"], w=["apT%d%d" % (sbuf_i, j)])

                def emit_pv(si_, steps=steps, lbs=lbs):
                    u, kb = steps[si_]
                    lb = lbs[u]
                    sbuf_i = si_ % 2
                    first = (si_ == 0)
                    last = (si_ == len(steps) - 1)
                    for j in range(2):
                        P.op("pe", lambda e, sbuf_i=sbuf_i, j=j, lb=lb, kb=kb, first=first, last=last: e.matmul(
                            ps_o[j][:], lhsT=vtb[lb][:, kb, :], rhs=pT[sbuf_i][j][:], start=first, stop=last),
                            r=["avt%d" % lb, "apT%d%d" % (sbuf_i, j)], w=["apo%d" % j])
                        P.op("pe", lambda e, sbuf_i=sbuf_i, j=j, first=first, last=last: e.matmul(
                            ps_l[j][:], lhsT=C.ones_bf[:], rhs=pT[sbuf_i][j][:], start=first, stop=last),
                            r=["ones_bf", "apT%d%d" % (sbuf_i, j)], w=["apl%d" % j])

                emit_qk(0)
                for si_ in range(len(steps)):
                    if si_ + 1 < len(steps):
                        emit_qk(si_ + 1)
                    emit_pv(si_)
                for j in range(2):
                    P.op("dve", lambda e, j=j: e.reciprocal(out=t32[j][:], in_=ps_l[j][:]), r=["apl%d" % j], w=["at32%d" % j])
                    P.op("dve", lambda e, j=j: e.tensor_tensor(out=t32[j][:], in0=ps_o[j][:], in1=t32[j][:], op=ALU.mult),
                         r=["apo%d" % j, "at32%d" % j], w=["at32%d" % j])
                P.op("dve", lambda e: e.scalar_tensor_tensor(out=t32[2][:], in0=t32[1][:], scalar=lsc[:, 4:5], in1=t32[0][:],
                                                              op0=ALU.mult, op1=ALU.add), r=["at320", "at321", "alsc"], w=["at322"])
                P.op("act", lambda e: e.activation(out=sqb[:], in_=t32[2][:], func=AF.Square), r=["at322"], w=["asqb"])
                P.op("pe", lambda e: e.matmul(ps_s[0][0][:], lhsT=C.ones_bf[:], rhs=sqb[:], start=True, stop=True),
                     r=["ones_bf", "asqb"], w=["aps00"])
                P.op("act", lambda e: e.activation(out=t32[3][:], in_=ps_s[0][0][:], func=AF.Sqrt, bias=C.eps_t[:, 0:1], scale=1.0 / 128),
                     r=["aps00"], w=["at323"])
                P.op("dve", lambda e: e.reciprocal(out=t32[3][:], in_=t32[3][:]), r=["at323"], w=["at323"])
                P.op("dve", lambda e, h=h: e.scalar_tensor_tensor(out=oT[:, h, :], in0=t32[2][:], scalar=gsub[:, 0:1], in1=t32[3][:],
                                                                   op0=ALU.mult, op1=ALU.mult), r=["at322", "at323", "agsub"], w=[("aoT", h)])
            for f in range(8):
                pb = ps_s[1][f % 2]
                pk = "aps1%d" % (f % 2)
                for h in range(8):
                    P.op("pe", lambda e, pb=pb, f=f, h=h: e.matmul(pb[:], lhsT=wo[:, h, f * 128:(f + 1) * 128], rhs=oT[:, h, :],
                                                                   start=(h == 0), stop=(h == 7)), r=["awo", "aoT"], w=[pk])
                P.op("dve", lambda e, pb=pb, f=f: e.scalar_tensor_tensor(
                    out=xa[:, f, :], in0=pb[:], scalar=mod[:, g0 + f:g0 + f + 1], in1=xa[:, f, :], op0=ALU.mult, op1=ALU.add),
                    r=[pk, "axa"], w=["axa"])
            P.op("sp", lambda e, si=si: e.dma_start(out=xout_v[:, :, si * 512:(si + 1) * 512], in_=xa[:]), r=["axa"], w=["x3T"], dma="axo")
        P.barrier()


def stage_final(C, x_in, x_out, T):
    nc, P, d = C.nc, C.P, C.d
    xin_v = x_in.rearrange("(k p) t -> p k t", p=128)
    xout_v = x_out.rearrange("(k p) t -> p k t", p=128)
    with ExitStack() as ls:
        fg = sb(nc, ls, "ffg", [128, 8], F32)
        xt = [sb(nc, ls, "fxt%d" % i, [128, 8, 512], F32) for i in range(2)]
        sq = sb(nc, ls, "fsq", [128, 8, 512], BF16)
        sqv = sb(nc, ls, "fsqv", [128, 512], F32)
        ps = ls.enter_context(nc.psum_tensor("fps", [128, 512], F32))
        P.op("sp", lambda e: e.dma_start(out=fg[:], in_=d["final_gT"][:, :]), w=["ffg"], dma="ffg")
        for t in range(T // 512):
            b = t % 2
            xk = "fxt%d" % b
            P.op("sp", lambda e, b=b, t=t: e.dma_start(out=xt[b][:], in_=xin_v[:, :, t * 512:(t + 1) * 512]), w=[xk], dma="fx%d" % b)
            P.op("act", lambda e, b=b: e.activation(out=sq[:], in_=xt[b][:], func=AF.Square), r=[xk], w=["fsq"])
            for k in range(8):
                P.op("pe", lambda e, k=k: e.matmul(ps[:], lhsT=C.ones_bf[:], rhs=sq[:, k, :], start=(k == 0), stop=(k == 7)),
                     r=["fsq", "ones_bf"], w=["fps"])
            P.op("act", lambda e: e.activation(out=sqv[:], in_=ps[:], func=AF.Sqrt, bias=C.eps_t[:, 0:1], scale=1.0 / D),
                 r=["fps"], w=["fsqv"])
            P.op("dve", lambda e: e.reciprocal(out=sqv[:], in_=sqv[:]), r=["fsqv"], w=["fsqv"])
            for k in range(8):
                P.op("dve", lambda e, b=b, k=k: e.scalar_tensor_tensor(
                    out=xt[b][:, k, :], in0=xt[b][:, k, :], scalar=fg[:, k:k + 1], in1=sqv[:], op0=ALU.mult, op1=ALU.mult),
                    r=[xk, "fsqv", "ffg"], w=[xk])
            P.op("sp", lambda e, b=b, t=t: e.dma_start(out=xout_v[:, :, t * 512:(t + 1) * 512], in_=xt[b][:]), r=[xk], w=["outT"], dma="fxo%d" % b)
        P.barrier()


M_IN = 6176
DBG = set()


def stage_m1(C, x_in, S, mod, part):
    nc, P, d = C.nc, C.P, C.d
    xin_v = x_in.rearrange("(k p) t -> p k t", p=128)
    wv = d["m_w_in"].rearrange("(k p) f -> p k f", p=128)
    nm = "m" + part
    with ExitStack() as ls:
        xt = [sb(nc, ls, nm + "xt%d" % i, [128, 8, 512], F32) for i in range(2)]
        hT2 = [sb(nc, ls, nm + "hT%d" % i, [128, 8, 512], BF16) for i in range(2)]
        sq = sb(nc, ls, nm + "sq", [128, 8, 512], BF16)
        tmp = [sb(nc, ls, nm + "tmp%d" % i, [128, 512], F32) for i in range(2)]
        sqv = sb(nc, ls, nm + "sqv", [128, 512], F32)
        ps_x = ls.enter_context(nc.psum_tensor(nm + "psx", [128, 512], F32))
        ps = [ls.enter_context(nc.psum_tensor(nm + "ps%d" % i, [128, 512], F32)) for i in range(3)]
        if part == "z":
            w = sb(nc, ls, nm + "w", [128, 8, 2048 + 32], BF16)
            zt = [sb(nc, ls, nm + "zt%d" % i, [128, 2048], BF16) for i in range(2)]
            dtt = [sb(nc, ls, nm + "dtt%d" % i, [128, 32], F32) for i in range(2)]
            for i in range(2):
                P.op("pool", lambda e, i=i: e.dma_start(out=w[:, :, i * 1024:(i + 1) * 1024], in_=wv[:, :, i * 1024:(i + 1) * 1024]),
                     w=[(nm + "w", i)], dma=nm + "w%d" % i)
            P.op("pool", lambda e: e.dma_start(out=w[:, :, 2048:2080], in_=wv[:, :, 6144:6176]), w=[(nm + "w", 2)], dma=nm + "w2")
            zv = d["zs"].rearrange("(n p) e -> p n e", p=128)
            dv = d["dtr"].rearrange("(n p) e -> p n e", p=128)
        else:
            w = sb(nc, ls, nm + "w", [128, 8, 4096], BF16)
            diag = sb(nc, ls, nm + "diag", [128, 4, 32, 128], BF16)
            cw = sb(nc, ls, nm + "cw", [128, 32, 4], F32)
            cb = sb(nc, ls, nm + "cb", [128, 32], F32)
            halo = sb(nc, ls, nm + "halo", [128, 32, 4], BF16)
            xraw = [sb(nc, ls, nm + "xraw%d" % i, [128, 516], BF16) for i in range(2)]
            xc = [sb(nc, ls, nm + "xc%d" % i, [128, 4, 512], BF16) for i in range(2)]
            xtok = [sb(nc, ls, nm + "xtok%d" % i, [128, 512], BF16) for i in range(2)]
            ps_t32 = [ls.enter_context(nc.psum_tensor(nm + "pst%d" % i, [128, 512], F32)) for i in range(2)]
            ps_t = [p_[:].bitcast(BF16) for p_ in ps_t32]
            for i in range(4):
                P.op("pool", lambda e, i=i: e.dma_start(out=w[:, :, i * 1024:(i + 1) * 1024], in_=wv[:, :, 2048 + i * 1024:2048 + (i + 1) * 1024]),
                     w=[(nm + "w", i)], dma=nm + "w%d" % i)
            P.op("sp", lambda e: e.dma_start(out=cw[:], in_=d["convwT"][:, :, :]), w=[nm + "cw"], dma=nm + "cw")
            P.op("sp", lambda e: e.dma_start(out=cb[:], in_=d["convbT"][:, :]), w=[nm + "cb"], dma=nm + "cb")
            P.op("dve", lambda e: e.memset(halo[:], 0.0), w=[nm + "halo"])
            for j in range(4):
                for cc in range(32):
                    P.op("pool" if cc % 2 else "dve", lambda e, j=j, cc=cc: e.tensor_scalar(
                        out=diag[:, j, cc, :], in0=C.ident_f[:], scalar1=cw[:, cc, j:j + 1], scalar2=None, op0=ALU.mult),
                        r=["ident_f", nm + "cw"], w=[(nm + "diag", cc)])
            xsv = d["xsB"].rearrange("(n p) e -> p n e", p=128)
            btv = d["BT"].rearrange("(c p) t -> p c t", p=128)
            ctv = d["CT"].rearrange("(c p) t -> p c t", p=128)
        pi = 0
        NT_ = S // 512

        def emit_norm(t):
            b = t % 2
            P.op("sp", lambda e, b=b, t=t: e.dma_start(out=xt[b][:], in_=xin_v[:, :, t * 512:(t + 1) * 512]),
                 w=[nm + "xt%d" % b], dma=nm + "x%d" % b)
            norm_mod(C, xt[b][:], hT2[b], ps_x, sq, tmp, sqv, mod, 0, nm, nm + "xt%d" % b, nm + "hT%d" % b)

        emit_norm(0)
        for t in range(NT_):
            b = t % 2
            hT = hT2[b]
            hk = nm + "hT%d" % b
            if t + 1 < NT_:
                emit_norm(t + 1)
            if part == "z":
                for tc in range(4):
                    zb = (t * 4 + tc) % 2
                    for cg in range(4):
                        pb = pi % 3
                        pi += 1
                        for k in range(8):
                            P.op("pe", lambda e, pb=pb, k=k, tc=tc, cg=cg, hT=hT: e.matmul(
                                ps[pb][:], lhsT=hT[:, k, tc * 128:(tc + 1) * 128], rhs=w[:, k, cg * 512:(cg + 1) * 512],
                                start=(k == 0), stop=(k == 7)), r=[nm + "w", hk], w=[nm + "ps%d" % pb])
                        P.op("act", lambda e, pb=pb, zb=zb, cg=cg: e.activation(
                            out=zt[zb][:, cg * 512:(cg + 1) * 512], in_=ps[pb][:], func=AF.Silu),
                            r=[nm + "ps%d" % pb], w=[(nm + "zt%d" % zb, cg)])
                    pb = pi % 3
                    pi += 1
                    for k in range(8):
                        P.op("pe", lambda e, pb=pb, k=k, tc=tc, hT=hT: e.matmul(
                            ps[pb][:, 0:32], lhsT=hT[:, k, tc * 128:(tc + 1) * 128], rhs=w[:, k, 2048:2080],
                            start=(k == 0), stop=(k == 7)), r=[nm + "w", hk], w=[nm + "ps%d" % pb])
                    P.op("dve", lambda e, pb=pb, zb=zb: e.tensor_copy(out=dtt[zb][:], in_=ps[pb][:, 0:32]),
                         r=[nm + "ps%d" % pb], w=[nm + "dtt%d" % zb])
                    n = t * 4 + tc
                    P.op("sp", lambda e, zb=zb, n=n: e.dma_start(out=zv[:, n, :], in_=zt[zb][:]), r=[nm + "zt%d" % zb], w=["zs"], dma=nm + "zo%d" % zb)
                    P.op("sp", lambda e, zb=zb, n=n: e.dma_start(out=dv[:, n, :], in_=dtt[zb][:]), r=[nm + "dtt%d" % zb], w=["dtr"], dma=nm + "do%d" % zb)
            else:
                def proj(cc, hT=hT, hk=hk):
                    nonlocal pi
                    rb = cc % 2
                    pb = pi % 3
                    pi += 1
                    for k in range(8):
                        P.op("pe", lambda e, pb=pb, k=k, cc=cc: e.matmul(
                            ps[pb][:], lhsT=w[:, k, cc * 128:(cc + 1) * 128], rhs=hT[:, k, :],
                            start=(k == 0), stop=(k == 7)), r=[nm + "w", hk], w=[nm + "ps%d" % pb])
                    P.op("dve", lambda e, rb=rb, cc=cc: e.tensor_copy(out=xraw[rb][:, 0:4], in_=halo[:, cc, :]),
                         r=[(nm + "halo", cc)], w=[nm + "xraw%d" % rb])
                    P.op("act", lambda e, rb=rb, pb=pb: e.activation(out=xraw[rb][:, 4:516], in_=ps[pb][:], func=AF.Copy),
                         r=[nm + "ps%d" % pb], w=[nm + "xraw%d" % rb])
                    P.op("dve", lambda e, rb=rb, cc=cc: e.tensor_copy(out=halo[:, cc, :], in_=xraw[rb][:, 512:516]),
                         r=[nm + "xraw%d" % rb], w=[(nm + "halo", cc)])

                def conv(cc):
                    nonlocal pi
                    rb = cc % 2
                    g4_, ci = divmod(cc, 4)
                    xb_i = g4_ % 2
                    pb2 = pi % 3
                    pi += 1
                    for j in range(4):
                        P.op("pe", lambda e, pb2=pb2, j=j, cc=cc, rb=rb: e.matmul(
                            ps[pb2][:], lhsT=diag[:, j, cc, :], rhs=xraw[rb][:, 1 + j:513 + j], start=(j == 0), stop=(j == 3)),
                            r=[(nm + "diag", cc), nm + "xraw%d" % rb], w=[nm + "ps%d" % pb2])
                    P.op("act", lambda e, pb2=pb2, xb_i=xb_i, ci=ci, cc=cc: e.activation(
                        out=xc[xb_i][:, ci, :], in_=ps[pb2][:], func=AF.Silu, bias=cb[:, cc:cc + 1], scale=1.0),
                        r=[nm + "ps%d" % pb2, nm + "cb"], w=[(nm + "xc%d" % xb_i, ci)])

                proj(0)
                for g4 in range(8):
                    xb_i = g4 % 2
                    for ci in range(4):
                        cc = g4 * 4 + ci
                        if cc + 1 < 32:
                            proj(cc + 1)
                        conv(cc)
                    if g4 >= 4:
                        dst = btv if g4 < 6 else ctv
                        c0 = (g4 - 4) * 4 if g4 < 6 else (g4 - 6) * 4
                        P.op("sp", lambda e, xb_i=xb_i, dst=dst, c0=c0, t=t: e.dma_start(
                            out=dst[:, c0:c0 + 4, t * 512:(t + 1) * 512], in_=xc[xb_i][:]),
                            r=[nm + "xc%d" % xb_i], w=["BTCT"], dma=nm + "bo%d" % xb_i)
                    if g4 < 6:
                        for tc in range(4):
                            tb = (g4 * 4 + tc) % 2
                            for ci in range(4):
                                P.op("pe", lambda e, tb=tb, ci=ci, xb_i=xb_i, tc=tc: e.transpose(
                                    out=ps_t[tb][:, ci * 128:(ci + 1) * 128], in_=xc[xb_i][:, ci, tc * 128:(tc + 1) * 128],
                                    identity=C.ident_bf[:]), r=[(nm + "xc%d" % xb_i, ci), "ident_bf"], w=[nm + "pst%d" % tb])
                            P.op("dve" if tc % 2 else "pool" if False else "dve", lambda e, tb=tb: e.tensor_copy(out=xtok[tb][:], in_=ps_t[tb][:, 0:512]),
                                 r=[nm + "pst%d" % tb], w=[nm + "xtok%d" % tb])
                            n = t * 4 + tc
                            P.op("sp", lambda e, tb=tb, n=n, g4=g4: e.dma_start(out=xsv[:, n, g4 * 512:(g4 + 1) * 512], in_=xtok[tb][:]),
                                 r=[nm + "xtok%d" % tb], w=["xsB"], dma=nm + "xo%d" % tb)
        P.barrier()


def stage_m2(C, x_in, x_out, S, mod):
    nc, P, d = C.nc, C.P, C.d
    xin_v = x_in.rearrange("(k p) t -> p k t", p=128)
    xout_v = x_out.rearrange("(k p) t -> p k t", p=128)
    zv = d["zs"].rearrange("(n p) e -> p n e", p=128)
    dv = d["dtr"].rearrange("(n p) e -> p n e", p=128)
    xsv = d["xsB"].rearrange("(n p) e -> p n e", p=128)
    btv = d["BT"].rearrange("(c p) t -> p c t", p=128)
    ctv = d["CT"].rearrange("(c p) t -> p c t", p=128)
    g0 = 2 * 8
    with ExitStack() as ls:
        wout = sb(nc, ls, "swout", [128, 16, 1024], BF16)
        tri = sb(nc, ls, "stri", [128, 128], F32)
        negm = sb(nc, ls, "snegm", [128, 4, 128], BF16)
        dtb = sb(nc, ls, "sdtb", [128, 32], F32)
        aneg = sb(nc, ls, "saneg", [128, 32], F32)
        dsk = sb(nc, ls, "sdsk", [128, 32], F32)
        dI = sb(nc, ls, "sdI", [128, 32, 128], BF16)
        normg = sb(nc, ls, "snormg", [128, 2048], F32)
        blkT = sb(nc, ls, "sblkT", [32, 4096], BF16)
        Rm = sb(nc, ls, "sRm", [64, 4096], F32)
        Lm = sb(nc, ls, "sLm", [64, 128], F32)
        Tin = sb(nc, ls, "sTin", [128, 64], F32)
        acsT = sb(nc, ls, "sacsT", [32, 128], F32)
        S32 = sb(nc, ls, "sS32", [128, 2048], F32)
        Sbf = [sb(nc, ls, "sSbf%d" % i, [128, 2048], BF16) for i in range(2)]
        zt = [sb(nc, ls, "szt%d" % i, [128, 2048], BF16) for i in range(2)]
        xb = [sb(nc, ls, "sxb%d" % i, [128, 3072], BF16) for i in range(2)]
        BTc = [sb(nc, ls, "sBT%d" % i, [128, 8, 128], BF16) for i in range(2)]
        CTc = [sb(nc, ls, "sCT%d" % i, [128, 8, 128], BF16) for i in range(2)]
        dtr = [sb(nc, ls, "sdtr%d" % i, [128, 32], F32) for i in range(2)]
        xt = [sb(nc, ls, "sxt%d" % i, [128, 8, 128], F32) for i in range(2)]
        sm = sb(nc, ls, "ssm", [128, 64], F32)
        v32 = [sb(nc, ls, "sv32%d" % i, [128, 8, 32], F32) for i in range(2)]
        xtl = [sb(nc, ls, "sxtl%d" % i, [128, 2048], BF16) for i in range(2)]
        xts = [sb(nc, ls, "sxts%d" % i, [128, 2048], BF16) for i in range(2)]
        CBm = [sb(nc, ls, "sCBm%d" % i, [128, 128], F32) for i in range(2)]
        LT = [sb(nc, ls, "sLT%d" % i, [128, 512], F32) for i in range(2)]
        MT = [sb(nc, ls, "sMT%d" % i, [128, 8, 512], BF16) for i in range(2)]
        ty = [sb(nc, ls, "sty%d" % i, [128, 256], F32) for i in range(2)]
        y32 = sb(nc, ls, "sy32", [128, 2048], F32)
        junk = sb(nc, ls, "sjunk", [128, 256], F32)
        ssq = sb(nc, ls, "sssq", [128, 8], F32)
        yn = sb(nc, ls, "syn", [128, 2048], BF16)
        ynT = sb(nc, ls, "synT", [128, 16, 128], BF16)
        pmc = ls.enter_context(nc.psum_tensor("spmc", [128, 512], F32))
        pD = [ls.enter_context(nc.psum_tensor("spD%d" % i, [128, 512], F32)) for i in range(2)]
        pY = [ls.enter_context(nc.psum_tensor("spY%d" % i, [128, 512], F32)) for i in range(2)]
        pS = ls.enter_context(nc.psum_tensor("spS", [128, 512], F32))
        pAB = [ls.enter_context(nc.psum_tensor("spAB%d" % i, [128, 512], F32)) for i in range(2)]
        pABb = [p_[:].bitcast(BF16) for p_ in pAB]
        wov = d["m_w_out"].rearrange("(j p) f -> p j f", p=128)
        for i in range(2):
            P.op("pool", lambda e, i=i: e.dma_start(out=wout[:, i * 8:(i + 1) * 8, :], in_=wov[:, i * 8:(i + 1) * 8, :]),
                 w=[("swout", i)], dma="swout%d" % i)
        for (t_, nme) in ((tri, "tri"), (dtb, "dtbT"), (aneg, "alogT"), (dsk, "dskT"), (normg, "normgT")):
            P.op("sp", lambda e, t_=t_, nme=nme: e.dma_start(out=t_[:], in_=d[nme][:, :]), w=["s_" + nme], dma="s_" + nme)
        P.op("pool", lambda e: e.dma_start(out=negm[:], in_=d["negm4"][:, :, :]), w=["s_negm"], dma="s_negm")
        P.op("pool", lambda e: e.dma_start(out=blkT[:, 0:2048], in_=d["blk"][0:32, 0:2048]), w=[("s_blkT", 0)], dma="s_blkT")
        P.op("pool", lambda e: e.dma_start(out=blkT[:, 2048:4096], in_=d["blk"][0:32, 2048:4096]), w=[("s_blkT", 1)], dma="s_blkT2")
        P.op("sp", lambda e: e.dma_start(out=Rm[32:64, :], in_=d["blk"][32:64, :]), w=["sRm_b"], dma="s_Rmb")
        P.op("act", lambda e: e.activation(out=aneg[:], in_=aneg[:], func=AF.Exp), r=["s_alogT"], w=["s_alogT"])
        P.op("dve", lambda e: e.tensor_scalar(out=aneg[:], in0=aneg[:], scalar1=-1.0, scalar2=None, op0=ALU.mult), r=["s_alogT"], w=["s_alogT"])
        P.op("dve", lambda e: e.memset(S32[:], 0.0), w=["sS32"])
        P.op("pool", lambda e: e.memset(Sbf[0][:], 0.0), w=["sSbf0"])
        P.op("dve", lambda e: e.memset(Tin[:, 0:32], 1.0), w=["sTin_a"])
        for r_ in range(32):
            P.op("dve" if r_ % 2 else "pool", lambda e, r_=r_: e.tensor_scalar(out=dI[:, r_, :], in0=C.ident_f[:], scalar1=dsk[:, r_:r_ + 1], scalar2=None, op0=ALU.mult),
                 r=["ident_f", "s_dskT"], w=[("sdI", r_)])
        one = C.ones_f[:, 0:1]
        NC_ = S // 128
        gcount = [0, 0]

        def front_pre(c):
            b = c % 2
            sl = slice(c * 128, (c + 1) * 128)
            V = lambda i: v32[b][:, i, :]
            sv = "sv%d" % b
            P.op("sp", lambda e: e.dma_start(out=dtr[b][:], in_=dv[:, c, :]), r=["dtr"], w=["sdtr%d" % b], dma="sdtr%d" % b)
            P.op("sp", lambda e: e.dma_start(out=BTc[b][:], in_=btv[:, :, sl]), r=["BTCT"], w=["sBT%d" % b], dma="sBT%d" % b)
            P.op("sp", lambda e: e.dma_start(out=CTc[b][:], in_=ctv[:, :, sl]), r=["BTCT"], w=["sCT%d" % b], dma="sCT%d" % b)
            P.op("sp", lambda e: e.dma_start(out=xb[b][:], in_=xsv[:, c, :]), r=["xsB"], w=["sxb%d" % b], dma="sxb%d" % b)
            P.op("dve", lambda e: e.tensor_tensor(out=V(0), in0=dtr[b][:], in1=dtb[:], op=ALU.add), r=["sdtr%d" % b, "s_dtbT"], w=[(sv, 0)])
            P.op("act", lambda e: e.activation(out=V(1), in_=V(0), func=AF.Exp), r=[(sv, 0)], w=[(sv, 1)])
            P.op("act", lambda e: e.activation(out=V(2), in_=V(1), func=AF.Ln, bias=one, scale=1.0), r=[(sv, 1), "ones_f"], w=[(sv, 2)])
            P.op("dve", lambda e: e.tensor_tensor(out=V(3), in0=V(2), in1=aneg[:], op=ALU.mult), r=[(sv, 2), "s_alogT"], w=[(sv, 3)])
            P.op("pe", lambda e: e.matmul(pmc[:, 0:32], lhsT=tri[:], rhs=V(3), start=True, stop=True), r=["s_tri", (sv, 3)], w=["spmc"])
            P.op("pe", lambda e: e.matmul(pmc[:, 32:64], lhsT=C.ones_f[:], rhs=V(3), start=True, stop=True), r=["ones_f", (sv, 3)], w=["spmc"])
            P.op("dve", lambda e: e.tensor_copy(out=sm[:], in_=pmc[:, 0:64]), r=["spmc"], w=["ssm"])
            P.op("dve", lambda e: e.tensor_scalar(out=Tin[:, 32:64], in0=sm[:, 0:32], scalar1=-1.0, scalar2=None, op0=ALU.mult), r=["ssm"], w=["sTin_b"])
            P.op("pe", lambda e: e.transpose(out=pmc[0:64, 256:384], in_=Tin[:], identity=C.ident_f[:]), r=["sTin_a", "sTin_b", "ident_f"], w=["spmc"])
            P.op("pe", lambda e: e.transpose(out=pmc[0:32, 384:512], in_=sm[:, 0:32], identity=C.ident_f[:]), r=["ssm", "ident_f"], w=["spmc"])
            P.op("act", lambda e: e.activation(out=Lm[:], in_=pmc[0:64, 256:384], func=AF.Copy), r=["spmc"], w=["sLm"])
            P.op("act", lambda e: e.activation(out=acsT[:], in_=pmc[0:32, 384:512], func=AF.Copy), r=["spmc"], w=["sacsT"])
            for hf, eng_ in ((0, "dve"), (1, "pool")):
                P.op(eng_, lambda e, hf=hf: e.tensor_tensor(
                    out=Rm[0:32, hf * 2048:(hf + 1) * 2048].rearrange("p (r q) -> p r q", q=128),
                    in0=blkT[:, hf * 2048:(hf + 1) * 2048].rearrange("p (r q) -> p r q", q=128),
                    in1=acsT[:].unsqueeze(1).broadcast_to([32, 16, 128]), op=ALU.mult),
                    r=["sacsT", "s_blkT"], w=[("sRm_a", hf)])
            P.op("act", lambda e: e.activation(out=V(4), in_=sm[:, 0:32], func=AF.Exp), r=["ssm"], w=[(sv, 4)])
            P.op("dve", lambda e: e.tensor_tensor(out=V(5), in0=sm[:, 32:64], in1=sm[:, 0:32], op=ALU.subtract), r=["ssm"], w=[(sv, 5)])
            P.op("act", lambda e: e.activation(out=V(5), in_=V(5), func=AF.Exp), r=[(sv, 5)], w=[(sv, 5)])
            P.op("act", lambda e: e.activation(out=V(6), in_=sm[:, 32:64], func=AF.Exp), r=["ssm"], w=[(sv, 6)])
            P.op("dve", lambda e: e.tensor_tensor(out=V(7), in0=V(2), in1=V(5), op=ALU.mult), r=[(sv, 2), (sv, 5)], w=[(sv, 7)])
            xs3 = xb[b][:, 0:2048].rearrange("p (h e) -> p h e", e=64)
            P.op("dve", lambda e: e.tensor_tensor(out=xtl[b][:].rearrange("p (h e) -> p h e", e=64), in0=xs3,
                                                  in1=V(2).unsqueeze(2).broadcast_to([128, 32, 64]), op=ALU.mult),
                 r=["sxb%d" % b, (sv, 2)], w=["sxtl%d" % b])
            P.op("pool", lambda e: e.tensor_tensor(out=xts[b][:].rearrange("p (h e) -> p h e", e=64), in0=xs3,
                                                   in1=V(7).unsqueeze(2).broadcast_to([128, 32, 64]), op=ALU.mult),
                 r=["sxb%d" % b, (sv, 7)], w=["sxts%d" % b])
        def front_group(c, g):
            b = c % 2
            sv = "sv%d" % b
            gb = gcount[0] % 2
            gcount[0] += 1
            P.op("pe", lambda e, g=g: e.matmul(pmc[:, 128:256], lhsT=BTc[b][:, g, :], rhs=CTc[b][:, g, :], start=True, stop=True),
                 r=["sBT%d" % b, "sCT%d" % b], w=["spmc"])
            P.op("act", lambda e, gb=gb: e.activation(out=CBm[gb][:], in_=pmc[:, 128:256], func=AF.Copy), r=["spmc"], w=["sCBm%d" % gb])
            P.op("pe", lambda e, g=g, gb=gb: e.matmul(pD[gb][:], lhsT=Lm[:], rhs=Rm[:, g * 512:(g + 1) * 512], start=True, stop=False),
                 r=["sLm", "sRm_a", "sRm_b"], w=["spD%d" % gb])
            P.op("pe", lambda e, gb=gb: e.matmul(pD[gb][:], lhsT=C.ident_bf[:], rhs=negm[:].rearrange("p r q -> p (r q)"), start=False, stop=True),
                 r=["ident_bf", "s_negm"], w=["spD%d" % gb])
            P.op("act", lambda e, gb=gb: e.activation(out=LT[gb][:], in_=pD[gb][:], func=AF.Exp), r=["spD%d" % gb], w=["sLT%d" % gb])
            P.op("dve" if g % 3 else "pool", lambda e, gb=gb, g=g: e.tensor_tensor(out=MT[b][:, g, :].rearrange("p (r q) -> p r q", q=128),
                                                              in0=LT[gb][:].rearrange("p (r q) -> p r q", q=128),
                                                              in1=CBm[gb][:].unsqueeze(1).broadcast_to([128, 4, 128]), op=ALU.mult),
                 r=["sLT%d" % gb, "sCBm%d" % gb], w=[("sMT%d" % b, g)])

        def back_pre(c):
            b = c % 2
            sl = slice(c * 128, (c + 1) * 128)
            sv = "sv%d" % b
            Sb_old, Sb_new = Sbf[c % 2], Sbf[(c + 1) % 2]
            ko, kn = "sSbf%d" % (c % 2), "sSbf%d" % ((c + 1) % 2)
            P.op("sp", lambda e: e.dma_start(out=zt[b][:], in_=zv[:, c, :]), r=["zs"], w=["szt%d" % b], dma="szt%d" % b)
            P.op("sp", lambda e: e.dma_start(out=xt[b][:], in_=xin_v[:, :, sl]), w=["sxt%d" % b], dma="sxt%d" % b)
            P.op("pool", lambda e: e.tensor_tensor(out=S32[:].rearrange("p (h e) -> p h e", e=64), in0=S32[:].rearrange("p (h e) -> p h e", e=64),
                                                   in1=v32[b][:, 6, :].unsqueeze(2).broadcast_to([128, 32, 64]), op=ALU.mult),
                 r=["sS32", (sv, 6)], w=["sS32"])
        def back_group(c, g):
            b = c % 2
            sl = slice(c * 128, (c + 1) * 128)
            sv = "sv%d" % b
            Sb_old, Sb_new = Sbf[c % 2], Sbf[(c + 1) % 2]
            ko, kn = "sSbf%d" % (c % 2), "sSbf%d" % ((c + 1) % 2)
            gb = gcount[1] % 2
            gcount[1] += 1
            for r_ in range(4):
                hd = 4 * g + r_
                P.op("pe", lambda e, r_=r_, hd=hd, gb=gb, g=g: e.matmul(pY[gb][:, r_ * 64:(r_ + 1) * 64], lhsT=MT[b][:, g, r_ * 128:(r_ + 1) * 128],
                                                                        rhs=xtl[b][:, hd * 64:(hd + 1) * 64], start=True, stop=False),
                     r=[("sMT%d" % b, g), "sxtl%d" % b], w=["spY%d" % gb])
                P.op("pe", lambda e, r_=r_, hd=hd, gb=gb: e.matmul(pY[gb][:, r_ * 64:(r_ + 1) * 64], lhsT=dI[:, hd, :],
                                                                   rhs=xb[b][:, hd * 64:(hd + 1) * 64], start=False, stop=True),
                     r=[("sdI", hd), "sxb%d" % b], w=["spY%d" % gb])
            P.op("pe", lambda e, g=g, gb=gb: e.matmul(pY[gb][:, 256:512], lhsT=CTc[b][:, g, :], rhs=Sb_old[:, g * 256:(g + 1) * 256], start=True, stop=True),
                 r=["sCT%d" % b, ko], w=["spY%d" % gb])
            P.op("dve", lambda e, g=g, gb=gb: e.tensor_tensor(out=ty[gb][:].rearrange("p (r e) -> p r e", e=64),
                                                              in0=pY[gb][:, 256:512].rearrange("p (r e) -> p r e", e=64),
                                                              in1=v32[b][:, 4, 4 * g:4 * g + 4].unsqueeze(2).broadcast_to([128, 4, 64]), op=ALU.mult),
                 r=["spY%d" % gb, (sv, 4)], w=["sty%d" % gb])
            P.op("dve", lambda e, g=g, gb=gb: e.tensor_tensor(out=y32[:, g * 256:(g + 1) * 256], in0=pY[gb][:, 0:256], in1=ty[gb][:], op=ALU.add),
                 r=["spY%d" % gb, "sty%d" % gb], w=[("sy32", g)])
            P.op("pe", lambda e, g=g: e.matmul(pS[:, 0:256], lhsT=xb[b][:, 2048 + g * 128:2048 + (g + 1) * 128], rhs=xts[b][:, g * 256:(g + 1) * 256],
                                               start=True, stop=True), r=["sxb%d" % b, "sxts%d" % b], w=["spS"])
            P.op("dve", lambda e, g=g: e.tensor_tensor(out=S32[:, g * 256:(g + 1) * 256], in0=pS[:, 0:256], in1=S32[:, g * 256:(g + 1) * 256], op=ALU.add),
                 r=["spS", "sS32"], w=[("sS32", g)])
        def back_post_ew(c):
            b = c % 2
            sl = slice(c * 128, (c + 1) * 128)
            sv = "sv%d" % b
            Sb_old, Sb_new = Sbf[c % 2], Sbf[(c + 1) % 2]
            ko, kn = "sSbf%d" % (c % 2), "sSbf%d" % ((c + 1) % 2)
            P.op("act", lambda e: e.activation(out=Sb_new[:], in_=S32[:], func=AF.Copy), r=["sS32"], w=[kn])
            P.op("pool", lambda e: e.tensor_tensor(out=y32[:], in0=y32[:], in1=zt[b][:], op=ALU.mult), r=["sy32", "szt%d" % b], w=["sy32"])
            for g in range(8):
                P.op("act", lambda e, g=g: e.activation(out=junk[:], in_=y32[:, g * 256:(g + 1) * 256], func=AF.Square, accum_out=ssq[:, g:g + 1]),
                     r=["sy32"], w=["sjunk", ("sssq", g)])
            P.op("act", lambda e: e.activation(out=ssq[:], in_=ssq[:], func=AF.Sqrt, bias=C.eps_t[:, 0:1], scale=1.0 / 256), r=["sssq"], w=["sssq"])
            P.op("dve", lambda e: e.reciprocal(out=ssq[:], in_=ssq[:]), r=["sssq"], w=["sssq"])
            for g in range(8):
                P.op("dve", lambda e, g=g: e.scalar_tensor_tensor(out=yn[:, g * 256:(g + 1) * 256], in0=y32[:, g * 256:(g + 1) * 256],
                                                                   scalar=ssq[:, g:g + 1], in1=normg[:, g * 256:(g + 1) * 256], op0=ALU.mult, op1=ALU.mult),
                     r=["sy32", "sssq", "s_normgT"], w=[("syn", g)])
        def back_post_pe(c):
            b = c % 2
            sl = slice(c * 128, (c + 1) * 128)
            sv = "sv%d" % b
            Sb_old, Sb_new = Sbf[c % 2], Sbf[(c + 1) % 2]
            ko, kn = "sSbf%d" % (c % 2), "sSbf%d" % ((c + 1) % 2)
            for i in range(4):
                ab = i % 2
                for ci in range(4):
                    dc = i * 4 + ci
                    P.op("pe", lambda e, ci=ci, dc=dc, ab=ab: e.transpose(out=pABb[ab][:, ci * 128:(ci + 1) * 128], in_=yn[:, dc * 128:(dc + 1) * 128], identity=C.ident_bf[:]),
                         r=["syn", "ident_bf"], w=["spAB%d" % ab])
                P.op("act", lambda e, i=i, ab=ab: e.activation(out=ynT[:, i * 4:(i + 1) * 4, :].rearrange("p a b -> p (a b)"), in_=pABb[ab][:, 0:512], func=AF.Copy),
                     r=["spAB%d" % ab], w=[("synT", i)])
            for f in range(8):
                ab = f % 2
                for dc in range(16):
                    P.op("pe", lambda e, f=f, ab=ab, dc=dc: e.matmul(pAB[ab][:, 0:128], lhsT=wout[:, dc, f * 128:(f + 1) * 128], rhs=ynT[:, dc, :],
                                                                    start=(dc == 0), stop=(dc == 15)), r=["swout", "synT"], w=["spAB%d" % ab])
                P.op("dve", lambda e, f=f, ab=ab: e.scalar_tensor_tensor(out=xt[b][:, f, :], in0=pAB[ab][:, 0:128], scalar=mod[:, g0 + f:g0 + f + 1],
                                                                          in1=xt[b][:, f, :], op0=ALU.mult, op1=ALU.add),
                     r=["spAB%d" % ab, "sxt%d" % b], w=["sxt%d" % b])
            P.op("sp", lambda e: e.dma_start(out=xout_v[:, :, sl], in_=xt[b][:]), r=["sxt%d" % b], w=["x1T"], dma="sxo%d" % b)

        front_pre(0)
        for g in range(8):
            front_group(0, g)
        for c in range(NC_):
            back_pre(c)
            if c + 1 < NC_:
                front_pre(c + 1)
            for g in range(8):
                if c + 1 < NC_:
                    front_group(c + 1, g)
                back_group(c, g)
                if g == 3 and c >= 1:
                    back_post_pe(c - 1)
            back_post_ew(c)
        back_post_pe(NC_ - 1)
        P.barrier()


def build(S, stages=None, outs=("out_tok",), chain=True, only_inputs=None):
    nc = bass.Bass("TRN2", target_bir_lowering=False)
    C = Ctx()
    C.nc = nc
    C.S = S
    TO = S // 2
    allst = ["m1z", "m1x", "m2", "ffn0", "qkv", "attn", "moes"]
    if stages is None:
        stages = allst
    d = {}

    def din(name, shape, dt=F32):
        if only_inputs is not None and name not in only_inputs:
            return
        d[name] = nc.dram_tensor(name, list(shape), dt, kind="ExternalInput").ap()

    def dscr(name, shape, dt=F32):
        kind = "ExternalOutput" if name in outs else "Internal"
        d[name] = nc.dram_tensor(name, list(shape), dt, kind=kind).ap()

    din("xT", [D, S]); din("cT", [128, 8]); din("ident", [128, 128]); din("sel", [8, 8, 128])
    din("ada_w0", [D, 6 * D]); din("ada_bT0", [128, 48])
    din("ada_w1", [D, 6 * D]); din("ada_bT1", [128, 48])
    din("m_w_in", [D, M_IN]); din("convwT", [128, 32, 4]); din("convbT", [128, 32])
    din("dtbT", [128, 32]); din("alogT", [128, 32]); din("dskT", [128, 32]); din("normgT", [128, 2048])
    din("m_w_out", [2048, D]); din("tri", [128, 128]); din("negm4", [128, 4, 128]); din("blk", [64, 4096])
    din("ffn_w_gate", [D, FFN_DIM]); din("ffn_w_up", [D, FFN_DIM]); din("ffn_w_down", [FFN_DIM, D])
    din("a_w_qkv", [D, 3072]); din("lamv", [128, 4, 64]); din("sublnT", [128, 1]); din("a_w_o", [D, D])
    din("masks", [4, 128, 4, 512]); din("rolesel", [128, 2])
    din("moe_w_router", [D, N_EXP]); din("moe_w_gate", [N_EXP, D, EXP_DIM]); din("moe_w_up", [N_EXP, D, EXP_DIM])
    din("moe_w_down", [N_EXP, EXP_DIM, D]); din("final_gT", [128, 8])
    NUe = EXP_DIM // 512
    for nm_ in ("moe_wg_r0", "moe_wg_r1", "moe_wu_r0", "moe_wu_r1", "moe_wd_r0", "moe_wd_r1"):
        din(nm_, [N_EXP * NUe * 128, 2048])
    din("sul", [128, 128]); din("thr", [128, 17]); din("iotaU", [128, NUe]); din("final_gB", [128, 1024])
    TS = TO if chain else S
    NBs = (2 * TS) // MOE_BLK + N_EXP
    din("bstart", [128, NBs])
    dscr("Hrow", [TS, 1024], BF16); dscr("Xs", [NBs * MOE_BLK, 1024], BF16); dscr("Ys", [NBs * MOE_BLK, 1024]); dscr("out_tok", [TS, 1024])
    dscr("zs", [S, 2048], BF16); dscr("dtr", [S, 32]); dscr("xsB", [S, 3072], BF16)
    dscr("BT", [1024, S], BF16); dscr("CT", [1024, S], BF16)
    dscr("x1T", [D, S]); dscr("x2T", [D, S])
    dscr("QT", [1024, S], BF16); dscr("KT", [1024, S], BF16); dscr("V", [S, 1024], BF16)
    dscr("x3T", [D, TO]); dscr("x4T", [D, TO]); dscr("outT", [D, TO])
    C.d = d
    with ExitStack() as top:
        C.top = top
        C.P = Prog(nc, top)
        C.eps_t = sb(nc, top, "eps_t", [128, 1], F32)
        C.P.op("dve", lambda e: e.memset(C.eps_t[:], EPS), w=["eps_t"])
        stage_consts(C)
        x0 = d["xT"]
        if "m1z" in stages:
            stage_m1(C, x0, S, C.mod[0], "z")
        if "m1x" in stages:
            stage_m1(C, x0, S, C.mod[0], "x")
        if "m2" in stages:
            stage_m2(C, x0, d["x1T"], S, C.mod[0])
        if "ffn0" in stages:
            stage_ffn(C, d["x1T"] if chain else x0, d["x2T"], S, C.mod[0], d["ffn_w_gate"], d["ffn_w_up"], d["ffn_w_down"], FFN_DIM, "f0")
        xl1 = d["x2T"] if chain else x0
        if "qkv" in stages:
            stage_qkv(C, xl1, S, C.mod[1])
        if "attn" in stages:
            stage_attn(C, xl1, d["x3T"], S, C.mod[1])
        if "moe" in stages:
            stage_ffn(C, d["x3T"] if chain else x0, d["x4T"], TO if chain else S, C.mod[1], d["moe_w_gate"], d["moe_w_up"], d["moe_w_down"],
                      EXP_DIM, "m1", n_exp=N_EXP, wr=d["moe_w_router"])
        if "final" in stages:
            stage_final(C, d["x4T"], d["outT"], TO)
        if "moes" in stages:
            stage_moe_sparse(C, d["x3T"] if chain else x0, TO if chain else S, C.mod[1])
        C.P.emit(final_wait_keys=list(range(len(C.P.dma_cnt))))
    return nc


def host_consts(role):
    ident = np.eye(128, dtype=np.float32)
    sel = np.zeros((8, 8, 128), np.float32)
    for e in range(8):
        sel[e, e, :] = 1.0
    s_ = np.arange(128)
    tri = (s_[:, None] <= s_[None, :]).astype(np.float32)
    negm4 = np.ascontiguousarray(np.tile(((s_[:, None] > s_[None, :]) * -30000.0).astype(np.float32)[:, None, :], (1, 4, 1)))
    blk = np.zeros((64, 32, 128), np.float32)
    for r_ in range(32):
        blk[r_, r_, :] = 1.0
        blk[32 + r_, r_, :] = 1.0
    blk = blk.reshape(64, 4096)
    k_ = np.arange(512)
    trib = (k_[None, :] >= k_[:, None]).astype(np.float32).reshape(4, 128, 512).transpose(1, 0, 2)
    ones = np.ones_like(trib)
    zeros = np.zeros_like(trib)
    if role == 1:
        masks = np.stack([ones, trib, trib, zeros])
    else:
        masks = np.stack([trib, zeros, ones, trib])
    rolesel = np.zeros((128, 2), np.float32)
    rolesel[:, role] = 1.0
    sul = (s_[:, None] < s_[None, :]).astype(np.float32)
    thr = np.tile((np.arange(17) * float(MOE_BLK))[None, :], (128, 1)).astype(np.float32)
    NUe = EXP_DIM // 512
    iotaU = (np.arange(NUe)[None, :] * 128 + s_[:, None]).astype(np.float32)
    return {"sul": sul, "thr": thr, "iotaU": iotaU, "ident": ident, "sel": sel, "tri": tri, "negm4": negm4, "blk": blk, "masks": np.ascontiguousarray(masks), "rolesel": rolesel}


def make_in_maps(inp, S, dense_moe=False, TS=None):
    f = lambda a: np.ascontiguousarray(np.asarray(a, dtype=np.float32))
    col = lambda v, n: f(np.asarray(v).reshape(n, 128).T)
    til = lambda v: f(np.tile(np.asarray(v)[None, :], (128, 1)))
    shared = {
        "m_w_in": f(inp["m_w_in"]), "m_w_out": f(inp["m_w_out"]),
        "convwT": f(np.asarray(inp["m_conv_w"]).reshape(4, 32, 128).transpose(2, 1, 0)),
        "convbT": col(inp["m_conv_b"], 32), "dtbT": til(inp["m_dt_bias"]), "alogT": til(inp["m_a_log"]),
        "dskT": til(inp["m_d_skip"]), "normgT": til(inp["m_norm_g"]),
        "ffn_w_gate": f(inp["ffn_w_gate"]), "ffn_w_up": f(inp["ffn_w_up"]), "ffn_w_down": f(inp["ffn_w_down"]),
        "a_w_qkv": f(inp["a_w_qkv"]), "a_w_o": f(inp["a_w_o"]),
        "lamv": f(np.tile(np.stack([inp["a_lam_q1"], inp["a_lam_k1"], inp["a_lam_q2"], inp["a_lam_k2"]])[None], (128, 1, 1))),
        "sublnT": col(inp["a_subln_g"], 1),
        "moe_w_router": f(inp["moe_w_router"]), "final_gT": col(inp["final_g"], 8), "final_gB": til(inp["final_g"]),
    }
    if TS is None:
        TS = S // 2
    NBs = (2 * TS) // MOE_BLK + N_EXP
    shared["bstart"] = f(np.tile((np.arange(NBs) * float(MOE_BLK))[None, :], (128, 1)))
    if dense_moe:
        shared.update({"moe_w_gate": f(inp["moe_w_gate"]), "moe_w_up": f(inp["moe_w_up"]), "moe_w_down": f(inp["moe_w_down"])})
    else:
        NUe = EXP_DIM // 512
        for nm_, key in (("moe_wg_r", "moe_w_gate"), ("moe_wu_r", "moe_w_up")):
            w_ = np.asarray(inp[key], dtype=np.float32).reshape(N_EXP, 2, 4, 128, NUe, 512)
            w2_ = np.ascontiguousarray(w_.transpose(1, 0, 4, 3, 2, 5)).reshape(2, N_EXP * NUe * 128, 2048)
            shared[nm_ + "0"], shared[nm_ + "1"] = w2_[0], w2_[1]
        w_ = np.asarray(inp["moe_w_down"], dtype=np.float32).reshape(N_EXP, NUe, 2, 2, 128, 1024)
        w2_ = np.ascontiguousarray(w_.transpose(2, 0, 1, 4, 3, 5)).reshape(2, N_EXP * NUe * 128, 2048)
        shared["moe_wd_r0"], shared["moe_wd_r1"] = w2_[0], w2_[1]
    shared["ada_w0"] = f(inp["ada_w0"]); shared["ada_bT0"] = col(inp["ada_b0"], 48)
    shared["ada_w1"] = f(inp["ada_w1"]); shared["ada_bT1"] = col(inp["ada_b1"], 48)
    hc = [host_consts(0), host_consts(1)]
    maps = []
    for c in range(8):
        b, role = c // 2, c % 2
        m = dict(shared)
        m.update(hc[role])
        m["xT"] = f(np.asarray(inp["x"])[b, :S, :].T)
        m["cT"] = col(np.asarray(inp["c"])[b], 8)
        maps.append(m)
    return maps


def assemble(res, S, B=4):
    TO = S // 2
    n_slots = S // 1024
    TA, TB = own_tiles(n_slots)
    out = np.zeros((B, S, D), np.float32)
    for c in range(2 * B):
        b, role = c // 2, c % 2
        o = np.asarray(res[c]["out_tok"])
        tiles = TA if role == 0 else TB
        for si, t in enumerate(tiles):
            out[b, t * 512:(t + 1) * 512, :] = o[si * 512:(si + 1) * 512, :]
    return out


_NC_CACHE = {}
_USED_INPUTS = {"xT", "cT", "ident", "sel", "ada_w0", "ada_bT0", "ada_w1", "ada_bT1", "m_w_in", "convwT", "convbT", "dtbT", "alogT",
                "dskT", "normgT", "m_w_out", "tri", "negm4", "blk", "ffn_w_gate", "ffn_w_up", "ffn_w_down", "a_w_qkv", "lamv", "sublnT",
                "a_w_o", "masks", "rolesel", "moe_w_router", "moe_wg_r0", "moe_wg_r1", "moe_wu_r0", "moe_wu_r1", "moe_wd_r0", "moe_wd_r1", "sul", "thr", "iotaU", "final_gB", "bstart"}


def kernel(**inputs):
    S = int(np.asarray(inputs["x"]).shape[1])
    if S not in _NC_CACHE:
        _NC_CACHE[S] = build(S, only_inputs=_USED_INPUTS)
    nc = _NC_CACHE[S]
    maps = [{k: v for k, v in m.items() if k in _USED_INPUTS} for m in make_in_maps(inputs, S)]
    res = run_bass_kernel_spmd(nc, maps, core_ids=list(range(8)))
    return assemble(res.results, S)


U32 = mybir.dt.uint32
MOE_BLK = 512


def stage_moe_sparse(C, x_in, T, mod):
    nc, P, d = C.nc, C.P, C.d
    BLK = MOE_BLK
    NCH = T // 128
    NB = (2 * T) // BLK + N_EXP
    NR = NB * BLK
    HG = 4
    NU = EXP_DIM // (HG * 128)
    xin_v = x_in.rearrange("(k p) t -> p k t", p=128)
    g0 = 5 * 8
    st = ExitStack()
    with st as ls:
        S1A = sb(nc, ls, "eS1A", [128, NCH, 8], F32)
        S2A = sb(nc, ls, "eS2A", [128, NCH, 8], F32)
        POS = sb(nc, ls, "ePOS", [128, NCH, 8], F32)
        GW = sb(nc, ls, "eGW", [128, NCH, 2], F32)
        base = sb(nc, ls, "ebase", [128, 8], F32)
        D1f = sb(nc, ls, "eD1f", [128, NCH], F32)
        D2f = sb(nc, ls, "eD2f", [128, NCH], F32)
        D1 = sb(nc, ls, "eD1", [128, NCH], U32)
        D2 = sb(nc, ls, "eD2", [128, NCH], U32)
        WIf = sb(nc, ls, "eWIf", [128, NB, NU], F32)
        WI = sb(nc, ls, "eWI", [128, NB, NU], U32)
        sul = sb(nc, ls, "esul", [128, 128], F32)
        thr = sb(nc, ls, "ethr", [128, 17], F32)
        bst = sb(nc, ls, "ebst", [128, NB], F32)
        iop = sb(nc, ls, "eiop", [128, NU], F32)
        wrt = sb(nc, ls, "ewr", [128, 8, 8], F32)
        sel = sb(nc, ls, "esel", [8, 8, 128], F32)
        fgB = sb(nc, ls, "efgB", [128, 1024], F32)
        g2b = sb(nc, ls, "eg2b", [128, 1024], F32)
        P.op("sp", lambda e: e.dma_start(out=sul[:], in_=d["sul"][:, :]), w=["esul"], dma="e_sul")
        P.op("sp", lambda e: e.dma_start(out=thr[:], in_=d["thr"][:, :]), w=["ethr"], dma="e_thr")
        P.op("sp", lambda e: e.dma_start(out=bst[:], in_=d["bstart"][:, :]), w=["ebst"], dma="e_bst")
        P.op("sp", lambda e: e.dma_start(out=iop[:], in_=d["iotaU"][:, :]), w=["eiop"], dma="e_iop")
        P.op("sp", lambda e: e.dma_start(out=wrt[:], in_=d["moe_w_router"].rearrange("(k p) e -> p k e", p=128)), w=["ewr"], dma="e_wr")
        P.op("sp", lambda e: e.dma_start(out=sel[:], in_=d["sel"][:, :, :]), w=["esel"], dma="e_sel")
        P.op("sp", lambda e: e.dma_start(out=fgB[:], in_=d["final_gB"][:, :]), w=["efgB"], dma="e_fgB")
        P.op("dve", lambda e: e.memset(base[:], 0.0), w=["ebase"])
        hrow_v = d["Hrow"].rearrange("(n p) f -> p n f", p=128)
        with ExitStack() as l2:
            xt = [sb(nc, l2, "ext%d" % i, [128, 8, 512], F32) for i in range(2)]
            hT = sb(nc, l2, "ehT", [128, 8, 512], BF16)
            h32 = sb(nc, l2, "eh32", [128, 8, 512], F32)
            sq = sb(nc, l2, "esq", [128, 8, 512], BF16)
            tmp = [sb(nc, l2, "etmp%d" % i, [128, 512], F32) for i in range(2)]
            sqv = sb(nc, l2, "esqv", [128, 512], F32)
            lg = sb(nc, l2, "elg", [128, 8], F32)
            mx = sb(nc, l2, "emx", [128, 8], F32)
            gd = sb(nc, l2, "egd", [128, 2], F32)
            Sel = sb(nc, l2, "eSel", [128, 8], F32)
            hrow = [sb(nc, l2, "ehrow%d" % i, [128, 1024], BF16) for i in range(2)]
            ps_x = l2.enter_context(nc.psum_tensor("epsx", [128, 512], F32))
            ps_r = l2.enter_context(nc.psum_tensor("epsr", [128, 512], F32))
            ps_t32 = l2.enter_context(nc.psum_tensor("epst", [128, 512], F32))
            ps_tR = ps_t32[:].bitcast(BF16)
            for ti in range(T // 512):
                b = ti % 2
                P.op("sp", lambda e, b=b, ti=ti: e.dma_start(out=xt[b][:], in_=xin_v[:, :, ti * 512:(ti + 1) * 512]), w=["ext%d" % b], dma="e_x%d" % b)
                norm_mod(C, xt[b][:], hT, ps_x, sq, tmp, sqv, mod, 3, "e", "ext%d" % b, "ehT", h32=h32)
                for c4 in range(4):
                    c = ti * 4 + c4
                    hb = c % 2
                    for k in range(8):
                        P.op("pe", lambda e, c4=c4, k=k: e.matmul(ps_r[:, 0:8], lhsT=h32[:, k, c4 * 128:(c4 + 1) * 128], rhs=wrt[:, k, :],
                                                                  start=(k == 0), stop=(k == 7)), r=[("h32", k), "ewr"], w=["epsr"])
                    P.op("dve", lambda e: e.tensor_copy(out=lg[:], in_=ps_r[:, 0:8]), r=["epsr"], w=["elg"])
                    P.op("dve", lambda e: e.max(out=mx[:], in_=lg[:]), r=["elg"], w=["emx"])
                    P.op("dve", lambda e: e.tensor_tensor(out=gd[:, 0:1], in0=mx[:, 0:1], in1=mx[:, 1:2], op=ALU.subtract), r=["emx"], w=["egd"])
                    P.op("act", lambda e, c=c: e.activation(out=GW[:, c, 0:1], in_=gd[:, 0:1], func=AF.Sigmoid), r=["egd"], w=[("eGW", c)])
                    P.op("dve", lambda e, c=c: e.tensor_scalar(out=GW[:, c, 1:2], in0=GW[:, c, 0:1], scalar1=-1.0, scalar2=1.0, op0=ALU.mult, op1=ALU.add),
                         r=[("eGW", c)], w=[("eGW", c)])
                    P.op("dve", lambda e, c=c: e.tensor_scalar(out=S1A[:, c, :], in0=lg[:], scalar1=mx[:, 0:1], scalar2=None, op0=ALU.is_equal),
                         r=["elg", "emx"], w=[("eS1A", c)])
                    P.op("dve", lambda e, c=c: e.tensor_scalar(out=S2A[:, c, :], in0=lg[:], scalar1=mx[:, 1:2], scalar2=None, op0=ALU.is_equal),
                         r=["elg", "emx"], w=[("eS2A", c)])
                    P.op("dve", lambda e, c=c: e.tensor_tensor(out=Sel[:], in0=S1A[:, c, :], in1=S2A[:, c, :], op=ALU.add),
                         r=[("eS1A", c), ("eS2A", c)], w=["eSel"])
                    P.op("pe", lambda e: e.matmul(ps_r[:, 8:16], lhsT=sul[:], rhs=Sel[:], start=True, stop=True), r=["esul", "eSel"], w=["epsr"])
                    P.op("pe", lambda e: e.matmul(ps_r[:, 16:24], lhsT=C.ones_f[:], rhs=Sel[:], start=True, stop=True), r=["ones_f", "eSel"], w=["epsr"])
                    P.op("dve", lambda e, c=c: e.tensor_tensor(out=POS[:, c, :], in0=ps_r[:, 8:16], in1=base[:], op=ALU.add), r=["epsr", "ebase"], w=[("ePOS", c)])
                    P.op("dve", lambda e: e.tensor_tensor(out=base[:], in0=ps_r[:, 16:24], in1=base[:], op=ALU.add), r=["epsr", "ebase"], w=["ebase"])
                    for k in range(8):
                        P.op("pe", lambda e, k=k, c4=c4: e.transpose(out=ps_tR[:, k * 128:(k + 1) * 128], in_=hT[:, k, c4 * 128:(c4 + 1) * 128], identity=C.ident_bf[:]),
                             r=["ehT", "ident_bf"], w=["epst"])
                    P.op("act", lambda e, hb=hb: e.activation(out=hrow[hb][:], in_=ps_tR[:, 0:1024], func=AF.Copy), r=["epst"], w=["ehrow%d" % hb])
                    P.op("sp", lambda e, hb=hb, c=c: e.dma_start(out=hrow_v[:, c, :], in_=hrow[hb][:]), r=["ehrow%d" % hb], w=["Hrow"], dma="e_ho%d" % hb)
            cmp = sb(nc, l2, "ecmp", [128, 32], F32)
            nbk = sb(nc, l2, "enbk", [128, 8], F32)
            pend = sb(nc, l2, "epend", [128, 8], F32)
            pstart = sb(nc, l2, "epstart", [128, 8], F32)
            bexp = sb(nc, l2, "ebexp", [128, NB], F32)
            ptmp = sb(nc, l2, "eptmp", [128, NCH, 8], F32)
            for e_ in range(8):
                P.op("dve", lambda e, e_=e_: e.tensor_scalar(out=cmp[:, 0:17], in0=thr[:], scalar1=base[:, e_:e_ + 1], scalar2=None, op0=ALU.is_lt),
                     r=["ethr", "ebase"], w=["ecmp"])
                P.op("dve", lambda e, e_=e_: e.tensor_reduce(out=nbk[:, e_:e_ + 1], in_=cmp[:, 0:17], axis=AX.X, op=ALU.add), r=["ecmp"], w=["enbk"])
            P.op("dve", lambda e: e.tensor_scalar(out=nbk[:], in0=nbk[:], scalar1=float(BLK), scalar2=None, op0=ALU.mult), r=["enbk"], w=["enbk"])
            P.op("dve", lambda e: e.tensor_copy(out=pend[:, 0:1], in_=nbk[:, 0:1]), r=["enbk"], w=["epend"])
            for e_ in range(1, 8):
                P.op("dve", lambda e, e_=e_: e.tensor_tensor(out=pend[:, e_:e_ + 1], in0=pend[:, e_ - 1:e_], in1=nbk[:, e_:e_ + 1], op=ALU.add),
                     r=["epend", "enbk"], w=["epend"])
            P.op("dve", lambda e: e.tensor_tensor(out=pstart[:], in0=pend[:], in1=nbk[:], op=ALU.subtract), r=["epend", "enbk"], w=["epstart"])
            P.op("dve", lambda e: e.memset(bexp[:], 0.0), w=["ebexp"])
            for e_ in range(8):
                P.op("dve", lambda e, e_=e_: e.tensor_scalar(out=cmp[:, 0:NB], in0=bst[:], scalar1=pend[:, e_:e_ + 1], scalar2=None, op0=ALU.is_ge),
                     r=["ebst", "epend"], w=["ecmp"])
                P.op("dve", lambda e: e.tensor_tensor(out=bexp[:], in0=bexp[:], in1=cmp[:, 0:NB], op=ALU.add), r=["ebexp", "ecmp"], w=["ebexp"])
            P.op("dve", lambda e: e.tensor_scalar(out=bexp[:], in0=bexp[:], scalar1=float(N_EXP - 1), scalar2=float(NU * 128), op0=ALU.min, op1=ALU.mult),
                 r=["ebexp"], w=["ebexp"])
            P.op("dve", lambda e: e.tensor_tensor(out=WIf[:], in0=bexp[:].unsqueeze(2).broadcast_to([128, NB, NU]),
                                                  in1=iop[:].unsqueeze(1).broadcast_to([128, NB, NU]), op=ALU.add), r=["ebexp", "eiop"], w=["eWIf"])
            P.op("dve", lambda e: e.tensor_copy(out=WI[:], in_=WIf[:]), r=["eWIf"], w=["eWI"])
            P.op("dve", lambda e: e.tensor_tensor(out=ptmp[:], in0=POS[:], in1=pstart[:].unsqueeze(1).broadcast_to([128, NCH, 8]), op=ALU.add),
                 r=["ePOS", "epstart"], w=["eptmp"])
            for (SA, Df, Du, nm_) in ((S1A, D1f, D1, "1"), (S2A, D2f, D2, "2")):
                P.op("dve", lambda e, SA=SA: e.tensor_tensor(out=SA[:], in0=SA[:], in1=ptmp[:], op=ALU.mult), r=["eS1A", "eS2A", "eptmp"], w=["eS%sA" % nm_])
                P.op("dve", lambda e, SA=SA, Df=Df: e.tensor_reduce(out=Df[:], in_=SA[:], axis=AX.X, op=ALU.add), r=["eS%sA" % nm_], w=["eD%sf" % nm_])
                P.op("dve", lambda e, Df=Df: e.tensor_scalar(out=Df[:], in0=Df[:], scalar1=float(NR - 1), scalar2=None, op0=ALU.min),
                     r=["eD%sf" % nm_], w=["eD%sf" % nm_])
                P.op("dve", lambda e, Df=Df, Du=Du: e.tensor_copy(out=Du[:], in_=Df[:]), r=["eD%sf" % nm_], w=["eD%s" % nm_])
            zrow = sb(nc, l2, "ezrow", [128, 4, 1024], BF16)
            P.op("pool", lambda e: e.memset(zrow[:], 0.0), w=["ezrow"])
            xs_z = d["Xs"].rearrange("(n p) f -> p n f", p=128)
            for i in range(NB):
                P.op("sp", lambda e, i=i: e.dma_start(out=xs_z[:, i * 4:(i + 1) * 4, :], in_=zrow[:]), r=["ezrow"], w=[("Xz", i)], dma="e_xz%d" % (i % 4))
            hrow4 = hrow + [sb(nc, l2, "ehrow%d" % i, [128, 1024], BF16) for i in (2, 3)]
            for c in range(NCH if "noS" not in DBG else 0):
                hb = c % 4
                P.op("sp", lambda e, hb=hb, c=c: e.dma_start(out=hrow4[hb][:], in_=hrow_v[:, c, :]), r=["Hrow"], w=["ehrow%d" % hb], dma="e_hi%d" % hb)
                for (Du, nm_) in ((D1, "1"), (D2, "2")):
                    P.op("pool", lambda e, hb=hb, c=c, Du=Du: e.indirect_dma_start(
                        out=d["Xs"][:, :], out_offset=bass.IndirectOffsetOnAxis(ap=Du[:, c:c + 1], axis=0), in_=hrow4[hb][:], in_offset=None),
                        r=["ehrow%d" % hb, "eD%s" % nm_, "Xz"], w=[("Xsc", 2 * c + int(nm_))], dma="e_sc%s%d" % (nm_, hb))
        P.barrier()
        with ExitStack() as l2:
            g2T = sb(nc, l2, "eg2T", [8, 128], F32)
            pg = [l2.enter_context(nc.psum_tensor("epg%d" % i, [128, 512], F32)) for i in range(2)]
            P.op("pe", lambda e: e.transpose(out=pg[0][0:8, 0:128], in_=mod[:, g0:g0 + 8], identity=C.ident_f[:]), r=["ident_f"], w=["epg0"])
            P.op("act", lambda e: e.activation(out=g2T[:], in_=pg[0][0:8, 0:128], func=AF.Copy), r=["epg0"], w=["eg2T"])
            for k in range(8):
                P.op("pe", lambda e, k=k: e.matmul(pg[k // 4][:, (k % 4) * 128:(k % 4 + 1) * 128], lhsT=sel[:, k, :], rhs=g2T[:], start=True, stop=True),
                     r=["esel", "eg2T"], w=["epg%d" % (k // 4)])
            for hf in range(2):
                P.op("act", lambda e, hf=hf: e.activation(out=g2b[:, hf * 512:(hf + 1) * 512], in_=pg[hf][:], func=AF.Copy), r=["epg%d" % hf], w=["eg2b"])
            P.op("dve", lambda e: e.tensor_copy(out=g2T[:], in_=g2T[:]), r=["eg2b", "eg2T"], w=["eg2T"])
        P.barrier()
        with ExitStack() as l2:
            xrow = [sb(nc, l2, "exrow%d" % i, [128, 4, 1024], BF16) for i in range(2)]
            XT = sb(nc, l2, "eXT", [128, 8, 512], BF16)
            acc = sb(nc, l2, "eacc", [128, 8, 512], F32)
            yrow = sb(nc, l2, "eyrow", [128, 4, 1024], F32)
            wgb = [sb(nc, l2, "ewg%d" % i, [128, 8, HG * 128], BF16) for i in range(2)]
            wub = [sb(nc, l2, "ewu%d" % i, [128, 8, HG * 128], BF16) for i in range(2)]
            wdb = [sb(nc, l2, "ewd%d" % i, [128, HG, 1024], BF16) for i in range(2)]
            aT = [sb(nc, l2, "eaT%d" % i, [128, HG, 512], BF16) for i in range(2)]
            sg = [sb(nc, l2, "esg%d" % i, [128, 512], F32) for i in range(2)]
            ps_g = [l2.enter_context(nc.psum_tensor("epsg%d" % i, [128, 512], F32)) for i in range(2)]
            ps_u = [l2.enter_context(nc.psum_tensor("epsu%d" % i, [128, 512], F32)) for i in range(2)]
            ps_d = [l2.enter_context(nc.psum_tensor("epsd%d" % i, [128, 512], F32)) for i in range(2)]
            ps_t = [l2.enter_context(nc.psum_tensor("epstt%d" % i, [128, 512], F32)) for i in range(2)]
            ps_tb = [p_[:].bitcast(BF16) for p_ in ps_t]
            xs_v = d["Xs"].rearrange("(n p) f -> p n f", p=128)
            ys_v = d["Ys"].rearrange("(n p) f -> p n f", p=128)
            wgv = [d["moe_wg_r0"], d["moe_wg_r1"]]
            wuv = [d["moe_wu_r0"], d["moe_wu_r1"]]
            wdv = [d["moe_wd_r0"], d["moe_wd_r1"]]
            ug = 0
            tcnt = 0
            pend_dn = [None]
            for i in range(NB if "noE" not in DBG else 0):
                xb_ = i % 2
                P.op("sp", lambda e, xb_=xb_, i=i: e.dma_start(out=xrow[xb_][:], in_=xs_v[:, i * 4:(i + 1) * 4, :]), r=["Xs"], w=["exrow%d" % xb_], dma="e_xr%d" % xb_)
                for k in range(8):
                    tb = tcnt % 2
                    tcnt += 1
                    for n in range(4):
                        P.op("pe", lambda e, tb=tb, n=n, k=k, xb_=xb_: e.transpose(out=ps_tb[tb][:, n * 128:(n + 1) * 128], in_=xrow[xb_][:, n, k * 128:(k + 1) * 128],
                                                                                identity=C.ident_bf[:]), r=["exrow%d" % xb_, "ident_bf"], w=["epstt%d" % tb])
                    P.op("act" if k % 2 else "dve", (lambda e, tb=tb, k=k: e.activation(out=XT[:, k, :], in_=ps_tb[tb][:, 0:512], func=AF.Copy)) if k % 2 else
                         (lambda e, tb=tb, k=k: e.tensor_copy(out=XT[:, k, :], in_=ps_tb[tb][:, 0:512])), r=["epstt%d" % tb], w=[("eXT", k)])
                for uu in range(NU):
                    b = ug % 2
                    ug += 1
                    for hf in range(2):
                        P.op("pool", lambda e, b=b, i=i, uu=uu, hf=hf: e.indirect_dma_start(
                            out=wgb[b][:, hf * 4:(hf + 1) * 4, :].rearrange("p k f -> p (k f)"), out_offset=None, in_=wgv[hf][:, :],
                            in_offset=bass.IndirectOffsetOnAxis(ap=WI[:, i, uu:uu + 1], axis=0)),
                            r=["eWI"], w=[("ewg%d" % b, hf)], dma="e_wg%d%d" % (b, hf))
                        P.op("pool", lambda e, b=b, i=i, uu=uu, hf=hf: e.indirect_dma_start(
                            out=wub[b][:, hf * 4:(hf + 1) * 4, :].rearrange("p k f -> p (k f)"), out_offset=None, in_=wuv[hf][:, :],
                            in_offset=bass.IndirectOffsetOnAxis(ap=WI[:, i, uu:uu + 1], axis=0)),
                            r=["eWI"], w=[("ewu%d" % b, hf)], dma="e_wu%d%d" % (b, hf))
                        P.op("pool", lambda e, b=b, i=i, uu=uu, hf=hf: e.indirect_dma_start(
                            out=wdb[b][:, hf * 2:(hf + 1) * 2, :].rearrange("p k f -> p (k f)"), out_offset=None, in_=wdv[hf][:, :],
                            in_offset=bass.IndirectOffsetOnAxis(ap=WI[:, i, uu:uu + 1], axis=0)),
                            r=["eWI"], w=[("ewd%d" % b, hf)], dma="e_wd%d%d" % (b, hf))
                    ab = ug % 2
                    for j in range(HG):
                        pb = j % 2
                        for k in range(8):
                            P.op("pe", lambda e, b=b, pb=pb, j=j, k=k: e.matmul(ps_g[pb][:], lhsT=wgb[b][:, k, j * 128:(j + 1) * 128], rhs=XT[:, k, :],
                                                                                start=(k == 0), stop=(k == 7)), r=["ewg%d" % b, "eXT"], w=["epsg%d" % pb])
                        for k in range(8):
                            P.op("pe", lambda e, b=b, pb=pb, j=j, k=k: e.matmul(ps_u[pb][:], lhsT=wub[b][:, k, j * 128:(j + 1) * 128], rhs=XT[:, k, :],
                                                                                start=(k == 0), stop=(k == 7)), r=["ewu%d" % b, "eXT"], w=["epsu%d" % pb])
                        P.op("act", lambda e, pb=pb: e.activation(out=sg[pb][:], in_=ps_g[pb][:], func=AF.Silu), r=["epsg%d" % pb], w=["esg%d" % pb])
                        P.op("dve", lambda e, pb=pb, ab=ab, j=j: e.tensor_tensor(out=aT[ab][:, j, :], in0=sg[pb][:], in1=ps_u[pb][:], op=ALU.mult),
                             r=["esg%d" % pb, "epsu%d" % pb], w=[("eaT%d" % ab, j)])
                    def _down(b=b, ab=ab, uu=uu):
                        for f in range(8):
                            db = f % 2
                            for j in range(HG):
                                P.op("pe", lambda e, b=b, db=db, j=j, f=f, ab=ab: e.matmul(ps_d[db][:], lhsT=wdb[b][:, j, f * 128:(f + 1) * 128], rhs=aT[ab][:, j, :],
                                                                                           start=(j == 0), stop=(j == HG - 1)), r=["ewd%d" % b, ("eaT%d" % ab, j)], w=["epsd%d" % db])
                            if uu == 0:
                                P.op("act", lambda e, db=db, f=f: e.activation(out=acc[:, f, :], in_=ps_d[db][:], func=AF.Copy), r=["epsd%d" % db], w=[("eacc", f)])
                            else:
                                P.op("dve", lambda e, db=db, f=f: e.tensor_tensor(out=acc[:, f, :], in0=ps_d[db][:], in1=acc[:, f, :], op=ALU.add),
                                     r=["epsd%d" % db, ("eacc", f)], w=[("eacc", f)])
                    if pend_dn[0] is not None:
                        pend_dn[0]()
                    pend_dn[0] = _down
                pend_dn[0]()
                pend_dn[0] = None
                for n in range(4):
                    for hf in range(2):
                        tb = tcnt % 2
                        tcnt += 1
                        for kk in range(4):
                            k = hf * 4 + kk
                            P.op("pe", lambda e, tb=tb, kk=kk, k=k, n=n: e.transpose(out=ps_t[tb][:, kk * 128:(kk + 1) * 128], in_=acc[:, k, n * 128:(n + 1) * 128],
                                                                                     identity=C.ident_f[:]), r=[("eacc", k), "ident_f"], w=["epstt%d" % tb])
                        P.op("act" if hf else "dve", (lambda e, tb=tb, n=n, hf=hf: e.activation(out=yrow[:, n, hf * 512:(hf + 1) * 512], in_=ps_t[tb][:], func=AF.Copy)) if hf else
                             (lambda e, tb=tb, n=n, hf=hf: e.tensor_copy(out=yrow[:, n, hf * 512:(hf + 1) * 512], in_=ps_t[tb][:])), r=["epstt%d" % tb], w=[("eyrow", n)])
                P.op("sp", lambda e, i=i: e.dma_start(out=ys_v[:, i * 4:(i + 1) * 4, :], in_=yrow[:]), r=["eyrow"], w=["Ys"], dma="e_yo")
        P.barrier()
        with ExitStack() as l2:
            xc = [sb(nc, l2, "exc%d" % i, [128, 8, 128], F32) for i in range(2)]
            xr = [sb(nc, l2, "exr%d" % i, [128, 1024], F32) for i in range(2)]
            y1 = [sb(nc, l2, "ey1%d" % i, [128, 1024], F32) for i in range(2)]
            y2 = [sb(nc, l2, "ey2%d" % i, [128, 1024], F32) for i in range(2)]
            junk = sb(nc, l2, "ejunk", [128, 1024], F32)
            ss = sb(nc, l2, "ess", [128, 2], F32)
            pc = [l2.enter_context(nc.psum_tensor("epc%d" % i, [128, 512], F32)) for i in range(4)]
            out_v = d["out_tok"].rearrange("(n p) f -> p n f", p=128)
            for c in range(NCH if "noC" not in DBG else 0):
                b = c % 2
                P.op("sp", lambda e, b=b, c=c: e.dma_start(out=xc[b][:], in_=xin_v[:, :, c * 128:(c + 1) * 128]), w=["exc%d" % b], dma="e_xc%d" % b)
                P.op("pool", lambda e, b=b, c=c: e.indirect_dma_start(out=y1[b][:], out_offset=None, in_=d["Ys"][:, :],
                                                                      in_offset=bass.IndirectOffsetOnAxis(ap=D1[:, c:c + 1], axis=0)),
                     r=["Ys", "eD1"], w=["ey1%d" % b], dma="e_g1%d" % b)
                P.op("pool", lambda e, b=b, c=c: e.indirect_dma_start(out=y2[b][:], out_offset=None, in_=d["Ys"][:, :],
                                                                      in_offset=bass.IndirectOffsetOnAxis(ap=D2[:, c:c + 1], axis=0)),
                     r=["Ys", "eD2"], w=["ey2%d" % b], dma="e_g2%d" % b)
                for hf in range(2):
                    pcb = pc[(c % 2) * 2 + hf]
                    pk = "epc%d" % ((c % 2) * 2 + hf)
                    for kk in range(4):
                        k = hf * 4 + kk
                        P.op("pe", lambda e, pcb=pcb, kk=kk, k=k, b=b: e.transpose(out=pcb[:, kk * 128:(kk + 1) * 128], in_=xc[b][:, k, :], identity=C.ident_f[:]),
                             r=["exc%d" % b, "ident_f"], w=[pk])
                    P.op("act", lambda e, pcb=pcb, hf=hf, b=b: e.activation(out=xr[b][:, hf * 512:(hf + 1) * 512], in_=pcb[:], func=AF.Copy), r=[pk], w=[("exr%d" % b, hf)])
                P.op("dve", lambda e, b=b, c=c: e.tensor_scalar(out=y1[b][:], in0=y1[b][:], scalar1=GW[:, c, 0:1], scalar2=None, op0=ALU.mult),
                     r=["ey1%d" % b, "eGW"], w=["ey1%d" % b])
                P.op("dve", lambda e, b=b, c=c: e.scalar_tensor_tensor(out=y1[b][:], in0=y2[b][:], scalar=GW[:, c, 1:2], in1=y1[b][:], op0=ALU.mult, op1=ALU.add),
                     r=["ey1%d" % b, "ey2%d" % b, "eGW"], w=["ey1%d" % b])
                P.op("pool", lambda e, b=b: e.tensor_tensor(out=y1[b][:], in0=y1[b][:], in1=g2b[:], op=ALU.mult), r=["ey1%d" % b, "eg2b"], w=["ey1%d" % b])
                P.op("dve", lambda e, b=b: e.tensor_tensor(out=xr[b][:], in0=xr[b][:], in1=y1[b][:], op=ALU.add), r=["exr%d" % b, "ey1%d" % b], w=["exr%d" % b])
                P.op("act", lambda e, b=b: e.activation(out=junk[:], in_=xr[b][:], func=AF.Square, accum_out=ss[:, 0:1]), r=["exr%d" % b], w=["ejunk", "ess"])
                P.op("act", lambda e: e.activation(out=ss[:, 1:2], in_=ss[:, 0:1], func=AF.Sqrt, bias=C.eps_t[:, 0:1], scale=1.0 / D), r=["ess"], w=["ess"])
                P.op("dve", lambda e: e.reciprocal(out=ss[:, 1:2], in_=ss[:, 1:2]), r=["ess"], w=["ess"])
                P.op("dve", lambda e, b=b: e.scalar_tensor_tensor(out=xr[b][:], in0=xr[b][:], scalar=ss[:, 1:2], in1=fgB[:], op0=ALU.mult, op1=ALU.mult),
                     r=["exr%d" % b, "ess", "efgB"], w=["exr%d" % b])
                P.op("sp", lambda e, b=b, c=c: e.dma_start(out=out_v[:, c, :], in_=xr[b][:]), r=["exr%d" % b], w=["out_tok"], dma="e_oo%d" % b)
        P.barrier()
```

```python
from contextlib import ExitStack
import numpy as np
import ml_dtypes
import concourse.bass as bass
import concourse.mybir as mybir
from concourse.bass_utils import run_bass_kernel_spmd

F32 = mybir.dt.float32
BF16 = mybir.dt.bfloat16
AF = mybir.ActivationFunctionType
ALU = mybir.AluOpType
AX = mybir.AxisListType

D = 1024
KC = 8
EPS = 1e-6
FFN_DIM = 2816
N_EXP = 8
EXP_DIM = 3584

ENGS = ["pe", "act", "dve", "pool", "sp"]


def _split(key):
    if isinstance(key, tuple):
        return key[0], key[1]
    return key, None


class Prog:
    def __init__(self, nc, stack):
        self.nc = nc
        self.stack = stack
        self.ops = {e: [] for e in ENGS}
        self.res = {}
        self.dma_cnt = []
        self.key2phys = {}
        self.free_phys = {True: [], False: []}
        self.phys_sw = []
        self.base = {}

    @staticmethod
    def _merge(d, ev):
        k = (ev[0], ev[1])
        if d.get(k, -1) < ev[2]:
            d[k] = ev[2]

    def _collect(self, deps, reads, writes):
        for key in reads:
            name, idx = _split(key)
            ent = self.res.get(name)
            if ent is None:
                continue
            for (i, ev) in ent["w"]:
                if i is None or idx is None or i == idx:
                    self._merge(deps, ev)
        for key in writes:
            name, idx = _split(key)
            ent = self.res.get(name)
            if ent is None:
                continue
            for (i, ev) in ent["w"] + ent["r"]:
                if i is None or idx is None or i == idx:
                    self._merge(deps, ev)

    def _update(self, ev, reads, writes):
        for key in reads:
            name, idx = _split(key)
            ent = self.res.setdefault(name, {"w": [], "r": []})
            ent["r"] = [(i, e) for (i, e) in ent["r"]
                        if not (i == idx and e[0] == ev[0] and e[1] == ev[1])]
            ent["r"].append((idx, ev))
        for key in writes:
            name, idx = _split(key)
            ent = self.res.setdefault(name, {"w": [], "r": []})
            if idx is None:
                ent["w"] = [(None, ev)]
                ent["r"] = []
            else:
                ent["w"] = [(i, e) for (i, e) in ent["w"] if i != idx]
                ent["w"].append((idx, ev))
                ent["r"] = [(i, e) for (i, e) in ent["r"] if i != idx]

    def op(self, eng, fn, r=(), w=(), dma=None):
        deps = dict(self.base)
        self._collect(deps, r, w)
        seq = len(self.ops[eng])
        if dma is not None:
            ph = self.key2phys.get(dma)
            if ph is None:
                sw = (eng == "pool")
                if self.free_phys[sw]:
                    ph = self.free_phys[sw].pop()
                else:
                    ph = len(self.dma_cnt)
                    self.dma_cnt.append(0)
                    self.phys_sw.append(sw)
                self.key2phys[dma] = ph
            self.dma_cnt[ph] += 1
            ev = ("d", ph, self.dma_cnt[ph])
        else:
            ev = ("c", eng, seq)
        o = {"fn": fn, "deps": deps, "ev": ev, "signal": False}
        self.ops[eng].append(o)
        self._update(ev, r, w)
        return o

    def barrier(self):
        fr = {}
        for e in ENGS:
            for s in range(len(self.ops[e]) - 1, -1, -1):
                if self.ops[e][s]["ev"][0] == "c":
                    fr[("c", e)] = s
                    break
        for k, c in enumerate(self.dma_cnt):
            fr[("d", k)] = c
        self.base = fr
        self.res = {}
        self.key2phys = {}
        self.free_phys = {True: [i for i in range(len(self.dma_cnt)) if self.phys_sw[i]],
                          False: [i for i in range(len(self.dma_cnt)) if not self.phys_sw[i]]}

    def emit(self, final_wait_keys=()):
        nc = self.nc
        for e in ENGS:
            for o in self.ops[e]:
                for (kind, src), val in o["deps"].items():
                    if kind == "c" and not (src == "pe" and e == "pe"):
                        self.ops[src][val]["signal"] = True
        for e in ENGS:
            cnt = 0
            for o in self.ops[e]:
                if o["signal"]:
                    cnt += 1
                o["sigval"] = cnt
        sems = {}
        for e in ENGS:
            sems[("c", e)] = self.stack.enter_context(nc.semaphore("s_" + e))
        for k in range(len(self.dma_cnt)):
            sems[("d", k)] = self.stack.enter_context(nc.semaphore("d_" + str(k)))
        block = self.stack.enter_context(nc.Block())
        prog = self

        def run(eng_name, handle):
            waited = {}
            for o in prog.ops[eng_name]:
                for (kind, src), val in o["deps"].items():
                    if kind == "c":
                        if src == "pe" and eng_name == "pe":
                            continue
                        target = prog.ops[src][val]["sigval"]
                    else:
                        target = 16 * val
                    if waited.get((kind, src), 0) >= target:
                        continue
                    waited[(kind, src)] = target
                    handle.wait_ge(sems[(kind, src)], target)
                ins = o["fn"](handle)
                if o["ev"][0] == "d":
                    ins.then_inc(sems[("d", o["ev"][1])], 16)
                elif o["signal"]:
                    ins.then_inc(sems[("c", eng_name)], 1)
            if eng_name == "sp":
                for k in final_wait_keys:
                    handle.wait_ge(sems[("d", k)], 16 * prog.dma_cnt[k])

        @block.tensor
        def _(h):
            run("pe", h)

        @block.scalar
        def _(h):
            run("act", h)

        @block.vector
        def _(h):
            run("dve", h)

        @block.gpsimd
        def _(h):
            run("pool", h)

        @block.sync
        def _(h):
            run("sp", h)


class Ctx:
    pass


def sb(nc, st, name, shape, dt):
    return st.enter_context(nc.sbuf_tensor("sb_" + name, list(shape), dt))


def stage_consts(C):
    nc, P = C.nc, C.P
    st = C.top
    C.ident_bf = sb(nc, st, "ident_bf", [128, 128], BF16)
    C.ident_f = sb(nc, st, "ident_f", [128, 128], F32)
    C.ones_bf = sb(nc, st, "ones_bf", [128, 128], BF16)
    C.ones_f = sb(nc, st, "ones_f", [128, 128], F32)
    P.op("pool", lambda e: e.dma_start(out=C.ident_bf[:], in_=C.d["ident"][:, :]), w=["ident_bf"], dma="c0")
    P.op("sp", lambda e: e.dma_start(out=C.ident_f[:], in_=C.d["ident"][:, :]), w=["ident_f"], dma="c1")
    P.op("dve", lambda e: e.memset(C.ones_bf[:], 1.0), w=["ones_bf"])
    P.op("dve", lambda e: e.memset(C.ones_f[:], 1.0), w=["ones_f"])
    C.mod = [sb(nc, st, "mod%d" % l, [128, 48], F32) for l in range(2)]
    ada_w_aps = [C.d["ada_w0"], C.d["ada_w1"]]
    ada_b_aps = [C.d["ada_bT0"], C.d["ada_bT1"]]
    with ExitStack() as ls:
        cT = sb(nc, ls, "cT", [128, 8], F32)
        s2 = sb(nc, ls, "s2", [128, 8, 2], F32)
        wb = [sb(nc, ls, "adaw%d" % i, [128, 8, 512], F32) for i in range(2)]
        bT = sb(nc, ls, "adab", [128, 48], F32)
        ps = ls.enter_context(nc.psum_tensor("ps_mod", [128, 48, 2], F32))
        P.op("sp", lambda e: e.dma_start(out=cT[:], in_=C.d["cT"][:, :]), w=["cT"], dma="c2")
        P.op("act", lambda e: e.activation(out=s2[:, :, 0], in_=cT[:], func=AF.Silu), r=["cT"], w=["s2"])
        P.op("act", lambda e: e.activation(out=s2[:, :, 1], in_=cT[:], func=AF.Silu), r=["cT"], w=["s2"])
        for l in range(2):
            mod = C.mod[l]
            aw = ada_w_aps[l].rearrange("(k p) f -> p k f", p=128)
            P.op("sp", lambda e, l=l: e.dma_start(out=bT[:], in_=ada_b_aps[l][:, :]), w=["adab"], dma="c3")
            for cb in range(12):
                buf = wb[cb % 2]
                bk = "adaw%d" % (cb % 2)
                P.op("sp", lambda e, buf=buf, aw=aw, cb=cb: e.dma_start(out=buf[:], in_=aw[:, :, cb * 512:(cb + 1) * 512]),
                     w=[bk], dma="aw%d" % (cb % 2))
                for fi in range(4):
                    f = cb * 4 + fi
                    for k in range(8):
                        P.op("pe", lambda e, buf=buf, f=f, fi=fi, k=k: e.matmul(
                            ps[:, f, :], lhsT=buf[:, k, fi * 128:(fi + 1) * 128], rhs=s2[:, k, :],
                            start=(k == 0), stop=(k == 7)), r=[bk, "s2"], w=["ps_mod"])
            P.op("dve", lambda e, mod=mod: e.tensor_tensor(out=mod[:], in0=ps[:, :, 0], in1=bT[:], op=ALU.add),
                 r=["ps_mod", "adab"], w=["mod%d" % l])
            for j in (1, 4):
                P.op("dve", lambda e, mod=mod, j=j: e.tensor_scalar(
                    out=mod[:, j * 8:(j + 1) * 8], in0=mod[:, j * 8:(j + 1) * 8], scalar1=1.0, scalar2=None,
                    op0=ALU.add), r=["mod%d" % l], w=["mod%d" % l])
        P.barrier()


def norm_mod(C, xsrc, hT, rs_ps, sq, tmp, sqv, mod, joff, nm, x_key, h_key, h32=None):
    nc, P = C.nc, C.P
    sh0, sc0 = joff * 8, (joff + 1) * 8
    P.op("act", lambda e: e.activation(out=sq[:], in_=xsrc, func=AF.Square), r=[x_key], w=[nm + "sq"])
    for k in range(8):
        P.op("pe", lambda e, k=k: e.matmul(rs_ps[:], lhsT=C.ones_bf[:], rhs=sq[:, k, :], start=(k == 0), stop=(k == 7)),
             r=[nm + "sq", "ones_bf"], w=[nm + "rs_ps"])
    P.op("act", lambda e: e.activation(out=sqv[:], in_=rs_ps[:], func=AF.Sqrt, bias=C.eps_t[:, 0:1], scale=1.0 / D),
         r=[nm + "rs_ps"], w=[nm + "sqv"])
    P.op("dve", lambda e: e.reciprocal(out=sqv[:], in_=sqv[:]), r=[nm + "sqv"], w=[nm + "sqv"])
    for k in range(8):
        t = tmp[k % 2]
        tk = nm + "tmp%d" % (k % 2)
        P.op("dve", lambda e, k=k, t=t: e.scalar_tensor_tensor(
            out=t[:], in0=xsrc[:, k, :], scalar=mod[:, sc0 + k:sc0 + k + 1], in1=sqv[:], op0=ALU.mult, op1=ALU.mult),
            r=[x_key, nm + "sqv"], w=[tk])
        P.op("act", lambda e, k=k, t=t: e.activation(out=hT[:, k, :], in_=t[:], func=AF.Identity,
                                                    bias=mod[:, sh0 + k:sh0 + k + 1], scale=1.0),
             r=[tk], w=[(h_key, k)])
        if h32 is not None:
            P.op("pool", lambda e, k=k, t=t: e.tensor_scalar(
                out=h32[:, k, :], in0=t[:], scalar1=mod[:, sh0 + k:sh0 + k + 1], scalar2=None, op0=ALU.add),
                r=[tk], w=[("h32", k)])


def stage_ffn(C, x_in, x_out, T, mod, wg, wu, wd, HID, nm, n_exp=1, wr=None):
    nc, P = C.nc, C.P
    HG = 4 if (HID // 128) % 4 == 0 else 2
    NT = 4 if T >= 2048 else T // 512
    n_super = T // (NT * 512)
    units_per_exp = HID // (HG * 128)
    assert HID % (HG * 128) == 0
    xin_v = x_in.rearrange("(k p) t -> p k t", p=128)
    xout_v = x_out.rearrange("(k p) t -> p k t", p=128)
    g0 = 5 * 8
    with ExitStack() as ls:
        acc = sb(nc, ls, nm + "acc", [128, NT, 8, 512], F32)
        hT = sb(nc, ls, nm + "hT", [128, NT, 8, 512], BF16)
        sq = sb(nc, ls, nm + "sq", [128, 8, 512], BF16)
        tmp = [sb(nc, ls, nm + "tmp%d" % i, [128, 512], F32) for i in range(2)]
        sqv = sb(nc, ls, nm + "sqv", [128, 512], F32)
        wgb = [sb(nc, ls, nm + "wg%d" % i, [128, 8, HG * 128], BF16) for i in range(2)]
        wub = [sb(nc, ls, nm + "wu%d" % i, [128, 8, HG * 128], BF16) for i in range(2)]
        wdb = [sb(nc, ls, nm + "wd%d" % i, [128, HG, 1024], BF16) for i in range(2)]
        aT = [sb(nc, ls, nm + "aT%d" % i, [128, HG, 512], BF16) for i in range(2)]
        sg = [sb(nc, ls, nm + "sg%d" % i, [128, 512], F32) for i in range(2)]
        ps_g = [ls.enter_context(nc.psum_tensor(nm + "psg%d" % i, [128, 512], F32)) for i in range(2)]
        ps_u = [ls.enter_context(nc.psum_tensor(nm + "psu%d" % i, [128, 512], F32)) for i in range(2)]
        ps_d = [ls.enter_context(nc.psum_tensor(nm + "psd%d" % i, [128, 512], F32)) for i in range(2)]
        ps_x = ls.enter_context(nc.psum_tensor(nm + "psx", [128, 512], F32))
        if n_exp > 1:
            h32 = sb(nc, ls, nm + "h32", [128, 8, 512], F32)
            wrt = sb(nc, ls, nm + "wr", [128, 8, 8], F32)
            GT = sb(nc, ls, nm + "GT", [8, NT * 512], F32)
            lg = sb(nc, ls, nm + "lg", [128, 8], F32)
            mx = sb(nc, ls, nm + "mx", [128, 8], F32)
            gw = sb(nc, ls, nm + "gw", [128, 4], F32)
            Gm = sb(nc, ls, nm + "Gm", [128, 8], F32)
            Gm2 = sb(nc, ls, nm + "Gm2", [128, 8], F32)
            sel = sb(nc, ls, nm + "sel", [8, 8, 128], F32)
            ps_gb = ls.enter_context(nc.psum_tensor(nm + "psgb", [128, 512], F32))
            P.op("sp", lambda e: e.dma_start(out=wrt[:], in_=wr.rearrange("(k p) e -> p k e", p=128)), w=["wr"], dma=nm + "wr")
            P.op("sp", lambda e: e.dma_start(out=sel[:], in_=C.d["sel"][:, :, :]), w=["sel"], dma=nm + "sel")
        pend_down = [None]
        for s_i in range(n_super):
            for ti in range(NT):
                t0 = (s_i * NT + ti) * 512
                P.op("sp", lambda e, ti=ti, t0=t0: e.dma_start(out=acc[:, ti], in_=xin_v[:, :, t0:t0 + 512]),
                     w=[("acc", ti)], dma=nm + "x%d" % ti)
                norm_mod(C, acc[:, ti], hT[:, ti], ps_x, sq, tmp, sqv, mod, 3, nm, ("acc", ti), "hT%d" % ti,
                         h32=(h32 if n_exp > 1 else None))
                if n_exp > 1:
                    for c4 in range(4):
                        for k in range(8):
                            P.op("pe", lambda e, c4=c4, k=k: e.matmul(
                                ps_gb[:, 0:8], lhsT=h32[:, k, c4 * 128:(c4 + 1) * 128], rhs=wrt[:, k, :],
                                start=(k == 0), stop=(k == 7)), r=[("h32", k), "wr"], w=["psgb"])
                        P.op("dve", lambda e: e.tensor_copy(out=lg[:], in_=ps_gb[:, 0:8]), r=["psgb"], w=["lg"])
                        P.op("dve", lambda e: e.max(out=mx[:], in_=lg[:]), r=["lg"], w=["mx"])
                        P.op("dve", lambda e: e.tensor_tensor(out=gw[:, 0:1], in0=mx[:, 0:1], in1=mx[:, 1:2], op=ALU.subtract),
                             r=["mx"], w=["gw"])
                        P.op("act", lambda e: e.activation(out=gw[:, 1:2], in_=gw[:, 0:1], func=AF.Sigmoid), r=["gw"], w=["gw"])
                        P.op("dve", lambda e: e.tensor_scalar(out=gw[:, 2:3], in0=gw[:, 1:2], scalar1=-1.0, scalar2=1.0,
                                                               op0=ALU.mult, op1=ALU.add), r=["gw"], w=["gw"])
                        P.op("dve", lambda e: e.tensor_scalar(out=Gm[:], in0=lg[:], scalar1=mx[:, 0:1], scalar2=gw[:, 1:2],
                                                               op0=ALU.is_equal, op1=ALU.mult), r=["lg", "mx", "gw"], w=["Gm"])
                        P.op("dve", lambda e: e.tensor_scalar(out=Gm2[:], in0=lg[:], scalar1=mx[:, 1:2], scalar2=gw[:, 2:3],
                                                               op0=ALU.is_equal, op1=ALU.mult), r=["lg", "mx", "gw"], w=["Gm2"])
                        P.op("dve", lambda e: e.tensor_tensor(out=Gm[:], in0=Gm[:], in1=Gm2[:], op=ALU.add),
                             r=["Gm", "Gm2"], w=["Gm"])
                        P.op("pe", lambda e: e.transpose(out=ps_gb[0:8, 128:256], in_=Gm[:], identity=C.ident_f[:]),
                             r=["Gm", "ident_f"], w=["psgb"])
                        P.op("dve", lambda e, ti=ti, c4=c4: e.tensor_copy(
                            out=GT[:, ti * 512 + c4 * 128: ti * 512 + (c4 + 1) * 128], in_=ps_gb[0:8, 128:256]),
                            r=["psgb"], w=[("GT", ti)])
            n_units = n_exp * units_per_exp
            for u in range(n_units):
                ex, uu = divmod(u, units_per_exp)
                b = u % 2
                h0 = uu * HG * 128
                wgs = (wg[ex] if n_exp > 1 else wg).rearrange("(k p) h -> p k h", p=128)
                wus = (wu[ex] if n_exp > 1 else wu).rearrange("(k p) h -> p k h", p=128)
                wds = (wd[ex] if n_exp > 1 else wd).rearrange("(j p) f -> p j f", p=128)
                P.op("pool", lambda e, b=b, wgs=wgs, h0=h0: e.dma_start(out=wgb[b][:], in_=wgs[:, :, h0:h0 + HG * 128]),
                     w=["wg%d" % b], dma=nm + "wg%d" % b)
                P.op("pool", lambda e, b=b, wus=wus, h0=h0: e.dma_start(out=wub[b][:], in_=wus[:, :, h0:h0 + HG * 128]),
                     w=["wu%d" % b], dma=nm + "wu%d" % b)
                P.op("pool", lambda e, b=b, wds=wds, uu=uu: e.dma_start(out=wdb[b][:], in_=wds[:, uu * HG:(uu + 1) * HG, :]),
                     w=["wd%d" % b], dma=nm + "wd%d" % b)
                for ti in range(NT):
                    ab = (u * NT + ti) % 2
                    if n_exp > 1:
                        for pp in range(1):
                            P.op("pe", lambda e, ex=ex, ti=ti: e.matmul(
                                ps_gb[:], lhsT=sel[:, ex, :], rhs=GT[:, ti * 512:(ti + 1) * 512], start=True, stop=True),
                                r=["sel", ("GT", ti)], w=["psgb"])
                    for j in range(HG):
                        pb = j % 2
                        for k in range(8):
                            P.op("pe", lambda e, b=b, pb=pb, j=j, k=k, ti=ti: e.matmul(
                                ps_g[pb][:], lhsT=wgb[b][:, k, j * 128:(j + 1) * 128], rhs=hT[:, ti, k, :],
                                start=(k == 0), stop=(k == 7)), r=["wg%d" % b, "hT%d" % ti], w=["psg%d" % pb])
                        for k in range(8):
                            P.op("pe", lambda e, b=b, pb=pb, j=j, k=k, ti=ti: e.matmul(
                                ps_u[pb][:], lhsT=wub[b][:, k, j * 128:(j + 1) * 128], rhs=hT[:, ti, k, :],
                                start=(k == 0), stop=(k == 7)), r=["wu%d" % b, "hT%d" % ti], w=["psu%d" % pb])
                        P.op("act", lambda e, pb=pb: e.activation(out=sg[pb][:], in_=ps_g[pb][:], func=AF.Silu),
                             r=["psg%d" % pb], w=["sg%d" % pb])
                        if n_exp > 1:
                            P.op("dve", lambda e, pb=pb: e.tensor_tensor(out=sg[pb][:], in0=sg[pb][:], in1=ps_u[pb][:], op=ALU.mult),
                                 r=["sg%d" % pb, "psu%d" % pb], w=["sg%d" % pb])
                            P.op("dve", lambda e, pb=pb, ab=ab, j=j: e.tensor_tensor(
                                out=aT[ab][:, j, :], in0=sg[pb][:], in1=ps_gb[:], op=ALU.mult),
                                r=["sg%d" % pb, "psgb"], w=[("aT%d" % ab, j)])
                        else:
                            P.op("dve", lambda e, pb=pb, ab=ab, j=j: e.tensor_tensor(
                                out=aT[ab][:, j, :], in0=sg[pb][:], in1=ps_u[pb][:], op=ALU.mult),
                                r=["sg%d" % pb, "psu%d" % pb], w=[("aT%d" % ab, j)])
                    def _down(b=b, ab=ab, ti=ti):
                        for f in range(8):
                            db = f % 2
                            for j in range(HG):
                                P.op("pe", lambda e, b=b, db=db, j=j, f=f, ab=ab: e.matmul(
                                    ps_d[db][:], lhsT=wdb[b][:, j, f * 128:(f + 1) * 128], rhs=aT[ab][:, j, :],
                                    start=(j == 0), stop=(j == HG - 1)), r=["wd%d" % b, ("aT%d" % ab, j)], w=["psd%d" % db])
                            P.op("dve", lambda e, db=db, f=f, ti=ti: e.scalar_tensor_tensor(
                                out=acc[:, ti, f, :], in0=ps_d[db][:], scalar=mod[:, g0 + f:g0 + f + 1], in1=acc[:, ti, f, :],
                                op0=ALU.mult, op1=ALU.add), r=["psd%d" % db, ("acc", ti)], w=[("acc", ti)])
                    if pend_down[0] is not None:
                        pend_down[0]()
                    pend_down[0] = _down
            if pend_down[0] is not None:
                pend_down[0]()
                pend_down[0] = None
            for ti in range(NT):
                t0 = (s_i * NT + ti) * 512
                P.op("sp", lambda e, ti=ti, t0=t0: e.dma_start(out=xout_v[:, :, t0:t0 + 512], in_=acc[:, ti]),
                     r=[("acc", ti)], w=[nm + "xout"], dma=nm + "xo%d" % ti)
        P.barrier()


LAMBDA_INIT = 0.8 - 0.6 * float(np.exp(-0.3 * 1))


def stage_qkv(C, x_in, S, mod):
    nc, P, d = C.nc, C.P, C.d
    xin_v = x_in.rearrange("(k p) t -> p k t", p=128)
    qv = d["QT"].rearrange("(c p) t -> p c t", p=128)
    kv = d["KT"].rearrange("(c p) t -> p c t", p=128)
    vv = d["V"].rearrange("(n p) e -> p n e", p=128)
    wq = d["a_w_qkv"].rearrange("(k p) f -> p k f", p=128)
    with ExitStack() as ls:
        w = sb(nc, ls, "qw", [128, 8, 3072], BF16)
        xt = [sb(nc, ls, "qxt%d" % i, [128, 8, 512], F32) for i in range(2)]
        hT2 = [sb(nc, ls, "qhT%d" % i, [128, 8, 512], BF16) for i in range(2)]
        sq = sb(nc, ls, "qsq", [128, 8, 512], BF16)
        tmp = [sb(nc, ls, "qtmp%d" % i, [128, 512], F32) for i in range(2)]
        sqv = sb(nc, ls, "qsqv", [128, 512], F32)
        qt = [sb(nc, ls, "qqt%d" % i, [128, 8, 512], BF16) for i in range(2)]
        kt = [sb(nc, ls, "qkt%d" % i, [128, 8, 512], BF16) for i in range(2)]
        vt = [sb(nc, ls, "qvt%d" % i, [128, 4, 1024], BF16) for i in range(2)]
        ps = [ls.enter_context(nc.psum_tensor("qps%d" % i, [128, 512], F32)) for i in range(4)]
        ps_x = ls.enter_context(nc.psum_tensor("qpsx", [128, 512], F32))
        for i in range(3):
            P.op("pool", lambda e, i=i: e.dma_start(out=w[:, :, i * 1024:(i + 1) * 1024], in_=wq[:, :, i * 1024:(i + 1) * 1024]),
                 w=[("qw", i)], dma="qw%d" % i)
        pi = 0
        NT_ = S // 512

        def emit_norm(t):
            b = t % 2
            P.op("sp", lambda e, b=b, t=t: e.dma_start(out=xt[b][:], in_=xin_v[:, :, t * 512:(t + 1) * 512]),
                 w=["qxt%d" % b], dma="qx%d" % b)
            norm_mod(C, xt[b][:], hT2[b], ps_x, sq, tmp, sqv, mod, 0, "q", "qxt%d" % b, "qhT%d" % b)

        emit_norm(0)
        for t in range(NT_):
            b = t % 2
            hT = hT2[b]
            hk = "qhT%d" % b
            if t + 1 < NT_:
                emit_norm(t + 1)
            for (dst, dkey, coff, scale) in ((qt[b], "qqt%d" % b, 0, 0.125), (kt[b], "qkt%d" % b, 1024, 1.0)):
                for c in range(8):
                    pb = pi % 4
                    pi += 1
                    for k in range(8):
                        P.op("pe", lambda e, pb=pb, k=k, c=c, coff=coff, hT=hT: e.matmul(
                            ps[pb][:], lhsT=w[:, k, coff + c * 128: coff + (c + 1) * 128], rhs=hT[:, k, :],
                            start=(k == 0), stop=(k == 7)), r=["qw", hk], w=["qps%d" % pb])
                    P.op("act", lambda e, pb=pb, dst=dst, c=c, scale=scale: e.activation(
                        out=dst[:, c, :], in_=ps[pb][:], func=AF.Copy, scale=scale), r=["qps%d" % pb], w=[(dkey, c)])
            for tc in range(4):
                for cg in range(2):
                    pb = pi % 4
                    pi += 1
                    for k in range(8):
                        P.op("pe", lambda e, pb=pb, k=k, tc=tc, cg=cg, hT=hT: e.matmul(
                            ps[pb][:], lhsT=hT[:, k, tc * 128:(tc + 1) * 128], rhs=w[:, k, 2048 + cg * 512: 2048 + (cg + 1) * 512],
                            start=(k == 0), stop=(k == 7)), r=["qw", hk], w=["qps%d" % pb])
                    P.op("dve", lambda e, pb=pb, b=b, tc=tc, cg=cg: e.tensor_copy(
                        out=vt[b][:, tc, cg * 512:(cg + 1) * 512], in_=ps[pb][:]), r=["qps%d" % pb], w=[("qvt%d" % b, tc)])
            P.op("sp", lambda e, b=b, t=t: e.dma_start(out=qv[:, :, t * 512:(t + 1) * 512], in_=qt[b][:]),
                 r=["qqt%d" % b], w=["QT"], dma="qqo%d" % b)
            P.op("sp", lambda e, b=b, t=t: e.dma_start(out=kv[:, :, t * 512:(t + 1) * 512], in_=kt[b][:]),
                 r=["qkt%d" % b], w=["KT"], dma="qko%d" % b)
            P.op("sp", lambda e, b=b, t=t: e.dma_start(out=vv[:, t * 4:(t + 1) * 4, :], in_=vt[b][:]),
                 r=["qvt%d" % b], w=["V"], dma="qvo%d" % b)
        P.barrier()


def own_tiles(n_slots):
    A = [2 * i if i % 2 == 0 else 2 * i + 1 for i in range(n_slots)]
    B = [2 * i + 1 if i % 2 == 0 else 2 * i for i in range(n_slots)]
    return A, B


def stage_attn(C, x_in, x_out, S, mod):
    nc, P, d = C.nc, C.P, C.d
    n_slots = S // 1024
    TA, TB = own_tiles(n_slots)
    xin_v = x_in.rearrange("(k p) t -> p k t", p=128)
    xout_v = x_out.rearrange("(k p) t -> p k t", p=128)
    qv = d["QT"].rearrange("(c p) t -> p c t", p=128)
    g0 = 2 * 8
    with ExitStack() as ls:
        wo = sb(nc, ls, "awo", [128, 8, 1024], BF16)
        masks = sb(nc, ls, "amask", [128, 4, 4, 512], BF16)
        rsel = sb(nc, ls, "arsel", [128, 2], F32)
        lamv = sb(nc, ls, "alamv", [128, 4, 64], F32)
        lsc = sb(nc, ls, "alsc", [128, 8], F32)
        gsub = sb(nc, ls, "agsub", [128, 1], F32)
        qa = sb(nc, ls, "aqa", [128, 8, 512], BF16)
        qb = sb(nc, ls, "aqb", [128, 8, 512], BF16)
        q = sb(nc, ls, "aq", [128, 8, 512], BF16)
        xa = sb(nc, ls, "axa", [128, 8, 512], F32)
        xb = sb(nc, ls, "axb", [128, 8, 512], F32)
        ktb = [sb(nc, ls, "akt%d" % i, [128, 512], BF16) for i in range(2)]
        vtb = [sb(nc, ls, "avt%d" % i, [128, 4, 128], BF16) for i in range(2)]
        pT = [[sb(nc, ls, "apT%d%d" % (i, j), [128, 512], BF16) for j in range(2)] for i in range(2)]
        oT = sb(nc, ls, "aoT", [128, 8, 512], BF16)
        t32 = [sb(nc, ls, "at32%d" % i, [128, 512], F32) for i in range(4)]
        sqb = sb(nc, ls, "asqb", [128, 512], BF16)
        ps_s = [[ls.enter_context(nc.psum_tensor("aps%d%d" % (i, j), [128, 512], F32)) for j in range(2)] for i in range(2)]
        ps_o = [ls.enter_context(nc.psum_tensor("apo%d" % j, [128, 512], F32)) for j in range(2)]
        ps_l = [ls.enter_context(nc.psum_tensor("apl%d" % j, [128, 512], F32)) for j in range(2)]
        P.op("pool", lambda e: e.dma_start(out=wo[:], in_=d["a_w_o"].rearrange("(k p) f -> p k f", p=128)), w=["awo"], dma="awo")
        for i in range(4):
            P.op("pool", lambda e, i=i: e.dma_start(out=masks[:, i], in_=d["masks"][i]), w=[("amask", i)], dma="amask%d" % i)
        P.op("sp", lambda e: e.dma_start(out=rsel[:], in_=d["rolesel"][:, :]), w=["arsel"], dma="arsel")
        P.op("sp", lambda e: e.dma_start(out=lamv[:], in_=d["lamv"][:, :, :]), w=["alamv"], dma="alamv")
        P.op("sp", lambda e: e.dma_start(out=gsub[:], in_=d["sublnT"][:, :]), w=["agsub"], dma="agsub")
        for i in range(2):
            P.op("dve", lambda e, i=i: e.tensor_tensor(out=lamv[:, 2 * i, :], in0=lamv[:, 2 * i, :], in1=lamv[:, 2 * i + 1, :], op=ALU.mult),
                 r=["alamv"], w=["alamv"])
            P.op("dve", lambda e, i=i: e.tensor_reduce(out=lsc[:, i:i + 1], in_=lamv[:, 2 * i, :], axis=AX.X, op=ALU.add),
                 r=["alamv"], w=["alsc"])
            P.op("act", lambda e, i=i: e.activation(out=lsc[:, 2 + i:3 + i], in_=lsc[:, i:i + 1], func=AF.Exp), r=["alsc"], w=["alsc"])
        P.op("dve", lambda e: e.tensor_tensor(out=lsc[:, 4:5], in0=lsc[:, 3:4], in1=lsc[:, 2:3], op=ALU.subtract), r=["alsc"], w=["alsc"])
        P.op("dve", lambda e: e.tensor_scalar(out=lsc[:, 4:5], in0=lsc[:, 4:5], scalar1=-LAMBDA_INIT, scalar2=None, op0=ALU.add),
             r=["alsc"], w=["alsc"])
        P.op("dve", lambda e: e.tensor_scalar(out=gsub[:], in0=gsub[:], scalar1=1.0 - LAMBDA_INIT, scalar2=None, op0=ALU.mult),
             r=["agsub"], w=["agsub"])
        step = 0
        for si in range(n_slots):
            ta, tb = TA[si], TB[si]
            P.op("sp", lambda e, ta=ta: e.dma_start(out=qa[:], in_=qv[:, :, ta * 512:(ta + 1) * 512]), w=["aqa"], dma="aqa")
            P.op("sp", lambda e, tb=tb: e.dma_start(out=qb[:], in_=qv[:, :, tb * 512:(tb + 1) * 512]), w=["aqb"], dma="aqb")
            P.op("sp", lambda e, ta=ta: e.dma_start(out=xa[:], in_=xin_v[:, :, ta * 512:(ta + 1) * 512]), w=["axa"], dma="axa")
            P.op("sp", lambda e, tb=tb: e.dma_start(out=xb[:], in_=xin_v[:, :, tb * 512:(tb + 1) * 512]), w=["axb"], dma="axb")
            P.op("dve", lambda e: e.tensor_scalar(out=qa[:], in0=qa[:], scalar1=rsel[:, 0:1], scalar2=None, op0=ALU.mult),
                 r=["aqa", "arsel"], w=["aqa"])
            P.op("dve", lambda e: e.scalar_tensor_tensor(out=q[:], in0=qb[:], scalar=rsel[:, 1:2], in1=qa[:], op0=ALU.mult, op1=ALU.add),
                 r=["aqa", "aqb", "arsel"], w=["aq"])
            P.op("pool", lambda e: e.tensor_scalar(out=xa[:], in0=xa[:], scalar1=rsel[:, 0:1], scalar2=None, op0=ALU.mult),
                 r=["axa", "arsel"], w=["axa"])
            P.op("dve", lambda e: e.scalar_tensor_tensor(out=xa[:], in0=xb[:], scalar=rsel[:, 1:2], in1=xa[:], op0=ALU.mult, op1=ALU.add),
                 r=["axa", "axb", "arsel"], w=["axa"])
            n_units = 2 * si + 2
            par = si % 2
            for h in range(8):
                steps = [(u, kb) for u in range(n_units) for kb in range(4)]
                lbs = {}

                def emit_qk(si_, h=h, steps=steps, lbs=lbs, n_units=n_units, par=par):
                    nonlocal step
                    u, kb = steps[si_]
                    if kb == 0:
                        lb = step % 2
                        step += 1
                        lbs[u] = lb
                        P.op("sp", lambda e, lb=lb, h=h, u=u: e.dma_start(
                            out=ktb[lb][:], in_=d["KT"][h * 128:(h + 1) * 128, u * 512:(u + 1) * 512]), r=["KT"], w=["akt%d" % lb], dma="akt%d" % lb)
                        P.op("sp", lambda e, lb=lb, h=h, u=u: e.dma_start(
                            out=vtb[lb][:], in_=d["V"].rearrange("(n p) e -> p n e", p=128)[:, u * 4:(u + 1) * 4, h * 128:(h + 1) * 128]),
                            r=["V"], w=["avt%d" % lb], dma="avt%d" % lb)
                    lb = lbs[u]
                    mk = None
                    if u == n_units - 2:
                        mk = 2 * par
                    elif u == n_units - 1:
                        mk = 2 * par + 1
                    sbuf_i = si_ % 2
                    for j in range(2):
                        P.op("pe", lambda e, sbuf_i=sbuf_i, j=j, lb=lb, kb=kb, h=h: e.matmul(
                            ps_s[sbuf_i][j][:], lhsT=ktb[lb][j * 64:(j + 1) * 64, kb * 128:(kb + 1) * 128],
                            rhs=q[j * 64:(j + 1) * 64, h, :], start=True, stop=True),
                            r=["akt%d" % lb, "aq"], w=["aps%d%d" % (sbuf_i, j)])
                        P.op("act", lambda e, sbuf_i=sbuf_i, j=j: e.activation(
                            out=pT[sbuf_i][j][:], in_=ps_s[sbuf_i][j][:], func=AF.Exp),
                            r=["aps%d%d" % (sbuf_i, j)], w=["apT%d%d" % (sbuf_i, j)])
                        if mk is not None:
                            P.op("dve" if j == 0 else "pool", lambda e, sbuf_i=sbuf_i, j=j, mk=mk, kb=kb: e.tensor_tensor(
                                out=pT[sbuf_i][j][:], in0=pT[sbuf_i][j][:], in1=masks[:, mk, kb, :], op=ALU.mult),
                                r=["apT%d%d" % (sbuf_i, j), "amask"], w=["apT%d%d" % (sbuf_i, j)])

                def emit_pv(si_, steps=steps, lbs=lbs):
                    u, kb = steps[si_]
                    lb = lbs[u]
                    sbuf_i = si_ % 2
                    first = (si_ == 0)
                    last = (si_ == len(steps) - 1)
                    for j in range(2):
                        P.op("pe", lambda e, sbuf_i=sbuf_i, j=j, lb=lb, kb=kb, first=first, last=last: e.matmul(
                            ps_o[j][:], lhsT=vtb[lb][:, kb, :], rhs=pT[sbuf_i][j][:], start=first, stop=last),
                            r=["avt%d" % lb, "apT%d%d" % (sbuf_i, j)], w=["apo%d" % j])
                        P.op("pe", lambda e, sbuf_i=sbuf_i, j=j, first=first, last=last: e.matmul(
                            ps_l[j][:], lhsT=C.ones_bf[:], rhs=pT[sbuf_i][j][:], start=first, stop=last),
                            r=["ones_bf", "apT%d%d" % (sbuf_i, j)], w=["apl%d" % j])

                emit_qk(0)
                for si_ in range(len(steps)):
                    if si_ + 1 < len(steps):
                        emit_qk(si_ + 1)
                    emit_pv(si_)
                for j in range(2):
                    P.op("dve", lambda e, j=j: e.reciprocal(out=t32[j][:], in_=ps_l[j][:]), r=["apl%d" % j], w=["at32%d" % j])
                    P.op("dve", lambda e, j=j: e.tensor_tensor(out=t32[j][:], in0=ps_o[j][:], in1=t32[j][:], op=ALU.mult),
                         r=["apo%d" % j, "at32%d" % j], w=["at32%d" % j])
                P.op("dve", lambda e: e.scalar_tensor_tensor(out=t32[2][:], in0=t32[1][:], scalar=lsc[:, 4:5], in1=t32[0][:],
                                                              op0=ALU.mult, op1=ALU.add), r=["at320", "at321", "alsc"], w=["at322"])
                P.op("act", lambda e: e.activation(out=sqb[:], in_=t32[2][:], func=AF.Square), r=["at322"], w=["asqb"])
                P.op("pe", lambda e: e.matmul(ps_s[0][0][:], lhsT=C.ones_bf[:], rhs=sqb[:], start=True, stop=True),
                     r=["ones_bf", "asqb"], w=["aps00"])
                P.op("act", lambda e: e.activation(out=t32[3][:], in_=ps_s[0][0][:], func=AF.Sqrt, bias=C.eps_t[:, 0:1], scale=1.0 / 128),
                     r=["aps00"], w=["at323"])
                P.op("dve", lambda e: e.reciprocal(out=t32[3][:], in_=t32[3][:]), r=["at323"], w=["at323"])
                P.op("dve", lambda e, h=h: e.scalar_tensor_tensor(out=oT[:, h, :], in0=t32[2][:], scalar=gsub[:, 0:1], in1=t32[3][:],
                                                                   op0=ALU.mult, op1=ALU.mult), r=["at322", "at323", "agsub"], w=[("aoT", h)])
            for f in range(8):
                pb = ps_s[1][f % 2]
                pk = "aps1%d" % (f % 2)
                for h in range(8):
                    P.op("pe", lambda e, pb=pb, f=f, h=h: e.matmul(pb[:], lhsT=wo[:, h, f * 128:(f + 1) * 128], rhs=oT[:, h, :],
                                                                   start=(h == 0), stop=(h == 7)), r=["awo", "aoT"], w=[pk])
                P.op("dve", lambda e, pb=pb, f=f: e.scalar_tensor_tensor(
                    out=xa[:, f, :], in0=pb[:], scalar=mod[:, g0 + f:g0 + f + 1], in1=xa[:, f, :], op0=ALU.mult, op1=ALU.add),
                    r=[pk, "axa"], w=["axa"])
            P.op("sp", lambda e, si=si: e.dma_start(out=xout_v[:, :, si * 512:(si + 1) * 512], in_=xa[:]), r=["axa"], w=["x3T"], dma="axo")
        P.barrier()


def stage_final(C, x_in, x_out, T):
    nc, P, d = C.nc, C.P, C.d
    xin_v = x_in.rearrange("(k p) t -> p k t", p=128)
    xout_v = x_out.rearrange("(k p) t -> p k t", p=128)
    with ExitStack() as ls:
        fg = sb(nc, ls, "ffg", [128, 8], F32)
        xt = [sb(nc, ls, "fxt%d" % i, [128, 8, 512], F32) for i in range(2)]
        sq = sb(nc, ls, "fsq", [128, 8, 512], BF16)
        sqv = sb(nc, ls, "fsqv", [128, 512], F32)
        ps = ls.enter_context(nc.psum_tensor("fps", [128, 512], F32))
        P.op("sp", lambda e: e.dma_start(out=fg[:], in_=d["final_gT"][:, :]), w=["ffg"], dma="ffg")
        for t in range(T // 512):
            b = t % 2
            xk = "fxt%d" % b
            P.op("sp", lambda e, b=b, t=t: e.dma_start(out=xt[b][:], in_=xin_v[:, :, t * 512:(t + 1) * 512]), w=[xk], dma="fx%d" % b)
            P.op("act", lambda e, b=b: e.activation(out=sq[:], in_=xt[b][:], func=AF.Square), r=[xk], w=["fsq"])
            for k in range(8):
                P.op("pe", lambda e, k=k: e.matmul(ps[:], lhsT=C.ones_bf[:], rhs=sq[:, k, :], start=(k == 0), stop=(k == 7)),
                     r=["fsq", "ones_bf"], w=["fps"])
            P.op("act", lambda e: e.activation(out=sqv[:], in_=ps[:], func=AF.Sqrt, bias=C.eps_t[:, 0:1], scale=1.0 / D),
                 r=["fps"], w=["fsqv"])
            P.op("dve", lambda e: e.reciprocal(out=sqv[:], in_=sqv[:]), r=["fsqv"], w=["fsqv"])
            for k in range(8):
                P.op("dve", lambda e, b=b, k=k: e.scalar_tensor_tensor(
                    out=xt[b][:, k, :], in0=xt[b][:, k, :], scalar=fg[:, k:k + 1], in1=sqv[:], op0=ALU.mult, op1=ALU.mult),
                    r=[xk, "fsqv", "ffg"], w=[xk])
            P.op("sp", lambda e, b=b, t=t: e.dma_start(out=xout_v[:, :, t * 512:(t + 1) * 512], in_=xt[b][:]), r=[xk], w=["outT"], dma="fxo%d" % b)
        P.barrier()


M_IN = 6176
DBG = set()


def stage_m1(C, x_in, S, mod, part):
    nc, P, d = C.nc, C.P, C.d
    xin_v = x_in.rearrange("(k p) t -> p k t", p=128)
    wv = d["m_w_in"].rearrange("(k p) f -> p k f", p=128)
    nm = "m" + part
    with ExitStack() as ls:
        xt = [sb(nc, ls, nm + "xt%d" % i, [128, 8, 512], F32) for i in range(2)]
        hT2 = [sb(nc, ls, nm + "hT%d" % i, [128, 8, 512], BF16) for i in range(2)]
        sq = sb(nc, ls, nm + "sq", [128, 8, 512], BF16)
        tmp = [sb(nc, ls, nm + "tmp%d" % i, [128, 512], F32) for i in range(2)]
        sqv = sb(nc, ls, nm + "sqv", [128, 512], F32)
        ps_x = ls.enter_context(nc.psum_tensor(nm + "psx", [128, 512], F32))
        ps = [ls.enter_context(nc.psum_tensor(nm + "ps%d" % i, [128, 512], F32)) for i in range(3)]
        if part == "z":
            w = sb(nc, ls, nm + "w", [128, 8, 2048 + 32], BF16)
            zt = [sb(nc, ls, nm + "zt%d" % i, [128, 2048], BF16) for i in range(2)]
            dtt = [sb(nc, ls, nm + "dtt%d" % i, [128, 32], F32) for i in range(2)]
            for i in range(2):
                P.op("pool", lambda e, i=i: e.dma_start(out=w[:, :, i * 1024:(i + 1) * 1024], in_=wv[:, :, i * 1024:(i + 1) * 1024]),
                     w=[(nm + "w", i)], dma=nm + "w%d" % i)
            P.op("pool", lambda e: e.dma_start(out=w[:, :, 2048:2080], in_=wv[:, :, 6144:6176]), w=[(nm + "w", 2)], dma=nm + "w2")
            zv = d["zs"].rearrange("(n p) e -> p n e", p=128)
            dv = d["dtr"].rearrange("(n p) e -> p n e", p=128)
        else:
            w = sb(nc, ls, nm + "w", [128, 8, 4096], BF16)
            diag = sb(nc, ls, nm + "diag", [128, 4, 32, 128], BF16)
            cw = sb(nc, ls, nm + "cw", [128, 32, 4], F32)
            cb = sb(nc, ls, nm + "cb", [128, 32], F32)
            halo = sb(nc, ls, nm + "halo", [128, 32, 4], BF16)
            xraw = [sb(nc, ls, nm + "xraw%d" % i, [128, 516], BF16) for i in range(2)]
            xc = [sb(nc, ls, nm + "xc%d" % i, [128, 4, 512], BF16) for i in range(2)]
            xtok = [sb(nc, ls, nm + "xtok%d" % i, [128, 512], BF16) for i in range(2)]
            ps_t32 = [ls.enter_context(nc.psum_tensor(nm + "pst%d" % i, [128, 512], F32)) for i in range(2)]
            ps_t = [p_[:].bitcast(BF16) for p_ in ps_t32]
            for i in range(4):
                P.op("pool", lambda e, i=i: e.dma_start(out=w[:, :, i * 1024:(i + 1) * 1024], in_=wv[:, :, 2048 + i * 1024:2048 + (i + 1) * 1024]),
                     w=[(nm + "w", i)], dma=nm + "w%d" % i)
            P.op("sp", lambda e: e.dma_start(out=cw[:], in_=d["convwT"][:, :, :]), w=[nm + "cw"], dma=nm + "cw")
            P.op("sp", lambda e: e.dma_start(out=cb[:], in_=d["convbT"][:, :]), w=[nm + "cb"], dma=nm + "cb")
            P.op("dve", lambda e: e.memset(halo[:], 0.0), w=[nm + "halo"])
            for j in range(4):
                for cc in range(32):
                    P.op("pool" if cc % 2 else "dve", lambda e, j=j, cc=cc: e.tensor_scalar(
                        out=diag[:, j, cc, :], in0=C.ident_f[:], scalar1=cw[:, cc, j:j + 1], scalar2=None, op0=ALU.mult),
                        r=["ident_f", nm + "cw"], w=[(nm + "diag", cc)])
            xsv = d["xsB"].rearrange("(n p) e -> p n e", p=128)
            btv = d["BT"].rearrange("(c p) t -> p c t", p=128)
            ctv = d["CT"].rearrange("(c p) t -> p c t", p=128)
        pi = 0
        NT_ = S // 512

        def emit_norm(t):
            b = t % 2
            P.op("sp", lambda e, b=b, t=t: e.dma_start(out=xt[b][:], in_=xin_v[:, :, t * 512:(t + 1) * 512]),
                 w=[nm + "xt%d" % b], dma=nm + "x%d" % b)
            norm_mod(C, xt[b][:], hT2[b], ps_x, sq, tmp, sqv, mod, 0, nm, nm + "xt%d" % b, nm + "hT%d" % b)

        emit_norm(0)
        for t in range(NT_):
            b = t % 2
            hT = hT2[b]
            hk = nm + "hT%d" % b
            if t + 1 < NT_:
                emit_norm(t + 1)
            if part == "z":
                for tc in range(4):
                    zb = (t * 4 + tc) % 2
                    for cg in range(4):
                        pb = pi % 3
                        pi += 1
                        for k in range(8):
                            P.op("pe", lambda e, pb=pb, k=k, tc=tc, cg=cg, hT=hT: e.matmul(
                                ps[pb][:], lhsT=hT[:, k, tc * 128:(tc + 1) * 128], rhs=w[:, k, cg * 512:(cg + 1) * 512],
                                start=(k == 0), stop=(k == 7)), r=[nm + "w", hk], w=[nm + "ps%d" % pb])
                        P.op("act", lambda e, pb=pb, zb=zb, cg=cg: e.activation(
                            out=zt[zb][:, cg * 512:(cg + 1) * 512], in_=ps[pb][:], func=AF.Silu),
                            r=[nm + "ps%d" % pb], w=[(nm + "zt%d" % zb, cg)])
                    pb = pi % 3
                    pi += 1
                    for k in range(8):
                        P.op("pe", lambda e, pb=pb, k=k, tc=tc, hT=hT: e.matmul(
                            ps[pb][:, 0:32], lhsT=hT[:, k, tc * 128:(tc + 1) * 128], rhs=w[:, k, 2048:2080],
                            start=(k == 0), stop=(k == 7)), r=[nm + "w", hk], w=[nm + "ps%d" % pb])
                    P.op("dve", lambda e, pb=pb, zb=zb: e.tensor_copy(out=dtt[zb][:], in_=ps[pb][:, 0:32]),
                         r=[nm + "ps%d" % pb], w=[nm + "dtt%d" % zb])
                    n = t * 4 + tc
                    P.op("sp", lambda e, zb=zb, n=n: e.dma_start(out=zv[:, n, :], in_=zt[zb][:]), r=[nm + "zt%d" % zb], w=["zs"], dma=nm + "zo%d" % zb)
                    P.op("sp", lambda e, zb=zb, n=n: e.dma_start(out=dv[:, n, :], in_=dtt[zb][:]), r=[nm + "dtt%d" % zb], w=["dtr"], dma=nm + "do%d" % zb)
            else:
                def proj(cc, hT=hT, hk=hk):
                    nonlocal pi
                    rb = cc % 2
                    pb = pi % 3
                    pi += 1
                    for k in range(8):
                        P.op("pe", lambda e, pb=pb, k=k, cc=cc: e.matmul(
                            ps[pb][:], lhsT=w[:, k, cc * 128:(cc + 1) * 128], rhs=hT[:, k, :],
                            start=(k == 0), stop=(k == 7)), r=[nm + "w", hk], w=[nm + "ps%d" % pb])
                    P.op("dve", lambda e, rb=rb, cc=cc: e.tensor_copy(out=xraw[rb][:, 0:4], in_=halo[:, cc, :]),
                         r=[(nm + "halo", cc)], w=[nm + "xraw%d" % rb])
                    P.op("act", lambda e, rb=rb, pb=pb: e.activation(out=xraw[rb][:, 4:516], in_=ps[pb][:], func=AF.Copy),
                         r=[nm + "ps%d" % pb], w=[nm + "xraw%d" % rb])
                    P.op("dve", lambda e, rb=rb, cc=cc: e.tensor_copy(out=halo[:, cc, :], in_=xraw[rb][:, 512:516]),
                         r=[nm + "xraw%d" % rb], w=[(nm + "halo", cc)])

                def conv(cc):
                    nonlocal pi
                    rb = cc % 2
                    g4_, ci = divmod(cc, 4)
                    xb_i = g4_ % 2
                    pb2 = pi % 3
                    pi += 1
                    for j in range(4):
                        P.op("pe", lambda e, pb2=pb2, j=j, cc=cc, rb=rb: e.matmul(
                            ps[pb2][:], lhsT=diag[:, j, cc, :], rhs=xraw[rb][:, 1 + j:513 + j], start=(j == 0), stop=(j == 3)),
                            r=[(nm + "diag", cc), nm + "xraw%d" % rb], w=[nm + "ps%d" % pb2])
                    P.op("act", lambda e, pb2=pb2, xb_i=xb_i, ci=ci, cc=cc: e.activation(
                        out=xc[xb_i][:, ci, :], in_=ps[pb2][:], func=AF.Silu, bias=cb[:, cc:cc + 1], scale=1.0),
                        r=[nm + "ps%d" % pb2, nm + "cb"], w=[(nm + "xc%d" % xb_i, ci)])

                proj(0)
                for g4 in range(8):
                    xb_i = g4 % 2
                    for ci in range(4):
                        cc = g4 * 4 + ci
                        if cc + 1 < 32:
                            proj(cc + 1)
                        conv(cc)
                    if g4 >= 4:
                        dst = btv if g4 < 6 else ctv
                        c0 = (g4 - 4) * 4 if g4 < 6 else (g4 - 6) * 4
                        P.op("sp", lambda e, xb_i=xb_i, dst=dst, c0=c0, t=t: e.dma_start(
                            out=dst[:, c0:c0 + 4, t * 512:(t + 1) * 512], in_=xc[xb_i][:]),
                            r=[nm + "xc%d" % xb_i], w=["BTCT"], dma=nm + "bo%d" % xb_i)
                    if g4 < 6:
                        for tc in range(4):
                            tb = (g4 * 4 + tc) % 2
                            for ci in range(4):
                                P.op("pe", lambda e, tb=tb, ci=ci, xb_i=xb_i, tc=tc: e.transpose(
                                    out=ps_t[tb][:, ci * 128:(ci + 1) * 128], in_=xc[xb_i][:, ci, tc * 128:(tc + 1) * 128],
                                    identity=C.ident_bf[:]), r=[(nm + "xc%d" % xb_i, ci), "ident_bf"], w=[nm + "pst%d" % tb])
                            P.op("dve" if tc % 2 else "pool" if False else "dve", lambda e, tb=tb: e.tensor_copy(out=xtok[tb][:], in_=ps_t[tb][:, 0:512]),
                                 r=[nm + "pst%d" % tb], w=[nm + "xtok%d" % tb])
                            n = t * 4 + tc
                            P.op("sp", lambda e, tb=tb, n=n, g4=g4: e.dma_start(out=xsv[:, n, g4 * 512:(g4 + 1) * 512], in_=xtok[tb][:]),
                                 r=[nm + "xtok%d" % tb], w=["xsB"], dma=nm + "xo%d" % tb)
        P.barrier()


def stage_m2(C, x_in, x_out, S, mod):
    nc, P, d = C.nc, C.P, C.d
    xin_v = x_in.rearrange("(k p) t -> p k t", p=128)
    xout_v = x_out.rearrange("(k p) t -> p k t", p=128)
    zv = d["zs"].rearrange("(n p) e -> p n e", p=128)
    dv = d["dtr"].rearrange("(n p) e -> p n e", p=128)
    xsv = d["xsB"].rearrange("(n p) e -> p n e", p=128)
    btv = d["BT"].rearrange("(c p) t -> p c t", p=128)
    ctv = d["CT"].rearrange("(c p) t -> p c t", p=128)
    g0 = 2 * 8
    with ExitStack() as ls:
        wout = sb(nc, ls, "swout", [128, 16, 1024], BF16)
        tri = sb(nc, ls, "stri", [128, 128], F32)
        negm = sb(nc, ls, "snegm", [128, 4, 128], BF16)
        dtb = sb(nc, ls, "sdtb", [128, 32], F32)
        aneg = sb(nc, ls, "saneg", [128, 32], F32)
        dsk = sb(nc, ls, "sdsk", [128, 32], F32)
        dI = sb(nc, ls, "sdI", [128, 32, 128], BF16)
        normg = sb(nc, ls, "snormg", [128, 2048], F32)
        blkT = sb(nc, ls, "sblkT", [32, 4096], BF16)
        Rm = sb(nc, ls, "sRm", [64, 4096], F32)
        Lm = sb(nc, ls, "sLm", [64, 128], F32)
        Tin = sb(nc, ls, "sTin", [128, 64], F32)
        acsT = sb(nc, ls, "sacsT", [32, 128], F32)
        S32 = sb(nc, ls, "sS32", [128, 2048], F32)
        Sbf = [sb(nc, ls, "sSbf%d" % i, [128, 2048], BF16) for i in range(2)]
        zt = [sb(nc, ls, "szt%d" % i, [128, 2048], BF16) for i in range(2)]
        xb = [sb(nc, ls, "sxb%d" % i, [128, 3072], BF16) for i in range(2)]
        BTc = [sb(nc, ls, "sBT%d" % i, [128, 8, 128], BF16) for i in range(2)]
        CTc = [sb(nc, ls, "sCT%d" % i, [128, 8, 128], BF16) for i in range(2)]
        dtr = [sb(nc, ls, "sdtr%d" % i, [128, 32], F32) for i in range(2)]
        xt = [sb(nc, ls, "sxt%d" % i, [128, 8, 128], F32) for i in range(2)]
        sm = sb(nc, ls, "ssm", [128, 64], F32)
        v32 = [sb(nc, ls, "sv32%d" % i, [128, 8, 32], F32) for i in range(2)]
        xtl = [sb(nc, ls, "sxtl%d" % i, [128, 2048], BF16) for i in range(2)]
        xts = [sb(nc, ls, "sxts%d" % i, [128, 2048], BF16) for i in range(2)]
        CBm = [sb(nc, ls, "sCBm%d" % i, [128, 128], F32) for i in range(2)]
        LT = [sb(nc, ls, "sLT%d" % i, [128, 512], F32) for i in range(2)]
        MT = [sb(nc, ls, "sMT%d" % i, [128, 8, 512], BF16) for i in range(2)]
        ty = [sb(nc, ls, "sty%d" % i, [128, 256], F32) for i in range(2)]
        y32 = sb(nc, ls, "sy32", [128, 2048], F32)
        junk = sb(nc, ls, "sjunk", [128, 256], F32)
        ssq = sb(nc, ls, "sssq", [128, 8], F32)
        yn = sb(nc, ls, "syn", [128, 2048], BF16)
        ynT = sb(nc, ls, "synT", [128, 16, 128], BF16)
        pmc = ls.enter_context(nc.psum_tensor("spmc", [128, 512], F32))
        pD = [ls.enter_context(nc.psum_tensor("spD%d" % i, [128, 512], F32)) for i in range(2)]
        pY = [ls.enter_context(nc.psum_tensor("spY%d" % i, [128, 512], F32)) for i in range(2)]
        pS = ls.enter_context(nc.psum_tensor("spS", [128, 512], F32))
        pAB = [ls.enter_context(nc.psum_tensor("spAB%d" % i, [128, 512], F32)) for i in range(2)]
        pABb = [p_[:].bitcast(BF16) for p_ in pAB]
        wov = d["m_w_out"].rearrange("(j p) f -> p j f", p=128)
        for i in range(2):
            P.op("pool", lambda e, i=i: e.dma_start(out=wout[:, i * 8:(i + 1) * 8, :], in_=wov[:, i * 8:(i + 1) * 8, :]),
                 w=[("swout", i)], dma="swout%d" % i)
        for (t_, nme) in ((tri, "tri"), (dtb, "dtbT"), (aneg, "alogT"), (dsk, "dskT"), (normg, "normgT")):
            P.op("sp", lambda e, t_=t_, nme=nme: e.dma_start(out=t_[:], in_=d[nme][:, :]), w=["s_" + nme], dma="s_" + nme)
        P.op("pool", lambda e: e.dma_start(out=negm[:], in_=d["negm4"][:, :, :]), w=["s_negm"], dma="s_negm")
        P.op("pool", lambda e: e.dma_start(out=blkT[:, 0:2048], in_=d["blk"][0:32, 0:2048]), w=[("s_blkT", 0)], dma="s_blkT")
        P.op("pool", lambda e: e.dma_start(out=blkT[:, 2048:4096], in_=d["blk"][0:32, 2048:4096]), w=[("s_blkT", 1)], dma="s_blkT2")
        P.op("sp", lambda e: e.dma_start(out=Rm[32:64, :], in_=d["blk"][32:64, :]), w=["sRm_b"], dma="s_Rmb")
        P.op("act", lambda e: e.activation(out=aneg[:], in_=aneg[:], func=AF.Exp), r=["s_alogT"], w=["s_alogT"])
        P.op("dve", lambda e: e.tensor_scalar(out=aneg[:], in0=aneg[:], scalar1=-1.0, scalar2=None, op0=ALU.mult), r=["s_alogT"], w=["s_alogT"])
        P.op("dve", lambda e: e.memset(S32[:], 0.0), w=["sS32"])
        P.op("pool", lambda e: e.memset(Sbf[0][:], 0.0), w=["sSbf0"])
        P.op("dve", lambda e: e.memset(Tin[:, 0:32], 1.0), w=["sTin_a"])
        for r_ in range(32):
            P.op("dve" if r_ % 2 else "pool", lambda e, r_=r_: e.tensor_scalar(out=dI[:, r_, :], in0=C.ident_f[:], scalar1=dsk[:, r_:r_ + 1], scalar2=None, op0=ALU.mult),
                 r=["ident_f", "s_dskT"], w=[("sdI", r_)])
        one = C.ones_f[:, 0:1]
        NC_ = S // 128
        gcount = [0, 0]

        def front_pre(c):
            b = c % 2
            sl = slice(c * 128, (c + 1) * 128)
            V = lambda i: v32[b][:, i, :]
            sv = "sv%d" % b
            P.op("sp", lambda e: e.dma_start(out=dtr[b][:], in_=dv[:, c, :]), r=["dtr"], w=["sdtr%d" % b], dma="sdtr%d" % b)
            P.op("sp", lambda e: e.dma_start(out=BTc[b][:], in_=btv[:, :, sl]), r=["BTCT"], w=["sBT%d" % b], dma="sBT%d" % b)
            P.op("sp", lambda e: e.dma_start(out=CTc[b][:], in_=ctv[:, :, sl]), r=["BTCT"], w=["sCT%d" % b], dma="sCT%d" % b)
            P.op("sp", lambda e: e.dma_start(out=xb[b][:], in_=xsv[:, c, :]), r=["xsB"], w=["sxb%d" % b], dma="sxb%d" % b)
            P.op("dve", lambda e: e.tensor_tensor(out=V(0), in0=dtr[b][:], in1=dtb[:], op=ALU.add), r=["sdtr%d" % b, "s_dtbT"], w=[(sv, 0)])
            P.op("act", lambda e: e.activation(out=V(1), in_=V(0), func=AF.Exp), r=[(sv, 0)], w=[(sv, 1)])
            P.op("act", lambda e: e.activation(out=V(2), in_=V(1), func=AF.Ln, bias=one, scale=1.0), r=[(sv, 1), "ones_f"], w=[(sv, 2)])
            P.op("dve", lambda e: e.tensor_tensor(out=V(3), in0=V(2), in1=aneg[:], op=ALU.mult), r=[(sv, 2), "s_alogT"], w=[(sv, 3)])
            P.op("pe", lambda e: e.matmul(pmc[:, 0:32], lhsT=tri[:], rhs=V(3), start=True, stop=True), r=["s_tri", (sv, 3)], w=["spmc"])
            P.op("pe", lambda e: e.matmul(pmc[:, 32:64], lhsT=C.ones_f[:], rhs=V(3), start=True, stop=True), r=["ones_f", (sv, 3)], w=["spmc"])
            P.op("dve", lambda e: e.tensor_copy(out=sm[:], in_=pmc[:, 0:64]), r=["spmc"], w=["ssm"])
            P.op("dve", lambda e: e.tensor_scalar(out=Tin[:, 32:64], in0=sm[:, 0:32], scalar1=-1.0, scalar2=None, op0=ALU.mult), r=["ssm"], w=["sTin_b"])
            P.op("pe", lambda e: e.transpose(out=pmc[0:64, 256:384], in_=Tin[:], identity=C.ident_f[:]), r=["sTin_a", "sTin_b", "ident_f"], w=["spmc"])
            P.op("pe", lambda e: e.transpose(out=pmc[0:32, 384:512], in_=sm[:, 0:32], identity=C.ident_f[:]), r=["ssm", "ident_f"], w=["spmc"])
            P.op("act", lambda e: e.activation(out=Lm[:], in_=pmc[0:64, 256:384], func=AF.Copy), r=["spmc"], w=["sLm"])
            P.op("act", lambda e: e.activation(out=acsT[:], in_=pmc[0:32, 384:512], func=AF.Copy), r=["spmc"], w=["sacsT"])
            for hf, eng_ in ((0, "dve"), (1, "pool")):
                P.op(eng_, lambda e, hf=hf: e.tensor_tensor(
                    out=Rm[0:32, hf * 2048:(hf + 1) * 2048].rearrange("p (r q) -> p r q", q=128),
                    in0=blkT[:, hf * 2048:(hf + 1) * 2048].rearrange("p (r q) -> p r q", q=128),
                    in1=acsT[:].unsqueeze(1).broadcast_to([32, 16, 128]), op=ALU.mult),
                    r=["sacsT", "s_blkT"], w=[("sRm_a", hf)])
            P.op("act", lambda e: e.activation(out=V(4), in_=sm[:, 0:32], func=AF.Exp), r=["ssm"], w=[(sv, 4)])
            P.op("dve", lambda e: e.tensor_tensor(out=V(5), in0=sm[:, 32:64], in1=sm[:, 0:32], op=ALU.subtract), r=["ssm"], w=[(sv, 5)])
            P.op("act", lambda e: e.activation(out=V(5), in_=V(5), func=AF.Exp), r=[(sv, 5)], w=[(sv, 5)])
            P.op("act", lambda e: e.activation(out=V(6), in_=sm[:, 32:64], func=AF.Exp), r=["ssm"], w=[(sv, 6)])
            P.op("dve", lambda e: e.tensor_tensor(out=V(7), in0=V(2), in1=V(5), op=ALU.mult), r=[(sv, 2), (sv, 5)], w=[(sv, 7)])
            xs3 = xb[b][:, 0:2048].rearrange("p (h e) -> p h e", e=64)
            P.op("dve", lambda e: e.tensor_tensor(out=xtl[b][:].rearrange("p (h e) -> p h e", e=64), in0=xs3,
                                                  in1=V(2).unsqueeze(2).broadcast_to([128, 32, 64]), op=ALU.mult),
                 r=["sxb%d" % b, (sv, 2)], w=["sxtl%d" % b])
            P.op("pool", lambda e: e.tensor_tensor(out=xts[b][:].rearrange("p (h e) -> p h e", e=64), in0=xs3,
                                                   in1=V(7).unsqueeze(2).broadcast_to([128, 32, 64]), op=ALU.mult),
                 r=["sxb%d" % b, (sv, 7)], w=["sxts%d" % b])
        def front_group(c, g):
            b = c % 2
            sv = "sv%d" % b
            gb = gcount[0] % 2
            gcount[0] += 1
            P.op("pe", lambda e, g=g: e.matmul(pmc[:, 128:256], lhsT=BTc[b][:, g, :], rhs=CTc[b][:, g, :], start=True, stop=True),
                 r=["sBT%d" % b, "sCT%d" % b], w=["spmc"])
            P.op("act", lambda e, gb=gb: e.activation(out=CBm[gb][:], in_=pmc[:, 128:256], func=AF.Copy), r=["spmc"], w=["sCBm%d" % gb])
            P.op("pe", lambda e, g=g, gb=gb: e.matmul(pD[gb][:], lhsT=Lm[:], rhs=Rm[:, g * 512:(g + 1) * 512], start=True, stop=False),
                 r=["sLm", "sRm_a", "sRm_b"], w=["spD%d" % gb])
            P.op("pe", lambda e, gb=gb: e.matmul(pD[gb][:], lhsT=C.ident_bf[:], rhs=negm[:].rearrange("p r q -> p (r q)"), start=False, stop=True),
                 r=["ident_bf", "s_negm"], w=["spD%d" % gb])
            P.op("act", lambda e, gb=gb: e.activation(out=LT[gb][:], in_=pD[gb][:], func=AF.Exp), r=["spD%d" % gb], w=["sLT%d" % gb])
            P.op("dve" if g % 3 else "pool", lambda e, gb=gb, g=g: e.tensor_tensor(out=MT[b][:, g, :].rearrange("p (r q) -> p r q", q=128),
                                                              in0=LT[gb][:].rearrange("p (r q) -> p r q", q=128),
                                                              in1=CBm[gb][:].unsqueeze(1).broadcast_to([128, 4, 128]), op=ALU.mult),
                 r=["sLT%d" % gb, "sCBm%d" % gb], w=[("sMT%d" % b, g)])

        def back_pre(c):
            b = c % 2
            sl = slice(c * 128, (c + 1) * 128)
            sv = "sv%d" % b
            Sb_old, Sb_new = Sbf[c % 2], Sbf[(c + 1) % 2]
            ko, kn = "sSbf%d" % (c % 2), "sSbf%d" % ((c + 1) % 2)
            P.op("sp", lambda e: e.dma_start(out=zt[b][:], in_=zv[:, c, :]), r=["zs"], w=["szt%d" % b], dma="szt%d" % b)
            P.op("sp", lambda e: e.dma_start(out=xt[b][:], in_=xin_v[:, :, sl]), w=["sxt%d" % b], dma="sxt%d" % b)
            P.op("pool", lambda e: e.tensor_tensor(out=S32[:].rearrange("p (h e) -> p h e", e=64), in0=S32[:].rearrange("p (h e) -> p h e", e=64),
                                                   in1=v32[b][:, 6, :].unsqueeze(2).broadcast_to([128, 32, 64]), op=ALU.mult),
                 r=["sS32", (sv, 6)], w=["sS32"])
        def back_group(c, g):
            b = c % 2
            sl = slice(c * 128, (c + 1) * 128)
            sv = "sv%d" % b
            Sb_old, Sb_new = Sbf[c % 2], Sbf[(c + 1) % 2]
            ko, kn = "sSbf%d" % (c % 2), "sSbf%d" % ((c + 1) % 2)
            gb = gcount[1] % 2
            gcount[1] += 1
            for r_ in range(4):
                hd = 4 * g + r_
                P.op("pe", lambda e, r_=r_, hd=hd, gb=gb, g=g: e.matmul(pY[gb][:, r_ * 64:(r_ + 1) * 64], lhsT=MT[b][:, g, r_ * 128:(r_ + 1) * 128],
                                                                        rhs=xtl[b][:, hd * 64:(hd + 1) * 64], start=True, stop=False),
                     r=[("sMT%d" % b, g), "sxtl%d" % b], w=["spY%d" % gb])
                P.op("pe", lambda e, r_=r_, hd=hd, gb=gb: e.matmul(pY[gb][:, r_ * 64:(r_ + 1) * 64], lhsT=dI[:, hd, :],
                                                                   rhs=xb[b][:, hd * 64:(hd + 1) * 64], start=False, stop=True),
                     r=[("sdI", hd), "sxb%d" % b], w=["spY%d" % gb])
            P.op("pe", lambda e, g=g, gb=gb: e.matmul(pY[gb][:, 256:512], lhsT=CTc[b][:, g, :], rhs=Sb_old[:, g * 256:(g + 1) * 256], start=True, stop=True),
                 r=["sCT%d" % b, ko], w=["spY%d" % gb])
            P.op("dve", lambda e, g=g, gb=gb: e.tensor_tensor(out=ty[gb][:].rearrange("p (r e) -> p r e", e=64),
                                                              in0=pY[gb][:, 256:512].rearrange("p (r e) -> p r e", e=64),
                                                              in1=v32[b][:, 4, 4 * g:4 * g + 4].unsqueeze(2).broadcast_to([128, 4, 64]), op=ALU.mult),
                 r=["spY%d" % gb, (sv, 4)], w=["sty%d" % gb])
            P.op("dve", lambda e, g=g, gb=gb: e.tensor_tensor(out=y32[:, g * 256:(g + 1) * 256], in0=pY[gb][:, 0:256], in1=ty[gb][:], op=ALU.add),
                 r=["spY%d" % gb, "sty%d" % gb], w=[("sy32", g)])
            P.op("pe", lambda e, g=g: e.matmul(pS[:, 0:256], lhsT=xb[b][:, 2048 + g * 128:2048 + (g + 1) * 128], rhs=xts[b][:, g * 256:(g + 1) * 256],
                                               start=True, stop=True), r=["sxb%d" % b, "sxts%d" % b], w=["spS"])
            P.op("dve", lambda e, g=g: e.tensor_tensor(out=S32[:, g * 256:(g + 1) * 256], in0=pS[:, 0:256], in1=S32[:, g * 256:(g + 1) * 256], op=ALU.add),
                 r=["spS", "sS32"], w=[("sS32", g)])
        def back_post_ew(c):
            b = c % 2
            sl = slice(c * 128, (c + 1) * 128)
            sv = "sv%d" % b
            Sb_old, Sb_new = Sbf[c % 2], Sbf[(c + 1) % 2]
            ko, kn = "sSbf%d" % (c % 2), "sSbf%d" % ((c + 1) % 2)
            P.op("act", lambda e: e.activation(out=Sb_new[:], in_=S32[:], func=AF.Copy), r=["sS32"], w=[kn])
            P.op("pool", lambda e: e.tensor_tensor(out=y32[:], in0=y32[:], in1=zt[b][:], op=ALU.mult), r=["sy32", "szt%d" % b], w=["sy32"])
            for g in range(8):
                P.op("act", lambda e, g=g: e.activation(out=junk[:], in_=y32[:, g * 256:(g + 1) * 256], func=AF.Square, accum_out=ssq[:, g:g + 1]),
                     r=["sy32"], w=["sjunk", ("sssq", g)])
            P.op("act", lambda e: e.activation(out=ssq[:], in_=ssq[:], func=AF.Sqrt, bias=C.eps_t[:, 0:1], scale=1.0 / 256), r=["sssq"], w=["sssq"])
            P.op("dve", lambda e: e.reciprocal(out=ssq[:], in_=ssq[:]), r=["sssq"], w=["sssq"])
            for g in range(8):
                P.op("dve", lambda e, g=g: e.scalar_tensor_tensor(out=yn[:, g * 256:(g + 1) * 256], in0=y32[:, g * 256:(g + 1) * 256],
                                                                   scalar=ssq[:, g:g + 1], in1=normg[:, g * 256:(g + 1) * 256], op0=ALU.mult, op1=ALU.mult),
                     r=["sy32", "sssq", "s_normgT"], w=[("syn", g)])
        def back_post_pe(c):
            b = c % 2
            sl = slice(c * 128, (c + 1) * 128)
            sv = "sv%d" % b
            Sb_old, Sb_new = Sbf[c % 2], Sbf[(c + 1) % 2]
            ko, kn = "sSbf%d" % (c % 2), "sSbf%d" % ((c + 1) % 2)
            for i in range(4):
                ab = i % 2
                for ci in range(4):
                    dc = i * 4 + ci
                    P.op("pe", lambda e, ci=ci, dc=dc, ab=ab: e.transpose(out=pABb[ab][:, ci * 128:(ci + 1) * 128], in_=yn[:, dc * 128:(dc + 1) * 128], identity=C.ident_bf[:]),
                         r=["syn", "ident_bf"], w=["spAB%d" % ab])
                P.op("act", lambda e, i=i, ab=ab: e.activation(out=ynT[:, i * 4:(i + 1) * 4, :].rearrange("p a b -> p (a b)"), in_=pABb[ab][:, 0:512], func=AF.Copy),
                     r=["spAB%d" % ab], w=[("synT", i)])
            for f in range(8):
                ab = f % 2
                for dc in range(16):
                    P.op("pe", lambda e, f=f, ab=ab, dc=dc: e.matmul(pAB[ab][:, 0:128], lhsT=wout[:, dc, f * 128:(f + 1) * 128], rhs=ynT[:, dc, :],
                                                                    start=(dc == 0), stop=(dc == 15)), r=["swout", "synT"], w=["spAB%d" % ab])
                P.op("dve", lambda e, f=f, ab=ab: e.scalar_tensor_tensor(out=xt[b][:, f, :], in0=pAB[ab][:, 0:128], scalar=mod[:, g0 + f:g0 + f + 1],
                                                                          in1=xt[b][:, f, :], op0=ALU.mult, op1=ALU.add),
                     r=["spAB%d" % ab, "sxt%d" % b], w=["sxt%d" % b])
            P.op("sp", lambda e: e.dma_start(out=xout_v[:, :, sl], in_=xt[b][:]), r=["sxt%d" % b], w=["x1T"], dma="sxo%d" % b)

        front_pre(0)
        for g in range(8):
            front_group(0, g)
        for c in range(NC_):
            back_pre(c)
            if c + 1 < NC_:
                front_pre(c + 1)
            for g in range(8):
                if c + 1 < NC_:
                    front_group(c + 1, g)
                back_group(c, g)
                if g == 3 and c >= 1:
                    back_post_pe(c - 1)
            back_post_ew(c)
        back_post_pe(NC_ - 1)
        P.barrier()


def build(S, stages=None, outs=("out_tok",), chain=True, only_inputs=None):
    nc = bass.Bass("TRN2", target_bir_lowering=False)
    C = Ctx()
    C.nc = nc
    C.S = S
    TO = S // 2
    allst = ["m1z", "m1x", "m2", "ffn0", "qkv", "attn", "moes"]
    if stages is None:
        stages = allst
    d = {}

    def din(name, shape, dt=F32):
        if only_inputs is not None and name not in only_inputs:
            return
        d[name] = nc.dram_tensor(name, list(shape), dt, kind="ExternalInput").ap()

    def dscr(name, shape, dt=F32):
        kind = "ExternalOutput" if name in outs else "Internal"
        d[name] = nc.dram_tensor(name, list(shape), dt, kind=kind).ap()

    din("xT", [D, S]); din("cT", [128, 8]); din("ident", [128, 128]); din("sel", [8, 8, 128])
    din("ada_w0", [D, 6 * D]); din("ada_bT0", [128, 48])
    din("ada_w1", [D, 6 * D]); din("ada_bT1", [128, 48])
    din("m_w_in", [D, M_IN]); din("convwT", [128, 32, 4]); din("convbT", [128, 32])
    din("dtbT", [128, 32]); din("alogT", [128, 32]); din("dskT", [128, 32]); din("normgT", [128, 2048])
    din("m_w_out", [2048, D]); din("tri", [128, 128]); din("negm4", [128, 4, 128]); din("blk", [64, 4096])
    din("ffn_w_gate", [D, FFN_DIM]); din("ffn_w_up", [D, FFN_DIM]); din("ffn_w_down", [FFN_DIM, D])
    din("a_w_qkv", [D, 3072]); din("lamv", [128, 4, 64]); din("sublnT", [128, 1]); din("a_w_o", [D, D])
    din("masks", [4, 128, 4, 512]); din("rolesel", [128, 2])
    din("moe_w_router", [D, N_EXP]); din("moe_w_gate", [N_EXP, D, EXP_DIM]); din("moe_w_up", [N_EXP, D, EXP_DIM])
    din("moe_w_down", [N_EXP, EXP_DIM, D]); din("final_gT", [128, 8])
    NUe = EXP_DIM // 512
    for nm_ in ("moe_wg_r0", "moe_wg_r1", "moe_wu_r0", "moe_wu_r1", "moe_wd_r0", "moe_wd_r1"):
        din(nm_, [N_EXP * NUe * 128, 2048])
    din("sul", [128, 128]); din("thr", [128, 17]); din("iotaU", [128, NUe]); din("final_gB", [128, 1024])
    TS = TO if chain else S
    NBs = (2 * TS) // MOE_BLK + N_EXP
    din("bstart", [128, NBs])
    dscr("Hrow", [TS, 1024], BF16); dscr("Xs", [NBs * MOE_BLK, 1024], BF16); dscr("Ys", [NBs * MOE_BLK, 1024]); dscr("out_tok", [TS, 1024])
    dscr("zs", [S, 2048], BF16); dscr("dtr", [S, 32]); dscr("xsB", [S, 3072], BF16)
    dscr("BT", [1024, S], BF16); dscr("CT", [1024, S], BF16)
    dscr("x1T", [D, S]); dscr("x2T", [D, S])
    dscr("QT", [1024, S], BF16); dscr("KT", [1024, S], BF16); dscr("V", [S, 1024], BF16)
    dscr("x3T", [D, TO]); dscr("x4T", [D, TO]); dscr("outT", [D, TO])
    C.d = d
    with ExitStack() as top:
        C.top = top
        C.P = Prog(nc, top)
        C.eps_t = sb(nc, top, "eps_t", [128, 1], F32)
        C.P.op("dve", lambda e: e.memset(C.eps_t[:], EPS), w=["eps_t"])
        stage_consts(C)
        x0 = d["xT"]
        if "m1z" in stages:
            stage_m1(C, x0, S, C.mod[0], "z")
        if "m1x" in stages:
            stage_m1(C, x0, S, C.mod[0], "x")
        if "m2" in stages:
            stage_m2(C, x0, d["x1T"], S, C.mod[0])
        if "ffn0" in stages:
            stage_ffn(C, d["x1T"] if chain else x0, d["x2T"], S, C.mod[0], d["ffn_w_gate"], d["ffn_w_up"], d["ffn_w_down"], FFN_DIM, "f0")
        xl1 = d["x2T"] if chain else x0
        if "qkv" in stages:
            stage_qkv(C, xl1, S, C.mod[1])
        if "attn" in stages:
            stage_attn(C, xl1, d["x3T"], S, C.mod[1])
        if "moe" in stages:
            stage_ffn(C, d["x3T"] if chain else x0, d["x4T"], TO if chain else S, C.mod[1], d["moe_w_gate"], d["moe_w_up"], d["moe_w_down"],
                      EXP_DIM, "m1", n_exp=N_EXP, wr=d["moe_w_router"])
        if "final" in stages:
            stage_final(C, d["x4T"], d["outT"], TO)
        if "moes" in stages:
            stage_moe_sparse(C, d["x3T"] if chain else x0, TO if chain else S, C.mod[1])
        C.P.emit(final_wait_keys=list(range(len(C.P.dma_cnt))))
    return nc


def host_consts(role):
    ident = np.eye(128, dtype=np.float32)
    sel = np.zeros((8, 8, 128), np.float32)
    for e in range(8):
        sel[e, e, :] = 1.0
    s_ = np.arange(128)
    tri = (s_[:, None] <= s_[None, :]).astype(np.float32)
    negm4 = np.ascontiguousarray(np.tile(((s_[:, None] > s_[None, :]) * -30000.0).astype(np.float32)[:, None, :], (1, 4, 1)))
    blk = np.zeros((64, 32, 128), np.float32)
    for r_ in range(32):
        blk[r_, r_, :] = 1.0
        blk[32 + r_, r_, :] = 1.0
    blk = blk.reshape(64, 4096)
    k_ = np.arange(512)
    trib = (k_[None, :] >= k_[:, None]).astype(np.float32).reshape(4, 128, 512).transpose(1, 0, 2)
    ones = np.ones_like(trib)
    zeros = np.zeros_like(trib)
    if role == 1:
        masks = np.stack([ones, trib, trib, zeros])
    else:
        masks = np.stack([trib, zeros, ones, trib])
    rolesel = np.zeros((128, 2), np.float32)
    rolesel[:, role] = 1.0
    sul = (s_[:, None] < s_[None, :]).astype(np.float32)
    thr = np.tile((np.arange(17) * float(MOE_BLK))[None, :], (128, 1)).astype(np.float32)
    NUe = EXP_DIM // 512
    iotaU = (np.arange(NUe)[None, :] * 128 + s_[:, None]).astype(np.float32)
    return {"sul": sul, "thr": thr, "iotaU": iotaU, "ident": ident, "sel": sel, "tri": tri, "negm4": negm4, "blk": blk, "masks": np.ascontiguousarray(masks), "rolesel": rolesel}


def make_in_maps(inp, S, dense_moe=False, TS=None):
    f = lambda a: np.ascontiguousarray(np.asarray(a, dtype=np.float32))
    col = lambda v, n: f(np.asarray(v).reshape(n, 128).T)
    til = lambda v: f(np.tile(np.asarray(v)[None, :], (128, 1)))
    shared = {
        "m_w_in": f(inp["m_w_in"]), "m_w_out": f(inp["m_w_out"]),
        "convwT": f(np.asarray(inp["m_conv_w"]).reshape(4, 32, 128).transpose(2, 1, 0)),
        "convbT": col(inp["m_conv_b"], 32), "dtbT": til(inp["m_dt_bias"]), "alogT": til(inp["m_a_log"]),
        "dskT": til(inp["m_d_skip"]), "normgT": til(inp["m_norm_g"]),
        "ffn_w_gate": f(inp["ffn_w_gate"]), "ffn_w_up": f(inp["ffn_w_up"]), "ffn_w_down": f(inp["ffn_w_down"]),
        "a_w_qkv": f(inp["a_w_qkv"]), "a_w_o": f(inp["a_w_o"]),
        "lamv": f(np.tile(np.stack([inp["a_lam_q1"], inp["a_lam_k1"], inp["a_lam_q2"], inp["a_lam_k2"]])[None], (128, 1, 1))),
        "sublnT": col(inp["a_subln_g"], 1),
        "moe_w_router": f(inp["moe_w_router"]), "final_gT": col(inp["final_g"], 8), "final_gB": til(inp["final_g"]),
    }
    if TS is None:
        TS = S // 2
    NBs = (2 * TS) // MOE_BLK + N_EXP
    shared["bstart"] = f(np.tile((np.arange(NBs) * float(MOE_BLK))[None, :], (128, 1)))
    if dense_moe:
        shared.update({"moe_w_gate": f(inp["moe_w_gate"]), "moe_w_up": f(inp["moe_w_up"]), "moe_w_down": f(inp["moe_w_down"])})
    else:
        NUe = EXP_DIM // 512
        for nm_, key in (("moe_wg_r", "moe_w_gate"), ("moe_wu_r", "moe_w_up")):
            w_ = np.asarray(inp[key], dtype=np.float32).reshape(N_EXP, 2, 4, 128, NUe, 512)
            w2_ = np.ascontiguousarray(w_.transpose(1, 0, 4, 3, 2, 5)).reshape(2, N_EXP * NUe * 128, 2048)
            shared[nm_ + "0"], shared[nm_ + "1"] = w2_[0], w2_[1]
        w_ = np.asarray(inp["moe_w_down"], dtype=np.float32).reshape(N_EXP, NUe, 2, 2, 128, 1024)
        w2_ = np.ascontiguousarray(w_.transpose(2, 0, 1, 4, 3, 5)).reshape(2, N_EXP * NUe * 128, 2048)
        shared["moe_wd_r0"], shared["moe_wd_r1"] = w2_[0], w2_[1]
    shared["ada_w0"] = f(inp["ada_w0"]); shared["ada_bT0"] = col(inp["ada_b0"], 48)
    shared["ada_w1"] = f(inp["ada_w1"]); shared["ada_bT1"] = col(inp["ada_b1"], 48)
    hc = [host_consts(0), host_consts(1)]
    maps = []
    for c in range(8):
        b, role = c // 2, c % 2
        m = dict(shared)
        m.update(hc[role])
        m["xT"] = f(np.asarray(inp["x"])[b, :S, :].T)
        m["cT"] = col(np.asarray(inp["c"])[b], 8)
        maps.append(m)
    return maps


def assemble(res, S, B=4):
    TO = S // 2
    n_slots = S // 1024
    TA, TB = own_tiles(n_slots)
    out = np.zeros((B, S, D), np.float32)
    for c in range(2 * B):
        b, role = c // 2, c % 2
        o = np.asarray(res[c]["out_tok"])
        tiles = TA if role == 0 else TB
        for si, t in enumerate(tiles):
            out[b, t * 512:(t + 1) * 512, :] = o[si * 512:(si + 1) * 512, :]
    return out


_NC_CACHE = {}
_USED_INPUTS = {"xT", "cT", "ident", "sel", "ada_w0", "ada_bT0", "ada_w1", "ada_bT1", "m_w_in", "convwT", "convbT", "dtbT", "alogT",
                "dskT", "normgT", "m_w_out", "tri", "negm4", "blk", "ffn_w_gate", "ffn_w_up", "ffn_w_down", "a_w_qkv", "lamv", "sublnT",
                "a_w_o", "masks", "rolesel", "moe_w_router", "moe_wg_r0", "moe_wg_r1", "moe_wu_r0", "moe_wu_r1", "moe_wd_r0", "moe_wd_r1", "sul", "thr", "iotaU", "final_gB", "bstart"}


def kernel(**inputs):
    S = int(np.asarray(inputs["x"]).shape[1])
    if S not in _NC_CACHE:
        _NC_CACHE[S] = build(S, only_inputs=_USED_INPUTS)
    nc = _NC_CACHE[S]
    maps = [{k: v for k, v in m.items() if k in _USED_INPUTS} for m in make_in_maps(inputs, S)]
    res = run_bass_kernel_spmd(nc, maps, core_ids=list(range(8)))
    return assemble(res.results, S)


U32 = mybir.dt.uint32
MOE_BLK = 512


def stage_moe_sparse(C, x_in, T, mod):
    nc, P, d = C.nc, C.P, C.d
    BLK = MOE_BLK
    NCH = T // 128
    NB = (2 * T) // BLK + N_EXP
    NR = NB * BLK
    HG = 4
    NU = EXP_DIM // (HG * 128)
    xin_v = x_in.rearrange("(k p) t -> p k t", p=128)
    g0 = 5 * 8
    st = ExitStack()
    with st as ls:
        S1A = sb(nc, ls, "eS1A", [128, NCH, 8], F32)
        S2A = sb(nc, ls, "eS2A", [128, NCH, 8], F32)
        POS = sb(nc, ls, "ePOS", [128, NCH, 8], F32)
        GW = sb(nc, ls, "eGW", [128, NCH, 2], F32)
        base = sb(nc, ls, "ebase", [128, 8], F32)
        D1f = sb(nc, ls, "eD1f", [128, NCH], F32)
        D2f = sb(nc, ls, "eD2f", [128, NCH], F32)
        D1 = sb(nc, ls, "eD1", [128, NCH], U32)
        D2 = sb(nc, ls, "eD2", [128, NCH], U32)
        WIf = sb(nc, ls, "eWIf", [128, NB, NU], F32)
        WI = sb(nc, ls, "eWI", [128, NB, NU], U32)
        sul = sb(nc, ls, "esul", [128, 128], F32)
        thr = sb(nc, ls, "ethr", [128, 17], F32)
        bst = sb(nc, ls, "ebst", [128, NB], F32)
        iop = sb(nc, ls, "eiop", [128, NU], F32)
        wrt = sb(nc, ls, "ewr", [128, 8, 8], F32)
        sel = sb(nc, ls, "esel", [8, 8, 128], F32)
        fgB = sb(nc, ls, "efgB", [128, 1024], F32)
        g2b = sb(nc, ls, "eg2b", [128, 1024], F32)
        P.op("sp", lambda e: e.dma_start(out=sul[:], in_=d["sul"][:, :]), w=["esul"], dma="e_sul")
        P.op("sp", lambda e: e.dma_start(out=thr[:], in_=d["thr"][:, :]), w=["ethr"], dma="e_thr")
        P.op("sp", lambda e: e.dma_start(out=bst[:], in_=d["bstart"][:, :]), w=["ebst"], dma="e_bst")
        P.op("sp", lambda e: e.dma_start(out=iop[:], in_=d["iotaU"][:, :]), w=["eiop"], dma="e_iop")
        P.op("sp", lambda e: e.dma_start(out=wrt[:], in_=d["moe_w_router"].rearrange("(k p) e -> p k e", p=128)), w=["ewr"], dma="e_wr")
        P.op("sp", lambda e: e.dma_start(out=sel[:], in_=d["sel"][:, :, :]), w=["esel"], dma="e_sel")
        P.op("sp", lambda e: e.dma_start(out=fgB[:], in_=d["final_gB"][:, :]), w=["efgB"], dma="e_fgB")
        P.op("dve", lambda e: e.memset(base[:], 0.0), w=["ebase"])
        hrow_v = d["Hrow"].rearrange("(n p) f -> p n f", p=128)
        with ExitStack() as l2:
            xt = [sb(nc, l2, "ext%d" % i, [128, 8, 512], F32) for i in range(2)]
            hT = sb(nc, l2, "ehT", [128, 8, 512], BF16)
            h32 = sb(nc, l2, "eh32", [128, 8, 512], F32)
            sq = sb(nc, l2, "esq", [128, 8, 512], BF16)
            tmp = [sb(nc, l2, "etmp%d" % i, [128, 512], F32) for i in range(2)]
            sqv = sb(nc, l2, "esqv", [128, 512], F32)
            lg = sb(nc, l2, "elg", [128, 8], F32)
            mx = sb(nc, l2, "emx", [128, 8], F32)
            gd = sb(nc, l2, "egd", [128, 2], F32)
            Sel = sb(nc, l2, "eSel", [128, 8], F32)
            hrow = [sb(nc, l2, "ehrow%d" % i, [128, 1024], BF16) for i in range(2)]
            ps_x = l2.enter_context(nc.psum_tensor("epsx", [128, 512], F32))
            ps_r = l2.enter_context(nc.psum_tensor("epsr", [128, 512], F32))
            ps_t32 = l2.enter_context(nc.psum_tensor("epst", [128, 512], F32))
            ps_tR = ps_t32[:].bitcast(BF16)
            for ti in range(T // 512):
                b = ti % 2
                P.op("sp", lambda e, b=b, ti=ti: e.dma_start(out=xt[b][:], in_=xin_v[:, :, ti * 512:(ti + 1) * 512]), w=["ext%d" % b], dma="e_x%d" % b)
                norm_mod(C, xt[b][:], hT, ps_x, sq, tmp, sqv, mod, 3, "e", "ext%d" % b, "ehT", h32=h32)
                for c4 in range(4):
                    c = ti * 4 + c4
                    hb = c % 2
                    for k in range(8):
                        P.op("pe", lambda e, c4=c4, k=k: e.matmul(ps_r[:, 0:8], lhsT=h32[:, k, c4 * 128:(c4 + 1) * 128], rhs=wrt[:, k, :],
                                                                  start=(k == 0), stop=(k == 7)), r=[("h32", k), "ewr"], w=["epsr"])
                    P.op("dve", lambda e: e.tensor_copy(out=lg[:], in_=ps_r[:, 0:8]), r=["epsr"], w=["elg"])
                    P.op("dve", lambda e: e.max(out=mx[:], in_=lg[:]), r=["elg"], w=["emx"])
                    P.op("dve", lambda e: e.tensor_tensor(out=gd[:, 0:1], in0=mx[:, 0:1], in1=mx[:, 1:2], op=ALU.subtract), r=["emx"], w=["egd"])
                    P.op("act", lambda e, c=c: e.activation(out=GW[:, c, 0:1], in_=gd[:, 0:1], func=AF.Sigmoid), r=["egd"], w=[("eGW", c)])
                    P.op("dve", lambda e, c=c: e.tensor_scalar(out=GW[:, c, 1:2], in0=GW[:, c, 0:1], scalar1=-1.0, scalar2=1.0, op0=ALU.mult, op1=ALU.add),
                         r=[("eGW", c)], w=[("eGW", c)])
                    P.op("dve", lambda e, c=c: e.tensor_scalar(out=S1A[:, c, :], in0=lg[:], scalar1=mx[:, 0:1], scalar2=None, op0=ALU.is_equal),
                         r=["elg", "emx"], w=[("eS1A", c)])
                    P.op("dve", lambda e, c=c: e.tensor_scalar(out=S2A[:, c, :], in0=lg[:], scalar1=mx[:, 1:2], scalar2=None, op0=ALU.is_equal),
                         r=["elg", "emx"], w=[("eS2A", c)])
                    P.op("dve", lambda e, c=c: e.tensor_tensor(out=Sel[:], in0=S1A[:, c, :], in1=S2A[:, c, :], op=ALU.add),
                         r=[("eS1A", c), ("eS2A", c)], w=["eSel"])
                    P.op("pe", lambda e: e.matmul(ps_r[:, 8:16], lhsT=sul[:], rhs=Sel[:], start=True, stop=True), r=["esul", "eSel"], w=["epsr"])
                    P.op("pe", lambda e: e.matmul(ps_r[:, 16:24], lhsT=C.ones_f[:], rhs=Sel[:], start=True, stop=True), r=["ones_f", "eSel"], w=["epsr"])
                    P.op("dve", lambda e, c=c: e.tensor_tensor(out=POS[:, c, :], in0=ps_r[:, 8:16], in1=base[:], op=ALU.add), r=["epsr", "ebase"], w=[("ePOS", c)])
                    P.op("dve", lambda e: e.tensor_tensor(out=base[:], in0=ps_r[:, 16:24], in1=base[:], op=ALU.add), r=["epsr", "ebase"], w=["ebase"])
                    for k in range(8):
                        P.op("pe", lambda e, k=k, c4=c4: e.transpose(out=ps_tR[:, k * 128:(k + 1) * 128], in_=hT[:, k, c4 * 128:(c4 + 1) * 128], identity=C.ident_bf[:]),
                             r=["ehT", "ident_bf"], w=["epst"])
                    P.op("act", lambda e, hb=hb: e.activation(out=hrow[hb][:], in_=ps_tR[:, 0:1024], func=AF.Copy), r=["epst"], w=["ehrow%d" % hb])
                    P.op("sp", lambda e, hb=hb, c=c: e.dma_start(out=hrow_v[:, c, :], in_=hrow[hb][:]), r=["ehrow%d" % hb], w=["Hrow"], dma="e_ho%d" % hb)
            cmp = sb(nc, l2, "ecmp", [128, 32], F32)
            nbk = sb(nc, l2, "enbk", [128, 8], F32)
            pend = sb(nc, l2, "epend", [128, 8], F32)
            pstart = sb(nc, l2, "epstart", [128, 8], F32)
            bexp = sb(nc, l2, "ebexp", [128, NB], F32)
            ptmp = sb(nc, l2, "eptmp", [128, NCH, 8], F32)
            for e_ in range(8):
                P.op("dve", lambda e, e_=e_: e.tensor_scalar(out=cmp[:, 0:17], in0=thr[:], scalar1=base[:, e_:e_ + 1], scalar2=None, op0=ALU.is_lt),
                     r=["ethr", "ebase"], w=["ecmp"])
                P.op("dve", lambda e, e_=e_: e.tensor_reduce(out=nbk[:, e_:e_ + 1], in_=cmp[:, 0:17], axis=AX.X, op=ALU.add), r=["ecmp"], w=["enbk"])
            P.op("dve", lambda e: e.tensor_scalar(out=nbk[:], in0=nbk[:], scalar1=float(BLK), scalar2=None, op0=ALU.mult), r=["enbk"], w=["enbk"])
            P.op("dve", lambda e: e.tensor_copy(out=pend[:, 0:1], in_=nbk[:, 0:1]), r=["enbk"], w=["epend"])
            for e_ in range(1, 8):
                P.op("dve", lambda e, e_=e_: e.tensor_tensor(out=pend[:, e_:e_ + 1], in0=pend[:, e_ - 1:e_], in1=nbk[:, e_:e_ + 1], op=ALU.add),
                     r=["epend", "enbk"], w=["epend"])
            P.op("dve", lambda e: e.tensor_tensor(out=pstart[:], in0=pend[:], in1=nbk[:], op=ALU.subtract), r=["epend", "enbk"], w=["epstart"])
            P.op("dve", lambda e: e.memset(bexp[:], 0.0), w=["ebexp"])
            for e_ in range(8):
                P.op("dve", lambda e, e_=e_: e.tensor_scalar(out=cmp[:, 0:NB], in0=bst[:], scalar1=pend[:, e_:e_ + 1], scalar2=None, op0=ALU.is_ge),
                     r=["ebst", "epend"], w=["ecmp"])
                P.op("dve", lambda e: e.tensor_tensor(out=bexp[:], in0=bexp[:], in1=cmp[:, 0:NB], op=ALU.add), r=["ebexp", "ecmp"], w=["ebexp"])
            P.op("dve", lambda e: e.tensor_scalar(out=bexp[:], in0=bexp[:], scalar1=float(N_EXP - 1), scalar2=float(NU * 128), op0=ALU.min, op1=ALU.mult),
                 r=["ebexp"], w=["ebexp"])
            P.op("dve", lambda e: e.tensor_tensor(out=WIf[:], in0=bexp[:].unsqueeze(2).broadcast_to([128, NB, NU]),
                                                  in1=iop[:].unsqueeze(1).broadcast_to([128, NB, NU]), op=ALU.add), r=["ebexp", "eiop"], w=["eWIf"])
            P.op("dve", lambda e: e.tensor_copy(out=WI[:], in_=WIf[:]), r=["eWIf"], w=["eWI"])
            P.op("dve", lambda e: e.tensor_tensor(out=ptmp[:], in0=POS[:], in1=pstart[:].unsqueeze(1).broadcast_to([128, NCH, 8]), op=ALU.add),
                 r=["ePOS", "epstart"], w=["eptmp"])
            for (SA, Df, Du, nm_) in ((S1A, D1f, D1, "1"), (S2A, D2f, D2, "2")):
                P.op("dve", lambda e, SA=SA: e.tensor_tensor(out=SA[:], in0=SA[:], in1=ptmp[:], op=ALU.mult), r=["eS1A", "eS2A", "eptmp"], w=["eS%sA" % nm_])
                P.op("dve", lambda e, SA=SA, Df=Df: e.tensor_reduce(out=Df[:], in_=SA[:], axis=AX.X, op=ALU.add), r=["eS%sA" % nm_], w=["eD%sf" % nm_])
                P.op("dve", lambda e, Df=Df: e.tensor_scalar(out=Df[:], in0=Df[:], scalar1=float(NR - 1), scalar2=None, op0=ALU.min),
                     r=["eD%sf" % nm_], w=["eD%sf" % nm_])
                P.op("dve", lambda e, Df=Df, Du=Du: e.tensor_copy(out=Du[:], in_=Df[:]), r=["eD%sf" % nm_], w=["eD%s" % nm_])
            zrow = sb(nc, l2, "ezrow", [128, 4, 1024], BF16)
            P.op("pool", lambda e: e.memset(zrow[:], 0.0), w=["ezrow"])
            xs_z = d["Xs"].rearrange("(n p) f -> p n f", p=128)
            for i in range(NB):
                P.op("sp", lambda e, i=i: e.dma_start(out=xs_z[:, i * 4:(i + 1) * 4, :], in_=zrow[:]), r=["ezrow"], w=[("Xz", i)], dma="e_xz%d" % (i % 4))
            hrow4 = hrow + [sb(nc, l2, "ehrow%d" % i, [128, 1024], BF16) for i in (2, 3)]
            for c in range(NCH if "noS" not in DBG else 0):
                hb = c % 4
                P.op("sp", lambda e, hb=hb, c=c: e.dma_start(out=hrow4[hb][:], in_=hrow_v[:, c, :]), r=["Hrow"], w=["ehrow%d" % hb], dma="e_hi%d" % hb)
                for (Du, nm_) in ((D1, "1"), (D2, "2")):
                    P.op("pool", lambda e, hb=hb, c=c, Du=Du: e.indirect_dma_start(
                        out=d["Xs"][:, :], out_offset=bass.IndirectOffsetOnAxis(ap=Du[:, c:c + 1], axis=0), in_=hrow4[hb][:], in_offset=None),
                        r=["ehrow%d" % hb, "eD%s" % nm_, "Xz"], w=[("Xsc", 2 * c + int(nm_))], dma="e_sc%s%d" % (nm_, hb))
        P.barrier()
        with ExitStack() as l2:
            g2T = sb(nc, l2, "eg2T", [8, 128], F32)
            pg = [l2.enter_context(nc.psum_tensor("epg%d" % i, [128, 512], F32)) for i in range(2)]
            P.op("pe", lambda e: e.transpose(out=pg[0][0:8, 0:128], in_=mod[:, g0:g0 + 8], identity=C.ident_f[:]), r=["ident_f"], w=["epg0"])
            P.op("act", lambda e: e.activation(out=g2T[:], in_=pg[0][0:8, 0:128], func=AF.Copy), r=["epg0"], w=["eg2T"])
            for k in range(8):
                P.op("pe", lambda e, k=k: e.matmul(pg[k // 4][:, (k % 4) * 128:(k % 4 + 1) * 128], lhsT=sel[:, k, :], rhs=g2T[:], start=True, stop=True),
                     r=["esel", "eg2T"], w=["epg%d" % (k // 4)])
            for hf in range(2):
                P.op("act", lambda e, hf=hf: e.activation(out=g2b[:, hf * 512:(hf + 1) * 512], in_=pg[hf][:], func=AF.Copy), r=["epg%d" % hf], w=["eg2b"])
            P.op("dve", lambda e: e.tensor_copy(out=g2T[:], in_=g2T[:]), r=["eg2b", "eg2T"], w=["eg2T"])
        P.barrier()
        with ExitStack() as l2:
            xrow = [sb(nc, l2, "exrow%d" % i, [128, 4, 1024], BF16) for i in range(2)]
            XT = sb(nc, l2, "eXT", [128, 8, 512], BF16)
            acc = sb(nc, l2, "eacc", [128, 8, 512], F32)
            yrow = sb(nc, l2, "eyrow", [128, 4, 1024], F32)
            wgb = [sb(nc, l2, "ewg%d" % i, [128, 8, HG * 128], BF16) for i in range(2)]
            wub = [sb(nc, l2, "ewu%d" % i, [128, 8, HG * 128], BF16) for i in range(2)]
            wdb = [sb(nc, l2, "ewd%d" % i, [128, HG, 1024], BF16) for i in range(2)]
            aT = [sb(nc, l2, "eaT%d" % i, [128, HG, 512], BF16) for i in range(2)]
            sg = [sb(nc, l2, "esg%d" % i, [128, 512], F32) for i in range(2)]
            ps_g = [l2.enter_context(nc.psum_tensor("epsg%d" % i, [128, 512], F32)) for i in range(2)]
            ps_u = [l2.enter_context(nc.psum_tensor("epsu%d" % i, [128, 512], F32)) for i in range(2)]
            ps_d = [l2.enter_context(nc.psum_tensor("epsd%d" % i, [128, 512], F32)) for i in range(2)]
            ps_t = [l2.enter_context(nc.psum_tensor("epstt%d" % i, [128, 512], F32)) for i in range(2)]
            ps_tb = [p_[:].bitcast(BF16) for p_ in ps_t]
            xs_v = d["Xs"].rearrange("(n p) f -> p n f", p=128)
            ys_v = d["Ys"].rearrange("(n p) f -> p n f", p=128)
            wgv = [d["moe_wg_r0"], d["moe_wg_r1"]]
            wuv = [d["moe_wu_r0"], d["moe_wu_r1"]]
            wdv = [d["moe_wd_r0"], d["moe_wd_r1"]]
            ug = 0
            tcnt = 0
            pend_dn = [None]
            for i in range(NB if "noE" not in DBG else 0):
                xb_ = i % 2
                P.op("sp", lambda e, xb_=xb_, i=i: e.dma_start(out=xrow[xb_][:], in_=xs_v[:, i * 4:(i + 1) * 4, :]), r=["Xs"], w=["exrow%d" % xb_], dma="e_xr%d" % xb_)
                for k in range(8):
                    tb = tcnt % 2
                    tcnt += 1
                    for n in range(4):
                        P.op("pe", lambda e, tb=tb, n=n, k=k, xb_=xb_: e.transpose(out=ps_tb[tb][:, n * 128:(n + 1) * 128], in_=xrow[xb_][:, n, k * 128:(k + 1) * 128],
                                                                                identity=C.ident_bf[:]), r=["exrow%d" % xb_, "ident_bf"], w=["epstt%d" % tb])
                    P.op("act" if k % 2 else "dve", (lambda e, tb=tb, k=k: e.activation(out=XT[:, k, :], in_=ps_tb[tb][:, 0:512], func=AF.Copy)) if k % 2 else
                         (lambda e, tb=tb, k=k: e.tensor_copy(out=XT[:, k, :], in_=ps_tb[tb][:, 0:512])), r=["epstt%d" % tb], w=[("eXT", k)])
                for uu in range(NU):
                    b = ug % 2
                    ug += 1
                    for hf in range(2):
                        P.op("pool", lambda e, b=b, i=i, uu=uu, hf=hf: e.indirect_dma_start(
                            out=wgb[b][:, hf * 4:(hf + 1) * 4, :].rearrange("p k f -> p (k f)"), out_offset=None, in_=wgv[hf][:, :],
                            in_offset=bass.IndirectOffsetOnAxis(ap=WI[:, i, uu:uu + 1], axis=0)),
                            r=["eWI"], w=[("ewg%d" % b, hf)], dma="e_wg%d%d" % (b, hf))
                        P.op("pool", lambda e, b=b, i=i, uu=uu, hf=hf: e.indirect_dma_start(
                            out=wub[b][:, hf * 4:(hf + 1) * 4, :].rearrange("p k f -> p (k f)"), out_offset=None, in_=wuv[hf][:, :],
                            in_offset=bass.IndirectOffsetOnAxis(ap=WI[:, i, uu:uu + 1], axis=0)),
                            r=["eWI"], w=[("ewu%d" % b, hf)], dma="e_wu%d%d" % (b, hf))
                        P.op("pool", lambda e, b=b, i=i, uu=uu, hf=hf: e.indirect_dma_start(
                            out=wdb[b][:, hf * 2:(hf + 1) * 2, :].rearrange("p k f -> p (k f)"), out_offset=None, in_=wdv[hf][:, :],
                            in_offset=bass.IndirectOffsetOnAxis(ap=WI[:, i, uu:uu + 1], axis=0)),
                            r=["eWI"], w=[("ewd%d" % b, hf)], dma="e_wd%d%d" % (b, hf))
                    ab = ug % 2
                    for j in range(HG):
                        pb = j % 2
                        for k in range(8):
                            P.op("pe", lambda e, b=b, pb=pb, j=j, k=k: e.matmul(ps_g[pb][:], lhsT=wgb[b][:, k, j * 128:(j + 1) * 128], rhs=XT[:, k, :],
                                                                                start=(k == 0), stop=(k == 7)), r=["ewg%d" % b, "eXT"], w=["epsg%d" % pb])
                        for k in range(8):
                            P.op("pe", lambda e, b=b, pb=pb, j=j, k=k: e.matmul(ps_u[pb][:], lhsT=wub[b][:, k, j * 128:(j + 1) * 128], rhs=XT[:, k, :],
                                                                                start=(k == 0), stop=(k == 7)), r=["ewu%d" % b, "eXT"], w=["epsu%d" % pb])
                        P.op("act", lambda e, pb=pb: e.activation(out=sg[pb][:], in_=ps_g[pb][:], func=AF.Silu), r=["epsg%d" % pb], w=["esg%d" % pb])
                        P.op("dve", lambda e, pb=pb, ab=ab, j=j: e.tensor_tensor(out=aT[ab][:, j, :], in0=sg[pb][:], in1=ps_u[pb][:], op=ALU.mult),
                             r=["esg%d" % pb, "epsu%d" % pb], w=[("eaT%d" % ab, j)])
                    def _down(b=b, ab=ab, uu=uu):
                        for f in range(8):
                            db = f % 2
                            for j in range(HG):
                                P.op("pe", lambda e, b=b, db=db, j=j, f=f, ab=ab: e.matmul(ps_d[db][:], lhsT=wdb[b][:, j, f * 128:(f + 1) * 128], rhs=aT[ab][:, j, :],
                                                                                           start=(j == 0), stop=(j == HG - 1)), r=["ewd%d" % b, ("eaT%d" % ab, j)], w=["epsd%d" % db])
                            if uu == 0:
                                P.op("act", lambda e, db=db, f=f: e.activation(out=acc[:, f, :], in_=ps_d[db][:], func=AF.Copy), r=["epsd%d" % db], w=[("eacc", f)])
                            else:
                                P.op("dve", lambda e, db=db, f=f: e.tensor_tensor(out=acc[:, f, :], in0=ps_d[db][:], in1=acc[:, f, :], op=ALU.add),
                                     r=["epsd%d" % db, ("eacc", f)], w=[("eacc", f)])
                    _down()
                for n in range(4):
                    for hf in range(2):
                        tb = tcnt % 2
                        tcnt += 1
                        for kk in range(4):
                            k = hf * 4 + kk
                            P.op("pe", lambda e, tb=tb, kk=kk, k=k, n=n: e.transpose(out=ps_t[tb][:, kk * 128:(kk + 1) * 128], in_=acc[:, k, n * 128:(n + 1) * 128],
                                                                                     identity=C.ident_f[:]), r=[("eacc", k), "ident_f"], w=["epstt%d" % tb])
                        P.op("act" if hf else "dve", (lambda e, tb=tb, n=n, hf=hf: e.activation(out=yrow[:, n, hf * 512:(hf + 1) * 512], in_=ps_t[tb][:], func=AF.Copy)) if hf else
                             (lambda e, tb=tb, n=n, hf=hf: e.tensor_copy(out=yrow[:, n, hf * 512:(hf + 1) * 512], in_=ps_t[tb][:])), r=["epstt%d" % tb], w=[("eyrow", n)])
                P.op("sp", lambda e, i=i: e.dma_start(out=ys_v[:, i * 4:(i + 1) * 4, :], in_=yrow[:]), r=["eyrow"], w=["Ys"], dma="e_yo")
        P.barrier()
        with ExitStack() as l2:
            xc = [sb(nc, l2, "exc%d" % i, [128, 8, 128], F32) for i in range(2)]
            xr = [sb(nc, l2, "exr%d" % i, [128, 1024], F32) for i in range(2)]
            y1 = [sb(nc, l2, "ey1%d" % i, [128, 1024], F32) for i in range(2)]
            y2 = [sb(nc, l2, "ey2%d" % i, [128, 1024], F32) for i in range(2)]
            junk = sb(nc, l2, "ejunk", [128, 1024], F32)
            ss = sb(nc, l2, "ess", [128, 2], F32)
            pc = [l2.enter_context(nc.psum_tensor("epc%d" % i, [128, 512], F32)) for i in range(4)]
            out_v = d["out_tok"].rearrange("(n p) f -> p n f", p=128)
            for c in range(NCH if "noC" not in DBG else 0):
                b = c % 2
                P.op("sp", lambda e, b=b, c=c: e.dma_start(out=xc[b][:], in_=xin_v[:, :, c * 128:(c + 1) * 128]), w=["exc%d" % b], dma="e_xc%d" % b)
                P.op("pool", lambda e, b=b, c=c: e.indirect_dma_start(out=y1[b][:], out_offset=None, in_=d["Ys"][:, :],
                                                                      in_offset=bass.IndirectOffsetOnAxis(ap=D1[:, c:c + 1], axis=0)),
                     r=["Ys", "eD1"], w=["ey1%d" % b], dma="e_g1%d" % b)
                P.op("pool", lambda e, b=b, c=c: e.indirect_dma_start(out=y2[b][:], out_offset=None, in_=d["Ys"][:, :],
                                                                      in_offset=bass.IndirectOffsetOnAxis(ap=D2[:, c:c + 1], axis=0)),
                     r=["Ys", "eD2"], w=["ey2%d" % b], dma="e_g2%d" % b)
                for hf in range(2):
                    pcb = pc[(c % 2) * 2 + hf]
                    pk = "epc%d" % ((c % 2) * 2 + hf)
                    for kk in range(4):
                        k = hf * 4 + kk
                        P.op("pe", lambda e, pcb=pcb, kk=kk, k=k, b=b: e.transpose(out=pcb[:, kk * 128:(kk + 1) * 128], in_=xc[b][:, k, :], identity=C.ident_f[:]),
                             r=["exc%d" % b, "ident_f"], w=[pk])
                    P.op("act", lambda e, pcb=pcb, hf=hf, b=b: e.activation(out=xr[b][:, hf * 512:(hf + 1) * 512], in_=pcb[:], func=AF.Copy), r=[pk], w=[("exr%d" % b, hf)])
                P.op("dve", lambda e, b=b, c=c: e.tensor_scalar(out=y1[b][:], in0=y1[b][:], scalar1=GW[:, c, 0:1], scalar2=None, op0=ALU.mult),
                     r=["ey1%d" % b, "eGW"], w=["ey1%d" % b])
                P.op("dve", lambda e, b=b, c=c: e.scalar_tensor_tensor(out=y1[b][:], in0=y2[b][:], scalar=GW[:, c, 1:2], in1=y1[b][:], op0=ALU.mult, op1=ALU.add),
                     r=["ey1%d" % b, "ey2%d" % b, "eGW"], w=["ey1%d" % b])
                P.op("pool", lambda e, b=b: e.tensor_tensor(out=y1[b][:], in0=y1[b][:], in1=g2b[:], op=ALU.mult), r=["ey1%d" % b, "eg2b"], w=["ey1%d" % b])
                P.op("dve", lambda e, b=b: e.tensor_tensor(out=xr[b][:], in0=xr[b][:], in1=y1[b][:], op=ALU.add), r=["exr%d" % b, "ey1%d" % b], w=["exr%d" % b])
                P.op("act", lambda e, b=b: e.activation(out=junk[:], in_=xr[b][:], func=AF.Square, accum_out=ss[:, 0:1]), r=["exr%d" % b], w=["ejunk", "ess"])
                P.op("act", lambda e: e.activation(out=ss[:, 1:2], in_=ss[:, 0:1], func=AF.Sqrt, bias=C.eps_t[:, 0:1], scale=1.0 / D), r=["ess"], w=["ess"])
                P.op("dve", lambda e: e.reciprocal(out=ss[:, 1:2], in_=ss[:, 1:2]), r=["ess"], w=["ess"])
                P.op("dve", lambda e, b=b: e.scalar_tensor_tensor(out=xr[b][:], in0=xr[b][:], scalar=ss[:, 1:2], in1=fgB[:], op0=ALU.mult, op1=ALU.mult),
                     r=["exr%d" % b, "ess", "efgB"], w=["exr%d" % b])
                P.op("sp", lambda e, b=b, c=c: e.dma_start(out=out_v[:, c, :], in_=xr[b][:]), r=["exr%d" % b], w=["out_tok"], dma="e_oo%d" % b)
        P.barrier()
```

```python
from contextlib import ExitStack
import numpy as np
import ml_dtypes
import concourse.bass as bass
import concourse.mybir as mybir
from concourse.bass_utils import run_bass_kernel_spmd

F32 = mybir.dt.float32
BF16 = mybir.dt.bfloat16
AF = mybir.ActivationFunctionType
ALU = mybir.AluOpType
AX = mybir.AxisListType

D = 1024
KC = 8
EPS = 1e-6
FFN_DIM = 2816
N_EXP = 8
EXP_DIM = 3584

ENGS = ["pe", "act", "dve", "pool", "sp"]


def _split(key):
    if isinstance(key, tuple):
        return key[0], key[1]
    return key, None


class Prog:
    def __init__(self, nc, stack):
        self.nc = nc
        self.stack = stack
        self.ops = {e: [] for e in ENGS}
        self.res = {}
        self.dma_cnt = []
        self.key2phys = {}
        self.free_phys = {True: [], False: []}
        self.phys_sw = []
        self.base = {}

    @staticmethod
    def _merge(d, ev):
        k = (ev[0], ev[1])
        if d.get(k, -1) < ev[2]:
            d[k] = ev[2]

    def _collect(self, deps, reads, writes):
        for key in reads:
            name, idx = _split(key)
            ent = self.res.get(name)
            if ent is None:
                continue
            for (i, ev) in ent["w"]:
                if i is None or idx is None or i == idx:
                    self._merge(deps, ev)
        for key in writes:
            name, idx = _split(key)
            ent = self.res.get(name)
            if ent is None:
                continue
            for (i, ev) in ent["w"] + ent["r"]:
                if i is None or idx is None or i == idx:
                    self._merge(deps, ev)

    def _update(self, ev, reads, writes):
        for key in reads:
            name, idx = _split(key)
            ent = self.res.setdefault(name, {"w": [], "r": []})
            ent["r"] = [(i, e) for (i, e) in ent["r"]
                        if not (i == idx and e[0] == ev[0] and e[1] == ev[1])]
            ent["r"].append((idx, ev))
        for key in writes:
            name, idx = _split(key)
            ent = self.res.setdefault(name, {"w": [], "r": []})
            if idx is None:
                ent["w"] = [(None, ev)]
                ent["r"] = []
            else:
                ent["w"] = [(i, e) for (i, e) in ent["w"] if i != idx]
                ent["w"].append((idx, ev))
                ent["r"] = [(i, e) for (i, e) in ent["r"] if i != idx]

    def op(self, eng, fn, r=(), w=(), dma=None):
        deps = dict(self.base)
        self._collect(deps, r, w)
        seq = len(self.ops[eng])
        if dma is not None:
            ph = self.key2phys.get(dma)
            if ph is None:
                sw = (eng == "pool")
                if self.free_phys[sw]:
                    ph = self.free_phys[sw].pop()
                else:
                    ph = len(self.dma_cnt)
                    self.dma_cnt.append(0)
                    self.phys_sw.append(sw)
                self.key2phys[dma] = ph
            self.dma_cnt[ph] += 1
            ev = ("d", ph, self.dma_cnt[ph])
        else:
            ev = ("c", eng, seq)
        o = {"fn": fn, "deps": deps, "ev": ev, "signal": False}
        self.ops[eng].append(o)
        self._update(ev, r, w)
        return o

    def barrier(self):
        fr = {}
        for e in ENGS:
            for s in range(len(self.ops[e]) - 1, -1, -1):
                if self.ops[e][s]["ev"][0] == "c":
                    fr[("c", e)] = s
                    break
        for k, c in enumerate(self.dma_cnt):
            fr[("d", k)] = c
        self.base = fr
        self.res = {}
        self.key2phys = {}
        self.free_phys = {True: [i for i in range(len(self.dma_cnt)) if self.phys_sw[i]],
                          False: [i for i in range(len(self.dma_cnt)) if not self.phys_sw[i]]}

    def emit(self, final_wait_keys=()):
        nc = self.nc
        for e in ENGS:
            for o in self.ops[e]:
                for (kind, src), val in o["deps"].items():
                    if kind == "c" and not (src == "pe" and e == "pe"):
                        self.ops[src][val]["signal"] = True
        for e in ENGS:
            cnt = 0
            for o in self.ops[e]:
                if o["signal"]:
                    cnt += 1
                o["sigval"] = cnt
        sems = {}
        for e in ENGS:
            sems[("c", e)] = self.stack.enter_context(nc.semaphore("s_" + e))
        for k in range(len(self.dma_cnt)):
            sems[("d", k)] = self.stack.enter_context(nc.semaphore("d_" + str(k)))
        block = self.stack.enter_context(nc.Block())
        prog = self

        def run(eng_name, handle):
            waited = {}
            for o in prog.ops[eng_name]:
                for (kind, src), val in o["deps"].items():
                    if kind == "c":
                        if src == "pe" and eng_name == "pe":
                            continue
                        target = prog.ops[src][val]["sigval"]
                    else:
                        target = 16 * val
                    if waited.get((kind, src), 0) >= target:
                        continue
                    waited[(kind, src)] = target
                    handle.wait_ge(sems[(kind, src)], target)
                ins = o["fn"](handle)
                if o["ev"][0] == "d":
                    ins.then_inc(sems[("d", o["ev"][1])], 16)
                elif o["signal"]:
                    ins.then_inc(sems[("c", eng_name)], 1)
            if eng_name == "sp":
                for k in final_wait_keys:
                    handle.wait_ge(sems[("d", k)], 16 * prog.dma_cnt[k])

        @block.tensor
        def _(h):
            run("pe", h)

        @block.scalar
        def _(h):
            run("act", h)

        @block.vector
        def _(h):
            run("dve", h)

        @block.gpsimd
        def _(h):
            run("pool", h)

        @block.sync
        def _(h):
            run("sp", h)


class Ctx:
    pass


def sb(nc, st, name, shape, dt):
    return st.enter_context(nc.sbuf_tensor("sb_" + name, list(shape), dt))


def stage_consts(C):
    nc, P = C.nc, C.P
    st = C.top
    C.ident_bf = sb(nc, st, "ident_bf", [128, 128], BF16)
    C.ident_f = sb(nc, st, "ident_f", [128, 128], F32)
    C.ones_bf = sb(nc, st, "ones_bf", [128, 128], BF16)
    C.ones_f = sb(nc, st, "ones_f", [128, 128], F32)
    P.op("pool", lambda e: e.dma_start(out=C.ident_bf[:], in_=C.d["ident"][:, :]), w=["ident_bf"], dma="c0")
    P.op("sp", lambda e: e.dma_start(out=C.ident_f[:], in_=C.d["ident"][:, :]), w=["ident_f"], dma="c1")
    P.op("dve", lambda e: e.memset(C.ones_bf[:], 1.0), w=["ones_bf"])
    P.op("dve", lambda e: e.memset(C.ones_f[:], 1.0), w=["ones_f"])
    C.mod = [sb(nc, st, "mod%d" % l, [128, 48], F32) for l in range(2)]
    ada_w_aps = [C.d["ada_w0"], C.d["ada_w1"]]
    ada_b_aps = [C.d["ada_bT0"], C.d["ada_bT1"]]
    with ExitStack() as ls:
        cT = sb(nc, ls, "cT", [128, 8], F32)
        s2 = sb(nc, ls, "s2", [128, 8, 2], F32)
        wb = [sb(nc, ls, "adaw%d" % i, [128, 8, 512], F32) for i in range(2)]
        bT = sb(nc, ls, "adab", [128, 48], F32)
        ps = ls.enter_context(nc.psum_tensor("ps_mod", [128, 48, 2], F32))
        P.op("sp", lambda e: e.dma_start(out=cT[:], in_=C.d["cT"][:, :]), w=["cT"], dma="c2")
        P.op("act", lambda e: e.activation(out=s2[:, :, 0], in_=cT[:], func=AF.Silu), r=["cT"], w=["s2"])
        P.op("act", lambda e: e.activation(out=s2[:, :, 1], in_=cT[:], func=AF.Silu), r=["cT"], w=["s2"])
        for l in range(2):
            mod = C.mod[l]
            aw = ada_w_aps[l].rearrange("(k p) f -> p k f", p=128)
            P.op("sp", lambda e, l=l: e.dma_start(out=bT[:], in_=ada_b_aps[l][:, :]), w=["adab"], dma="c3")
            for cb in range(12):
                buf = wb[cb % 2]
                bk = "adaw%d" % (cb % 2)
                P.op("sp", lambda e, buf=buf, aw=aw, cb=cb: e.dma_start(out=buf[:], in_=aw[:, :, cb * 512:(cb + 1) * 512]),
                     w=[bk], dma="aw%d" % (cb % 2))
                for fi in range(4):
                    f = cb * 4 + fi
                    for k in range(8):
                        P.op("pe", lambda e, buf=buf, f=f, fi=fi, k=k: e.matmul(
                            ps[:, f, :], lhsT=buf[:, k, fi * 128:(fi + 1) * 128], rhs=s2[:, k, :],
                            start=(k == 0), stop=(k == 7)), r=[bk, "s2"], w=["ps_mod"])
            P.op("dve", lambda e, mod=mod: e.tensor_tensor(out=mod[:], in0=ps[:, :, 0], in1=bT[:], op=ALU.add),
                 r=["ps_mod", "adab"], w=["mod%d" % l])
            for j in (1, 4):
                P.op("dve", lambda e, mod=mod, j=j: e.tensor_scalar(
                    out=mod[:, j * 8:(j + 1) * 8], in0=mod[:, j * 8:(j + 1) * 8], scalar1=1.0, scalar2=None,
                    op0=ALU.add), r=["mod%d" % l], w=["mod%d" % l])
        P.barrier()


def norm_mod(C, xsrc, hT, rs_ps, sq, tmp, sqv, mod, joff, nm, x_key, h_key, h32=None):
    nc, P = C.nc, C.P
    sh0, sc0 = joff * 8, (joff + 1) * 8
    P.op("act", lambda e: e.activation(out=sq[:], in_=xsrc, func=AF.Square), r=[x_key], w=[nm + "sq"])
    for k in range(8):
        P.op("pe", lambda e, k=k: e.matmul(rs_ps[:], lhsT=C.ones_bf[:], rhs=sq[:, k, :], start=(k == 0), stop=(k == 7)),
             r=[nm + "sq", "ones_bf"], w=[nm + "rs_ps"])
    P.op("act", lambda e: e.activation(out=sqv[:], in_=rs_ps[:], func=AF.Sqrt, bias=C.eps_t[:, 0:1], scale=1.0 / D),
         r=[nm + "rs_ps"], w=[nm + "sqv"])
    P.op("dve", lambda e: e.reciprocal(out=sqv[:], in_=sqv[:]), r=[nm + "sqv"], w=[nm + "sqv"])
    for k in range(8):
        t = tmp[k % 2]
        tk = nm + "tmp%d" % (k % 2)
        P.op("dve", lambda e, k=k, t=t: e.scalar_tensor_tensor(
            out=t[:], in0=xsrc[:, k, :], scalar=mod[:, sc0 + k:sc0 + k + 1], in1=sqv[:], op0=ALU.mult, op1=ALU.mult),
            r=[x_key, nm + "sqv"], w=[tk])
        P.op("act", lambda e, k=k, t=t: e.activation(out=hT[:, k, :], in_=t[:], func=AF.Identity,
                                                    bias=mod[:, sh0 + k:sh0 + k + 1], scale=1.0),
             r=[tk], w=[(h_key, k)])
        if h32 is not None:
            P.op("pool", lambda e, k=k, t=t: e.tensor_scalar(
                out=h32[:, k, :], in0=t[:], scalar1=mod[:, sh0 + k:sh0 + k + 1], scalar2=None, op0=ALU.add),
                r=[tk], w=[("h32", k)])


def stage_ffn(C, x_in, x_out, T, mod, wg, wu, wd, HID, nm, n_exp=1, wr=None):
    nc, P = C.nc, C.P
    HG = 4 if (HID // 128) % 4 == 0 else 2
    NT = 4 if T >= 2048 else T // 512
    n_super = T // (NT * 512)
    units_per_exp = HID // (HG * 128)
    assert HID % (HG * 128) == 0
    xin_v = x_in.rearrange("(k p) t -> p k t", p=128)
    xout_v = x_out.rearrange("(k p) t -> p k t", p=128)
    g0 = 5 * 8
    with ExitStack() as ls:
        acc = sb(nc, ls, nm + "acc", [128, NT, 8, 512], F32)
        hT = sb(nc, ls, nm + "hT", [128, NT, 8, 512], BF16)
        sq = sb(nc, ls, nm + "sq", [128, 8, 512], BF16)
        tmp = [sb(nc, ls, nm + "tmp%d" % i, [128, 512], F32) for i in range(2)]
        sqv = sb(nc, ls, nm + "sqv", [128, 512], F32)
        wgb = [sb(nc, ls, nm + "wg%d" % i, [128, 8, HG * 128], BF16) for i in range(2)]
        wub = [sb(nc, ls, nm + "wu%d" % i, [128, 8, HG * 128], BF16) for i in range(2)]
        wdb = [sb(nc, ls, nm + "wd%d" % i, [128, HG, 1024], BF16) for i in range(2)]
        aT = [sb(nc, ls, nm + "aT%d" % i, [128, HG, 512], BF16) for i in range(2)]
        sg = [sb(nc, ls, nm + "sg%d" % i, [128, 512], F32) for i in range(2)]
        ps_g = [ls.enter_context(nc.psum_tensor(nm + "psg%d" % i, [128, 512], F32)) for i in range(2)]
        ps_u = [ls.enter_context(nc.psum_tensor(nm + "psu%d" % i, [128, 512], F32)) for i in range(2)]
        ps_d = [ls.enter_context(nc.psum_tensor(nm + "psd%d" % i, [128, 512], F32)) for i in range(2)]
        ps_x = ls.enter_context(nc.psum_tensor(nm + "psx", [128, 512], F32))
        if n_exp > 1:
            h32 = sb(nc, ls, nm + "h32", [128, 8, 512], F32)
            wrt = sb(nc, ls, nm + "wr", [128, 8, 8], F32)
            GT = sb(nc, ls, nm + "GT", [8, NT * 512], F32)
            lg = sb(nc, ls, nm + "lg", [128, 8], F32)
            mx = sb(nc, ls, nm + "mx", [128, 8], F32)
            gw = sb(nc, ls, nm + "gw", [128, 4], F32)
            Gm = sb(nc, ls, nm + "Gm", [128, 8], F32)
            Gm2 = sb(nc, ls, nm + "Gm2", [128, 8], F32)
            sel = sb(nc, ls, nm + "sel", [8, 8, 128], F32)
            ps_gb = ls.enter_context(nc.psum_tensor(nm + "psgb", [128, 512], F32))
            P.op("sp", lambda e: e.dma_start(out=wrt[:], in_=wr.rearrange("(k p) e -> p k e", p=128)), w=["wr"], dma=nm + "wr")
            P.op("sp", lambda e: e.dma_start(out=sel[:], in_=C.d["sel"][:, :, :]), w=["sel"], dma=nm + "sel")
        pend_down = [None]
        for s_i in range(n_super):
            for ti in range(NT):
                t0 = (s_i * NT + ti) * 512
                P.op("sp", lambda e, ti=ti, t0=t0: e.dma_start(out=acc[:, ti], in_=xin_v[:, :, t0:t0 + 512]),
                     w=[("acc", ti)], dma=nm + "x%d" % ti)
                norm_mod(C, acc[:, ti], hT[:, ti], ps_x, sq, tmp, sqv, mod, 3, nm, ("acc", ti), "hT%d" % ti,
                         h32=(h32 if n_exp > 1 else None))
                if n_exp > 1:
                    for c4 in range(4):
                        for k in range(8):
                            P.op("pe", lambda e, c4=c4, k=k: e.matmul(
                                ps_gb[:, 0:8], lhsT=h32[:, k, c4 * 128:(c4 + 1) * 128], rhs=wrt[:, k, :],
                                start=(k == 0), stop=(k == 7)), r=[("h32", k), "wr"], w=["psgb"])
                        P.op("dve", lambda e: e.tensor_copy(out=lg[:], in_=ps_gb[:, 0:8]), r=["psgb"], w=["lg"])
                        P.op("dve", lambda e: e.max(out=mx[:], in_=lg[:]), r=["lg"], w=["mx"])
                        P.op("dve", lambda e: e.tensor_tensor(out=gw[:, 0:1], in0=mx[:, 0:1], in1=mx[:, 1:2], op=ALU.subtract),
                             r=["mx"], w=["gw"])
                        P.op("act", lambda e: e.activation(out=gw[:, 1:2], in_=gw[:, 0:1], func=AF.Sigmoid), r=["gw"], w=["gw"])
                        P.op("dve", lambda e: e.tensor_scalar(out=gw[:, 2:3], in0=gw[:, 1:2], scalar1=-1.0, scalar2=1.0,
                                                               op0=ALU.mult, op1=ALU.add), r=["gw"], w=["gw"])
                        P.op("dve", lambda e: e.tensor_scalar(out=Gm[:], in0=lg[:], scalar1=mx[:, 0:1], scalar2=gw[:, 1:2],
                                                               op0=ALU.is_equal, op1=ALU.mult), r=["lg", "mx", "gw"], w=["Gm"])
                        P.op("dve", lambda e: e.tensor_scalar(out=Gm2[:], in0=lg[:], scalar1=mx[:, 1:2], scalar2=gw[:, 2:3],
                                                               op0=ALU.is_equal, op1=ALU.mult), r=["lg", "mx", "gw"], w=["Gm2"])
                        P.op("dve", lambda e: e.tensor_tensor(out=Gm[:], in0=Gm[:], in1=Gm2[:], op=ALU.add),
                             r=["Gm", "Gm2"], w=["Gm"])
                        P.op("pe", lambda e: e.transpose(out=ps_gb[0:8, 128:256], in_=Gm[:], identity=C.ident_f[:]),
                             r=["Gm", "ident_f"], w=["psgb"])
                        P.op("dve", lambda e, ti=ti, c4=c4: e.tensor_copy(
                            out=GT[:, ti * 512 + c4 * 128: ti * 512 + (c4 + 1) * 128], in_=ps_gb[0:8, 128:256]),
                            r=["psgb"], w=[("GT", ti)])
            n_units = n_exp * units_per_exp
            for u in range(n_units):
                ex, uu = divmod(u, units_per_exp)
                b = u % 2
                h0 = uu * HG * 128
                wgs = (wg[ex] if n_exp > 1 else wg).rearrange("(k p) h -> p k h", p=128)
                wus = (wu[ex] if n_exp > 1 else wu).rearrange("(k p) h -> p k h", p=128)
                wds = (wd[ex] if n_exp > 1 else wd).rearrange("(j p) f -> p j f", p=128)
                P.op("pool", lambda e, b=b, wgs=wgs, h0=h0: e.dma_start(out=wgb[b][:], in_=wgs[:, :, h0:h0 + HG * 128]),
                     w=["wg%d" % b], dma=nm + "wg%d" % b)
                P.op("pool", lambda e, b=b, wus=wus, h0=h0: e.dma_start(out=wub[b][:], in_=wus[:, :, h0:h0 + HG * 128]),
                     w=["wu%d" % b], dma=nm + "wu%d" % b)
                P.op("pool", lambda e, b=b, wds=wds, uu=uu: e.dma_start(out=wdb[b][:], in_=wds[:, uu * HG:(uu + 1) * HG, :]),
                     w=["wd%d" % b], dma=nm + "wd%d" % b)
                for ti in range(NT):
                    ab = (u * NT + ti) % 2
                    if n_exp > 1:
                        for pp in range(1):
                            P.op("pe", lambda e, ex=ex, ti=ti: e.matmul(
                                ps_gb[:], lhsT=sel[:, ex, :], rhs=GT[:, ti * 512:(ti + 1) * 512], start=True, stop=True),
                                r=["sel", ("GT", ti)], w=["psgb"])
                    for j in range(HG):
                        pb = j % 2
                        for k in range(8):
                            P.op("pe", lambda e, b=b, pb=pb, j=j, k=k, ti=ti: e.matmul(
                                ps_g[pb][:], lhsT=wgb[b][:, k, j * 128:(j + 1) * 128], rhs=hT[:, ti, k, :],
                                start=(k == 0), stop=(k == 7)), r=["wg%d" % b, "hT%d" % ti], w=["psg%d" % pb])
                        for k in range(8):
                            P.op("pe", lambda e, b=b, pb=pb, j=j, k=k, ti=ti: e.matmul(
                                ps_u[pb][:], lhsT=wub[b][:, k, j * 128:(j + 1) * 128], rhs=hT[:, ti, k, :],
                                start=(k == 0), stop=(k == 7)), r=["wu%d" % b, "hT%d" % ti], w=["psu%d" % pb])
                        P.op("act", lambda e, pb=pb: e.activation(out=sg[pb][:], in_=ps_g[pb][:], func=AF.Silu),
                             r=["psg%d" % pb], w=["sg%d" % pb])
                        if n_exp > 1:
                            P.op("dve", lambda e, pb=pb: e.tensor_tensor(out=sg[pb][:], in0=sg[pb][:], in1=ps_u[pb][:], op=ALU.mult),
                                 r=["sg%d" % pb, "psu%d" % pb], w=["sg%d" % pb])
                            P.op("dve", lambda e, pb=pb, ab=ab, j=j: e.tensor_tensor(
                                out=aT[ab][:, j, :], in0=sg[pb][:], in1=ps_gb[:], op=ALU.mult),
                                r=["sg%d" % pb, "psgb"], w=[("aT%d" % ab, j)])
                        else:
                            P.op("dve", lambda e, pb=pb, ab=ab, j=j: e.tensor_tensor(
                                out=aT[ab][:, j, :], in0=sg[pb][:], in1=ps_u[pb][:], op=ALU.mult),
                                r=["sg%d" % pb, "psu%d" % pb], w=[("aT%d" % ab, j)])
                    def _down(b=b, ab=ab, ti=ti):
                        for f in range(8):
                            db = f % 2
                            for j in range(HG):
                                P.op("pe", lambda e, b=b, db=db, j=j, f=f, ab=ab: e.matmul(
                                    ps_d[db][:], lhsT=wdb[b][:, j, f * 128:(f + 1) * 128], rhs=aT[ab][:, j, :],
                                    start=(j == 0), stop=(j == HG - 1)), r=["wd%d" % b, ("aT%d" % ab, j)], w=["psd%d" % db])
                            P.op("dve", lambda e, db=db, f=f, ti=ti: e.scalar_tensor_tensor(
                                out=acc[:, ti, f, :], in0=ps_d[db][:], scalar=mod[:, g0 + f:g0 + f + 1], in1=acc[:, ti, f, :],
                                op0=ALU.mult, op1=ALU.add), r=["psd%d" % db, ("acc", ti)], w=[("acc", ti)])
                    if pend_down[0] is not None:
                        pend_down[0]()
                    pend_down[0] = _down
            if pend_down[0] is not None:
                pend_down[0]()
                pend_down[0] = None
            for ti in range(NT):
                t0 = (s_i * NT + ti) * 512
                P.op("sp", lambda e, ti=ti, t0=t0: e.dma_start(out=xout_v[:, :, t0:t0 + 512], in_=acc[:, ti]),
                     r=[("acc", ti)], w=[nm + "xout"], dma=nm + "xo%d" % ti)
        P.barrier()


LAMBDA_INIT = 0.8 - 0.6 * float(np.exp(-0.3 * 1))


def stage_qkv(C, x_in, S, mod):
    nc, P, d = C.nc, C.P, C.d
    xin_v = x_in.rearrange("(k p) t -> p k t", p=128)
    qv = d["QT"].rearrange("(c p) t -> p c t", p=128)
    kv = d["KT"].rearrange("(c p) t -> p c t", p=128)
    vv = d["V"].rearrange("(n p) e -> p n e", p=128)
    wq = d["a_w_qkv"].rearrange("(k p) f -> p k f", p=128)
    with ExitStack() as ls:
        w = sb(nc, ls, "qw", [128, 8, 3072], BF16)
        xt = [sb(nc, ls, "qxt%d" % i, [128, 8, 512], F32) for i in range(2)]
        hT2 = [sb(nc, ls, "qhT%d" % i, [128, 8, 512], BF16) for i in range(2)]
        sq = sb(nc, ls, "qsq", [128, 8, 512], BF16)
        tmp = [sb(nc, ls, "qtmp%d" % i, [128, 512], F32) for i in range(2)]
        sqv = sb(nc, ls, "qsqv", [128, 512], F32)
        qt = [sb(nc, ls, "qqt%d" % i, [128, 8, 512], BF16) for i in range(2)]
        kt = [sb(nc, ls, "qkt%d" % i, [128, 8, 512], BF16) for i in range(2)]
        vt = [sb(nc, ls, "qvt%d" % i, [128, 4, 1024], BF16) for i in range(2)]
        ps = [ls.enter_context(nc.psum_tensor("qps%d" % i, [128, 512], F32)) for i in range(4)]
        ps_x = ls.enter_context(nc.psum_tensor("qpsx", [128, 512], F32))
        for i in range(3):
            P.op("pool", lambda e, i=i: e.dma_start(out=w[:, :, i * 1024:(i + 1) * 1024], in_=wq[:, :, i * 1024:(i + 1) * 1024]),
                 w=[("qw", i)], dma="qw%d" % i)
        pi = 0
        NT_ = S // 512

        def emit_norm(t):
            b = t % 2
            P.op("sp", lambda e, b=b, t=t: e.dma_start(out=xt[b][:], in_=xin_v[:, :, t * 512:(t + 1) * 512]),
                 w=["qxt%d" % b], dma="qx%d" % b)
            norm_mod(C, xt[b][:], hT2[b], ps_x, sq, tmp, sqv, mod, 0, "q", "qxt%d" % b, "qhT%d" % b)

        emit_norm(0)
        for t in range(NT_):
            b = t % 2
            hT = hT2[b]
            hk = "qhT%d" % b
            if t + 1 < NT_:
                emit_norm(t + 1)
            for (dst, dkey, coff, scale) in ((qt[b], "qqt%d" % b, 0, 0.125), (kt[b], "qkt%d" % b, 1024, 1.0)):
                for c in range(8):
                    pb = pi % 4
                    pi += 1
                    for k in range(8):
                        P.op("pe", lambda e, pb=pb, k=k, c=c, coff=coff, hT=hT: e.matmul(
                            ps[pb][:], lhsT=w[:, k, coff + c * 128: coff + (c + 1) * 128], rhs=hT[:, k, :],
                            start=(k == 0), stop=(k == 7)), r=["qw", hk], w=["qps%d" % pb])
                    P.op("act", lambda e, pb=pb, dst=dst, c=c, scale=scale: e.activation(
                        out=dst[:, c, :], in_=ps[pb][:], func=AF.Copy, scale=scale), r=["qps%d" % pb], w=[(dkey, c)])
            for tc in range(4):
                for cg in range(2):
                    pb = pi % 4
                    pi += 1
                    for k in range(8):
                        P.op("pe", lambda e, pb=pb, k=k, tc=tc, cg=cg, hT=hT: e.matmul(
                            ps[pb][:], lhsT=hT[:, k, tc * 128:(tc + 1) * 128], rhs=w[:, k, 2048 + cg * 512: 2048 + (cg + 1) * 512],
                            start=(k == 0), stop=(k == 7)), r=["qw", hk], w=["qps%d" % pb])
                    P.op("dve", lambda e, pb=pb, b=b, tc=tc, cg=cg: e.tensor_copy(
                        out=vt[b][:, tc, cg * 512:(cg + 1) * 512], in_=ps[pb][:]), r=["qps%d" % pb], w=[("qvt%d" % b, tc)])
            P.op("sp", lambda e, b=b, t=t: e.dma_start(out=qv[:, :, t * 512:(t + 1) * 512], in_=qt[b][:]),
                 r=["qqt%d" % b], w=["QT"], dma="qqo%d" % b)
            P.op("sp", lambda e, b=b, t=t: e.dma_start(out=kv[:, :, t * 512:(t + 1) * 512], in_=kt[b][:]),
                 r=["qkt%d" % b], w=["KT"], dma="qko%d" % b)
            P.op("sp", lambda e, b=b, t=t: e.dma_start(out=vv[:, t * 4:(t + 1) * 4, :], in_=vt[b][:]),
                 r=["qvt%d" % b], w=["V"], dma="qvo%d" % b)
        P.barrier()


def own_tiles(n_slots):
    A = [2 * i if i % 2 == 0 else 2 * i + 1 for i in range(n_slots)]
    B = [2 * i + 1 if i % 2 == 0 else 2 * i for i in range(n_slots)]
    return A, B


def stage_attn(C, x_in, x_out, S, mod):
    nc, P, d = C.nc, C.P, C.d
    n_slots = S // 1024
    TA, TB = own_tiles(n_slots)
    xin_v = x_in.rearrange("(k p) t -> p k t", p=128)
    xout_v = x_out.rearrange("(k p) t -> p k t", p=128)
    qv = d["QT"].rearrange("(c p) t -> p c t", p=128)
    g0 = 2 * 8
    with ExitStack() as ls:
        wo = sb(nc, ls, "awo", [128, 8, 1024], BF16)
        masks = sb(nc, ls, "amask", [128, 4, 4, 512], BF16)
        rsel = sb(nc, ls, "arsel", [128, 2], F32)
        lamv = sb(nc, ls, "alamv", [128, 4, 64], F32)
        lsc = sb(nc, ls, "alsc", [128, 8], F32)
        gsub = sb(nc, ls, "agsub", [128, 1], F32)
        qa = sb(nc, ls, "aqa", [128, 8, 512], BF16)
        qb = sb(nc, ls, "aqb", [128, 8, 512], BF16)
        q = sb(nc, ls, "aq", [128, 8, 512], BF16)
        xa = sb(nc, ls, "axa", [128, 8, 512], F32)
        xb = sb(nc, ls, "axb", [128, 8, 512], F32)
        ktb = [sb(nc, ls, "akt%d" % i, [128, 512], BF16) for i in range(2)]
        vtb = [sb(nc, ls, "avt%d" % i, [128, 4, 128], BF16) for i in range(2)]
        pT = [[sb(nc, ls, "apT%d%d" % (i, j), [128, 512], BF16) for j in range(2)] for i in range(2)]
        oT = sb(nc, ls, "aoT", [128, 8, 512], BF16)
        t32 = [sb(nc, ls, "at32%d" % i, [128, 512], F32) for i in range(4)]
        sqb = sb(nc, ls, "asqb", [128, 512], BF16)
        ps_s = [[ls.enter_context(nc.psum_tensor("aps%d%d" % (i, j), [128, 512], F32)) for j in range(2)] for i in range(2)]
        ps_o = [ls.enter_context(nc.psum_tensor("apo%d" % j, [128, 512], F32)) for j in range(2)]
        ps_l = [ls.enter_context(nc.psum_tensor("apl%d" % j, [128, 512], F32)) for j in range(2)]
        P.op("pool", lambda e: e.dma_start(out=wo[:], in_=d["a_w_o"].rearrange("(k p) f -> p k f", p=128)), w=["awo"], dma="awo")
        for i in range(4):
            P.op("pool", lambda e, i=i: e.dma_start(out=masks[:, i], in_=d["masks"][i]), w=[("amask", i)], dma="amask%d" % i)
        P.op("sp", lambda e: e.dma_start(out=rsel[:], in_=d["rolesel"][:, :]), w=["arsel"], dma="arsel")
        P.op("sp", lambda e: e.dma_start(out=lamv[:], in_=d["lamv"][:, :, :]), w=["alamv"], dma="alamv")
        P.op("sp", lambda e: e.dma_start(out=gsub[:], in_=d["sublnT"][:, :]), w=["agsub"], dma="agsub")
        for i in range(2):
            P.op("dve", lambda e, i=i: e.tensor_tensor(out=lamv[:, 2 * i, :], in0=lamv[:, 2 * i, :], in1=lamv[:, 2 * i + 1, :], op=ALU.mult),
                 r=["alamv"], w=["alamv"])
            P.op("dve", lambda e, i=i: e.tensor_reduce(out=lsc[:, i:i + 1], in_=lamv[:, 2 * i, :], axis=AX.X, op=ALU.add),
                 r=["alamv"], w=["alsc"])
            P.op("act", lambda e, i=i: e.activation(out=lsc[:, 2 + i:3 + i], in_=lsc[:, i:i + 1], func=AF.Exp), r=["alsc"], w=["alsc"])
        P.op("dve", lambda e: e.tensor_tensor(out=lsc[:, 4:5], in0=lsc[:, 3:4], in1=lsc[:, 2:3], op=ALU.subtract), r=["alsc"], w=["alsc"])
        P.op("dve", lambda e: e.tensor_scalar(out=lsc[:, 4:5], in0=lsc[:, 4:5], scalar1=-LAMBDA_INIT, scalar2=None, op0=ALU.add),
             r=["alsc"], w=["alsc"])
        P.op("dve", lambda e: e.tensor_scalar(out=gsub[:], in0=gsub[:], scalar1=1.0 - LAMBDA_INIT, scalar2=None, op0=ALU.mult),
             r=["agsub"], w=["agsub"])
        step = 0
        for si in range(n_slots):
            ta, tb = TA[si], TB[si]
            P.op("sp", lambda e, ta=ta: e.dma_start(out=qa[:], in_=qv[:, :, ta * 512:(ta + 1) * 512]), w=["aqa"], dma="aqa")
            P.op("sp", lambda e, tb=tb: e.dma_start(out=qb[:], in_=qv[:, :, tb * 512:(tb + 1) * 512]), w=["aqb"], dma="aqb")
            P.op("sp", lambda e, ta=ta: e.dma_start(out=xa[:], in_=xin_v[:, :, ta * 512:(ta + 1) * 512]), w=["axa"], dma="axa")
            P.op("sp", lambda e, tb=tb: e.dma_start(out=xb[:], in_=xin_v[:, :, tb * 512:(tb + 1) * 512]), w=["axb"], dma="axb")
            P.op("dve", lambda e: e.tensor_scalar(out=qa[:], in0=qa[:], scalar1=rsel[:, 0:1], scalar2=None, op0=ALU.mult),
                 r=["aqa", "arsel"], w=["aqa"])
            P.op("dve", lambda e: e.scalar_tensor_tensor(out=q[:], in0=qb[:], scalar=rsel[:, 1:2], in1=qa[:], op0=ALU.mult, op1=ALU.add),
                 r=["aqa", "aqb", "arsel"], w=["aq"])
            P.op("pool", lambda e: e.tensor_scalar(out=xa[:], in0=xa[:], scalar1=rsel[:, 0:1], scalar2=None, op0=ALU.mult),
                 r=["axa", "arsel"], w=["axa"])
            P.op("dve", lambda e: e.scalar_tensor_tensor(out=xa[:], in0=xb[:], scalar=rsel[:, 1:2], in1=xa[:], op0=ALU.mult, op1=ALU.add),
                 r=["axa", "axb", "arsel"], w=["axa"])
            n_units = 2 * si + 2
            par = si % 2
            for h in range(8):
                steps = [(u, kb) for u in range(n_units) for kb in range(4)]
                lbs = {}

                def emit_qk(si_, h=h, steps=steps, lbs=lbs, n_units=n_units, par=par):
                    nonlocal step
                    u, kb = steps[si_]
                    if kb == 0:
                        lb = step % 2
                        step += 1
                        lbs[u] = lb
                        P.op("sp", lambda e, lb=lb, h=h, u=u: e.dma_start(
                            out=ktb[lb][:], in_=d["KT"][h * 128:(h + 1) * 128, u * 512:(u + 1) * 512]), r=["KT"], w=["akt%d" % lb], dma="akt%d" % lb)
                        P.op("sp", lambda e, lb=lb, h=h, u=u: e.dma_start(
                            out=vtb[lb][:], in_=d["V"].rearrange("(n p) e -> p n e", p=128)[:, u * 4:(u + 1) * 4, h * 128:(h + 1) * 128]),
                            r=["V"], w=["avt%d" % lb], dma="avt%d" % lb)
                    lb = lbs[u]
                    mk = None
                    if u == n_units - 2:
                        mk = 2 * par
                    elif u == n_units - 1:
                        mk = 2 * par + 1
                    sbuf_i = si_ % 2
                    for j in range(2):
                        P.op("pe", lambda e, sbuf_i=sbuf_i, j=j, lb=lb, kb=kb, h=h: e.matmul(
                            ps_s[sbuf_i][j][:], lhsT=ktb[lb][j * 64:(j + 1) * 64, kb * 128:(kb + 1) * 128],
                            rhs=q[j * 64:(j + 1) * 64, h, :], start=True, stop=True),
                            r=["akt%d" % lb, "aq"], w=["aps%d%d" % (sbuf_i, j)])
                        P.op("act", lambda e, sbuf_i=sbuf_i, j=j: e.activation(
                            out=pT[sbuf_i][j][:], in_=ps_s[sbuf_i][j][:], func=AF.Exp),
                            r=["aps%d%d" % (sbuf_i, j)], w=["apT%d%d" % (sbuf_i, j)])
                        if mk is not None:
                            P.op("dve" if j == 0 else "pool", lambda e, sbuf_i=sbuf_i, j=j, mk=mk, kb=kb: e.tensor_tensor(
                                out=pT[sbuf_i][j][:], in0=pT[sbuf_i][j][:], in1=masks[:, mk, kb, :], op=ALU.mult),
                                r=["apT%d%d" % (sbuf_i, j), "amask"], w=["apT%d%d" % (sbuf_i, j)])

                def emit_pv(si_, steps=steps, lbs=lbs):
                    u, kb = steps[si_]
                    lb = lbs[u]
                    sbuf_i = si_ % 2
                    first = (si_ == 0)
                    last = (si_ == len(steps) - 1)
                    for j in range(2):
                        P.op("pe", lambda e, sbuf_i=sbuf_i, j=j, lb=lb, kb=kb, first=first, last=last: e.matmul(
                            ps_o[j][:], lhsT=vtb[lb][:, kb, :], rhs=pT[sbuf_i][j][:], start=first, stop=last),
                            r=["avt%d" % lb, "apT%d%d" % (sbuf_i, j)], w=["apo%d" % j])
                        P.op("pe", lambda e, sbuf_i=sbuf_i, j=j, first=first, last=last: e.matmul(
                            ps_l[j][:], lhsT=C.ones_bf[:], rhs=pT[sbuf_i][j][:], start=first, stop=last),
                            r=["ones_bf", "apT%d%d" % (sbuf_i, j)], w=["apl%d" % j])

                emit_qk(0)
                for si_ in range(len(steps)):
                    if si_ + 1 < len(steps):
                        emit_qk(si_ + 1)
                    emit_pv(si_)
                for j in range(2):
                    P.op("dve", lambda e, j=j: e.reciprocal(out=t32[j][:], in_=ps_l[j][:]), r=["apl%d" % j], w=["at32%d" % j])
                    P.op("dve", lambda e, j=j: e.tensor_tensor(out=t32[j][:], in0=ps_o[j][:], in1=t32[j][:], op=ALU.mult),
                         r=["apo%d" % j, "at32%d" % j], w=["at32%d" % j])
                P.op("dve", lambda e: e.scalar_tensor_tensor(out=t32[2][:], in0=t32[1][:], scalar=lsc[:, 4:5], in1=t32[0][:],
                                                              op0=ALU.mult, op1=ALU.add), r=["at320", "at321", "alsc"], w=["at322"])
                P.op("act", lambda e: e.activation(out=sqb[:], in_=t32[2][:], func=AF.Square), r=["at322"], w=["asqb"])
                P.op("pe", lambda e: e.matmul(ps_s[0][0][:], lhsT=C.ones_bf[:], rhs=sqb[:], start=True, stop=True),
                     r=["ones_bf", "asqb"], w=["aps00"])
                P.op("act", lambda e: e.activation(out=t32[3][:], in_=ps_s[0][0][:], func=AF.Sqrt, bias=C.eps_t[:, 0:1], scale=1.0 / 128),
                     r=["aps00"], w=["at323"])
                P.op("dve", lambda e: e.reciprocal(out=t32[3][:], in_=t32[3][:]), r=["at323"], w=["at323"])
                P.op("dve", lambda e, h=h: e.scalar_tensor_tensor(out=oT[:, h, :], in0=t32[2][:], scalar=gsub[:, 0:1], in1=t32[3][:],
                                                                   op0=ALU.mult, op1=ALU.mult), r=["at322", "at323", "agsub"], w=[("aoT", h)])
            for f in range(8):
                pb = ps_s[1][f % 2]
                pk = "aps1%d" % (f % 2)
                for h in range(8):
                    P.op("pe", lambda e, pb=pb, f=f, h=h: e.matmul(pb[:], lhsT=wo[:, h, f * 128:(f + 1) * 128], rhs=oT[:, h, :],
                                                                   start=(h == 0), stop=(h == 7)), r=["awo", "aoT"], w=[pk])
                P.op("dve", lambda e, pb=pb, f=f: e.scalar_tensor_tensor(
                    out=xa[:, f, :], in0=pb[:], scalar=mod[:, g0 + f:g0 + f + 1], in1=xa[:, f, :], op0=ALU.mult, op1=ALU.add),
                    r=[pk, "axa"], w=["axa"])
            P.op("sp", lambda e, si=si: e.dma_start(out=xout_v[:, :, si * 512:(si + 1) * 512], in_=xa[:]), r=["axa"], w=["x3T"], dma="axo")
        P.barrier()


def stage_final(C, x_in, x_out, T):
    nc, P, d = C.nc, C.P, C.d
    xin_v = x_in.rearrange("(k p) t -> p k t", p=128)
    xout_v = x_out.rearrange("(k p) t -> p k t", p=128)
    with ExitStack() as ls:
        fg = sb(nc, ls, "ffg", [128, 8], F32)
        xt = [sb(nc, ls, "fxt%d" % i, [128, 8, 512], F32) for i in range(2)]
        sq = sb(nc, ls, "fsq", [128, 8, 512], BF16)
        sqv = sb(nc, ls, "fsqv", [128, 512], F32)
        ps = ls.enter_context(nc.psum_tensor("fps", [128, 512], F32))
        P.op("sp", lambda e: e.dma_start(out=fg[:], in_=d["final_gT"][:, :]), w=["ffg"], dma="ffg")
        for t in range(T // 512):
            b = t % 2
            xk = "fxt%d" % b
            P.op("sp", lambda e, b=b, t=t: e.dma_start(out=xt[b][:], in_=xin_v[:, :, t * 512:(t + 1) * 512]), w=[xk], dma="fx%d" % b)
            P.op("act", lambda e, b=b: e.activation(out=sq[:], in_=xt[b][:], func=AF.Square), r=[xk], w=["fsq"])
            for k in range(8):
                P.op("pe", lambda e, k=k: e.matmul(ps[:], lhsT=C.ones_bf[:], rhs=sq[:, k, :], start=(k == 0), stop=(k == 7)),
                     r=["fsq", "ones_bf"], w=["fps"])
            P.op("act", lambda e: e.activation(out=sqv[:], in_=ps[:], func=AF.Sqrt, bias=C.eps_t[:, 0:1], scale=1.0 / D),
                 r=["fps"], w=["fsqv"])
            P.op("dve", lambda e: e.reciprocal(out=sqv[:], in_=sqv[:]), r=["fsqv"], w=["fsqv"])
            for k in range(8):
                P.op("dve", lambda e, b=b, k=k: e.scalar_tensor_tensor(
                    out=xt[b][:, k, :], in0=xt[b][:, k, :], scalar=fg[:, k:k + 1], in1=sqv[:], op0=ALU.mult, op1=ALU.mult),
                    r=[xk, "fsqv", "ffg"], w=[xk])
            P.op("sp", lambda e, b=b, t=t: e.dma_start(out=xout_v[:, :, t * 512:(t + 1) * 512], in_=xt[b][:]), r=[xk], w=["outT"], dma="fxo%d" % b)
        P.barrier()


M_IN = 6176
DBG = set()


def stage_m1(C, x_in, S, mod, part):
    nc, P, d = C.nc, C.P, C.d
    xin_v = x_in.rearrange("(k p) t -> p k t", p=128)
    wv = d["m_w_in"].rearrange("(k p) f -> p k f", p=128)
    nm = "m" + part
    with ExitStack() as ls:
        xt = [sb(nc, ls, nm + "xt%d" % i, [128, 8, 512], F32) for i in range(2)]
        hT2 = [sb(nc, ls, nm + "hT%d" % i, [128, 8, 512], BF16) for i in range(2)]
        sq = sb(nc, ls, nm + "sq", [128, 8, 512], BF16)
        tmp = [sb(nc, ls, nm + "tmp%d" % i, [128, 512], F32) for i in range(2)]
        sqv = sb(nc, ls, nm + "sqv", [128, 512], F32)
        ps_x = ls.enter_context(nc.psum_tensor(nm + "psx", [128, 512], F32))
        ps = [ls.enter_context(nc.psum_tensor(nm + "ps%d" % i, [128, 512], F32)) for i in range(3)]
        if part == "z":
            w = sb(nc, ls, nm + "w", [128, 8, 2048 + 32], BF16)
            zt = [sb(nc, ls, nm + "zt%d" % i, [128, 2048], BF16) for i in range(2)]
            dtt = [sb(nc, ls, nm + "dtt%d" % i, [128, 32], F32) for i in range(2)]
            for i in range(2):
                P.op("pool", lambda e, i=i: e.dma_start(out=w[:, :, i * 1024:(i + 1) * 1024], in_=wv[:, :, i * 1024:(i + 1) * 1024]),
                     w=[(nm + "w", i)], dma=nm + "w%d" % i)
            P.op("pool", lambda e: e.dma_start(out=w[:, :, 2048:2080], in_=wv[:, :, 6144:6176]), w=[(nm + "w", 2)], dma=nm + "w2")
            zv = d["zs"].rearrange("(n p) e -> p n e", p=128)
            dv = d["dtr"].rearrange("(n p) e -> p n e", p=128)
        else:
            w = sb(nc, ls, nm + "w", [128, 8, 4096], BF16)
            diag = sb(nc, ls, nm + "diag", [128, 4, 32, 128], BF16)
            cw = sb(nc, ls, nm + "cw", [128, 32, 4], F32)
            cb = sb(nc, ls, nm + "cb", [128, 32], F32)
            halo = sb(nc, ls, nm + "halo", [128, 32, 4], BF16)
            xraw = [sb(nc, ls, nm + "xraw%d" % i, [128, 516], BF16) for i in range(2)]
            xc = [sb(nc, ls, nm + "xc%d" % i, [128, 4, 512], BF16) for i in range(2)]
            xtok = [sb(nc, ls, nm + "xtok%d" % i, [128, 512], BF16) for i in range(2)]
            ps_t32 = [ls.enter_context(nc.psum_tensor(nm + "pst%d" % i, [128, 512], F32)) for i in range(2)]
            ps_t = [p_[:].bitcast(BF16) for p_ in ps_t32]
            for i in range(4):
                P.op("pool", lambda e, i=i: e.dma_start(out=w[:, :, i * 1024:(i + 1) * 1024], in_=wv[:, :, 2048 + i * 1024:2048 + (i + 1) * 1024]),
                     w=[(nm + "w", i)], dma=nm + "w%d" % i)
            P.op("sp", lambda e: e.dma_start(out=cw[:], in_=d["convwT"][:, :, :]), w=[nm + "cw"], dma=nm + "cw")
            P.op("sp", lambda e: e.dma_start(out=cb[:], in_=d["convbT"][:, :]), w=[nm + "cb"], dma=nm + "cb")
            P.op("dve", lambda e: e.memset(halo[:], 0.0), w=[nm + "halo"])
            for j in range(4):
                for cc in range(32):
                    P.op("pool" if cc % 2 else "dve", lambda e, j=j, cc=cc: e.tensor_scalar(
                        out=diag[:, j, cc, :], in0=C.ident_f[:], scalar1=cw[:, cc, j:j + 1], scalar2=None, op0=ALU.mult),
                        r=["ident_f", nm + "cw"], w=[(nm + "diag", cc)])
            xsv = d["xsB"].rearrange("(n p) e -> p n e", p=128)
            btv = d["BT"].rearrange("(c p) t -> p c t", p=128)
            ctv = d["CT"].rearrange("(c p) t -> p c t", p=128)
        pi = 0
        NT_ = S // 512

        def emit_norm(t):
            b = t % 2
            P.op("sp", lambda e, b=b, t=t: e.dma_start(out=xt[b][:], in_=xin_v[:, :, t * 512:(t + 1) * 512]),
                 w=[nm + "xt%d" % b], dma=nm + "x%d" % b)
            norm_mod(C, xt[b][:], hT2[b], ps_x, sq, tmp, sqv, mod, 0, nm, nm + "xt%d" % b, nm + "hT%d" % b)

        emit_norm(0)
        for t in range(NT_):
            b = t % 2
            hT = hT2[b]
            hk = nm + "hT%d" % b
            if t + 1 < NT_:
                emit_norm(t + 1)
            if part == "z":
                for tc in range(4):
                    zb = (t * 4 + tc) % 2
                    for cg in range(4):
                        pb = pi % 3
                        pi += 1
                        for k in range(8):
                            P.op("pe", lambda e, pb=pb, k=k, tc=tc, cg=cg, hT=hT: e.matmul(
                                ps[pb][:], lhsT=hT[:, k, tc * 128:(tc + 1) * 128], rhs=w[:, k, cg * 512:(cg + 1) * 512],
                                start=(k == 0), stop=(k == 7)), r=[nm + "w", hk], w=[nm + "ps%d" % pb])
                        P.op("act", lambda e, pb=pb, zb=zb, cg=cg: e.activation(
                            out=zt[zb][:, cg * 512:(cg + 1) * 512], in_=ps[pb][:], func=AF.Silu),
                            r=[nm + "ps%d" % pb], w=[(nm + "zt%d" % zb, cg)])
                    pb = pi % 3
                    pi += 1
                    for k in range(8):
                        P.op("pe", lambda e, pb=pb, k=k, tc=tc, hT=hT: e.matmul(
                            ps[pb][:, 0:32], lhsT=hT[:, k, tc * 128:(tc + 1) * 128], rhs=w[:, k, 2048:2080],
                            start=(k == 0), stop=(k == 7)), r=[nm + "w", hk], w=[nm + "ps%d" % pb])
                    P.op("dve", lambda e, pb=pb, zb=zb: e.tensor_copy(out=dtt[zb][:], in_=ps[pb][:, 0:32]),
                         r=[nm + "ps%d" % pb], w=[nm + "dtt%d" % zb])
                    n = t * 4 + tc
                    P.op("sp", lambda e, zb=zb, n=n: e.dma_start(out=zv[:, n, :], in_=zt[zb][:]), r=[nm + "zt%d" % zb], w=["zs"], dma=nm + "zo%d" % zb)
                    P.op("sp", lambda e, zb=zb, n=n: e.dma_start(out=dv[:, n, :], in_=dtt[zb][:]), r=[nm + "dtt%d" % zb], w=["dtr"], dma=nm + "do%d" % zb)
            else:
                def proj(cc, hT=hT, hk=hk):
                    nonlocal pi
                    rb = cc % 2
                    pb = pi % 3
                    pi += 1
                    for k in range(8):
                        P.op("pe", lambda e, pb=pb, k=k, cc=cc: e.matmul(
                            ps[pb][:], lhsT=w[:, k, cc * 128:(cc + 1) * 128], rhs=hT[:, k, :],
                            start=(k == 0), stop=(k == 7)), r=[nm + "w", hk], w=[nm + "ps%d" % pb])
                    P.op("dve", lambda e, rb=rb, cc=cc: e.tensor_copy(out=xraw[rb][:, 0:4], in_=halo[:, cc, :]),
                         r=[(nm + "halo", cc)], w=[nm + "xraw%d" % rb])
                    P.op("act", lambda e, rb=rb, pb=pb: e.activation(out=xraw[rb][:, 4:516], in_=ps[pb][:], func=AF.Copy),
                         r=[nm + "ps%d" % pb], w=[nm + "xraw%d" % rb])
                    P.op("dve", lambda e, rb=rb, cc=cc: e.tensor_copy(out=halo[:, cc, :], in_=xraw[rb][:, 512:516]),
                         r=[nm + "xraw%d" % rb], w=[(nm + "halo", cc)])

                def conv(cc):
                    nonlocal pi
                    rb = cc % 2
                    g4_, ci = divmod(cc, 4)
                    xb_i = g4_ % 2
                    pb2 = pi % 3
                    pi += 1
                    for j in range(4):
                        P.op("pe", lambda e, pb2=pb2, j=j, cc=cc, rb=rb: e.matmul(
                            ps[pb2][:], lhsT=diag[:, j, cc, :], rhs=xraw[rb][:, 1 + j:513 + j], start=(j == 0), stop=(j == 3)),
                            r=[(nm + "diag", cc), nm + "xraw%d" % rb], w=[nm + "ps%d" % pb2])
                    P.op("act", lambda e, pb2=pb2, xb_i=xb_i, ci=ci, cc=cc: e.activation(
                        out=xc[xb_i][:, ci, :], in_=ps[pb2][:], func=AF.Silu, bias=cb[:, cc:cc + 1], scale=1.0),
                        r=[nm + "ps%d" % pb2, nm + "cb"], w=[(nm + "xc%d" % xb_i, ci)])

                proj(0)
                for g4 in range(8):
                    xb_i = g4 % 2
                    for ci in range(4):
                        cc = g4 * 4 + ci
                        if cc + 1 < 32:
                            proj(cc + 1)
                        conv(cc)
                    if g4 >= 4:
                        dst = btv if g4 < 6 else ctv
                        c0 = (g4 - 4) * 4 if g4 < 6 else (g4 - 6) * 4
                        P.op("sp", lambda e, xb_i=xb_i, dst=dst, c0=c0, t=t: e.dma_start(
                            out=dst[:, c0:c0 + 4, t * 512:(t + 1) * 512], in_=xc[xb_i][:]),
                            r=[nm + "xc%d" % xb_i], w=["BTCT"], dma=nm + "bo%d" % xb_i)
                    if g4 < 6:
                        for tc in range(4):
                            tb = (g4 * 4 + tc) % 2
                            for ci in range(4):
                                P.op("pe", lambda e, tb=tb, ci=ci, xb_i=xb_i, tc=tc: e.transpose(
                                    out=ps_t[tb][:, ci * 128:(ci + 1) * 128], in_=xc[xb_i][:, ci, tc * 128:(tc + 1) * 128],
                                    identity=C.ident_bf[:]), r=[(nm + "xc%d" % xb_i, ci), "ident_bf"], w=[nm + "pst%d" % tb])
                            P.op("dve" if tc % 2 else "pool" if False else "dve", lambda e, tb=tb: e.tensor_copy(out=xtok[tb][:], in_=ps_t[tb][:, 0:512]),
                                 r=[nm + "pst%d" % tb], w=[nm + "xtok%d" % tb])
                            n = t * 4 + tc
                            P.op("sp", lambda e, tb=tb, n=n, g4=g4: e.dma_start(out=xsv[:, n, g4 * 512:(g4 + 1) * 512], in_=xtok[tb][:]),
                                 r=[nm + "xtok%d" % tb], w=["xsB"], dma=nm + "xo%d" % tb)
        P.barrier()


def stage_m2(C, x_in, x_out, S, mod):
    nc, P, d = C.nc, C.P, C.d
    xin_v = x_in.rearrange("(k p) t -> p k t", p=128)
    xout_v = x_out.rearrange("(k p) t -> p k t", p=128)
    zv = d["zs"].rearrange("(n p) e -> p n e", p=128)
    dv = d["dtr"].rearrange("(n p) e -> p n e", p=128)
    xsv = d["xsB"].rearrange("(n p) e -> p n e", p=128)
    btv = d["BT"].rearrange("(c p) t -> p c t", p=128)
    ctv = d["CT"].rearrange("(c p) t -> p c t", p=128)
    g0 = 2 * 8
    with ExitStack() as ls:
        wout = sb(nc, ls, "swout", [128, 16, 1024], BF16)
        tri = sb(nc, ls, "stri", [128, 128], F32)
        negm = sb(nc, ls, "snegm", [128, 4, 128], BF16)
        dtb = sb(nc, ls, "sdtb", [128, 32], F32)
        aneg = sb(nc, ls, "saneg", [128, 32], F32)
        dsk = sb(nc, ls, "sdsk", [128, 32], F32)
        dI = sb(nc, ls, "sdI", [128, 32, 128], BF16)
        normg = sb(nc, ls, "snormg", [128, 2048], F32)
        blkT = sb(nc, ls, "sblkT", [32, 4096], BF16)
        Rm = sb(nc, ls, "sRm", [64, 4096], F32)
        Lm = sb(nc, ls, "sLm", [64, 128], F32)
        Tin = sb(nc, ls, "sTin", [128, 64], F32)
        acsT = sb(nc, ls, "sacsT", [32, 128], F32)
        S32 = sb(nc, ls, "sS32", [128, 2048], F32)
        Sbf = [sb(nc, ls, "sSbf%d" % i, [128, 2048], BF16) for i in range(2)]
        zt = [sb(nc, ls, "szt%d" % i, [128, 2048], BF16) for i in range(2)]
        xb = [sb(nc, ls, "sxb%d" % i, [128, 3072], BF16) for i in range(2)]
        BTc = [sb(nc, ls, "sBT%d" % i, [128, 8, 128], BF16) for i in range(2)]
        CTc = [sb(nc, ls, "sCT%d" % i, [128, 8, 128], BF16) for i in range(2)]
        dtr = [sb(nc, ls, "sdtr%d" % i, [128, 32], F32) for i in range(2)]
        xt = [sb(nc, ls, "sxt%d" % i, [128, 8, 128], F32) for i in range(2)]
        sm = sb(nc, ls, "ssm", [128, 64], F32)
        v32 = [sb(nc, ls, "sv32%d" % i, [128, 8, 32], F32) for i in range(2)]
        xtl = [sb(nc, ls, "sxtl%d" % i, [128, 2048], BF16) for i in range(2)]
        xts = [sb(nc, ls, "sxts%d" % i, [128, 2048], BF16) for i in range(2)]
        CBm = [sb(nc, ls, "sCBm%d" % i, [128, 128], F32) for i in range(2)]
        LT = [sb(nc, ls, "sLT%d" % i, [128, 512], F32) for i in range(2)]
        MT = [sb(nc, ls, "sMT%d" % i, [128, 8, 512], BF16) for i in range(2)]
        ty = [sb(nc, ls, "sty%d" % i, [128, 256], F32) for i in range(2)]
        y32 = sb(nc, ls, "sy32", [128, 2048], F32)
        junk = sb(nc, ls, "sjunk", [128, 256], F32)
        ssq = sb(nc, ls, "sssq", [128, 8], F32)
        yn = sb(nc, ls, "syn", [128, 2048], BF16)
        ynT = sb(nc, ls, "synT", [128, 16, 128], BF16)
        pmc = ls.enter_context(nc.psum_tensor("spmc", [128, 512], F32))
        pD = [ls.enter_context(nc.psum_tensor("spD%d" % i, [128, 512], F32)) for i in range(2)]
        pY = [ls.enter_context(nc.psum_tensor("spY%d" % i, [128, 512], F32)) for i in range(2)]
        pS = ls.enter_context(nc.psum_tensor("spS", [128, 512], F32))
        pAB = [ls.enter_context(nc.psum_tensor("spAB%d" % i, [128, 512], F32)) for i in range(2)]
        pABb = [p_[:].bitcast(BF16) for p_ in pAB]
        wov = d["m_w_out"].rearrange("(j p) f -> p j f", p=128)
        for i in range(2):
            P.op("pool", lambda e, i=i: e.dma_start(out=wout[:, i * 8:(i + 1) * 8, :], in_=wov[:, i * 8:(i + 1) * 8, :]),
                 w=[("swout", i)], dma="swout%d" % i)
        for (t_, nme) in ((tri, "tri"), (dtb, "dtbT"), (aneg, "alogT"), (dsk, "dskT"), (normg, "normgT")):
            P.op("sp", lambda e, t_=t_, nme=nme: e.dma_start(out=t_[:], in_=d[nme][:, :]), w=["s_" + nme], dma="s_" + nme)
        P.op("pool", lambda e: e.dma_start(out=negm[:], in_=d["negm4"][:, :, :]), w=["s_negm"], dma="s_negm")
        P.op("pool", lambda e: e.dma_start(out=blkT[:, 0:2048], in_=d["blk"][0:32, 0:2048]), w=[("s_blkT", 0)], dma="s_blkT")
        P.op("pool", lambda e: e.dma_start(out=blkT[:, 2048:4096], in_=d["blk"][0:32, 2048:4096]), w=[("s_blkT", 1)], dma="s_blkT2")
        P.op("sp", lambda e: e.dma_start(out=Rm[32:64, :], in_=d["blk"][32:64, :]), w=["sRm_b"], dma="s_Rmb")
        P.op("act", lambda e: e.activation(out=aneg[:], in_=aneg[:], func=AF.Exp), r=["s_alogT"], w=["s_alogT"])
        P.op("dve", lambda e: e.tensor_scalar(out=aneg[:], in0=aneg[:], scalar1=-1.0, scalar2=None, op0=ALU.mult), r=["s_alogT"], w=["s_alogT"])
        P.op("dve", lambda e: e.memset(S32[:], 0.0), w=["sS32"])
        P.op("pool", lambda e: e.memset(Sbf[0][:], 0.0), w=["sSbf0"])
        P.op("dve", lambda e: e.memset(Tin[:, 0:32], 1.0), w=["sTin_a"])
        for r_ in range(32):
            P.op("dve" if r_ % 2 else "pool", lambda e, r_=r_: e.tensor_scalar(out=dI[:, r_, :], in0=C.ident_f[:], scalar1=dsk[:, r_:r_ + 1], scalar2=None, op0=ALU.mult),
                 r=["ident_f", "s_dskT"], w=[("sdI", r_)])
        one = C.ones_f[:, 0:1]
        NC_ = S // 128
        gcount = [0, 0]

        def front_pre(c):
            b = c % 2
            sl = slice(c * 128, (c + 1) * 128)
            V = lambda i: v32[b][:, i, :]
            sv = "sv%d" % b
            P.op("sp", lambda e: e.dma_start(out=dtr[b][:], in_=dv[:, c, :]), r=["dtr"], w=["sdtr%d" % b], dma="sdtr%d" % b)
            P.op("sp", lambda e: e.dma_start(out=BTc[b][:], in_=btv[:, :, sl]), r=["BTCT"], w=["sBT%d" % b], dma="sBT%d" % b)
            P.op("sp", lambda e: e.dma_start(out=CTc[b][:], in_=ctv[:, :, sl]), r=["BTCT"], w=["sCT%d" % b], dma="sCT%d" % b)
            P.op("sp", lambda e: e.dma_start(out=xb[b][:], in_=xsv[:, c, :]), r=["xsB"], w=["sxb%d" % b], dma="sxb%d" % b)
            P.op("dve", lambda e: e.tensor_tensor(out=V(0), in0=dtr[b][:], in1=dtb[:], op=ALU.add), r=["sdtr%d" % b, "s_dtbT"], w=[(sv, 0)])
            P.op("act", lambda e: e.activation(out=V(1), in_=V(0), func=AF.Exp), r=[(sv, 0)], w=[(sv, 1)])
            P.op("act", lambda e: e.activation(out=V(2), in_=V(1), func=AF.Ln, bias=one, scale=1.0), r=[(sv, 1), "ones_f"], w=[(sv, 2)])
            P.op("dve", lambda e: e.tensor_tensor(out=V(3), in0=V(2), in1=aneg[:], op=ALU.mult), r=[(sv, 2), "s_alogT"], w=[(sv, 3)])
            P.op("pe", lambda e: e.matmul(pmc[:, 0:32], lhsT=tri[:], rhs=V(3), start=True, stop=True), r=["s_tri", (sv, 3)], w=["spmc"])
            P.op("pe", lambda e: e.matmul(pmc[:, 32:64], lhsT=C.ones_f[:], rhs=V(3), start=True, stop=True), r=["ones_f", (sv, 3)], w=["spmc"])
            P.op("dve", lambda e: e.tensor_copy(out=sm[:], in_=pmc[:, 0:64]), r=["spmc"], w=["ssm"])
            P.op("dve", lambda e: e.tensor_scalar(out=Tin[:, 32:64], in0=sm[:, 0:32], scalar1=-1.0, scalar2=None, op0=ALU.mult), r=["ssm"], w=["sTin_b"])
            P.op("pe", lambda e: e.transpose(out=pmc[0:64, 256:384], in_=Tin[:], identity=C.ident_f[:]), r=["sTin_a", "sTin_b", "ident_f"], w=["spmc"])
            P.op("pe", lambda e: e.transpose(out=pmc[0:32, 384:512], in_=sm[:, 0:32], identity=C.ident_f[:]), r=["ssm", "ident_f"], w=["spmc"])
            P.op("act", lambda e: e.activation(out=Lm[:], in_=pmc[0:64, 256:384], func=AF.Copy), r=["spmc"], w=["sLm"])
            P.op("act", lambda e: e.activation(out=acsT[:], in_=pmc[0:32, 384:512], func=AF.Copy), r=["spmc"], w=["sacsT"])
            for hf, eng_ in ((0, "dve"), (1, "pool")):
                P.op(eng_, lambda e, hf=hf: e.tensor_tensor(
                    out=Rm[0:32, hf * 2048:(hf + 1) * 2048].rearrange("p (r q) -> p r q", q=128),
                    in0=blkT[:, hf * 2048:(hf + 1) * 2048].rearrange("p (r q) -> p r q", q=128),
                    in1=acsT[:].unsqueeze(1).broadcast_to([32, 16, 128]), op=ALU.mult),
                    r=["sacsT", "s_blkT"], w=[("sRm_a", hf)])
            P.op("act", lambda e: e.activation(out=V(4), in_=sm[:, 0:32], func=AF.Exp), r=["ssm"], w=[(sv, 4)])
            P.op("dve", lambda e: e.tensor_tensor(out=V(5), in0=sm[:, 32:64], in1=sm[:, 0:32], op=ALU.subtract), r=["ssm"], w=[(sv, 5)])
            P.op("act", lambda e: e.activation(out=V(5), in_=V(5), func=AF.Exp), r=[(sv, 5)], w=[(sv, 5)])
            P.op("act", lambda e: e.activation(out=V(6), in_=sm[:, 32:64], func=AF.Exp), r=["ssm"], w=[(sv, 6)])
            P.op("dve", lambda e: e.tensor_tensor(out=V(7), in0=V(2), in1=V(5), op=ALU.mult), r=[(sv, 2), (sv, 5)], w=[(sv, 7)])
            xs3 = xb[b][:, 0:2048].rearrange("p (h e) -> p h e", e=64)
            P.op("dve", lambda e: e.tensor_tensor(out=xtl[b][:].rearrange("p (h e) -> p h e", e=64), in0=xs3,
                                                  in1=V(2).unsqueeze(2).broadcast_to([128, 32, 64]), op=ALU.mult),
                 r=["sxb%d" % b, (sv, 2)], w=["sxtl%d" % b])
            P.op("pool", lambda e: e.tensor_tensor(out=xts[b][:].rearrange("p (h e) -> p h e", e=64), in0=xs3,
                                                   in1=V(7).unsqueeze(2).broadcast_to([128, 32, 64]), op=ALU.mult),
                 r=["sxb%d" % b, (sv, 7)], w=["sxts%d" % b])
        def front_group(c, g):
            b = c % 2
            sv = "sv%d" % b
            gb = gcount[0] % 2
            gcount[0] += 1
            P.op("pe", lambda e, g=g: e.matmul(pmc[:, 128:256], lhsT=BTc[b][:, g, :], rhs=CTc[b][:, g, :], start=True, stop=True),
                 r=["sBT%d" % b, "sCT%d" % b], w=["spmc"])
            P.op("act", lambda e, gb=gb: e.activation(out=CBm[gb][:], in_=pmc[:, 128:256], func=AF.Copy), r=["spmc"], w=["sCBm%d" % gb])
            P.op("pe", lambda e, g=g, gb=gb: e.matmul(pD[gb][:], lhsT=Lm[:], rhs=Rm[:, g * 512:(g + 1) * 512], start=True, stop=False),
                 r=["sLm", "sRm_a", "sRm_b"], w=["spD%d" % gb])
            P.op("pe", lambda e, gb=gb: e.matmul(pD[gb][:], lhsT=C.ident_bf[:], rhs=negm[:].rearrange("p r q -> p (r q)"), start=False, stop=True),
                 r=["ident_bf", "s_negm"], w=["spD%d" % gb])
            P.op("act", lambda e, gb=gb: e.activation(out=LT[gb][:], in_=pD[gb][:], func=AF.Exp), r=["spD%d" % gb], w=["sLT%d" % gb])
            P.op("dve" if g % 3 else "pool", lambda e, gb=gb, g=g: e.tensor_tensor(out=MT[b][:, g, :].rearrange("p (r q) -> p r q", q=128),
                                                              in0=LT[gb][:].rearrange("p (r q) -> p r q", q=128),
                                                              in1=CBm[gb][:].unsqueeze(1).broadcast_to([128, 4, 128]), op=ALU.mult),
                 r=["sLT%d" % gb, "sCBm%d" % gb], w=[("sMT%d" % b, g)])

        def back_pre(c):
            b = c % 2
            sl = slice(c * 128, (c + 1) * 128)
            sv = "sv%d" % b
            Sb_old, Sb_new = Sbf[c % 2], Sbf[(c + 1) % 2]
            ko, kn = "sSbf%d" % (c % 2), "sSbf%d" % ((c + 1) % 2)
            P.op("sp", lambda e: e.dma_start(out=zt[b][:], in_=zv[:, c, :]), r=["zs"], w=["szt%d" % b], dma="szt%d" % b)
            P.op("sp", lambda e: e.dma_start(out=xt[b][:], in_=xin_v[:, :, sl]), w=["sxt%d" % b], dma="sxt%d" % b)
            P.op("pool", lambda e: e.tensor_tensor(out=S32[:].rearrange("p (h e) -> p h e", e=64), in0=S32[:].rearrange("p (h e) -> p h e", e=64),
                                                   in1=v32[b][:, 6, :].unsqueeze(2).broadcast_to([128, 32, 64]), op=ALU.mult),
                 r=["sS32", (sv, 6)], w=["sS32"])
        def back_group(c, g):
            b = c % 2
            sl = slice(c * 128, (c + 1) * 128)
            sv = "sv%d" % b
            Sb_old, Sb_new = Sbf[c % 2], Sbf[(c + 1) % 2]
            ko, kn = "sSbf%d" % (c % 2), "sSbf%d" % ((c + 1) % 2)
            gb = gcount[1] % 2
            gcount[1] += 1
            for r_ in range(4):
                hd = 4 * g + r_
                P.op("pe", lambda e, r_=r_, hd=hd, gb=gb, g=g: e.matmul(pY[gb][:, r_ * 64:(r_ + 1) * 64], lhsT=MT[b][:, g, r_ * 128:(r_ + 1) * 128],
                                                                        rhs=xtl[b][:, hd * 64:(hd + 1) * 64], start=True, stop=False),
                     r=[("sMT%d" % b, g), "sxtl%d" % b], w=["spY%d" % gb])
                P.op("pe", lambda e, r_=r_, hd=hd, gb=gb: e.matmul(pY[gb][:, r_ * 64:(r_ + 1) * 64], lhsT=dI[:, hd, :],
                                                                   rhs=xb[b][:, hd * 64:(hd + 1) * 64], start=False, stop=True),
                     r=[("sdI", hd), "sxb%d" % b], w=["spY%d" % gb])
            P.op("pe", lambda e, g=g, gb=gb: e.matmul(pY[gb][:, 256:512], lhsT=CTc[b][:, g, :], rhs=Sb_old[:, g * 256:(g + 1) * 256], start=True, stop=True),
                 r=["sCT%d" % b, ko], w=["spY%d" % gb])
            P.op("dve", lambda e, g=g, gb=gb: e.tensor_tensor(out=ty[gb][:].rearrange("p (r e) -> p r e", e=64),
                                                              in0=pY[gb][:, 256:512].rearrange("p (r e) -> p r e", e=64),
                                                              in1=v32[b][:, 4, 4 * g:4 * g + 4].unsqueeze(2).broadcast_to([128, 4, 64]), op=ALU.mult),
                 r=["spY%d" % gb, (sv, 4)], w=["sty%d" % gb])
            P.op("dve", lambda e, g=g, gb=gb: e.tensor_tensor(out=y32[:, g * 256:(g + 1) * 256], in0=pY[gb][:, 0:256], in1=ty[gb][:], op=ALU.add),
                 r=["spY%d" % gb, "sty%d" % gb], w=[("sy32", g)])
            P.op("pe", lambda e, g=g: e.matmul(pS[:, 0:256], lhsT=xb[b][:, 2048 + g * 128:2048 + (g + 1) * 128], rhs=xts[b][:, g * 256:(g + 1) * 256],
                                               start=True, stop=True), r=["sxb%d" % b, "sxts%d" % b], w=["spS"])
            P.op("dve", lambda e, g=g: e.tensor_tensor(out=S32[:, g * 256:(g + 1) * 256], in0=pS[:, 0:256], in1=S32[:, g * 256:(g + 1) * 256], op=ALU.add),
                 r=["spS", "sS32"], w=[("sS32", g)])
        def back_post_ew(c):
            b = c % 2
            sl = slice(c * 128, (c + 1) * 128)
            sv = "sv%d" % b
            Sb_old, Sb_new = Sbf[c % 2], Sbf[(c + 1) % 2]
            ko, kn = "sSbf%d" % (c % 2), "sSbf%d" % ((c + 1) % 2)
            P.op("act", lambda e: e.activation(out=Sb_new[:], in_=S32[:], func=AF.Copy), r=["sS32"], w=[kn])
            P.op("pool", lambda e: e.tensor_tensor(out=y32[:], in0=y32[:], in1=zt[b][:], op=ALU.mult), r=["sy32", "szt%d" % b], w=["sy32"])
            for g in range(8):
                P.op("act", lambda e, g=g: e.activation(out=junk[:], in_=y32[:, g * 256:(g + 1) * 256], func=AF.Square, accum_out=ssq[:, g:g + 1]),
                     r=["sy32"], w=["sjunk", ("sssq", g)])
            P.op("act", lambda e: e.activation(out=ssq[:], in_=ssq[:], func=AF.Sqrt, bias=C.eps_t[:, 0:1], scale=1.0 / 256), r=["sssq"], w=["sssq"])
            P.op("dve", lambda e: e.reciprocal(out=ssq[:], in_=ssq[:]), r=["sssq"], w=["sssq"])
            for g in range(8):
                P.op("dve", lambda e, g=g: e.scalar_tensor_tensor(out=yn[:, g * 256:(g + 1) * 256], in0=y32[:, g * 256:(g + 1) * 256],
                                                                   scalar=ssq[:, g:g + 1], in1=normg[:, g * 256:(g + 1) * 256], op0=ALU.mult, op1=ALU.mult),
                     r=["sy32", "sssq", "s_normgT"], w=[("syn", g)])
        def back_post_pe(c):
            b = c % 2
            sl = slice(c * 128, (c + 1) * 128)
            sv = "sv%d" % b
            Sb_old, Sb_new = Sbf[c % 2], Sbf[(c + 1) % 2]
            ko, kn = "sSbf%d" % (c % 2), "sSbf%d" % ((c + 1) % 2)
            for i in range(4):
                ab = i % 2
                for ci in range(4):
                    dc = i * 4 + ci
                    P.op("pe", lambda e, ci=ci, dc=dc, ab=ab: e.transpose(out=pABb[ab][:, ci * 128:(ci + 1) * 128], in_=yn[:, dc * 128:(dc + 1) * 128], identity=C.ident_bf[:]),
                         r=["syn", "ident_bf"], w=["spAB%d" % ab])
                P.op("act", lambda e, i=i, ab=ab: e.activation(out=ynT[:, i * 4:(i + 1) * 4, :].rearrange("p a b -> p (a b)"), in_=pABb[ab][:, 0:512], func=AF.Copy),
                     r=["spAB%d" % ab], w=[("synT", i)])
            for f in range(8):
                ab = f % 2
                for dc in range(16):
                    P.op("pe", lambda e, f=f, ab=ab, dc=dc: e.matmul(pAB[ab][:, 0:128], lhsT=wout[:, dc, f * 128:(f + 1) * 128], rhs=ynT[:, dc, :],
                                                                    start=(dc == 0), stop=(dc == 15)), r=["swout", "synT"], w=["spAB%d" % ab])
                P.op("dve", lambda e, f=f, ab=ab: e.scalar_tensor_tensor(out=xt[b][:, f, :], in0=pAB[ab][:, 0:128], scalar=mod[:, g0 + f:g0 + f + 1],
                                                                          in1=xt[b][:, f, :], op0=ALU.mult, op1=ALU.add),
                     r=["spAB%d" % ab, "sxt%d" % b], w=["sxt%d" % b])
            P.op("sp", lambda e: e.dma_start(out=xout_v[:, :, sl], in_=xt[b][:]), r=["sxt%d" % b], w=["x1T"], dma="sxo%d" % b)

        front_pre(0)
        for g in range(8):
            front_group(0, g)
        for c in range(NC_):
            back_pre(c)
            if c + 1 < NC_:
                front_pre(c + 1)
            for g in range(8):
                if c + 1 < NC_:
                    front_group(c + 1, g)
                back_group(c, g)
                if g == 3 and c >= 1:
                    back_post_pe(c - 1)
            back_post_ew(c)
        back_post_pe(NC_ - 1)
        P.barrier()


def build(S, stages=None, outs=("out_tok",), chain=True, only_inputs=None):
    nc = bass.Bass("TRN2", target_bir_lowering=False)
    C = Ctx()
    C.nc = nc
    C.S = S
    TO = S // 2
    allst = ["m1z", "m1x", "m2", "ffn0", "qkv", "attn", "moes"]
    if stages is None:
        stages = allst
    d = {}

    def din(name, shape, dt=F32):
        if only_inputs is not None and name not in only_inputs:
            return
        d[name] = nc.dram_tensor(name, list(shape), dt, kind="ExternalInput").ap()

    def dscr(name, shape, dt=F32):
        kind = "ExternalOutput" if name in outs else "Internal"
        d[name] = nc.dram_tensor(name, list(shape), dt, kind=kind).ap()

    din("xT", [D, S]); din("cT", [128, 8]); din("ident", [128, 128]); din("sel", [8, 8, 128])
    din("ada_w0", [D, 6 * D]); din("ada_bT0", [128, 48])
    din("ada_w1", [D, 6 * D]); din("ada_bT1", [128, 48])
    din("m_w_in", [D, M_IN]); din("convwT", [128, 32, 4]); din("convbT", [128, 32])
    din("dtbT", [128, 32]); din("alogT", [128, 32]); din("dskT", [128, 32]); din("normgT", [128, 2048])
    din("m_w_out", [2048, D]); din("tri", [128, 128]); din("negm4", [128, 4, 128]); din("blk", [64, 4096])
    din("ffn_w_gate", [D, FFN_DIM]); din("ffn_w_up", [D, FFN_DIM]); din("ffn_w_down", [FFN_DIM, D])
    din("a_w_qkv", [D, 3072]); din("lamv", [128, 4, 64]); din("sublnT", [128, 1]); din("a_w_o", [D, D])
    din("masks", [4, 128, 4, 512]); din("rolesel", [128, 2])
    din("moe_w_router", [D, N_EXP]); din("moe_w_gate", [N_EXP, D, EXP_DIM]); din("moe_w_up", [N_EXP, D, EXP_DIM])
    din("moe_w_down", [N_EXP, EXP_DIM, D]); din("final_gT", [128, 8])
    NUe = EXP_DIM // 512
    for nm_ in ("moe_wg_r0", "moe_wg_r1", "moe_wu_r0", "moe_wu_r1", "moe_wd_r0", "moe_wd_r1"):
        din(nm_, [N_EXP * NUe * 128, 2048])
    din("sul", [128, 128]); din("thr", [128, 17]); din("iotaU", [128, NUe]); din("final_gB", [128, 1024])
    TS = TO if chain else S
    NBs = (2 * TS) // MOE_BLK + N_EXP
    din("bstart", [128, NBs])
    dscr("Hrow", [TS, 1024], BF16); dscr("HTs", [1024, TS], BF16); dscr("Xs", [NBs * MOE_BLK, 1024], BF16); dscr("Ys", [NBs * MOE_BLK, 1024]); dscr("out_tok", [TS, 1024])
    dscr("zs", [S, 2048], BF16); dscr("dtr", [S, 32]); dscr("xsB", [S, 3072], BF16)
    dscr("BT", [1024, S], BF16); dscr("CT", [1024, S], BF16)
    dscr("x1T", [D, S]); dscr("x2T", [D, S])
    dscr("QT", [1024, S], BF16); dscr("KT", [1024, S], BF16); dscr("V", [S, 1024], BF16)
    dscr("x3T", [D, TO]); dscr("x4T", [D, TO]); dscr("outT", [D, TO])
    C.d = d
    with ExitStack() as top:
        C.top = top
        C.P = Prog(nc, top)
        C.eps_t = sb(nc, top, "eps_t", [128, 1], F32)
        C.P.op("dve", lambda e: e.memset(C.eps_t[:], EPS), w=["eps_t"])
        stage_consts(C)
        x0 = d["xT"]
        if "m1z" in stages:
            stage_m1(C, x0, S, C.mod[0], "z")
        if "m1x" in stages:
            stage_m1(C, x0, S, C.mod[0], "x")
        if "m2" in stages:
            stage_m2(C, x0, d["x1T"], S, C.mod[0])
        if "ffn0" in stages:
            stage_ffn(C, d["x1T"] if chain else x0, d["x2T"], S, C.mod[0], d["ffn_w_gate"], d["ffn_w_up"], d["ffn_w_down"], FFN_DIM, "f0")
        xl1 = d["x2T"] if chain else x0
        if "qkv" in stages:
            stage_qkv(C, xl1, S, C.mod[1])
        if "attn" in stages:
            stage_attn(C, xl1, d["x3T"], S, C.mod[1])
        if "moe" in stages:
            stage_ffn(C, d["x3T"] if chain else x0, d["x4T"], TO if chain else S, C.mod[1], d["moe_w_gate"], d["moe_w_up"], d["moe_w_down"],
                      EXP_DIM, "m1", n_exp=N_EXP, wr=d["moe_w_router"])
        if "final" in stages:
            stage_final(C, d["x4T"], d["outT"], TO)
        if "moes" in stages:
            stage_moe_sparse(C, d["x3T"] if chain else x0, TO if chain else S, C.mod[1])
        C.P.emit(final_wait_keys=list(range(len(C.P.dma_cnt))))
    return nc


def host_consts(role):
    ident = np.eye(128, dtype=np.float32)
    sel = np.zeros((8, 8, 128), np.float32)
    for e in range(8):
        sel[e, e, :] = 1.0
    s_ = np.arange(128)
    tri = (s_[:, None] <= s_[None, :]).astype(np.float32)
    negm4 = np.ascontiguousarray(np.tile(((s_[:, None] > s_[None, :]) * -30000.0).astype(np.float32)[:, None, :], (1, 4, 1)))
    blk = np.zeros((64, 32, 128), np.float32)
    for r_ in range(32):
        blk[r_, r_, :] = 1.0
        blk[32 + r_, r_, :] = 1.0
    blk = blk.reshape(64, 4096)
    k_ = np.arange(512)
    trib = (k_[None, :] >= k_[:, None]).astype(np.float32).reshape(4, 128, 512).transpose(1, 0, 2)
    ones = np.ones_like(trib)
    zeros = np.zeros_like(trib)
    if role == 1:
        masks = np.stack([ones, trib, trib, zeros])
    else:
        masks = np.stack([trib, zeros, ones, trib])
    rolesel = np.zeros((128, 2), np.float32)
    rolesel[:, role] = 1.0
    sul = (s_[:, None] < s_[None, :]).astype(np.float32)
    thr = np.tile((np.arange(17) * float(MOE_BLK))[None, :], (128, 1)).astype(np.float32)
    NUe = EXP_DIM // 512
    iotaU = (np.arange(NUe)[None, :] * 128 + s_[:, None]).astype(np.float32)
    return {"sul": sul, "thr": thr, "iotaU": iotaU, "ident": ident, "sel": sel, "tri": tri, "negm4": negm4, "blk": blk, "masks": np.ascontiguousarray(masks), "rolesel": rolesel}


def make_in_maps(inp, S, dense_moe=False, TS=None):
    f = lambda a: np.ascontiguousarray(np.asarray(a, dtype=np.float32))
    col = lambda v, n: f(np.asarray(v).reshape(n, 128).T)
    til = lambda v: f(np.tile(np.asarray(v)[None, :], (128, 1)))
    shared = {
        "m_w_in": f(inp["m_w_in"]), "m_w_out": f(inp["m_w_out"]),
        "convwT": f(np.asarray(inp["m_conv_w"]).reshape(4, 32, 128).transpose(2, 1, 0)),
        "convbT": col(inp["m_conv_b"], 32), "dtbT": til(inp["m_dt_bias"]), "alogT": til(inp["m_a_log"]),
        "dskT": til(inp["m_d_skip"]), "normgT": til(inp["m_norm_g"]),
        "ffn_w_gate": f(inp["ffn_w_gate"]), "ffn_w_up": f(inp["ffn_w_up"]), "ffn_w_down": f(inp["ffn_w_down"]),
        "a_w_qkv": f(inp["a_w_qkv"]), "a_w_o": f(inp["a_w_o"]),
        "lamv": f(np.tile(np.stack([inp["a_lam_q1"], inp["a_lam_k1"], inp["a_lam_q2"], inp["a_lam_k2"]])[None], (128, 1, 1))),
        "sublnT": col(inp["a_subln_g"], 1),
        "moe_w_router": f(inp["moe_w_router"]), "final_gT": col(inp["final_g"], 8), "final_gB": til(inp["final_g"]),
    }
    if TS is None:
        TS = S // 2
    NBs = (2 * TS) // MOE_BLK + N_EXP
    shared["bstart"] = f(np.tile((np.arange(NBs) * float(MOE_BLK))[None, :], (128, 1)))
    if dense_moe:
        shared.update({"moe_w_gate": f(inp["moe_w_gate"]), "moe_w_up": f(inp["moe_w_up"]), "moe_w_down": f(inp["moe_w_down"])})
    else:
        NUe = EXP_DIM // 512
        for nm_, key in (("moe_wg_r", "moe_w_gate"), ("moe_wu_r", "moe_w_up")):
            w_ = np.asarray(inp[key], dtype=np.float32).reshape(N_EXP, 2, 4, 128, NUe, 512)
            w2_ = np.ascontiguousarray(w_.transpose(1, 0, 4, 3, 2, 5)).reshape(2, N_EXP * NUe * 128, 2048)
            shared[nm_ + "0"], shared[nm_ + "1"] = w2_[0], w2_[1]
        w_ = np.asarray(inp["moe_w_down"], dtype=np.float32).reshape(N_EXP, NUe, 2, 2, 128, 1024)
        w2_ = np.ascontiguousarray(w_.transpose(2, 0, 1, 4, 3, 5)).reshape(2, N_EXP * NUe * 128, 2048)
        shared["moe_wd_r0"], shared["moe_wd_r1"] = w2_[0], w2_[1]
    shared["ada_w0"] = f(inp["ada_w0"]); shared["ada_bT0"] = col(inp["ada_b0"], 48)
    shared["ada_w1"] = f(inp["ada_w1"]); shared["ada_bT1"] = col(inp["ada_b1"], 48)
    hc = [host_consts(0), host_consts(1)]
    maps = []
    for c in range(8):
        b, role = c // 2, c % 2
        m = dict(shared)
        m.update(hc[role])
        m["xT"] = f(np.asarray(inp["x"])[b, :S, :].T)
        m["cT"] = col(np.asarray(inp["c"])[b], 8)
        maps.append(m)
    return maps


def assemble(res, S, B=4):
    TO = S // 2
    n_slots = S // 1024
    TA, TB = own_tiles(n_slots)
    out = np.zeros((B, S, D), np.float32)
    for c in range(2 * B):
        b, role = c // 2, c % 2
        o = np.asarray(res[c]["out_tok"])
        tiles = TA if role == 0 else TB
        for si, t in enumerate(tiles):
            out[b, t * 512:(t + 1) * 512, :] = o[si * 512:(si + 1) * 512, :]
    return out


_NC_CACHE = {}
_USED_INPUTS = {"xT", "cT", "ident", "sel", "ada_w0", "ada_bT0", "ada_w1", "ada_bT1", "m_w_in", "convwT", "convbT", "dtbT", "alogT",
                "dskT", "normgT", "m_w_out", "tri", "negm4", "blk", "ffn_w_gate", "ffn_w_up", "ffn_w_down", "a_w_qkv", "lamv", "sublnT",
                "a_w_o", "masks", "rolesel", "moe_w_router", "moe_wg_r0", "moe_wg_r1", "moe_wu_r0", "moe_wu_r1", "moe_wd_r0", "moe_wd_r1", "sul", "thr", "iotaU", "final_gB", "bstart"}


def kernel(**inputs):
    S = int(np.asarray(inputs["x"]).shape[1])
    if S not in _NC_CACHE:
        _NC_CACHE[S] = build(S, only_inputs=_USED_INPUTS)
    nc = _NC_CACHE[S]
    maps = [{k: v for k, v in m.items() if k in _USED_INPUTS} for m in make_in_maps(inputs, S)]
    res = run_bass_kernel_spmd(nc, maps, core_ids=list(range(8)))
    return assemble(res.results, S)


U32 = mybir.dt.uint32
MOE_BLK = 512


def stage_moe_sparse(C, x_in, T, mod):
    nc, P, d = C.nc, C.P, C.d
    BLK = MOE_BLK
    NCH = T // 128
    NB = (2 * T) // BLK + N_EXP
    NR = NB * BLK
    HG = 4
    NU = EXP_DIM // (HG * 128)
    xin_v = x_in.rearrange("(k p) t -> p k t", p=128)
    g0 = 5 * 8
    st = ExitStack()
    with st as ls:
        S1A = sb(nc, ls, "eS1A", [128, NCH, 8], F32)
        S2A = sb(nc, ls, "eS2A", [128, NCH, 8], F32)
        POS = sb(nc, ls, "ePOS", [128, NCH, 8], F32)
        GW = sb(nc, ls, "eGW", [128, NCH, 2], F32)
        base = sb(nc, ls, "ebase", [128, 8], F32)
        D1f = sb(nc, ls, "eD1f", [128, NCH], F32)
        D2f = sb(nc, ls, "eD2f", [128, NCH], F32)
        D1 = sb(nc, ls, "eD1", [128, NCH], U32)
        D2 = sb(nc, ls, "eD2", [128, NCH], U32)
        WIf = sb(nc, ls, "eWIf", [128, NB, NU], F32)
        WI = sb(nc, ls, "eWI", [128, NB, NU], U32)
        sul = sb(nc, ls, "esul", [128, 128], F32)
        thr = sb(nc, ls, "ethr", [128, 17], F32)
        bst = sb(nc, ls, "ebst", [128, NB], F32)
        iop = sb(nc, ls, "eiop", [128, NU], F32)
        wrt = sb(nc, ls, "ewr", [128, 8, 8], F32)
        sel = sb(nc, ls, "esel", [8, 8, 128], F32)
        fgB = sb(nc, ls, "efgB", [128, 1024], F32)
        g2b = sb(nc, ls, "eg2b", [128, 1024], F32)
        P.op("sp", lambda e: e.dma_start(out=sul[:], in_=d["sul"][:, :]), w=["esul"], dma="e_sul")
        P.op("sp", lambda e: e.dma_start(out=thr[:], in_=d["thr"][:, :]), w=["ethr"], dma="e_thr")
        P.op("sp", lambda e: e.dma_start(out=bst[:], in_=d["bstart"][:, :]), w=["ebst"], dma="e_bst")
        P.op("sp", lambda e: e.dma_start(out=iop[:], in_=d["iotaU"][:, :]), w=["eiop"], dma="e_iop")
        P.op("sp", lambda e: e.dma_start(out=wrt[:], in_=d["moe_w_router"].rearrange("(k p) e -> p k e", p=128)), w=["ewr"], dma="e_wr")
        P.op("sp", lambda e: e.dma_start(out=sel[:], in_=d["sel"][:, :, :]), w=["esel"], dma="e_sel")
        P.op("sp", lambda e: e.dma_start(out=fgB[:], in_=d["final_gB"][:, :]), w=["efgB"], dma="e_fgB")
        P.op("dve", lambda e: e.memset(base[:], 0.0), w=["ebase"])
        hts_v = d["HTs"].rearrange("(k p) t -> p k t", p=128)
        with ExitStack() as l2:
            xt = [sb(nc, l2, "ext%d" % i, [128, 8, 512], F32) for i in range(2)]
            hT = sb(nc, l2, "ehT", [128, 8, 512], BF16)
            h32 = sb(nc, l2, "eh32", [128, 8, 512], F32)
            sq = sb(nc, l2, "esq", [128, 8, 512], BF16)
            tmp = [sb(nc, l2, "etmp%d" % i, [128, 512], F32) for i in range(2)]
            sqv = sb(nc, l2, "esqv", [128, 512], F32)
            lg = sb(nc, l2, "elg", [128, 8], F32)
            mx = sb(nc, l2, "emx", [128, 8], F32)
            gd = sb(nc, l2, "egd", [128, 2], F32)
            Sel = sb(nc, l2, "eSel", [128, 8], F32)
            hrow = [sb(nc, l2, "ehrow%d" % i, [128, 1024], BF16) for i in range(2)]
            ps_x = l2.enter_context(nc.psum_tensor("epsx", [128, 512], F32))
            ps_r = l2.enter_context(nc.psum_tensor("epsr", [128, 512], F32))
            ps_t32 = l2.enter_context(nc.psum_tensor("epst", [128, 512], F32))
            ps_tR = ps_t32[:].bitcast(BF16)
            zrow = sb(nc, l2, "ezrow", [128, 4, 1024], BF16)
            P.op("pool", lambda e: e.memset(zrow[:], 0.0), w=["ezrow"])
            xs_z = d["Xs"].rearrange("(n p) f -> p n f", p=128)
            for i in range(NB):
                P.op("sp", lambda e, i=i: e.dma_start(out=xs_z[:, i * 4:(i + 1) * 4, :], in_=zrow[:]), r=["ezrow"], w=[("Xz", i)], dma="e_xz%d" % (i % 4))
            for ti in range(T // 512):
                b = ti % 2
                P.op("sp", lambda e, b=b, ti=ti: e.dma_start(out=xt[b][:], in_=xin_v[:, :, ti * 512:(ti + 1) * 512]), w=["ext%d" % b], dma="e_x%d" % b)
                norm_mod(C, xt[b][:], hT, ps_x, sq, tmp, sqv, mod, 3, "e", "ext%d" % b, "ehT", h32=h32)
                P.op("sp", lambda e, ti=ti: e.dma_start(out=hts_v[:, :, ti * 512:(ti + 1) * 512], in_=hT[:]), r=["ehT"], w=["HTs"], dma="e_hts")
                for c4 in range(4):
                    c = ti * 4 + c4
                    hb = c % 2
                    for k in range(8):
                        P.op("pe", lambda e, c4=c4, k=k: e.matmul(ps_r[:, 0:8], lhsT=h32[:, k, c4 * 128:(c4 + 1) * 128], rhs=wrt[:, k, :],
                                                                  start=(k == 0), stop=(k == 7)), r=[("h32", k), "ewr"], w=["epsr"])
                    P.op("dve", lambda e: e.tensor_copy(out=lg[:], in_=ps_r[:, 0:8]), r=["epsr"], w=["elg"])
                    P.op("dve", lambda e: e.max(out=mx[:], in_=lg[:]), r=["elg"], w=["emx"])
                    P.op("dve", lambda e: e.tensor_tensor(out=gd[:, 0:1], in0=mx[:, 0:1], in1=mx[:, 1:2], op=ALU.subtract), r=["emx"], w=["egd"])
                    P.op("act", lambda e, c=c: e.activation(out=GW[:, c, 0:1], in_=gd[:, 0:1], func=AF.Sigmoid), r=["egd"], w=[("eGW", c)])
                    P.op("dve", lambda e, c=c: e.tensor_scalar(out=GW[:, c, 1:2], in0=GW[:, c, 0:1], scalar1=-1.0, scalar2=1.0, op0=ALU.mult, op1=ALU.add),
                         r=[("eGW", c)], w=[("eGW", c)])
                    P.op("dve", lambda e, c=c: e.tensor_scalar(out=S1A[:, c, :], in0=lg[:], scalar1=mx[:, 0:1], scalar2=None, op0=ALU.is_equal),
                         r=["elg", "emx"], w=[("eS1A", c)])
                    P.op("dve", lambda e, c=c: e.tensor_scalar(out=S2A[:, c, :], in0=lg[:], scalar1=mx[:, 1:2], scalar2=None, op0=ALU.is_equal),
                         r=["elg", "emx"], w=[("eS2A", c)])
                    P.op("dve", lambda e, c=c: e.tensor_tensor(out=Sel[:], in0=S1A[:, c, :], in1=S2A[:, c, :], op=ALU.add),
                         r=[("eS1A", c), ("eS2A", c)], w=["eSel"])
                    P.op("pe", lambda e: e.matmul(ps_r[:, 8:16], lhsT=sul[:], rhs=Sel[:], start=True, stop=True), r=["esul", "eSel"], w=["epsr"])
                    P.op("pe", lambda e: e.matmul(ps_r[:, 16:24], lhsT=C.ones_f[:], rhs=Sel[:], start=True, stop=True), r=["ones_f", "eSel"], w=["epsr"])
                    P.op("dve", lambda e, c=c: e.tensor_tensor(out=POS[:, c, :], in0=ps_r[:, 8:16], in1=base[:], op=ALU.add), r=["epsr", "ebase"], w=[("ePOS", c)])
                    P.op("dve", lambda e: e.tensor_tensor(out=base[:], in0=ps_r[:, 16:24], in1=base[:], op=ALU.add), r=["epsr", "ebase"], w=["ebase"])
            cmp = sb(nc, l2, "ecmp", [128, 32], F32)
            nbk = sb(nc, l2, "enbk", [128, 8], F32)
            pend = sb(nc, l2, "epend", [128, 8], F32)
            pstart = sb(nc, l2, "epstart", [128, 8], F32)
            bexp = sb(nc, l2, "ebexp", [128, NB], F32)
            ptmp = sb(nc, l2, "eptmp", [128, NCH, 8], F32)
            for e_ in range(8):
                P.op("dve", lambda e, e_=e_: e.tensor_scalar(out=cmp[:, 0:17], in0=thr[:], scalar1=base[:, e_:e_ + 1], scalar2=None, op0=ALU.is_lt),
                     r=["ethr", "ebase"], w=["ecmp"])
                P.op("dve", lambda e, e_=e_: e.tensor_reduce(out=nbk[:, e_:e_ + 1], in_=cmp[:, 0:17], axis=AX.X, op=ALU.add), r=["ecmp"], w=["enbk"])
            P.op("dve", lambda e: e.tensor_scalar(out=nbk[:], in0=nbk[:], scalar1=float(BLK), scalar2=None, op0=ALU.mult), r=["enbk"], w=["enbk"])
            P.op("dve", lambda e: e.tensor_copy(out=pend[:, 0:1], in_=nbk[:, 0:1]), r=["enbk"], w=["epend"])
            for e_ in range(1, 8):
                P.op("dve", lambda e, e_=e_: e.tensor_tensor(out=pend[:, e_:e_ + 1], in0=pend[:, e_ - 1:e_], in1=nbk[:, e_:e_ + 1], op=ALU.add),
                     r=["epend", "enbk"], w=["epend"])
            P.op("dve", lambda e: e.tensor_tensor(out=pstart[:], in0=pend[:], in1=nbk[:], op=ALU.subtract), r=["epend", "enbk"], w=["epstart"])
            P.op("dve", lambda e: e.memset(bexp[:], 0.0), w=["ebexp"])
            for e_ in range(8):
                P.op("dve", lambda e, e_=e_: e.tensor_scalar(out=cmp[:, 0:NB], in0=bst[:], scalar1=pend[:, e_:e_ + 1], scalar2=None, op0=ALU.is_ge),
                     r=["ebst", "epend"], w=["ecmp"])
                P.op("dve", lambda e: e.tensor_tensor(out=bexp[:], in0=bexp[:], in1=cmp[:, 0:NB], op=ALU.add), r=["ebexp", "ecmp"], w=["ebexp"])
            P.op("dve", lambda e: e.tensor_scalar(out=bexp[:], in0=bexp[:], scalar1=float(N_EXP - 1), scalar2=float(NU * 128), op0=ALU.min, op1=ALU.mult),
                 r=["ebexp"], w=["ebexp"])
            P.op("dve", lambda e: e.tensor_tensor(out=WIf[:], in0=bexp[:].unsqueeze(2).broadcast_to([128, NB, NU]),
                                                  in1=iop[:].unsqueeze(1).broadcast_to([128, NB, NU]), op=ALU.add), r=["ebexp", "eiop"], w=["eWIf"])
            P.op("dve", lambda e: e.tensor_copy(out=WI[:], in_=WIf[:]), r=["eWIf"], w=["eWI"])
            P.op("dve", lambda e: e.tensor_tensor(out=ptmp[:], in0=POS[:], in1=pstart[:].unsqueeze(1).broadcast_to([128, NCH, 8]), op=ALU.add),
                 r=["ePOS", "epstart"], w=["eptmp"])
            for (SA, Df, Du, nm_) in ((S1A, D1f, D1, "1"), (S2A, D2f, D2, "2")):
                P.op("dve", lambda e, SA=SA: e.tensor_tensor(out=SA[:], in0=SA[:], in1=ptmp[:], op=ALU.mult), r=["eS1A", "eS2A", "eptmp"], w=["eS%sA" % nm_])
                P.op("dve", lambda e, SA=SA, Df=Df: e.tensor_reduce(out=Df[:], in_=SA[:], axis=AX.X, op=ALU.add), r=["eS%sA" % nm_], w=["eD%sf" % nm_])
                P.op("dve", lambda e, Df=Df: e.tensor_scalar(out=Df[:], in0=Df[:], scalar1=float(NR - 1), scalar2=None, op0=ALU.min),
                     r=["eD%sf" % nm_], w=["eD%sf" % nm_])
                P.op("dve", lambda e, Df=Df, Du=Du: e.tensor_copy(out=Du[:], in_=Df[:]), r=["eD%sf" % nm_], w=["eD%s" % nm_])
            hrow4 = hrow + [sb(nc, l2, "ehrow%d" % i, [128, 1024], BF16) for i in (2, 3)]
            hcb = [sb(nc, l2, "ehc%d" % i, [128, 8, 128], BF16) for i in range(3)]
            ps_tS = [ps_tR, ps_x[:].bitcast(BF16)]
            ps_tSk = ["epst", "ers_ps"]
            for c in range(NCH if "noS" not in DBG else 0):
                hb = c % 4
                h3 = c % 3
                tb = c % 2
                P.op("sp", lambda e, h3=h3, c=c: e.dma_start(out=hcb[h3][:], in_=hts_v[:, :, c * 128:(c + 1) * 128]), r=["HTs"], w=["ehc%d" % h3], dma="e_hi%d" % h3)
                for k in range(8):
                    P.op("pe", lambda e, k=k, h3=h3, tb=tb: e.transpose(out=ps_tS[tb][:, k * 128:(k + 1) * 128], in_=hcb[h3][:, k, :], identity=C.ident_bf[:]),
                         r=["ehc%d" % h3, "ident_bf"], w=[ps_tSk[tb]])
                P.op("act", lambda e, hb=hb, tb=tb: e.activation(out=hrow4[hb][:], in_=ps_tS[tb][:, 0:1024], func=AF.Copy), r=[ps_tSk[tb]], w=["ehrow%d" % hb])
                for (Du, nm_) in ((D1, "1"), (D2, "2")):
                    P.op("pool", lambda e, hb=hb, c=c, Du=Du: e.indirect_dma_start(
                        out=d["Xs"][:, :], out_offset=bass.IndirectOffsetOnAxis(ap=Du[:, c:c + 1], axis=0), in_=hrow4[hb][:], in_offset=None),
                        r=["ehrow%d" % hb, "eD%s" % nm_, "Xz"], w=[("Xsc", 2 * c + int(nm_))], dma="e_sc%s%d" % (nm_, hb))
        P.barrier()
        with ExitStack() as l2:
            g2T = sb(nc, l2, "eg2T", [8, 128], F32)
            pg = [l2.enter_context(nc.psum_tensor("epg%d" % i, [128, 512], F32)) for i in range(2)]
            P.op("pe", lambda e: e.transpose(out=pg[0][0:8, 0:128], in_=mod[:, g0:g0 + 8], identity=C.ident_f[:]), r=["ident_f"], w=["epg0"])
            P.op("act", lambda e: e.activation(out=g2T[:], in_=pg[0][0:8, 0:128], func=AF.Copy), r=["epg0"], w=["eg2T"])
            for k in range(8):
                P.op("pe", lambda e, k=k: e.matmul(pg[k // 4][:, (k % 4) * 128:(k % 4 + 1) * 128], lhsT=sel[:, k, :], rhs=g2T[:], start=True, stop=True),
                     r=["esel", "eg2T"], w=["epg%d" % (k // 4)])
            for hf in range(2):
                P.op("act", lambda e, hf=hf: e.activation(out=g2b[:, hf * 512:(hf + 1) * 512], in_=pg[hf][:], func=AF.Copy), r=["epg%d" % hf], w=["eg2b"])
            P.op("dve", lambda e: e.tensor_copy(out=g2T[:], in_=g2T[:]), r=["eg2b", "eg2T"], w=["eg2T"])
        P.barrier()
        with ExitStack() as l2:
            xrow = [sb(nc, l2, "exrow%d" % i, [128, 4, 1024], BF16) for i in range(2)]
            XT = sb(nc, l2, "eXT", [128, 8, 512], BF16)
            acc = sb(nc, l2, "eacc", [128, 8, 512], F32)
            yrow = sb(nc, l2, "eyrow", [128, 4, 1024], F32)
            wgb = [sb(nc, l2, "ewg%d" % i, [128, 8, HG * 128], BF16) for i in range(2)]
            wub = [sb(nc, l2, "ewu%d" % i, [128, 8, HG * 128], BF16) for i in range(2)]
            wdb = [sb(nc, l2, "ewd%d" % i, [128, HG, 1024], BF16) for i in range(2)]
            aT = [sb(nc, l2, "eaT%d" % i, [128, HG, 512], BF16) for i in range(2)]
            sg = [sb(nc, l2, "esg%d" % i, [128, 512], F32) for i in range(2)]
            ps_g = [l2.enter_context(nc.psum_tensor("epsg%d" % i, [128, 512], F32)) for i in range(2)]
            ps_u = [l2.enter_context(nc.psum_tensor("epsu%d" % i, [128, 512], F32)) for i in range(2)]
            ps_d = [l2.enter_context(nc.psum_tensor("epsd%d" % i, [128, 512], F32)) for i in range(2)]
            ps_t = [l2.enter_context(nc.psum_tensor("epstt%d" % i, [128, 512], F32)) for i in range(2)]
            ps_tb = [p_[:].bitcast(BF16) for p_ in ps_t]
            xs_v = d["Xs"].rearrange("(n p) f -> p n f", p=128)
            ys_v = d["Ys"].rearrange("(n p) f -> p n f", p=128)
            wgv = [d["moe_wg_r0"], d["moe_wg_r1"]]
            wuv = [d["moe_wu_r0"], d["moe_wu_r1"]]
            wdv = [d["moe_wd_r0"], d["moe_wd_r1"]]
            ug = 0
            tcnt = 0
            pend_dn = [None]
            for i in range(NB if "noE" not in DBG else 0):
                xb_ = i % 2
                P.op("sp", lambda e, xb_=xb_, i=i: e.dma_start(out=xrow[xb_][:], in_=xs_v[:, i * 4:(i + 1) * 4, :]), r=["Xs"], w=["exrow%d" % xb_], dma="e_xr%d" % xb_)
                for k in range(8):
                    tb = tcnt % 2
                    tcnt += 1
                    for n in range(4):
                        P.op("pe", lambda e, tb=tb, n=n, k=k, xb_=xb_: e.transpose(out=ps_tb[tb][:, n * 128:(n + 1) * 128], in_=xrow[xb_][:, n, k * 128:(k + 1) * 128],
                                                                                identity=C.ident_bf[:]), r=["exrow%d" % xb_, "ident_bf"], w=["epstt%d" % tb])
                    P.op("act" if k % 2 else "dve", (lambda e, tb=tb, k=k: e.activation(out=XT[:, k, :], in_=ps_tb[tb][:, 0:512], func=AF.Copy)) if k % 2 else
                         (lambda e, tb=tb, k=k: e.tensor_copy(out=XT[:, k, :], in_=ps_tb[tb][:, 0:512])), r=["epstt%d" % tb], w=[("eXT", k)])
                for uu in range(NU):
                    b = ug % 2
                    ug += 1
                    for hf in range(2):
                        P.op("pool", lambda e, b=b, i=i, uu=uu, hf=hf: e.indirect_dma_start(
                            out=wgb[b][:, hf * 4:(hf + 1) * 4, :].rearrange("p k f -> p (k f)"), out_offset=None, in_=wgv[hf][:, :],
                            in_offset=bass.IndirectOffsetOnAxis(ap=WI[:, i, uu:uu + 1], axis=0)),
                            r=["eWI"], w=[("ewg%d" % b, hf)], dma="e_wg%d%d" % (b, hf))
                        P.op("pool", lambda e, b=b, i=i, uu=uu, hf=hf: e.indirect_dma_start(
                            out=wub[b][:, hf * 4:(hf + 1) * 4, :].rearrange("p k f -> p (k f)"), out_offset=None, in_=wuv[hf][:, :],
                            in_offset=bass.IndirectOffsetOnAxis(ap=WI[:, i, uu:uu + 1], axis=0)),
                            r=["eWI"], w=[("ewu%d" % b, hf)], dma="e_wu%d%d" % (b, hf))
                        P.op("pool", lambda e, b=b, i=i, uu=uu, hf=hf: e.indirect_dma_start(
                            out=wdb[b][:, hf * 2:(hf + 1) * 2, :].rearrange("p k f -> p (k f)"), out_offset=None, in_=wdv[hf][:, :],
                            in_offset=bass.IndirectOffsetOnAxis(ap=WI[:, i, uu:uu + 1], axis=0)),
                            r=["eWI"], w=[("ewd%d" % b, hf)], dma="e_wd%d%d" % (b, hf))
                    ab = ug % 2
                    for j in range(HG):
                        pb = j % 2
                        for k in range(8):
                            P.op("pe", lambda e, b=b, pb=pb, j=j, k=k: e.matmul(ps_g[pb][:], lhsT=wgb[b][:, k, j * 128:(j + 1) * 128], rhs=XT[:, k, :],
                                                                                start=(k == 0), stop=(k == 7)), r=["ewg%d" % b, "eXT"], w=["epsg%d" % pb])
                        for k in range(8):
                            P.op("pe", lambda e, b=b, pb=pb, j=j, k=k: e.matmul(ps_u[pb][:], lhsT=wub[b][:, k, j * 128:(j + 1) * 128], rhs=XT[:, k, :],
                                                                                start=(k == 0), stop=(k == 7)), r=["ewu%d" % b, "eXT"], w=["epsu%d" % pb])
                        P.op("act", lambda e, pb=pb: e.activation(out=sg[pb][:], in_=ps_g[pb][:], func=AF.Silu), r=["epsg%d" % pb], w=["esg%d" % pb])
                        P.op("dve", lambda e, pb=pb, ab=ab, j=j: e.tensor_tensor(out=aT[ab][:, j, :], in0=sg[pb][:], in1=ps_u[pb][:], op=ALU.mult),
                             r=["esg%d" % pb, "epsu%d" % pb], w=[("eaT%d" % ab, j)])
                    def _down(b=b, ab=ab, uu=uu):
                        for f in range(8):
                            db = f % 2
                            for j in range(HG):
                                P.op("pe", lambda e, b=b, db=db, j=j, f=f, ab=ab: e.matmul(ps_d[db][:], lhsT=wdb[b][:, j, f * 128:(f + 1) * 128], rhs=aT[ab][:, j, :],
                                                                                           start=(j == 0), stop=(j == HG - 1)), r=["ewd%d" % b, ("eaT%d" % ab, j)], w=["epsd%d" % db])
                            if uu == 0:
                                P.op("act", lambda e, db=db, f=f: e.activation(out=acc[:, f, :], in_=ps_d[db][:], func=AF.Copy), r=["epsd%d" % db], w=[("eacc", f)])
                            else:
                                P.op("dve", lambda e, db=db, f=f: e.tensor_tensor(out=acc[:, f, :], in0=ps_d[db][:], in1=acc[:, f, :], op=ALU.add),
                                     r=["epsd%d" % db, ("eacc", f)], w=[("eacc", f)])
                    _down()
                for n in range(4):
                    for hf in range(2):
                        tb = tcnt % 2
                        tcnt += 1
                        for kk in range(4):
                            k = hf * 4 + kk
                            P.op("pe", lambda e, tb=tb, kk=kk, k=k, n=n: e.transpose(out=ps_t[tb][:, kk * 128:(kk + 1) * 128], in_=acc[:, k, n * 128:(n + 1) * 128],
                                                                                     identity=C.ident_f[:]), r=[("eacc", k), "ident_f"], w=["epstt%d" % tb])
                        P.op("act" if hf else "dve", (lambda e, tb=tb, n=n, hf=hf: e.activation(out=yrow[:, n, hf * 512:(hf + 1) * 512], in_=ps_t[tb][:], func=AF.Copy)) if hf else
                             (lambda e, tb=tb, n=n, hf=hf: e.tensor_copy(out=yrow[:, n, hf * 512:(hf + 1) * 512], in_=ps_t[tb][:])), r=["epstt%d" % tb], w=[("eyrow", n)])
                P.op("sp", lambda e, i=i: e.dma_start(out=ys_v[:, i * 4:(i + 1) * 4, :], in_=yrow[:]), r=["eyrow"], w=["Ys"], dma="e_yo")
        P.barrier()
        with ExitStack() as l2:
            xc = [sb(nc, l2, "exc%d" % i, [128, 8, 128], F32) for i in range(2)]
            xr = [sb(nc, l2, "exr%d" % i, [128, 1024], F32) for i in range(2)]
            y1 = [sb(nc, l2, "ey1%d" % i, [128, 1024], F32) for i in range(2)]
            y2 = [sb(nc, l2, "ey2%d" % i, [128, 1024], F32) for i in range(2)]
            junk = sb(nc, l2, "ejunk", [128, 1024], F32)
            ss = sb(nc, l2, "ess", [128, 2], F32)
            pc = [l2.enter_context(nc.psum_tensor("epc%d" % i, [128, 512], F32)) for i in range(4)]
            out_v = d["out_tok"].rearrange("(n p) f -> p n f", p=128)
            for c in range(NCH if "noC" not in DBG else 0):
                b = c % 2
                P.op("sp", lambda e, b=b, c=c: e.dma_start(out=xc[b][:], in_=xin_v[:, :, c * 128:(c + 1) * 128]), w=["exc%d" % b], dma="e_xc%d" % b)
                P.op("pool", lambda e, b=b, c=c: e.indirect_dma_start(out=y1[b][:], out_offset=None, in_=d["Ys"][:, :],
                                                                      in_offset=bass.IndirectOffsetOnAxis(ap=D1[:, c:c + 1], axis=0)),
                     r=["Ys", "eD1"], w=["ey1%d" % b], dma="e_g1%d" % b)
                P.op("pool", lambda e, b=b, c=c: e.indirect_dma_start(out=y2[b][:], out_offset=None, in_=d["Ys"][:, :],
                                                                      in_offset=bass.IndirectOffsetOnAxis(ap=D2[:, c:c + 1], axis=0)),
                     r=["Ys", "eD2"], w=["ey2%d" % b], dma="e_g2%d" % b)
                for hf in range(2):
                    pcb = pc[(c % 2) * 2 + hf]
                    pk = "epc%d" % ((c % 2) * 2 + hf)
                    for kk in range(4):
                        k = hf * 4 + kk
                        P.op("pe", lambda e, pcb=pcb, kk=kk, k=k, b=b: e.transpose(out=pcb[:, kk * 128:(kk + 1) * 128], in_=xc[b][:, k, :], identity=C.ident_f[:]),
                             r=["exc%d" % b, "ident_f"], w=[pk])
                    P.op("act", lambda e, pcb=pcb, hf=hf, b=b: e.activation(out=xr[b][:, hf * 512:(hf + 1) * 512], in_=pcb[:], func=AF.Copy), r=[pk], w=[("exr%d" % b, hf)])
                P.op("dve", lambda e, b=b, c=c: e.tensor_scalar(out=y1[b][:], in0=y1[b][:], scalar1=GW[:, c, 0:1], scalar2=None, op0=ALU.mult),
                     r=["ey1%d" % b, "eGW"], w=["ey1%d" % b])
                P.op("dve", lambda e, b=b, c=c: e.scalar_tensor_tensor(out=y1[b][:], in0=y2[b][:], scalar=GW[:, c, 1:2], in1=y1[b][:], op0=ALU.mult, op1=ALU.add),
                     r=["ey1%d" % b, "ey2%d" % b, "eGW"], w=["ey1%d" % b])
                P.op("pool", lambda e, b=b: e.tensor_tensor(out=y1[b][:], in0=y1[b][:], in1=g2b[:], op=ALU.mult), r=["ey1%d" % b, "eg2b"], w=["ey1%d" % b])
                P.op("dve", lambda e, b=b: e.tensor_tensor(out=xr[b][:], in0=xr[b][:], in1=y1[b][:], op=ALU.add), r=["exr%d" % b, "ey1%d" % b], w=["exr%d" % b])
                P.op("act", lambda e, b=b: e.activation(out=junk[:], in_=xr[b][:], func=AF.Square, accum_out=ss[:, 0:1]), r=["exr%d" % b], w=["ejunk", "ess"])
                P.op("act", lambda e: e.activation(out=ss[:, 1:2], in_=ss[:, 0:1], func=AF.Sqrt, bias=C.eps_t[:, 0:1], scale=1.0 / D), r=["ess"], w=["ess"])
                P.op("dve", lambda e: e.reciprocal(out=ss[:, 1:2], in_=ss[:, 1:2]), r=["ess"], w=["ess"])
                P.op("dve", lambda e, b=b: e.scalar_tensor_tensor(out=xr[b][:], in0=xr[b][:], scalar=ss[:, 1:2], in1=fgB[:], op0=ALU.mult, op1=ALU.mult),
                     r=["exr%d" % b, "ess", "efgB"], w=["exr%d" % b])
                P.op("sp", lambda e, b=b, c=c: e.dma_start(out=out_v[:, c, :], in_=xr[b][:]), r=["exr%d" % b], w=["out_tok"], dma="e_oo%d" % b)
        P.barrier()
```

```python
from contextlib import ExitStack
import numpy as np
import ml_dtypes
import concourse.bass as bass
import concourse.mybir as mybir
from concourse.bass_utils import run_bass_kernel_spmd

F32 = mybir.dt.float32
BF16 = mybir.dt.bfloat16
AF = mybir.ActivationFunctionType
ALU = mybir.AluOpType
AX = mybir.AxisListType

D = 1024
KC = 8
EPS = 1e-6
FFN_DIM = 2816
N_EXP = 8
EXP_DIM = 3584

ENGS = ["pe", "act", "dve", "pool", "sp"]


def _split(key):
    if isinstance(key, tuple):
        return key[0], key[1]
    return key, None


class Prog:
    def __init__(self, nc, stack):
        self.nc = nc
        self.stack = stack
        self.ops = {e: [] for e in ENGS}
        self.res = {}
        self.dma_cnt = []
        self.key2phys = {}
        self.free_phys = {True: [], False: []}
        self.phys_sw = []
        self.base = {}

    @staticmethod
    def _merge(d, ev):
        k = (ev[0], ev[1])
        if d.get(k, -1) < ev[2]:
            d[k] = ev[2]

    def _collect(self, deps, reads, writes):
        for key in reads:
            name, idx = _split(key)
            ent = self.res.get(name)
            if ent is None:
                continue
            for (i, ev) in ent["w"]:
                if i is None or idx is None or i == idx:
                    self._merge(deps, ev)
        for key in writes:
            name, idx = _split(key)
            ent = self.res.get(name)
            if ent is None:
                continue
            for (i, ev) in ent["w"] + ent["r"]:
                if i is None or idx is None or i == idx:
                    self._merge(deps, ev)

    def _update(self, ev, reads, writes):
        for key in reads:
            name, idx = _split(key)
            ent = self.res.setdefault(name, {"w": [], "r": []})
            ent["r"] = [(i, e) for (i, e) in ent["r"]
                        if not (i == idx and e[0] == ev[0] and e[1] == ev[1])]
            ent["r"].append((idx, ev))
        for key in writes:
            name, idx = _split(key)
            ent = self.res.setdefault(name, {"w": [], "r": []})
            if idx is None:
                ent["w"] = [(None, ev)]
                ent["r"] = []
            else:
                ent["w"] = [(i, e) for (i, e) in ent["w"] if i != idx]
                ent["w"].append((idx, ev))
                ent["r"] = [(i, e) for (i, e) in ent["r"] if i != idx]

    def op(self, eng, fn, r=(), w=(), dma=None):
        deps = dict(self.base)
        self._collect(deps, r, w)
        seq = len(self.ops[eng])
        if dma is not None:
            ph = self.key2phys.get(dma)
            if ph is None:
                sw = (eng == "pool")
                if self.free_phys[sw]:
                    ph = self.free_phys[sw].pop()
                else:
                    ph = len(self.dma_cnt)
                    self.dma_cnt.append(0)
                    self.phys_sw.append(sw)
                self.key2phys[dma] = ph
            self.dma_cnt[ph] += 1
            ev = ("d", ph, self.dma_cnt[ph])
        else:
            ev = ("c", eng, seq)
        o = {"fn": fn, "deps": deps, "ev": ev, "signal": False}
        self.ops[eng].append(o)
        self._update(ev, r, w)
        return o

    def barrier(self):
        fr = {}
        for e in ENGS:
            for s in range(len(self.ops[e]) - 1, -1, -1):
                if self.ops[e][s]["ev"][0] == "c":
                    fr[("c", e)] = s
                    break
        for k, c in enumerate(self.dma_cnt):
            fr[("d", k)] = c
        self.base = fr
        self.res = {}
        self.key2phys = {}
        self.free_phys = {True: [i for i in range(len(self.dma_cnt)) if self.phys_sw[i]],
                          False: [i for i in range(len(self.dma_cnt)) if not self.phys_sw[i]]}

    def emit(self, final_wait_keys=()):
        nc = self.nc
        for e in ENGS:
            for o in self.ops[e]:
                for (kind, src), val in o["deps"].items():
                    if kind == "c" and not (src == "pe" and e == "pe"):
                        self.ops[src][val]["signal"] = True
        for e in ENGS:
            cnt = 0
            for o in self.ops[e]:
                if o["signal"]:
                    cnt += 1
                o["sigval"] = cnt
        sems = {}
        for e in ENGS:
            sems[("c", e)] = self.stack.enter_context(nc.semaphore("s_" + e))
        for k in range(len(self.dma_cnt)):
            sems[("d", k)] = self.stack.enter_context(nc.semaphore("d_" + str(k)))
        block = self.stack.enter_context(nc.Block())
        prog = self

        def run(eng_name, handle):
            waited = {}
            for o in prog.ops[eng_name]:
                for (kind, src), val in o["deps"].items():
                    if kind == "c":
                        if src == "pe" and eng_name == "pe":
                            continue
                        target = prog.ops[src][val]["sigval"]
                    else:
                        target = 16 * val
                    if waited.get((kind, src), 0) >= target:
                        continue
                    waited[(kind, src)] = target
                    handle.wait_ge(sems[(kind, src)], target)
                ins = o["fn"](handle)
                if o["ev"][0] == "d":
                    ins.then_inc(sems[("d", o["ev"][1])], 16)
                elif o["signal"]:
                    ins.then_inc(sems[("c", eng_name)], 1)
            if eng_name == "sp":
                for k in final_wait_keys:
                    handle.wait_ge(sems[("d", k)], 16 * prog.dma_cnt[k])

        @block.tensor
        def _(h):
            run("pe", h)

        @block.scalar
        def _(h):
            run("act", h)

        @block.vector
        def _(h):
            run("dve", h)

        @block.gpsimd
        def _(h):
            run("pool", h)

        @block.sync
        def _(h):
            run("sp", h)


class Ctx:
    pass


def sb(nc, st, name, shape, dt):
    return st.enter_context(nc.sbuf_tensor("sb_" + name, list(shape), dt))


def stage_consts(C):
    nc, P = C.nc, C.P
    st = C.top
    C.ident_bf = sb(nc, st, "ident_bf", [128, 128], BF16)
    C.ident_f = sb(nc, st, "ident_f", [128, 128], F32)
    C.ones_bf = sb(nc, st, "ones_bf", [128, 128], BF16)
    C.ones_f = sb(nc, st, "ones_f", [128, 128], F32)
    P.op("pool", lambda e: e.dma_start(out=C.ident_bf[:], in_=C.d["ident"][:, :]), w=["ident_bf"], dma="c0")
    P.op("sp", lambda e: e.dma_start(out=C.ident_f[:], in_=C.d["ident"][:, :]), w=["ident_f"], dma="c1")
    P.op("dve", lambda e: e.memset(C.ones_bf[:], 1.0), w=["ones_bf"])
    P.op("dve", lambda e: e.memset(C.ones_f[:], 1.0), w=["ones_f"])
    C.mod = [sb(nc, st, "mod%d" % l, [128, 48], F32) for l in range(2)]
    ada_w_aps = [C.d["ada_w0"], C.d["ada_w1"]]
    ada_b_aps = [C.d["ada_bT0"], C.d["ada_bT1"]]
    with ExitStack() as ls:
        cT = sb(nc, ls, "cT", [128, 8], F32)
        s2 = sb(nc, ls, "s2", [128, 8, 2], F32)
        wb = [sb(nc, ls, "adaw%d" % i, [128, 8, 512], F32) for i in range(2)]
        bT = sb(nc, ls, "adab", [128, 48], F32)
        ps = ls.enter_context(nc.psum_tensor("ps_mod", [128, 48, 2], F32))
        P.op("sp", lambda e: e.dma_start(out=cT[:], in_=C.d["cT"][:, :]), w=["cT"], dma="c2")
        P.op("act", lambda e: e.activation(out=s2[:, :, 0], in_=cT[:], func=AF.Silu), r=["cT"], w=["s2"])
        P.op("act", lambda e: e.activation(out=s2[:, :, 1], in_=cT[:], func=AF.Silu), r=["cT"], w=["s2"])
        for l in range(2):
            mod = C.mod[l]
            aw = ada_w_aps[l].rearrange("(k p) f -> p k f", p=128)
            P.op("sp", lambda e, l=l: e.dma_start(out=bT[:], in_=ada_b_aps[l][:, :]), w=["adab"], dma="c3")
            for cb in range(12):
                buf = wb[cb % 2]
                bk = "adaw%d" % (cb % 2)
                P.op("sp", lambda e, buf=buf, aw=aw, cb=cb: e.dma_start(out=buf[:], in_=aw[:, :, cb * 512:(cb + 1) * 512]),
                     w=[bk], dma="aw%d" % (cb % 2))
                for fi in range(4):
                    f = cb * 4 + fi
                    for k in range(8):
                        P.op("pe", lambda e, buf=buf, f=f, fi=fi, k=k: e.matmul(
                            ps[:, f, :], lhsT=buf[:, k, fi * 128:(fi + 1) * 128], rhs=s2[:, k, :],
                            start=(k == 0), stop=(k == 7)), r=[bk, "s2"], w=["ps_mod"])
            P.op("dve", lambda e, mod=mod: e.tensor_tensor(out=mod[:], in0=ps[:, :, 0], in1=bT[:], op=ALU.add),
                 r=["ps_mod", "adab"], w=["mod%d" % l])
            for j in (1, 4):
                P.op("dve", lambda e, mod=mod, j=j: e.tensor_scalar(
                    out=mod[:, j * 8:(j + 1) * 8], in0=mod[:, j * 8:(j + 1) * 8], scalar1=1.0, scalar2=None,
                    op0=ALU.add), r=["mod%d" % l], w=["mod%d" % l])
        P.barrier()


def norm_mod(C, xsrc, hT, rs_ps, sq, tmp, sqv, mod, joff, nm, x_key, h_key, h32=None):
    nc, P = C.nc, C.P
    sh0, sc0 = joff * 8, (joff + 1) * 8
    P.op("act", lambda e: e.activation(out=sq[:], in_=xsrc, func=AF.Square), r=[x_key], w=[nm + "sq"])
    for k in range(8):
        P.op("pe", lambda e, k=k: e.matmul(rs_ps[:], lhsT=C.ones_bf[:], rhs=sq[:, k, :], start=(k == 0), stop=(k == 7)),
             r=[nm + "sq", "ones_bf"], w=[nm + "rs_ps"])
    P.op("act", lambda e: e.activation(out=sqv[:], in_=rs_ps[:], func=AF.Sqrt, bias=C.eps_t[:, 0:1], scale=1.0 / D),
         r=[nm + "rs_ps"], w=[nm + "sqv"])
    P.op("dve", lambda e: e.reciprocal(out=sqv[:], in_=sqv[:]), r=[nm + "sqv"], w=[nm + "sqv"])
    for k in range(8):
        t = tmp[k % 2]
        tk = nm + "tmp%d" % (k % 2)
        P.op("dve", lambda e, k=k, t=t: e.scalar_tensor_tensor(
            out=t[:], in0=xsrc[:, k, :], scalar=mod[:, sc0 + k:sc0 + k + 1], in1=sqv[:], op0=ALU.mult, op1=ALU.mult),
            r=[x_key, nm + "sqv"], w=[tk])
        P.op("act", lambda e, k=k, t=t: e.activation(out=hT[:, k, :], in_=t[:], func=AF.Identity,
                                                    bias=mod[:, sh0 + k:sh0 + k + 1], scale=1.0),
             r=[tk], w=[(h_key, k)])
        if h32 is not None:
            P.op("pool", lambda e, k=k, t=t: e.tensor_scalar(
                out=h32[:, k, :], in0=t[:], scalar1=mod[:, sh0 + k:sh0 + k + 1], scalar2=None, op0=ALU.add),
                r=[tk], w=[("h32", k)])


def stage_ffn(C, x_in, x_out, T, mod, wg, wu, wd, HID, nm, n_exp=1, wr=None):
    nc, P = C.nc, C.P
    HG = 4 if (HID // 128) % 4 == 0 else 2
    NT = 4 if T >= 2048 else T // 512
    n_super = T // (NT * 512)
    units_per_exp = HID // (HG * 128)
    assert HID % (HG * 128) == 0
    xin_v = x_in.rearrange("(k p) t -> p k t", p=128)
    xout_v = x_out.rearrange("(k p) t -> p k t", p=128)
    g0 = 5 * 8
    with ExitStack() as ls:
        acc = sb(nc, ls, nm + "acc", [128, NT, 8, 512], F32)
        hT = sb(nc, ls, nm + "hT", [128, NT, 8, 512], BF16)
        sq = sb(nc, ls, nm + "sq", [128, 8, 512], BF16)
        tmp = [sb(nc, ls, nm + "tmp%d" % i, [128, 512], F32) for i in range(2)]
        sqv = sb(nc, ls, nm + "sqv", [128, 512], F32)
        wgb = [sb(nc, ls, nm + "wg%d" % i, [128, 8, HG * 128], BF16) for i in range(2)]
        wub = [sb(nc, ls, nm + "wu%d" % i, [128, 8, HG * 128], BF16) for i in range(2)]
        wdb = [sb(nc, ls, nm + "wd%d" % i, [128, HG, 1024], BF16) for i in range(2)]
        aT = [sb(nc, ls, nm + "aT%d" % i, [128, HG, 512], BF16) for i in range(2)]
        sg = [sb(nc, ls, nm + "sg%d" % i, [128, 512], F32) for i in range(2)]
        ps_g = [ls.enter_context(nc.psum_tensor(nm + "psg%d" % i, [128, 512], F32)) for i in range(2)]
        ps_u = [ls.enter_context(nc.psum_tensor(nm + "psu%d" % i, [128, 512], F32)) for i in range(2)]
        ps_d = [ls.enter_context(nc.psum_tensor(nm + "psd%d" % i, [128, 512], F32)) for i in range(2)]
        ps_x = ls.enter_context(nc.psum_tensor(nm + "psx", [128, 512], F32))
        if n_exp > 1:
            h32 = sb(nc, ls, nm + "h32", [128, 8, 512], F32)
            wrt = sb(nc, ls, nm + "wr", [128, 8, 8], F32)
            GT = sb(nc, ls, nm + "GT", [8, NT * 512], F32)
            lg = sb(nc, ls, nm + "lg", [128, 8], F32)
            mx = sb(nc, ls, nm + "mx", [128, 8], F32)
            gw = sb(nc, ls, nm + "gw", [128, 4], F32)
            Gm = sb(nc, ls, nm + "Gm", [128, 8], F32)
            Gm2 = sb(nc, ls, nm + "Gm2", [128, 8], F32)
            sel = sb(nc, ls, nm + "sel", [8, 8, 128], F32)
            ps_gb = ls.enter_context(nc.psum_tensor(nm + "psgb", [128, 512], F32))
            P.op("sp", lambda e: e.dma_start(out=wrt[:], in_=wr.rearrange("(k p) e -> p k e", p=128)), w=["wr"], dma=nm + "wr")
            P.op("sp", lambda e: e.dma_start(out=sel[:], in_=C.d["sel"][:, :, :]), w=["sel"], dma=nm + "sel")
        pend_down = [None]
        for s_i in range(n_super):
            for ti in range(NT):
                t0 = (s_i * NT + ti) * 512
                P.op("sp", lambda e, ti=ti, t0=t0: e.dma_start(out=acc[:, ti], in_=xin_v[:, :, t0:t0 + 512]),
                     w=[("acc", ti)], dma=nm + "x%d" % ti)
                norm_mod(C, acc[:, ti], hT[:, ti], ps_x, sq, tmp, sqv, mod, 3, nm, ("acc", ti), "hT%d" % ti,
                         h32=(h32 if n_exp > 1 else None))
                if n_exp > 1:
                    for c4 in range(4):
                        for k in range(8):
                            P.op("pe", lambda e, c4=c4, k=k: e.matmul(
                                ps_gb[:, 0:8], lhsT=h32[:, k, c4 * 128:(c4 + 1) * 128], rhs=wrt[:, k, :],
                                start=(k == 0), stop=(k == 7)), r=[("h32", k), "wr"], w=["psgb"])
                        P.op("dve", lambda e: e.tensor_copy(out=lg[:], in_=ps_gb[:, 0:8]), r=["psgb"], w=["lg"])
                        P.op("dve", lambda e: e.max(out=mx[:], in_=lg[:]), r=["lg"], w=["mx"])
                        P.op("dve", lambda e: e.tensor_tensor(out=gw[:, 0:1], in0=mx[:, 0:1], in1=mx[:, 1:2], op=ALU.subtract),
                             r=["mx"], w=["gw"])
                        P.op("act", lambda e: e.activation(out=gw[:, 1:2], in_=gw[:, 0:1], func=AF.Sigmoid), r=["gw"], w=["gw"])
                        P.op("dve", lambda e: e.tensor_scalar(out=gw[:, 2:3], in0=gw[:, 1:2], scalar1=-1.0, scalar2=1.0,
                                                               op0=ALU.mult, op1=ALU.add), r=["gw"], w=["gw"])
                        P.op("dve", lambda e: e.tensor_scalar(out=Gm[:], in0=lg[:], scalar1=mx[:, 0:1], scalar2=gw[:, 1:2],
                                                               op0=ALU.is_equal, op1=ALU.mult), r=["lg", "mx", "gw"], w=["Gm"])
                        P.op("dve", lambda e: e.tensor_scalar(out=Gm2[:], in0=lg[:], scalar1=mx[:, 1:2], scalar2=gw[:, 2:3],
                                                               op0=ALU.is_equal, op1=ALU.mult), r=["lg", "mx", "gw"], w=["Gm2"])
                        P.op("dve", lambda e: e.tensor_tensor(out=Gm[:], in0=Gm[:], in1=Gm2[:], op=ALU.add),
                             r=["Gm", "Gm2"], w=["Gm"])
                        P.op("pe", lambda e: e.transpose(out=ps_gb[0:8, 128:256], in_=Gm[:], identity=C.ident_f[:]),
                             r=["Gm", "ident_f"], w=["psgb"])
                        P.op("dve", lambda e, ti=ti, c4=c4: e.tensor_copy(
                            out=GT[:, ti * 512 + c4 * 128: ti * 512 + (c4 + 1) * 128], in_=ps_gb[0:8, 128:256]),
                            r=["psgb"], w=[("GT", ti)])
            n_units = n_exp * units_per_exp
            for u in range(n_units):
                ex, uu = divmod(u, units_per_exp)
                b = u % 2
                h0 = uu * HG * 128
                wgs = (wg[ex] if n_exp > 1 else wg).rearrange("(k p) h -> p k h", p=128)
                wus = (wu[ex] if n_exp > 1 else wu).rearrange("(k p) h -> p k h", p=128)
                wds = (wd[ex] if n_exp > 1 else wd).rearrange("(j p) f -> p j f", p=128)
                P.op("pool", lambda e, b=b, wgs=wgs, h0=h0: e.dma_start(out=wgb[b][:], in_=wgs[:, :, h0:h0 + HG * 128]),
                     w=["wg%d" % b], dma=nm + "wg%d" % b)
                P.op("pool", lambda e, b=b, wus=wus, h0=h0: e.dma_start(out=wub[b][:], in_=wus[:, :, h0:h0 + HG * 128]),
                     w=["wu%d" % b], dma=nm + "wu%d" % b)
                P.op("pool", lambda e, b=b, wds=wds, uu=uu: e.dma_start(out=wdb[b][:], in_=wds[:, uu * HG:(uu + 1) * HG, :]),
                     w=["wd%d" % b], dma=nm + "wd%d" % b)
                for ti in range(NT):
                    ab = (u * NT + ti) % 2
                    if n_exp > 1:
                        for pp in range(1):
                            P.op("pe", lambda e, ex=ex, ti=ti: e.matmul(
                                ps_gb[:], lhsT=sel[:, ex, :], rhs=GT[:, ti * 512:(ti + 1) * 512], start=True, stop=True),
                                r=["sel", ("GT", ti)], w=["psgb"])
                    for j in range(HG):
                        pb = j % 2
                        for k in range(8):
                            P.op("pe", lambda e, b=b, pb=pb, j=j, k=k, ti=ti: e.matmul(
                                ps_g[pb][:], lhsT=wgb[b][:, k, j * 128:(j + 1) * 128], rhs=hT[:, ti, k, :],
                                start=(k == 0), stop=(k == 7)), r=["wg%d" % b, "hT%d" % ti], w=["psg%d" % pb])
                        for k in range(8):
                            P.op("pe", lambda e, b=b, pb=pb, j=j, k=k, ti=ti: e.matmul(
                                ps_u[pb][:], lhsT=wub[b][:, k, j * 128:(j + 1) * 128], rhs=hT[:, ti, k, :],
                                start=(k == 0), stop=(k == 7)), r=["wu%d" % b, "hT%d" % ti], w=["psu%d" % pb])
                        P.op("act", lambda e, pb=pb: e.activation(out=sg[pb][:], in_=ps_g[pb][:], func=AF.Silu),
                             r=["psg%d" % pb], w=["sg%d" % pb])
                        if n_exp > 1:
                            P.op("dve", lambda e, pb=pb: e.tensor_tensor(out=sg[pb][:], in0=sg[pb][:], in1=ps_u[pb][:], op=ALU.mult),
                                 r=["sg%d" % pb, "psu%d" % pb], w=["sg%d" % pb])
                            P.op("dve", lambda e, pb=pb, ab=ab, j=j: e.tensor_tensor(
                                out=aT[ab][:, j, :], in0=sg[pb][:], in1=ps_gb[:], op=ALU.mult),
                                r=["sg%d" % pb, "psgb"], w=[("aT%d" % ab, j)])
                        else:
                            P.op("dve", lambda e, pb=pb, ab=ab, j=j: e.tensor_tensor(
                                out=aT[ab][:, j, :], in0=sg[pb][:], in1=ps_u[pb][:], op=ALU.mult),
                                r=["sg%d" % pb, "psu%d" % pb], w=[("aT%d" % ab, j)])
                    def _down(b=b, ab=ab, ti=ti):
                        for f in range(8):
                            db = f % 2
                            for j in range(HG):
                                P.op("pe", lambda e, b=b, db=db, j=j, f=f, ab=ab: e.matmul(
                                    ps_d[db][:], lhsT=wdb[b][:, j, f * 128:(f + 1) * 128], rhs=aT[ab][:, j, :],
                                    start=(j == 0), stop=(j == HG - 1)), r=["wd%d" % b, ("aT%d" % ab, j)], w=["psd%d" % db])
                            P.op("dve", lambda e, db=db, f=f, ti=ti: e.scalar_tensor_tensor(
                                out=acc[:, ti, f, :], in0=ps_d[db][:], scalar=mod[:, g0 + f:g0 + f + 1], in1=acc[:, ti, f, :],
                                op0=ALU.mult, op1=ALU.add), r=["psd%d" % db, ("acc", ti)], w=[("acc", ti)])
                    if pend_down[0] is not None:
                        pend_down[0]()
                    pend_down[0] = _down
            if pend_down[0] is not None:
                pend_down[0]()
                pend_down[0] = None
            for ti in range(NT):
                t0 = (s_i * NT + ti) * 512
                P.op("sp", lambda e, ti=ti, t0=t0: e.dma_start(out=xout_v[:, :, t0:t0 + 512], in_=acc[:, ti]),
                     r=[("acc", ti)], w=[nm + "xout"], dma=nm + "xo%d" % ti)
        P.barrier()


LAMBDA_INIT = 0.8 - 0.6 * float(np.exp(-0.3 * 1))


def stage_qkv(C, x_in, S, mod):
    nc, P, d = C.nc, C.P, C.d
    xin_v = x_in.rearrange("(k p) t -> p k t", p=128)
    qv = d["QT"].rearrange("(c p) t -> p c t", p=128)
    kv = d["KT"].rearrange("(c p) t -> p c t", p=128)
    vv = d["V"].rearrange("(n p) e -> p n e", p=128)
    wq = d["a_w_qkv"].rearrange("(k p) f -> p k f", p=128)
    with ExitStack() as ls:
        w = sb(nc, ls, "qw", [128, 8, 3072], BF16)
        xt = [sb(nc, ls, "qxt%d" % i, [128, 8, 512], F32) for i in range(2)]
        hT2 = [sb(nc, ls, "qhT%d" % i, [128, 8, 512], BF16) for i in range(2)]
        sq = sb(nc, ls, "qsq", [128, 8, 512], BF16)
        tmp = [sb(nc, ls, "qtmp%d" % i, [128, 512], F32) for i in range(2)]
        sqv = sb(nc, ls, "qsqv", [128, 512], F32)
        qt = [sb(nc, ls, "qqt%d" % i, [128, 8, 512], BF16) for i in range(2)]
        kt = [sb(nc, ls, "qkt%d" % i, [128, 8, 512], BF16) for i in range(2)]
        vt = [sb(nc, ls, "qvt%d" % i, [128, 4, 1024], BF16) for i in range(2)]
        ps = [ls.enter_context(nc.psum_tensor("qps%d" % i, [128, 512], F32)) for i in range(4)]
        ps_x = ls.enter_context(nc.psum_tensor("qpsx", [128, 512], F32))
        for i in range(3):
            P.op("pool", lambda e, i=i: e.dma_start(out=w[:, :, i * 1024:(i + 1) * 1024], in_=wq[:, :, i * 1024:(i + 1) * 1024]),
                 w=[("qw", i)], dma="qw%d" % i)
        pi = 0
        NT_ = S // 512

        def emit_norm(t):
            b = t % 2
            P.op("sp", lambda e, b=b, t=t: e.dma_start(out=xt[b][:], in_=xin_v[:, :, t * 512:(t + 1) * 512]),
                 w=["qxt%d" % b], dma="qx%d" % b)
            norm_mod(C, xt[b][:], hT2[b], ps_x, sq, tmp, sqv, mod, 0, "q", "qxt%d" % b, "qhT%d" % b)

        emit_norm(0)
        for t in range(NT_):
            b = t % 2
            hT = hT2[b]
            hk = "qhT%d" % b
            if t + 1 < NT_:
                emit_norm(t + 1)
            for (dst, dkey, coff, scale) in ((qt[b], "qqt%d" % b, 0, 0.125), (kt[b], "qkt%d" % b, 1024, 1.0)):
                for c in range(8):
                    pb = pi % 4
                    pi += 1
                    for k in range(8):
                        P.op("pe", lambda e, pb=pb, k=k, c=c, coff=coff, hT=hT: e.matmul(
                            ps[pb][:], lhsT=w[:, k, coff + c * 128: coff + (c + 1) * 128], rhs=hT[:, k, :],
                            start=(k == 0), stop=(k == 7)), r=["qw", hk], w=["qps%d" % pb])
                    P.op("act", lambda e, pb=pb, dst=dst, c=c, scale=scale: e.activation(
                        out=dst[:, c, :], in_=ps[pb][:], func=AF.Copy, scale=scale), r=["qps%d" % pb], w=[(dkey, c)])
            for tc in range(4):
                for cg in range(2):
                    pb = pi % 4
                    pi += 1
                    for k in range(8):
                        P.op("pe", lambda e, pb=pb, k=k, tc=tc, cg=cg, hT=hT: e.matmul(
                            ps[pb][:], lhsT=hT[:, k, tc * 128:(tc + 1) * 128], rhs=w[:, k, 2048 + cg * 512: 2048 + (cg + 1) * 512],
                            start=(k == 0), stop=(k == 7)), r=["qw", hk], w=["qps%d" % pb])
                    P.op("dve", lambda e, pb=pb, b=b, tc=tc, cg=cg: e.tensor_copy(
                        out=vt[b][:, tc, cg * 512:(cg + 1) * 512], in_=ps[pb][:]), r=["qps%d" % pb], w=[("qvt%d" % b, tc)])
            P.op("sp", lambda e, b=b, t=t: e.dma_start(out=qv[:, :, t * 512:(t + 1) * 512], in_=qt[b][:]),
                 r=["qqt%d" % b], w=["QT"], dma="qqo%d" % b)
            P.op("sp", lambda e, b=b, t=t: e.dma_start(out=kv[:, :, t * 512:(t + 1) * 512], in_=kt[b][:]),
                 r=["qkt%d" % b], w=["KT"], dma="qko%d" % b)
            P.op("sp", lambda e, b=b, t=t: e.dma_start(out=vv[:, t * 4:(t + 1) * 4, :], in_=vt[b][:]),
                 r=["qvt%d" % b], w=["V"], dma="qvo%d" % b)
        P.barrier()


def own_tiles(n_slots):
    A = [2 * i if i % 2 == 0 else 2 * i + 1 for i in range(n_slots)]
    B = [2 * i + 1 if i % 2 == 0 else 2 * i for i in range(n_slots)]
    return A, B


def stage_attn(C, x_in, x_out, S, mod):
    nc, P, d = C.nc, C.P, C.d
    n_slots = S // 1024
    TA, TB = own_tiles(n_slots)
    xin_v = x_in.rearrange("(k p) t -> p k t", p=128)
    xout_v = x_out.rearrange("(k p) t -> p k t", p=128)
    qv = d["QT"].rearrange("(c p) t -> p c t", p=128)
    g0 = 2 * 8
    with ExitStack() as ls:
        wo = sb(nc, ls, "awo", [128, 8, 1024], BF16)
        masks = sb(nc, ls, "amask", [128, 4, 4, 512], BF16)
        rsel = sb(nc, ls, "arsel", [128, 2], F32)
        lamv = sb(nc, ls, "alamv", [128, 4, 64], F32)
        lsc = sb(nc, ls, "alsc", [128, 8], F32)
        gsub = sb(nc, ls, "agsub", [128, 1], F32)
        qa = sb(nc, ls, "aqa", [128, 8, 512], BF16)
        qb = sb(nc, ls, "aqb", [128, 8, 512], BF16)
        q = sb(nc, ls, "aq", [128, 8, 512], BF16)
        xa = sb(nc, ls, "axa", [128, 8, 512], F32)
        xb = sb(nc, ls, "axb", [128, 8, 512], F32)
        ktb = [sb(nc, ls, "akt%d" % i, [128, 512], BF16) for i in range(2)]
        vtb = [sb(nc, ls, "avt%d" % i, [128, 4, 128], BF16) for i in range(2)]
        pT = [[sb(nc, ls, "apT%d%d" % (i, j), [128, 512], BF16) for j in range(2)] for i in range(2)]
        oT = sb(nc, ls, "aoT", [128, 8, 512], BF16)
        t32 = [sb(nc, ls, "at32%d" % i, [128, 512], F32) for i in range(4)]
        sqb = sb(nc, ls, "asqb", [128, 512], BF16)
        ps_s = [[ls.enter_context(nc.psum_tensor("aps%d%d" % (i, j), [128, 512], F32)) for j in range(2)] for i in range(2)]
        ps_o = [ls.enter_context(nc.psum_tensor("apo%d" % j, [128, 512], F32)) for j in range(2)]
        ps_l = [ls.enter_context(nc.psum_tensor("apl%d" % j, [128, 512], F32)) for j in range(2)]
        P.op("pool", lambda e: e.dma_start(out=wo[:], in_=d["a_w_o"].rearrange("(k p) f -> p k f", p=128)), w=["awo"], dma="awo")
        for i in range(4):
            P.op("pool", lambda e, i=i: e.dma_start(out=masks[:, i], in_=d["masks"][i]), w=[("amask", i)], dma="amask%d" % i)
        P.op("sp", lambda e: e.dma_start(out=rsel[:], in_=d["rolesel"][:, :]), w=["arsel"], dma="arsel")
        P.op("sp", lambda e: e.dma_start(out=lamv[:], in_=d["lamv"][:, :, :]), w=["alamv"], dma="alamv")
        P.op("sp", lambda e: e.dma_start(out=gsub[:], in_=d["sublnT"][:, :]), w=["agsub"], dma="agsub")
        for i in range(2):
            P.op("dve", lambda e, i=i: e.tensor_tensor(out=lamv[:, 2 * i, :], in0=lamv[:, 2 * i, :], in1=lamv[:, 2 * i + 1, :], op=ALU.mult),
                 r=["alamv"], w=["alamv"])
            P.op("dve", lambda e, i=i: e.tensor_reduce(out=lsc[:, i:i + 1], in_=lamv[:, 2 * i, :], axis=AX.X, op=ALU.add),
                 r=["alamv"], w=["alsc"])
            P.op("act", lambda e, i=i: e.activation(out=lsc[:, 2 + i:3 + i], in_=lsc[:, i:i + 1], func=AF.Exp), r=["alsc"], w=["alsc"])
        P.op("dve", lambda e: e.tensor_tensor(out=lsc[:, 4:5], in0=lsc[:, 3:4], in1=lsc[:, 2:3], op=ALU.subtract), r=["alsc"], w=["alsc"])
        P.op("dve", lambda e: e.tensor_scalar(out=lsc[:, 4:5], in0=lsc[:, 4:5], scalar1=-LAMBDA_INIT, scalar2=None, op0=ALU.add),
             r=["alsc"], w=["alsc"])
        P.op("dve", lambda e: e.tensor_scalar(out=gsub[:], in0=gsub[:], scalar1=1.0 - LAMBDA_INIT, scalar2=None, op0=ALU.mult),
             r=["agsub"], w=["agsub"])
        step = 0
        for si in range(n_slots):
            ta, tb = TA[si], TB[si]
            P.op("sp", lambda e, ta=ta: e.dma_start(out=qa[:], in_=qv[:, :, ta * 512:(ta + 1) * 512]), w=["aqa"], dma="aqa")
            P.op("sp", lambda e, tb=tb: e.dma_start(out=qb[:], in_=qv[:, :, tb * 512:(tb + 1) * 512]), w=["aqb"], dma="aqb")
            P.op("sp", lambda e, ta=ta: e.dma_start(out=xa[:], in_=xin_v[:, :, ta * 512:(ta + 1) * 512]), w=["axa"], dma="axa")
            P.op("sp", lambda e, tb=tb: e.dma_start(out=xb[:], in_=xin_v[:, :, tb * 512:(tb + 1) * 512]), w=["axb"], dma="axb")
            P.op("dve", lambda e: e.tensor_scalar(out=qa[:], in0=qa[:], scalar1=rsel[:, 0:1], scalar2=None, op0=ALU.mult),
                 r=["aqa", "arsel"], w=["aqa"])
            P.op("dve", lambda e: e.scalar_tensor_tensor(out=q[:], in0=qb[:], scalar=rsel[:, 1:2], in1=qa[:], op0=ALU.mult, op1=ALU.add),
                 r=["aqa", "aqb", "arsel"], w=["aq"])
            P.op("pool", lambda e: e.tensor_scalar(out=xa[:], in0=xa[:], scalar1=rsel[:, 0:1], scalar2=None, op0=ALU.mult),
                 r=["axa", "arsel"], w=["axa"])
            P.op("dve", lambda e: e.scalar_tensor_tensor(out=xa[:], in0=xb[:], scalar=rsel[:, 1:2], in1=xa[:], op0=ALU.mult, op1=ALU.add),
                 r=["axa", "axb", "arsel"], w=["axa"])
            n_units = 2 * si + 2
            par = si % 2
            for h in range(8):
                steps = [(u, kb) for u in range(n_units) for kb in range(4)]
                lbs = {}

                def emit_qk(si_, h=h, steps=steps, lbs=lbs, n_units=n_units, par=par):
                    nonlocal step
                    u, kb = steps[si_]
                    if kb == 0:
                        lb = step % 2
                        step += 1
                        lbs[u] = lb
                        P.op("sp", lambda e, lb=lb, h=h, u=u: e.dma_start(
                            out=ktb[lb][:], in_=d["KT"][h * 128:(h + 1) * 128, u * 512:(u + 1) * 512]), r=["KT"], w=["akt%d" % lb], dma="akt%d" % lb)
                        P.op("sp", lambda e, lb=lb, h=h, u=u: e.dma_start(
                            out=vtb[lb][:], in_=d["V"].rearrange("(n p) e -> p n e", p=128)[:, u * 4:(u + 1) * 4, h * 128:(h + 1) * 128]),
                            r=["V"], w=["avt%d" % lb], dma="avt%d" % lb)
                    lb = lbs[u]
                    mk = None
                    if u == n_units - 2:
                        mk = 2 * par
                    elif u == n_units - 1:
                        mk = 2 * par + 1
                    sbuf_i = si_ % 2
                    for j in range(2):
                        P.op("pe", lambda e, sbuf_i=sbuf_i, j=j, lb=lb, kb=kb, h=h: e.matmul(
                            ps_s[sbuf_i][j][:], lhsT=ktb[lb][j * 64:(j + 1) * 64, kb * 128:(kb + 1) * 128],
                            rhs=q[j * 64:(j + 1) * 64, h, :], start=True, stop=True),
                            r=["akt%d" % lb, "aq"], w=["aps%d%d" % (sbuf_i, j)])
                        P.op("act", lambda e, sbuf_i=sbuf_i, j=j: e.activation(
                            out=pT[sbuf_i][j][:], in_=ps_s[sbuf_i][j][:], func=AF.Exp),
                            r=["aps%d%d" % (sbuf_i, j)], w=["apT%d%d" % (sbuf_i, j)])
                        if mk is not None:
                            P.op("dve" if j == 0 else "pool", lambda e, sbuf_i=sbuf_i, j=j, mk=mk, kb=kb: e.tensor_tensor(
                                out=pT[sbuf_i][j][:], in0=pT[sbuf_i][j][:], in1=masks[:, mk, kb, :], op=ALU.mult),
                                r=["apT%d%d" % (sbuf_i, j), "amask"], w=["apT%d%d" % (sbuf_i, j)])

                def emit_pv(si_, steps=steps, lbs=lbs):
                    u, kb = steps[si_]
                    lb = lbs[u]
                    sbuf_i = si_ % 2
                    first = (si_ == 0)
                    last = (si_ == len(steps) - 1)
                    for j in range(2):
                        P.op("pe", lambda e, sbuf_i=sbuf_i, j=j, lb=lb, kb=kb, first=first, last=last: e.matmul(
                            ps_o[j][:], lhsT=vtb[lb][:, kb, :], rhs=pT[sbuf_i][j][:], start=first, stop=last),
                            r=["avt%d" % lb, "apT%d%d" % (sbuf_i, j)], w=["apo%d" % j])
                        P.op("pe", lambda e, sbuf_i=sbuf_i, j=j, first=first, last=last: e.matmul(
                            ps_l[j][:], lhsT=C.ones_bf[:], rhs=pT[sbuf_i][j][:], start=first, stop=last),
                            r=["ones_bf", "apT%d%d" % (sbuf_i, j)], w=["apl%d" % j])

                emit_qk(0)
                for si_ in range(len(steps)):
                    if si_ + 1 < len(steps):
                        emit_qk(si_ + 1)
                    emit_pv(si_)
                for j in range(2):
                    P.op("dve", lambda e, j=j: e.reciprocal(out=t32[j][:], in_=ps_l[j][:]), r=["apl%d" % j], w=["at32%d" % j])
                    P.op("dve", lambda e, j=j: e.tensor_tensor(out=t32[j][:], in0=ps_o[j][:], in1=t32[j][:], op=ALU.mult),
                         r=["apo%d" % j, "at32%d" % j], w=["at32%d" % j])
                P.op("dve", lambda e: e.scalar_tensor_tensor(out=t32[2][:], in0=t32[1][:], scalar=lsc[:, 4:5], in1=t32[0][:],
                                                              op0=ALU.mult, op1=ALU.add), r=["at320", "at321", "alsc"], w=["at322"])
                P.op("act", lambda e: e.activation(out=sqb[:], in_=t32[2][:], func=AF.Square), r=["at322"], w=["asqb"])
                P.op("pe", lambda e: e.matmul(ps_s[0][0][:], lhsT=C.ones_bf[:], rhs=sqb[:], start=True, stop=True),
                     r=["ones_bf", "asqb"], w=["aps00"])
                P.op("act", lambda e: e.activation(out=t32[3][:], in_=ps_s[0][0][:], func=AF.Sqrt, bias=C.eps_t[:, 0:1], scale=1.0 / 128),
                     r=["aps00"], w=["at323"])
                P.op("dve", lambda e: e.reciprocal(out=t32[3][:], in_=t32[3][:]), r=["at323"], w=["at323"])
                P.op("dve", lambda e, h=h: e.scalar_tensor_tensor(out=oT[:, h, :], in0=t32[2][:], scalar=gsub[:, 0:1], in1=t32[3][:],
                                                                   op0=ALU.mult, op1=ALU.mult), r=["at322", "at323", "agsub"], w=[("aoT", h)])
            for f in range(8):
                pb = ps_s[1][f % 2]
                pk = "aps1%d" % (f % 2)
                for h in range(8):
                    P.op("pe", lambda e, pb=pb, f=f, h=h: e.matmul(pb[:], lhsT=wo[:, h, f * 128:(f + 1) * 128], rhs=oT[:, h, :],
                                                                   start=(h == 0), stop=(h == 7)), r=["awo", "aoT"], w=[pk])
                P.op("dve", lambda e, pb=pb, f=f: e.scalar_tensor_tensor(
                    out=xa[:, f, :], in0=pb[:], scalar=mod[:, g0 + f:g0 + f + 1], in1=xa[:, f, :], op0=ALU.mult, op1=ALU.add),
                    r=[pk, "axa"], w=["axa"])
            P.op("sp", lambda e, si=si: e.dma_start(out=xout_v[:, :, si * 512:(si + 1) * 512], in_=xa[:]), r=["axa"], w=["x3T"], dma="axo")
        P.barrier()


def stage_final(C, x_in, x_out, T):
    nc, P, d = C.nc, C.P, C.d
    xin_v = x_in.rearrange("(k p) t -> p k t", p=128)
    xout_v = x_out.rearrange("(k p) t -> p k t", p=128)
    with ExitStack() as ls:
        fg = sb(nc, ls, "ffg", [128, 8], F32)
        xt = [sb(nc, ls, "fxt%d" % i, [128, 8, 512], F32) for i in range(2)]
        sq = sb(nc, ls, "fsq", [128, 8, 512], BF16)
        sqv = sb(nc, ls, "fsqv", [128, 512], F32)
        ps = ls.enter_context(nc.psum_tensor("fps", [128, 512], F32))
        P.op("sp", lambda e: e.dma_start(out=fg[:], in_=d["final_gT"][:, :]), w=["ffg"], dma="ffg")
        for t in range(T // 512):
            b = t % 2
            xk = "fxt%d" % b
            P.op("sp", lambda e, b=b, t=t: e.dma_start(out=xt[b][:], in_=xin_v[:, :, t * 512:(t + 1) * 512]), w=[xk], dma="fx%d" % b)
            P.op("act", lambda e, b=b: e.activation(out=sq[:], in_=xt[b][:], func=AF.Square), r=[xk], w=["fsq"])
            for k in range(8):
                P.op("pe", lambda e, k=k: e.matmul(ps[:], lhsT=C.ones_bf[:], rhs=sq[:, k, :], start=(k == 0), stop=(k == 7)),
                     r=["fsq", "ones_bf"], w=["fps"])
            P.op("act", lambda e: e.activation(out=sqv[:], in_=ps[:], func=AF.Sqrt, bias=C.eps_t[:, 0:1], scale=1.0 / D),
                 r=["fps"], w=["fsqv"])
            P.op("dve", lambda e: e.reciprocal(out=sqv[:], in_=sqv[:]), r=["fsqv"], w=["fsqv"])
            for k in range(8):
                P.op("dve", lambda e, b=b, k=k: e.scalar_tensor_tensor(
                    out=xt[b][:, k, :], in0=xt[b][:, k, :], scalar=fg[:, k:k + 1], in1=sqv[:], op0=ALU.mult, op1=ALU.mult),
                    r=[xk, "fsqv", "ffg"], w=[xk])
            P.op("sp", lambda e, b=b, t=t: e.dma_start(out=xout_v[:, :, t * 512:(t + 1) * 512], in_=xt[b][:]), r=[xk], w=["outT"], dma="fxo%d" % b)
        P.barrier()


M_IN = 6176
DBG = set()


def stage_m1(C, x_in, S, mod, part):
    nc, P, d = C.nc, C.P, C.d
    xin_v = x_in.rearrange("(k p) t -> p k t", p=128)
    wv = d["m_w_in"].rearrange("(k p) f -> p k f", p=128)
    nm = "m" + part
    with ExitStack() as ls:
        xt = [sb(nc, ls, nm + "xt%d" % i, [128, 8, 512], F32) for i in range(2)]
        hT2 = [sb(nc, ls, nm + "hT%d" % i, [128, 8, 512], BF16) for i in range(2)]
        sq = sb(nc, ls, nm + "sq", [128, 8, 512], BF16)
        tmp = [sb(nc, ls, nm + "tmp%d" % i, [128, 512], F32) for i in range(2)]
        sqv = sb(nc, ls, nm + "sqv", [128, 512], F32)
        ps_x = ls.enter_context(nc.psum_tensor(nm + "psx", [128, 512], F32))
        ps = [ls.enter_context(nc.psum_tensor(nm + "ps%d" % i, [128, 512], F32)) for i in range(3)]
        if part == "z":
            w = sb(nc, ls, nm + "w", [128, 8, 2048 + 32], BF16)
            zt = [sb(nc, ls, nm + "zt%d" % i, [128, 2048], BF16) for i in range(2)]
            dtt = [sb(nc, ls, nm + "dtt%d" % i, [128, 32], F32) for i in range(2)]
            for i in range(2):
                P.op("pool", lambda e, i=i: e.dma_start(out=w[:, :, i * 1024:(i + 1) * 1024], in_=wv[:, :, i * 1024:(i + 1) * 1024]),
                     w=[(nm + "w", i)], dma=nm + "w%d" % i)
            P.op("pool", lambda e: e.dma_start(out=w[:, :, 2048:2080], in_=wv[:, :, 6144:6176]), w=[(nm + "w", 2)], dma=nm + "w2")
            zv = d["zs"].rearrange("(n p) e -> p n e", p=128)
            dv = d["dtr"].rearrange("(n p) e -> p n e", p=128)
        else:
            w = sb(nc, ls, nm + "w", [128, 8, 4096], BF16)
            diag = sb(nc, ls, nm + "diag", [128, 4, 32, 128], BF16)
            cw = sb(nc, ls, nm + "cw", [128, 32, 4], F32)
            cb = sb(nc, ls, nm + "cb", [128, 32], F32)
            halo = sb(nc, ls, nm + "halo", [128, 32, 4], BF16)
            xraw = [sb(nc, ls, nm + "xraw%d" % i, [128, 516], BF16) for i in range(2)]
            xc = [sb(nc, ls, nm + "xc%d" % i, [128, 4, 512], BF16) for i in range(2)]
            xtok = [sb(nc, ls, nm + "xtok%d" % i, [128, 512], BF16) for i in range(2)]
            ps_t32 = [ls.enter_context(nc.psum_tensor(nm + "pst%d" % i, [128, 512], F32)) for i in range(2)]
            ps_t = [p_[:].bitcast(BF16) for p_ in ps_t32]
            for i in range(4):
                P.op("pool", lambda e, i=i: e.dma_start(out=w[:, :, i * 1024:(i + 1) * 1024], in_=wv[:, :, 2048 + i * 1024:2048 + (i + 1) * 1024]),
                     w=[(nm + "w", i)], dma=nm + "w%d" % i)
            P.op("sp", lambda e: e.dma_start(out=cw[:], in_=d["convwT"][:, :, :]), w=[nm + "cw"], dma=nm + "cw")
            P.op("sp", lambda e: e.dma_start(out=cb[:], in_=d["convbT"][:, :]), w=[nm + "cb"], dma=nm + "cb")
            P.op("dve", lambda e: e.memset(halo[:], 0.0), w=[nm + "halo"])
            for j in range(4):
                for cc in range(32):
                    P.op("pool" if cc % 2 else "dve", lambda e, j=j, cc=cc: e.tensor_scalar(
                        out=diag[:, j, cc, :], in0=C.ident_f[:], scalar1=cw[:, cc, j:j + 1], scalar2=None, op0=ALU.mult),
                        r=["ident_f", nm + "cw"], w=[(nm + "diag", cc)])
            xsv = d["xsB"].rearrange("(n p) e -> p n e", p=128)
            btv = d["BT"].rearrange("(c p) t -> p c t", p=128)
            ctv = d["CT"].rearrange("(c p) t -> p c t", p=128)
        pi = 0
        NT_ = S // 512

        def emit_norm(t):
            b = t % 2
            P.op("sp", lambda e, b=b, t=t: e.dma_start(out=xt[b][:], in_=xin_v[:, :, t * 512:(t + 1) * 512]),
                 w=[nm + "xt%d" % b], dma=nm + "x%d" % b)
            norm_mod(C, xt[b][:], hT2[b], ps_x, sq, tmp, sqv, mod, 0, nm, nm + "xt%d" % b, nm + "hT%d" % b)

        emit_norm(0)
        for t in range(NT_):
            b = t % 2
            hT = hT2[b]
            hk = nm + "hT%d" % b
            if t + 1 < NT_:
                emit_norm(t + 1)
            if part == "z":
                for tc in range(4):
                    zb = (t * 4 + tc) % 2
                    for cg in range(4):
                        pb = pi % 3
                        pi += 1
                        for k in range(8):
                            P.op("pe", lambda e, pb=pb, k=k, tc=tc, cg=cg, hT=hT: e.matmul(
                                ps[pb][:], lhsT=hT[:, k, tc * 128:(tc + 1) * 128], rhs=w[:, k, cg * 512:(cg + 1) * 512],
                                start=(k == 0), stop=(k == 7)), r=[nm + "w", hk], w=[nm + "ps%d" % pb])
                        P.op("act", lambda e, pb=pb, zb=zb, cg=cg: e.activation(
                            out=zt[zb][:, cg * 512:(cg + 1) * 512], in_=ps[pb][:], func=AF.Silu),
                            r=[nm + "ps%d" % pb], w=[(nm + "zt%d" % zb, cg)])
                    pb = pi % 3
                    pi += 1
                    for k in range(8):
                        P.op("pe", lambda e, pb=pb, k=k, tc=tc, hT=hT: e.matmul(
                            ps[pb][:, 0:32], lhsT=hT[:, k, tc * 128:(tc + 1) * 128], rhs=w[:, k, 2048:2080],
                            start=(k == 0), stop=(k == 7)), r=[nm + "w", hk], w=[nm + "ps%d" % pb])
                    P.op("dve", lambda e, pb=pb, zb=zb: e.tensor_copy(out=dtt[zb][:], in_=ps[pb][:, 0:32]),
                         r=[nm + "ps%d" % pb], w=[nm + "dtt%d" % zb])
                    n = t * 4 + tc
                    P.op("sp", lambda e, zb=zb, n=n: e.dma_start(out=zv[:, n, :], in_=zt[zb][:]), r=[nm + "zt%d" % zb], w=["zs"], dma=nm + "zo%d" % zb)
                    P.op("sp", lambda e, zb=zb, n=n: e.dma_start(out=dv[:, n, :], in_=dtt[zb][:]), r=[nm + "dtt%d" % zb], w=["dtr"], dma=nm + "do%d" % zb)
            else:
                def proj(cc, hT=hT, hk=hk):
                    nonlocal pi
                    rb = cc % 2
                    pb = pi % 3
                    pi += 1
                    for k in range(8):
                        P.op("pe", lambda e, pb=pb, k=k, cc=cc: e.matmul(
                            ps[pb][:], lhsT=w[:, k, cc * 128:(cc + 1) * 128], rhs=hT[:, k, :],
                            start=(k == 0), stop=(k == 7)), r=[nm + "w", hk], w=[nm + "ps%d" % pb])
                    P.op("dve", lambda e, rb=rb, cc=cc: e.tensor_copy(out=xraw[rb][:, 0:4], in_=halo[:, cc, :]),
                         r=[(nm + "halo", cc)], w=[nm + "xraw%d" % rb])
                    P.op("act", lambda e, rb=rb, pb=pb: e.activation(out=xraw[rb][:, 4:516], in_=ps[pb][:], func=AF.Copy),
                         r=[nm + "ps%d" % pb], w=[nm + "xraw%d" % rb])
                    P.op("dve", lambda e, rb=rb, cc=cc: e.tensor_copy(out=halo[:, cc, :], in_=xraw[rb][:, 512:516]),
                         r=[nm + "xraw%d" % rb], w=[(nm + "halo", cc)])

                def conv(cc):
                    nonlocal pi
                    rb = cc % 2
                    g4_, ci = divmod(cc, 4)
                    xb_i = g4_ % 2
                    pb2 = pi % 3
                    pi += 1
                    for j in range(4):
                        P.op("pe", lambda e, pb2=pb2, j=j, cc=cc, rb=rb: e.matmul(
                            ps[pb2][:], lhsT=diag[:, j, cc, :], rhs=xraw[rb][:, 1 + j:513 + j], start=(j == 0), stop=(j == 3)),
                            r=[(nm + "diag", cc), nm + "xraw%d" % rb], w=[nm + "ps%d" % pb2])
                    P.op("act", lambda e, pb2=pb2, xb_i=xb_i, ci=ci, cc=cc: e.activation(
                        out=xc[xb_i][:, ci, :], in_=ps[pb2][:], func=AF.Silu, bias=cb[:, cc:cc + 1], scale=1.0),
                        r=[nm + "ps%d" % pb2, nm + "cb"], w=[(nm + "xc%d" % xb_i, ci)])

                proj(0)
                for g4 in range(8):
                    xb_i = g4 % 2
                    for ci in range(4):
                        cc = g4 * 4 + ci
                        if cc + 1 < 32:
                            proj(cc + 1)
                        conv(cc)
                    if g4 >= 4:
                        dst = btv if g4 < 6 else ctv
                        c0 = (g4 - 4) * 4 if g4 < 6 else (g4 - 6) * 4
                        P.op("sp", lambda e, xb_i=xb_i, dst=dst, c0=c0, t=t: e.dma_start(
                            out=dst[:, c0:c0 + 4, t * 512:(t + 1) * 512], in_=xc[xb_i][:]),
                            r=[nm + "xc%d" % xb_i], w=["BTCT"], dma=nm + "bo%d" % xb_i)
                    if g4 < 6:
                        for tc in range(4):
                            tb = (g4 * 4 + tc) % 2
                            for ci in range(4):
                                P.op("pe", lambda e, tb=tb, ci=ci, xb_i=xb_i, tc=tc: e.transpose(
                                    out=ps_t[tb][:, ci * 128:(ci + 1) * 128], in_=xc[xb_i][:, ci, tc * 128:(tc + 1) * 128],
                                    identity=C.ident_bf[:]), r=[(nm + "xc%d" % xb_i, ci), "ident_bf"], w=[nm + "pst%d" % tb])
                            P.op("dve" if tc % 2 else "pool" if False else "dve", lambda e, tb=tb: e.tensor_copy(out=xtok[tb][:], in_=ps_t[tb][:, 0:512]),
                                 r=[nm + "pst%d" % tb], w=[nm + "xtok%d" % tb])
                            n = t * 4 + tc
                            P.op("sp", lambda e, tb=tb, n=n, g4=g4: e.dma_start(out=xsv[:, n, g4 * 512:(g4 + 1) * 512], in_=xtok[tb][:]),
                                 r=[nm + "xtok%d" % tb], w=["xsB"], dma=nm + "xo%d" % tb)
        P.barrier()


def stage_m2(C, x_in, x_out, S, mod):
    nc, P, d = C.nc, C.P, C.d
    xin_v = x_in.rearrange("(k p) t -> p k t", p=128)
    xout_v = x_out.rearrange("(k p) t -> p k t", p=128)
    zv = d["zs"].rearrange("(n p) e -> p n e", p=128)
    dv = d["dtr"].rearrange("(n p) e -> p n e", p=128)
    xsv = d["xsB"].rearrange("(n p) e -> p n e", p=128)
    btv = d["BT"].rearrange("(c p) t -> p c t", p=128)
    ctv = d["CT"].rearrange("(c p) t -> p c t", p=128)
    g0 = 2 * 8
    with ExitStack() as ls:
        wout = sb(nc, ls, "swout", [128, 16, 1024], BF16)
        tri = sb(nc, ls, "stri", [128, 128], F32)
        negm = sb(nc, ls, "snegm", [128, 4, 128], BF16)
        dtb = sb(nc, ls, "sdtb", [128, 32], F32)
        aneg = sb(nc, ls, "saneg", [128, 32], F32)
        dsk = sb(nc, ls, "sdsk", [128, 32], F32)
        dI = sb(nc, ls, "sdI", [128, 32, 128], BF16)
        normg = sb(nc, ls, "snormg", [128, 2048], F32)
        blkT = sb(nc, ls, "sblkT", [32, 4096], BF16)
        Rm = sb(nc, ls, "sRm", [64, 4096], F32)
        Lm = sb(nc, ls, "sLm", [64, 128], F32)
        Tin = sb(nc, ls, "sTin", [128, 64], F32)
        acsT = sb(nc, ls, "sacsT", [32, 128], F32)
        S32 = sb(nc, ls, "sS32", [128, 2048], F32)
        Sbf = [sb(nc, ls, "sSbf%d" % i, [128, 2048], BF16) for i in range(2)]
        zt = [sb(nc, ls, "szt%d" % i, [128, 2048], BF16) for i in range(2)]
        xb = [sb(nc, ls, "sxb%d" % i, [128, 3072], BF16) for i in range(2)]
        BTc = [sb(nc, ls, "sBT%d" % i, [128, 8, 128], BF16) for i in range(2)]
        CTc = [sb(nc, ls, "sCT%d" % i, [128, 8, 128], BF16) for i in range(2)]
        dtr = [sb(nc, ls, "sdtr%d" % i, [128, 32], F32) for i in range(2)]
        xt = [sb(nc, ls, "sxt%d" % i, [128, 8, 128], F32) for i in range(2)]
        sm = sb(nc, ls, "ssm", [128, 64], F32)
        v32 = [sb(nc, ls, "sv32%d" % i, [128, 8, 32], F32) for i in range(2)]
        xtl = [sb(nc, ls, "sxtl%d" % i, [128, 2048], BF16) for i in range(2)]
        xts = [sb(nc, ls, "sxts%d" % i, [128, 2048], BF16) for i in range(2)]
        CBm = [sb(nc, ls, "sCBm%d" % i, [128, 128], F32) for i in range(2)]
        LT = [sb(nc, ls, "sLT%d" % i, [128, 512], F32) for i in range(2)]
        MT = [sb(nc, ls, "sMT%d" % i, [128, 8, 512], BF16) for i in range(2)]
        ty = [sb(nc, ls, "sty%d" % i, [128, 256], F32) for i in range(2)]
        y32_2 = [sb(nc, ls, "sy32%d" % i, [128, 2048], F32) for i in range(2)]
        junk = sb(nc, ls, "sjunk", [128, 256], F32)
        ssq2 = [sb(nc, ls, "sssq%d" % i, [128, 8], F32) for i in range(2)]
        yn = sb(nc, ls, "syn", [128, 2048], BF16)
        ynT = sb(nc, ls, "synT", [128, 16, 128], BF16)
        pmc = ls.enter_context(nc.psum_tensor("spmc", [128, 512], F32))
        pD = [ls.enter_context(nc.psum_tensor("spD%d" % i, [128, 512], F32)) for i in range(2)]
        pY = [ls.enter_context(nc.psum_tensor("spY%d" % i, [128, 512], F32)) for i in range(2)]
        pS = ls.enter_context(nc.psum_tensor("spS", [128, 512], F32))
        pAB = [ls.enter_context(nc.psum_tensor("spAB%d" % i, [128, 512], F32)) for i in range(2)]
        pABb = [p_[:].bitcast(BF16) for p_ in pAB]
        wov = d["m_w_out"].rearrange("(j p) f -> p j f", p=128)
        for i in range(2):
            P.op("pool", lambda e, i=i: e.dma_start(out=wout[:, i * 8:(i + 1) * 8, :], in_=wov[:, i * 8:(i + 1) * 8, :]),
                 w=[("swout", i)], dma="swout%d" % i)
        for (t_, nme) in ((tri, "tri"), (dtb, "dtbT"), (aneg, "alogT"), (dsk, "dskT"), (normg, "normgT")):
            P.op("sp", lambda e, t_=t_, nme=nme: e.dma_start(out=t_[:], in_=d[nme][:, :]), w=["s_" + nme], dma="s_" + nme)
        P.op("pool", lambda e: e.dma_start(out=negm[:], in_=d["negm4"][:, :, :]), w=["s_negm"], dma="s_negm")
        P.op("pool", lambda e: e.dma_start(out=blkT[:, 0:2048], in_=d["blk"][0:32, 0:2048]), w=[("s_blkT", 0)], dma="s_blkT")
        P.op("pool", lambda e: e.dma_start(out=blkT[:, 2048:4096], in_=d["blk"][0:32, 2048:4096]), w=[("s_blkT", 1)], dma="s_blkT2")
        P.op("sp", lambda e: e.dma_start(out=Rm[32:64, :], in_=d["blk"][32:64, :]), w=["sRm_b"], dma="s_Rmb")
        P.op("act", lambda e: e.activation(out=aneg[:], in_=aneg[:], func=AF.Exp), r=["s_alogT"], w=["s_alogT"])
        P.op("dve", lambda e: e.tensor_scalar(out=aneg[:], in0=aneg[:], scalar1=-1.0, scalar2=None, op0=ALU.mult), r=["s_alogT"], w=["s_alogT"])
        P.op("dve", lambda e: e.memset(S32[:], 0.0), w=["sS32"])
        P.op("pool", lambda e: e.memset(Sbf[0][:], 0.0), w=["sSbf0"])
        P.op("dve", lambda e: e.memset(Tin[:, 0:32], 1.0), w=["sTin_a"])
        for r_ in range(32):
            P.op("dve" if r_ % 2 else "pool", lambda e, r_=r_: e.tensor_scalar(out=dI[:, r_, :], in0=C.ident_f[:], scalar1=dsk[:, r_:r_ + 1], scalar2=None, op0=ALU.mult),
                 r=["ident_f", "s_dskT"], w=[("sdI", r_)])
        one = C.ones_f[:, 0:1]
        NC_ = S // 128
        gcount = [0, 0]

        def front_pre(c):
            b = c % 2
            sl = slice(c * 128, (c + 1) * 128)
            V = lambda i: v32[b][:, i, :]
            sv = "sv%d" % b
            P.op("sp", lambda e: e.dma_start(out=dtr[b][:], in_=dv[:, c, :]), r=["dtr"], w=["sdtr%d" % b], dma="sdtr%d" % b)
            P.op("sp", lambda e: e.dma_start(out=BTc[b][:], in_=btv[:, :, sl]), r=["BTCT"], w=["sBT%d" % b], dma="sBT%d" % b)
            P.op("sp", lambda e: e.dma_start(out=CTc[b][:], in_=ctv[:, :, sl]), r=["BTCT"], w=["sCT%d" % b], dma="sCT%d" % b)
            P.op("sp", lambda e: e.dma_start(out=xb[b][:], in_=xsv[:, c, :]), r=["xsB"], w=["sxb%d" % b], dma="sxb%d" % b)
            P.op("dve", lambda e: e.tensor_tensor(out=V(0), in0=dtr[b][:], in1=dtb[:], op=ALU.add), r=["sdtr%d" % b, "s_dtbT"], w=[(sv, 0)])
            P.op("act", lambda e: e.activation(out=V(1), in_=V(0), func=AF.Exp), r=[(sv, 0)], w=[(sv, 1)])
            P.op("act", lambda e: e.activation(out=V(2), in_=V(1), func=AF.Ln, bias=one, scale=1.0), r=[(sv, 1), "ones_f"], w=[(sv, 2)])
            P.op("dve", lambda e: e.tensor_tensor(out=V(3), in0=V(2), in1=aneg[:], op=ALU.mult), r=[(sv, 2), "s_alogT"], w=[(sv, 3)])
            P.op("pe", lambda e: e.matmul(pmc[:, 0:32], lhsT=tri[:], rhs=V(3), start=True, stop=True), r=["s_tri", (sv, 3)], w=["spmc"])
            P.op("pe", lambda e: e.matmul(pmc[:, 32:64], lhsT=C.ones_f[:], rhs=V(3), start=True, stop=True), r=["ones_f", (sv, 3)], w=["spmc"])
            P.op("dve", lambda e: e.tensor_copy(out=sm[:], in_=pmc[:, 0:64]), r=["spmc"], w=["ssm"])
            P.op("dve", lambda e: e.tensor_scalar(out=Tin[:, 32:64], in0=sm[:, 0:32], scalar1=-1.0, scalar2=None, op0=ALU.mult), r=["ssm"], w=["sTin_b"])
            P.op("pe", lambda e: e.transpose(out=pmc[0:64, 256:384], in_=Tin[:], identity=C.ident_f[:]), r=["sTin_a", "sTin_b", "ident_f"], w=["spmc"])
            P.op("pe", lambda e: e.transpose(out=pmc[0:32, 384:512], in_=sm[:, 0:32], identity=C.ident_f[:]), r=["ssm", "ident_f"], w=["spmc"])
            P.op("act", lambda e: e.activation(out=Lm[:], in_=pmc[0:64, 256:384], func=AF.Copy), r=["spmc"], w=["sLm"])
            P.op("act", lambda e: e.activation(out=acsT[:], in_=pmc[0:32, 384:512], func=AF.Copy), r=["spmc"], w=["sacsT"])
            for hf, eng_ in ((0, "dve"), (1, "pool")):
                P.op(eng_, lambda e, hf=hf: e.tensor_tensor(
                    out=Rm[0:32, hf * 2048:(hf + 1) * 2048].rearrange("p (r q) -> p r q", q=128),
                    in0=blkT[:, hf * 2048:(hf + 1) * 2048].rearrange("p (r q) -> p r q", q=128),
                    in1=acsT[:].unsqueeze(1).broadcast_to([32, 16, 128]), op=ALU.mult),
                    r=["sacsT", "s_blkT"], w=[("sRm_a", hf)])
            P.op("act", lambda e: e.activation(out=V(4), in_=sm[:, 0:32], func=AF.Exp), r=["ssm"], w=[(sv, 4)])
            P.op("dve", lambda e: e.tensor_tensor(out=V(5), in0=sm[:, 32:64], in1=sm[:, 0:32], op=ALU.subtract), r=["ssm"], w=[(sv, 5)])
            P.op("act", lambda e: e.activation(out=V(5), in_=V(5), func=AF.Exp), r=[(sv, 5)], w=[(sv, 5)])
            P.op("act", lambda e: e.activation(out=V(6), in_=sm[:, 32:64], func=AF.Exp), r=["ssm"], w=[(sv, 6)])
            P.op("dve", lambda e: e.tensor_tensor(out=V(7), in0=V(2), in1=V(5), op=ALU.mult), r=[(sv, 2), (sv, 5)], w=[(sv, 7)])
            xs3 = xb[b][:, 0:2048].rearrange("p (h e) -> p h e", e=64)
            P.op("dve", lambda e: e.tensor_tensor(out=xtl[b][:].rearrange("p (h e) -> p h e", e=64), in0=xs3,
                                                  in1=V(2).unsqueeze(2).broadcast_to([128, 32, 64]), op=ALU.mult),
                 r=["sxb%d" % b, (sv, 2)], w=["sxtl%d" % b])
            P.op("pool", lambda e: e.tensor_tensor(out=xts[b][:].rearrange("p (h e) -> p h e", e=64), in0=xs3,
                                                   in1=V(7).unsqueeze(2).broadcast_to([128, 32, 64]), op=ALU.mult),
                 r=["sxb%d" % b, (sv, 7)], w=["sxts%d" % b])
        def front_group(c, g):
            b = c % 2
            sv = "sv%d" % b
            gb = gcount[0] % 2
            gcount[0] += 1
            P.op("pe", lambda e, g=g: e.matmul(pmc[:, 128:256], lhsT=BTc[b][:, g, :], rhs=CTc[b][:, g, :], start=True, stop=True),
                 r=["sBT%d" % b, "sCT%d" % b], w=["spmc"])
            P.op("act", lambda e, gb=gb: e.activation(out=CBm[gb][:], in_=pmc[:, 128:256], func=AF.Copy), r=["spmc"], w=["sCBm%d" % gb])
            P.op("pe", lambda e, g=g, gb=gb: e.matmul(pD[gb][:], lhsT=Lm[:], rhs=Rm[:, g * 512:(g + 1) * 512], start=True, stop=False),
                 r=["sLm", "sRm_a", "sRm_b"], w=["spD%d" % gb])
            P.op("pe", lambda e, gb=gb: e.matmul(pD[gb][:], lhsT=C.ident_bf[:], rhs=negm[:].rearrange("p r q -> p (r q)"), start=False, stop=True),
                 r=["ident_bf", "s_negm"], w=["spD%d" % gb])
            P.op("act", lambda e, gb=gb: e.activation(out=LT[gb][:], in_=pD[gb][:], func=AF.Exp), r=["spD%d" % gb], w=["sLT%d" % gb])
            P.op("dve" if g % 3 else "pool", lambda e, gb=gb, g=g: e.tensor_tensor(out=MT[b][:, g, :].rearrange("p (r q) -> p r q", q=128),
                                                              in0=LT[gb][:].rearrange("p (r q) -> p r q", q=128),
                                                              in1=CBm[gb][:].unsqueeze(1).broadcast_to([128, 4, 128]), op=ALU.mult),
                 r=["sLT%d" % gb, "sCBm%d" % gb], w=[("sMT%d" % b, g)])

        def back_pre(c):
            b = c % 2
            sl = slice(c * 128, (c + 1) * 128)
            sv = "sv%d" % b
            Sb_old, Sb_new = Sbf[c % 2], Sbf[(c + 1) % 2]
            ko, kn = "sSbf%d" % (c % 2), "sSbf%d" % ((c + 1) % 2)
            P.op("sp", lambda e: e.dma_start(out=zt[b][:], in_=zv[:, c, :]), r=["zs"], w=["szt%d" % b], dma="szt%d" % b)
            P.op("sp", lambda e: e.dma_start(out=xt[b][:], in_=xin_v[:, :, sl]), w=["sxt%d" % b], dma="sxt%d" % b)
            P.op("pool", lambda e: e.tensor_tensor(out=S32[:].rearrange("p (h e) -> p h e", e=64), in0=S32[:].rearrange("p (h e) -> p h e", e=64),
                                                   in1=v32[b][:, 6, :].unsqueeze(2).broadcast_to([128, 32, 64]), op=ALU.mult),
                 r=["sS32", (sv, 6)], w=["sS32"])
        def back_group(c, g):
            b = c % 2
            sl = slice(c * 128, (c + 1) * 128)
            sv = "sv%d" % b
            Sb_old, Sb_new = Sbf[c % 2], Sbf[(c + 1) % 2]
            ko, kn = "sSbf%d" % (c % 2), "sSbf%d" % ((c + 1) % 2)
            gb = gcount[1] % 2
            gcount[1] += 1
            for r_ in range(4):
                hd = 4 * g + r_
                P.op("pe", lambda e, r_=r_, hd=hd, gb=gb, g=g: e.matmul(pY[gb][:, r_ * 64:(r_ + 1) * 64], lhsT=MT[b][:, g, r_ * 128:(r_ + 1) * 128],
                                                                        rhs=xtl[b][:, hd * 64:(hd + 1) * 64], start=True, stop=False),
                     r=[("sMT%d" % b, g), "sxtl%d" % b], w=["spY%d" % gb])
                P.op("pe", lambda e, r_=r_, hd=hd, gb=gb: e.matmul(pY[gb][:, r_ * 64:(r_ + 1) * 64], lhsT=dI[:, hd, :],
                                                                   rhs=xb[b][:, hd * 64:(hd + 1) * 64], start=False, stop=True),
                     r=[("sdI", hd), "sxb%d" % b], w=["spY%d" % gb])
            P.op("pe", lambda e, g=g, gb=gb: e.matmul(pY[gb][:, 256:512], lhsT=CTc[b][:, g, :], rhs=Sb_old[:, g * 256:(g + 1) * 256], start=True, stop=True),
                 r=["sCT%d" % b, ko], w=["spY%d" % gb])
            P.op("dve", lambda e, g=g, gb=gb: e.tensor_tensor(out=ty[gb][:].rearrange("p (r e) -> p r e", e=64),
                                                              in0=pY[gb][:, 256:512].rearrange("p (r e) -> p r e", e=64),
                                                              in1=v32[b][:, 4, 4 * g:4 * g + 4].unsqueeze(2).broadcast_to([128, 4, 64]), op=ALU.mult),
                 r=["spY%d" % gb, (sv, 4)], w=["sty%d" % gb])
            P.op("dve", lambda e, g=g, gb=gb: e.tensor_tensor(out=y32_2[b][:, g * 256:(g + 1) * 256], in0=pY[gb][:, 0:256], in1=ty[gb][:], op=ALU.add),
                 r=["spY%d" % gb, "sty%d" % gb], w=[("sy32%d" % b, g)])
            P.op("pe", lambda e, g=g: e.matmul(pS[:, 0:256], lhsT=xb[b][:, 2048 + g * 128:2048 + (g + 1) * 128], rhs=xts[b][:, g * 256:(g + 1) * 256],
                                               start=True, stop=True), r=["sxb%d" % b, "sxts%d" % b], w=["spS"])
            P.op("dve", lambda e, g=g: e.tensor_tensor(out=S32[:, g * 256:(g + 1) * 256], in0=pS[:, 0:256], in1=S32[:, g * 256:(g + 1) * 256], op=ALU.add),
                 r=["spS", "sS32"], w=[("sS32", g)])
        def back_post_ew(c):
            b = c % 2
            sl = slice(c * 128, (c + 1) * 128)
            sv = "sv%d" % b
            Sb_old, Sb_new = Sbf[c % 2], Sbf[(c + 1) % 2]
            ko, kn = "sSbf%d" % (c % 2), "sSbf%d" % ((c + 1) % 2)
            P.op("act", lambda e: e.activation(out=Sb_new[:], in_=S32[:], func=AF.Copy), r=["sS32"], w=[kn])
            P.op("pool", lambda e: e.tensor_tensor(out=y32_2[b][:], in0=y32_2[b][:], in1=zt[b][:], op=ALU.mult), r=["sy32%d" % b, "szt%d" % b], w=["sy32%d" % b])
            for g in range(8):
                P.op("act", lambda e, g=g: e.activation(out=junk[:], in_=y32_2[b][:, g * 256:(g + 1) * 256], func=AF.Square, accum_out=ssq2[b][:, g:g + 1]),
                     r=["sy32%d" % b], w=["sjunk", ("sssq%d" % b, g)])
            P.op("act", lambda e: e.activation(out=ssq2[b][:], in_=ssq2[b][:], func=AF.Sqrt, bias=C.eps_t[:, 0:1], scale=1.0 / 256), r=["sssq%d" % b], w=["sssq%d" % b])
        def back_post_ew2(c):
            b = c % 2
            sl = slice(c * 128, (c + 1) * 128)
            sv = "sv%d" % b
            Sb_old, Sb_new = Sbf[c % 2], Sbf[(c + 1) % 2]
            ko, kn = "sSbf%d" % (c % 2), "sSbf%d" % ((c + 1) % 2)
            P.op("dve", lambda e: e.reciprocal(out=ssq2[b][:], in_=ssq2[b][:]), r=["sssq%d" % b], w=["sssq%d" % b])
            for g in range(8):
                P.op("dve", lambda e, g=g: e.scalar_tensor_tensor(out=yn[:, g * 256:(g + 1) * 256], in0=y32_2[b][:, g * 256:(g + 1) * 256],
                                                                   scalar=ssq2[b][:, g:g + 1], in1=normg[:, g * 256:(g + 1) * 256], op0=ALU.mult, op1=ALU.mult),
                     r=["sy32%d" % b, "sssq%d" % b, "s_normgT"], w=[("syn", g)])
        def back_post_pe(c):
            b = c % 2
            sl = slice(c * 128, (c + 1) * 128)
            sv = "sv%d" % b
            Sb_old, Sb_new = Sbf[c % 2], Sbf[(c + 1) % 2]
            ko, kn = "sSbf%d" % (c % 2), "sSbf%d" % ((c + 1) % 2)
            for i in range(4):
                ab = i % 2
                for ci in range(4):
                    dc = i * 4 + ci
                    P.op("pe", lambda e, ci=ci, dc=dc, ab=ab: e.transpose(out=pABb[ab][:, ci * 128:(ci + 1) * 128], in_=yn[:, dc * 128:(dc + 1) * 128], identity=C.ident_bf[:]),
                         r=["syn", "ident_bf"], w=["spAB%d" % ab])
                P.op("act", lambda e, i=i, ab=ab: e.activation(out=ynT[:, i * 4:(i + 1) * 4, :].rearrange("p a b -> p (a b)"), in_=pABb[ab][:, 0:512], func=AF.Copy),
                     r=["spAB%d" % ab], w=[("synT", i)])
            for f in range(8):
                ab = f % 2
                for dc in range(16):
                    P.op("pe", lambda e, f=f, ab=ab, dc=dc: e.matmul(pAB[ab][:, 0:128], lhsT=wout[:, dc, f * 128:(f + 1) * 128], rhs=ynT[:, dc, :],
                                                                    start=(dc == 0), stop=(dc == 15)), r=["swout", "synT"], w=["spAB%d" % ab])
                P.op("dve", lambda e, f=f, ab=ab: e.scalar_tensor_tensor(out=xt[b][:, f, :], in0=pAB[ab][:, 0:128], scalar=mod[:, g0 + f:g0 + f + 1],
                                                                          in1=xt[b][:, f, :], op0=ALU.mult, op1=ALU.add),
                     r=["spAB%d" % ab, "sxt%d" % b], w=["sxt%d" % b])
            P.op("sp", lambda e: e.dma_start(out=xout_v[:, :, sl], in_=xt[b][:]), r=["sxt%d" % b], w=["x1T"], dma="sxo%d" % b)

        front_pre(0)
        for g in range(8):
            front_group(0, g)
        for c in range(NC_):
            nxt = c + 1 < NC_
            back_pre(c)
            back_group(c, 0)
            if nxt:
                front_pre(c + 1)
            for g in range(1, 8):
                back_group(c, g)
                if g == 1 and c >= 1:
                    back_post_ew2(c - 1)
                if g == 3 and c >= 1:
                    back_post_pe(c - 1)
                if nxt and g >= 2:
                    front_group(c + 1, g - 2)
            if nxt:
                front_group(c + 1, 6)
                front_group(c + 1, 7)
            back_post_ew(c)
        back_post_ew2(NC_ - 1)
        back_post_pe(NC_ - 1)
        P.barrier()


def build(S, stages=None, outs=("out_tok",), chain=True, only_inputs=None):
    nc = bass.Bass("TRN2", target_bir_lowering=False)
    C = Ctx()
    C.nc = nc
    C.S = S
    TO = S // 2
    allst = ["m1z", "m1x", "m2", "ffn0", "qkv", "attn", "moes"]
    if stages is None:
        stages = allst
    d = {}

    def din(name, shape, dt=F32):
        if only_inputs is not None and name not in only_inputs:
            return
        d[name] = nc.dram_tensor(name, list(shape), dt, kind="ExternalInput").ap()

    def dscr(name, shape, dt=F32):
        kind = "ExternalOutput" if name in outs else "Internal"
        d[name] = nc.dram_tensor(name, list(shape), dt, kind=kind).ap()

    din("xT", [D, S]); din("cT", [128, 8]); din("ident", [128, 128]); din("sel", [8, 8, 128])
    din("ada_w0", [D, 6 * D]); din("ada_bT0", [128, 48])
    din("ada_w1", [D, 6 * D]); din("ada_bT1", [128, 48])
    din("m_w_in", [D, M_IN]); din("convwT", [128, 32, 4]); din("convbT", [128, 32])
    din("dtbT", [128, 32]); din("alogT", [128, 32]); din("dskT", [128, 32]); din("normgT", [128, 2048])
    din("m_w_out", [2048, D]); din("tri", [128, 128]); din("negm4", [128, 4, 128]); din("blk", [64, 4096])
    din("ffn_w_gate", [D, FFN_DIM]); din("ffn_w_up", [D, FFN_DIM]); din("ffn_w_down", [FFN_DIM, D])
    din("a_w_qkv", [D, 3072]); din("lamv", [128, 4, 64]); din("sublnT", [128, 1]); din("a_w_o", [D, D])
    din("masks", [4, 128, 4, 512]); din("rolesel", [128, 2])
    din("moe_w_router", [D, N_EXP]); din("moe_w_gate", [N_EXP, D, EXP_DIM]); din("moe_w_up", [N_EXP, D, EXP_DIM])
    din("moe_w_down", [N_EXP, EXP_DIM, D]); din("final_gT", [128, 8])
    NUe = EXP_DIM // 512
    for nm_ in ("moe_wg_r0", "moe_wg_r1", "moe_wu_r0", "moe_wu_r1", "moe_wd_r0", "moe_wd_r1"):
        din(nm_, [N_EXP * NUe * 128, 2048])
    din("sul", [128, 128]); din("thr", [128, 17]); din("iotaU", [128, NUe]); din("final_gB", [128, 1024])
    TS = TO if chain else S
    NBs = (2 * TS) // MOE_BLK + N_EXP
    din("bstart", [128, NBs])
    dscr("Hrow", [TS, 1024], BF16); dscr("HTs", [1024, TS], BF16); dscr("Xs", [NBs * MOE_BLK, 1024], BF16); dscr("Ys", [NBs * MOE_BLK, 1024]); dscr("out_tok", [TS, 1024])
    dscr("zs", [S, 2048], BF16); dscr("dtr", [S, 32]); dscr("xsB", [S, 3072], BF16)
    dscr("BT", [1024, S], BF16); dscr("CT", [1024, S], BF16)
    dscr("x1T", [D, S]); dscr("x2T", [D, S])
    dscr("QT", [1024, S], BF16); dscr("KT", [1024, S], BF16); dscr("V", [S, 1024], BF16)
    dscr("x3T", [D, TO]); dscr("x4T", [D, TO]); dscr("outT", [D, TO])
    C.d = d
    with ExitStack() as top:
        C.top = top
        C.P = Prog(nc, top)
        C.eps_t = sb(nc, top, "eps_t", [128, 1], F32)
        C.P.op("dve", lambda e: e.memset(C.eps_t[:], EPS), w=["eps_t"])
        stage_consts(C)
        x0 = d["xT"]
        if "m1z" in stages:
            stage_m1(C, x0, S, C.mod[0], "z")
        if "m1x" in stages:
            stage_m1(C, x0, S, C.mod[0], "x")
        if "m2" in stages:
            stage_m2(C, x0, d["x1T"], S, C.mod[0])
        if "ffn0" in stages:
            stage_ffn(C, d["x1T"] if chain else x0, d["x2T"], S, C.mod[0], d["ffn_w_gate"], d["ffn_w_up"], d["ffn_w_down"], FFN_DIM, "f0")
        xl1 = d["x2T"] if chain else x0
        if "qkv" in stages:
            stage_qkv(C, xl1, S, C.mod[1])
        if "attn" in stages:
            stage_attn(C, xl1, d["x3T"], S, C.mod[1])
        if "moe" in stages:
            stage_ffn(C, d["x3T"] if chain else x0, d["x4T"], TO if chain else S, C.mod[1], d["moe_w_gate"], d["moe_w_up"], d["moe_w_down"],
                      EXP_DIM, "m1", n_exp=N_EXP, wr=d["moe_w_router"])
        if "final" in stages:
            stage_final(C, d["x4T"], d["outT"], TO)
        if "moes" in stages:
            stage_moe_sparse(C, d["x3T"] if chain else x0, TO if chain else S, C.mod[1])
        C.P.emit(final_wait_keys=list(range(len(C.P.dma_cnt))))
    return nc


def host_consts(role):
    ident = np.eye(128, dtype=np.float32)
    sel = np.zeros((8, 8, 128), np.float32)
    for e in range(8):
        sel[e, e, :] = 1.0
    s_ = np.arange(128)
    tri = (s_[:, None] <= s_[None, :]).astype(np.float32)
    negm4 = np.ascontiguousarray(np.tile(((s_[:, None] > s_[None, :]) * -30000.0).astype(np.float32)[:, None, :], (1, 4, 1)))
    blk = np.zeros((64, 32, 128), np.float32)
    for r_ in range(32):
        blk[r_, r_, :] = 1.0
        blk[32 + r_, r_, :] = 1.0
    blk = blk.reshape(64, 4096)
    k_ = np.arange(512)
    trib = (k_[None, :] >= k_[:, None]).astype(np.float32).reshape(4, 128, 512).transpose(1, 0, 2)
    ones = np.ones_like(trib)
    zeros = np.zeros_like(trib)
    if role == 1:
        masks = np.stack([ones, trib, trib, zeros])
    else:
        masks = np.stack([trib, zeros, ones, trib])
    rolesel = np.zeros((128, 2), np.float32)
    rolesel[:, role] = 1.0
    sul = (s_[:, None] < s_[None, :]).astype(np.float32)
    thr = np.tile((np.arange(17) * float(MOE_BLK))[None, :], (128, 1)).astype(np.float32)
    NUe = EXP_DIM // 512
    iotaU = (np.arange(NUe)[None, :] * 128 + s_[:, None]).astype(np.float32)
    return {"sul": sul, "thr": thr, "iotaU": iotaU, "ident": ident, "sel": sel, "tri": tri, "negm4": negm4, "blk": blk, "masks": np.ascontiguousarray(masks), "rolesel": rolesel}


def make_in_maps(inp, S, dense_moe=False, TS=None):
    f = lambda a: np.ascontiguousarray(np.asarray(a, dtype=np.float32))
    col = lambda v, n: f(np.asarray(v).reshape(n, 128).T)
    til = lambda v: f(np.tile(np.asarray(v)[None, :], (128, 1)))
    shared = {
        "m_w_in": f(inp["m_w_in"]), "m_w_out": f(inp["m_w_out"]),
        "convwT": f(np.asarray(inp["m_conv_w"]).reshape(4, 32, 128).transpose(2, 1, 0)),
        "convbT": col(inp["m_conv_b"], 32), "dtbT": til(inp["m_dt_bias"]), "alogT": til(inp["m_a_log"]),
        "dskT": til(inp["m_d_skip"]), "normgT": til(inp["m_norm_g"]),
        "ffn_w_gate": f(inp["ffn_w_gate"]), "ffn_w_up": f(inp["ffn_w_up"]), "ffn_w_down": f(inp["ffn_w_down"]),
        "a_w_qkv": f(inp["a_w_qkv"]), "a_w_o": f(inp["a_w_o"]),
        "lamv": f(np.tile(np.stack([inp["a_lam_q1"], inp["a_lam_k1"], inp["a_lam_q2"], inp["a_lam_k2"]])[None], (128, 1, 1))),
        "sublnT": col(inp["a_subln_g"], 1),
        "moe_w_router": f(inp["moe_w_router"]), "final_gT": col(inp["final_g"], 8), "final_gB": til(inp["final_g"]),
    }
    if TS is None:
        TS = S // 2
    NBs = (2 * TS) // MOE_BLK + N_EXP
    shared["bstart"] = f(np.tile((np.arange(NBs) * float(MOE_BLK))[None, :], (128, 1)))
    if dense_moe:
        shared.update({"moe_w_gate": f(inp["moe_w_gate"]), "moe_w_up": f(inp["moe_w_up"]), "moe_w_down": f(inp["moe_w_down"])})
    else:
        NUe = EXP_DIM // 512
        for nm_, key in (("moe_wg_r", "moe_w_gate"), ("moe_wu_r", "moe_w_up")):
            w_ = np.asarray(inp[key], dtype=np.float32).reshape(N_EXP, 2, 4, 128, NUe, 512)
            w2_ = np.ascontiguousarray(w_.transpose(1, 0, 4, 3, 2, 5)).reshape(2, N_EXP * NUe * 128, 2048)
            shared[nm_ + "0"], shared[nm_ + "1"] = w2_[0], w2_[1]
        w_ = np.asarray(inp["moe_w_down"], dtype=np.float32).reshape(N_EXP, NUe, 2, 2, 128, 1024)
        w2_ = np.ascontiguousarray(w_.transpose(2, 0, 1, 4, 3, 5)).reshape(2, N_EXP * NUe * 128, 2048)
        shared["moe_wd_r0"], shared["moe_wd_r1"] = w2_[0], w2_[1]
    shared["ada_w0"] = f(inp["ada_w0"]); shared["ada_bT0"] = col(inp["ada_b0"], 48)
    shared["ada_w1"] = f(inp["ada_w1"]); shared["ada_bT1"] = col(inp["ada_b1"], 48)
    hc = [host_consts(0), host_consts(1)]
    maps = []
    for c in range(8):
        b, role = c // 2, c % 2
        m = dict(shared)
        m.update(hc[role])
        m["xT"] = f(np.asarray(inp["x"])[b, :S, :].T)
        m["cT"] = col(np.asarray(inp["c"])[b], 8)
        maps.append(m)
    return maps


def assemble(res, S, B=4):
    TO = S // 2
    n_slots = S // 1024
    TA, TB = own_tiles(n_slots)
    out = np.zeros((B, S, D), np.float32)
    for c in range(2 * B):
        b, role = c // 2, c % 2
        o = np.asarray(res[c]["out_tok"])
        tiles = TA if role == 0 else TB
        for si, t in enumerate(tiles):
            out[b, t * 512:(t + 1) * 512, :] = o[si * 512:(si + 1) * 512, :]
    return out


_NC_CACHE = {}
_USED_INPUTS = {"xT", "cT", "ident", "sel", "ada_w0", "ada_bT0", "ada_w1", "ada_bT1", "m_w_in", "convwT", "convbT", "dtbT", "alogT",
                "dskT", "normgT", "m_w_out", "tri", "negm4", "blk", "ffn_w_gate", "ffn_w_up", "ffn_w_down", "a_w_qkv", "lamv", "sublnT",
                "a_w_o", "masks", "rolesel", "moe_w_router", "moe_wg_r0", "moe_wg_r1", "moe_wu_r0", "moe_wu_r1", "moe_wd_r0", "moe_wd_r1", "sul", "thr", "iotaU", "final_gB", "bstart"}


def kernel(**inputs):
    S = int(np.asarray(inputs["x"]).shape[1])
    if S not in _NC_CACHE:
        _NC_CACHE[S] = build(S, only_inputs=_USED_INPUTS)
    nc = _NC_CACHE[S]
    maps = [{k: v for k, v in m.items() if k in _USED_INPUTS} for m in make_in_maps(inputs, S)]
    res = run_bass_kernel_spmd(nc, maps, core_ids=list(range(8)))
    return assemble(res.results, S)


U32 = mybir.dt.uint32
MOE_BLK = 512


def stage_moe_sparse(C, x_in, T, mod):
    nc, P, d = C.nc, C.P, C.d
    BLK = MOE_BLK
    NCH = T // 128
    NB = (2 * T) // BLK + N_EXP
    NR = NB * BLK
    HG = 4
    NU = EXP_DIM // (HG * 128)
    xin_v = x_in.rearrange("(k p) t -> p k t", p=128)
    g0 = 5 * 8
    st = ExitStack()
    with st as ls:
        S1A = sb(nc, ls, "eS1A", [128, NCH, 8], F32)
        S2A = sb(nc, ls, "eS2A", [128, NCH, 8], F32)
        POS = sb(nc, ls, "ePOS", [128, NCH, 8], F32)
        GW = sb(nc, ls, "eGW", [128, NCH, 2], F32)
        base = sb(nc, ls, "ebase", [128, 8], F32)
        D1f = sb(nc, ls, "eD1f", [128, NCH], F32)
        D2f = sb(nc, ls, "eD2f", [128, NCH], F32)
        D1 = sb(nc, ls, "eD1", [128, NCH], U32)
        D2 = sb(nc, ls, "eD2", [128, NCH], U32)
        WIf = sb(nc, ls, "eWIf", [128, NB, NU], F32)
        WI = sb(nc, ls, "eWI", [128, NB, NU], U32)
        sul = sb(nc, ls, "esul", [128, 128], F32)
        thr = sb(nc, ls, "ethr", [128, 17], F32)
        bst = sb(nc, ls, "ebst", [128, NB], F32)
        iop = sb(nc, ls, "eiop", [128, NU], F32)
        wrt = sb(nc, ls, "ewr", [128, 8, 8], F32)
        sel = sb(nc, ls, "esel", [8, 8, 128], F32)
        fgB = sb(nc, ls, "efgB", [128, 1024], F32)
        g2b = sb(nc, ls, "eg2b", [128, 1024], F32)
        P.op("sp", lambda e: e.dma_start(out=sul[:], in_=d["sul"][:, :]), w=["esul"], dma="e_sul")
        P.op("sp", lambda e: e.dma_start(out=thr[:], in_=d["thr"][:, :]), w=["ethr"], dma="e_thr")
        P.op("sp", lambda e: e.dma_start(out=bst[:], in_=d["bstart"][:, :]), w=["ebst"], dma="e_bst")
        P.op("sp", lambda e: e.dma_start(out=iop[:], in_=d["iotaU"][:, :]), w=["eiop"], dma="e_iop")
        P.op("sp", lambda e: e.dma_start(out=wrt[:], in_=d["moe_w_router"].rearrange("(k p) e -> p k e", p=128)), w=["ewr"], dma="e_wr")
        P.op("sp", lambda e: e.dma_start(out=sel[:], in_=d["sel"][:, :, :]), w=["esel"], dma="e_sel")
        P.op("sp", lambda e: e.dma_start(out=fgB[:], in_=d["final_gB"][:, :]), w=["efgB"], dma="e_fgB")
        P.op("dve", lambda e: e.memset(base[:], 0.0), w=["ebase"])
        hts_v = d["HTs"].rearrange("(k p) t -> p k t", p=128)
        with ExitStack() as l2:
            xt = [sb(nc, l2, "ext%d" % i, [128, 8, 512], F32) for i in range(2)]
            hT = sb(nc, l2, "ehT", [128, 8, 512], BF16)
            h32 = sb(nc, l2, "eh32", [128, 8, 512], F32)
            sq = sb(nc, l2, "esq", [128, 8, 512], BF16)
            tmp = [sb(nc, l2, "etmp%d" % i, [128, 512], F32) for i in range(2)]
            sqv = sb(nc, l2, "esqv", [128, 512], F32)
            lg = sb(nc, l2, "elg", [128, 8], F32)
            mx = sb(nc, l2, "emx", [128, 8], F32)
            gd = sb(nc, l2, "egd", [128, 2], F32)
            Sel = sb(nc, l2, "eSel", [128, 8], F32)
            hrow = [sb(nc, l2, "ehrow%d" % i, [128, 1024], BF16) for i in range(2)]
            ps_x = l2.enter_context(nc.psum_tensor("epsx", [128, 512], F32))
            ps_r = l2.enter_context(nc.psum_tensor("epsr", [128, 512], F32))
            ps_t32 = l2.enter_context(nc.psum_tensor("epst", [128, 512], F32))
            ps_tR = ps_t32[:].bitcast(BF16)
            zrow = sb(nc, l2, "ezrow", [128, 4, 1024], BF16)
            P.op("pool", lambda e: e.memset(zrow[:], 0.0), w=["ezrow"])
            xs_z = d["Xs"].rearrange("(n p) f -> p n f", p=128)
            for i in range(NB):
                P.op("sp", lambda e, i=i: e.dma_start(out=xs_z[:, i * 4:(i + 1) * 4, :], in_=zrow[:]), r=["ezrow"], w=[("Xz", i)], dma="e_xz%d" % (i % 4))
            for ti in range(T // 512):
                b = ti % 2
                P.op("sp", lambda e, b=b, ti=ti: e.dma_start(out=xt[b][:], in_=xin_v[:, :, ti * 512:(ti + 1) * 512]), w=["ext%d" % b], dma="e_x%d" % b)
                norm_mod(C, xt[b][:], hT, ps_x, sq, tmp, sqv, mod, 3, "e", "ext%d" % b, "ehT", h32=h32)
                P.op("sp", lambda e, ti=ti: e.dma_start(out=hts_v[:, :, ti * 512:(ti + 1) * 512], in_=hT[:]), r=["ehT"], w=["HTs"], dma="e_hts")
                for c4 in range(4):
                    c = ti * 4 + c4
                    hb = c % 2
                    for k in range(8):
                        P.op("pe", lambda e, c4=c4, k=k: e.matmul(ps_r[:, 0:8], lhsT=h32[:, k, c4 * 128:(c4 + 1) * 128], rhs=wrt[:, k, :],
                                                                  start=(k == 0), stop=(k == 7)), r=[("h32", k), "ewr"], w=["epsr"])
                    P.op("dve", lambda e: e.tensor_copy(out=lg[:], in_=ps_r[:, 0:8]), r=["epsr"], w=["elg"])
                    P.op("dve", lambda e: e.max(out=mx[:], in_=lg[:]), r=["elg"], w=["emx"])
                    P.op("dve", lambda e: e.tensor_tensor(out=gd[:, 0:1], in0=mx[:, 0:1], in1=mx[:, 1:2], op=ALU.subtract), r=["emx"], w=["egd"])
                    P.op("act", lambda e, c=c: e.activation(out=GW[:, c, 0:1], in_=gd[:, 0:1], func=AF.Sigmoid), r=["egd"], w=[("eGW", c)])
                    P.op("dve", lambda e, c=c: e.tensor_scalar(out=GW[:, c, 1:2], in0=GW[:, c, 0:1], scalar1=-1.0, scalar2=1.0, op0=ALU.mult, op1=ALU.add),
                         r=[("eGW", c)], w=[("eGW", c)])
                    P.op("dve", lambda e, c=c: e.tensor_scalar(out=S1A[:, c, :], in0=lg[:], scalar1=mx[:, 0:1], scalar2=None, op0=ALU.is_equal),
                         r=["elg", "emx"], w=[("eS1A", c)])
                    P.op("dve", lambda e, c=c: e.tensor_scalar(out=S2A[:, c, :], in0=lg[:], scalar1=mx[:, 1:2], scalar2=None, op0=ALU.is_equal),
                         r=["elg", "emx"], w=[("eS2A", c)])
                    P.op("dve", lambda e, c=c: e.tensor_tensor(out=Sel[:], in0=S1A[:, c, :], in1=S2A[:, c, :], op=ALU.add),
                         r=[("eS1A", c), ("eS2A", c)], w=["eSel"])
                    P.op("pe", lambda e: e.matmul(ps_r[:, 8:16], lhsT=sul[:], rhs=Sel[:], start=True, stop=True), r=["esul", "eSel"], w=["epsr"])
                    P.op("pe", lambda e: e.matmul(ps_r[:, 16:24], lhsT=C.ones_f[:], rhs=Sel[:], start=True, stop=True), r=["ones_f", "eSel"], w=["epsr"])
                    P.op("dve", lambda e, c=c: e.tensor_tensor(out=POS[:, c, :], in0=ps_r[:, 8:16], in1=base[:], op=ALU.add), r=["epsr", "ebase"], w=[("ePOS", c)])
                    P.op("dve", lambda e: e.tensor_tensor(out=base[:], in0=ps_r[:, 16:24], in1=base[:], op=ALU.add), r=["epsr", "ebase"], w=["ebase"])
            cmp = sb(nc, l2, "ecmp", [128, 32], F32)
            nbk = sb(nc, l2, "enbk", [128, 8], F32)
            pend = sb(nc, l2, "epend", [128, 8], F32)
            pstart = sb(nc, l2, "epstart", [128, 8], F32)
            bexp = sb(nc, l2, "ebexp", [128, NB], F32)
            ptmp = sb(nc, l2, "eptmp", [128, NCH, 8], F32)
            for e_ in range(8):
                P.op("dve", lambda e, e_=e_: e.tensor_scalar(out=cmp[:, 0:17], in0=thr[:], scalar1=base[:, e_:e_ + 1], scalar2=None, op0=ALU.is_lt),
                     r=["ethr", "ebase"], w=["ecmp"])
                P.op("dve", lambda e, e_=e_: e.tensor_reduce(out=nbk[:, e_:e_ + 1], in_=cmp[:, 0:17], axis=AX.X, op=ALU.add), r=["ecmp"], w=["enbk"])
            P.op("dve", lambda e: e.tensor_scalar(out=nbk[:], in0=nbk[:], scalar1=float(BLK), scalar2=None, op0=ALU.mult), r=["enbk"], w=["enbk"])
            P.op("dve", lambda e: e.tensor_copy(out=pend[:, 0:1], in_=nbk[:, 0:1]), r=["enbk"], w=["epend"])
            for e_ in range(1, 8):
                P.op("dve", lambda e, e_=e_: e.tensor_tensor(out=pend[:, e_:e_ + 1], in0=pend[:, e_ - 1:e_], in1=nbk[:, e_:e_ + 1], op=ALU.add),
                     r=["epend", "enbk"], w=["epend"])
            P.op("dve", lambda e: e.tensor_tensor(out=pstart[:], in0=pend[:], in1=nbk[:], op=ALU.subtract), r=["epend", "enbk"], w=["epstart"])
            P.op("dve", lambda e: e.memset(bexp[:], 0.0), w=["ebexp"])
            for e_ in range(8):
                P.op("dve", lambda e, e_=e_: e.tensor_scalar(out=cmp[:, 0:NB], in0=bst[:], scalar1=pend[:, e_:e_ + 1], scalar2=None, op0=ALU.is_ge),
                     r=["ebst", "epend"], w=["ecmp"])
                P.op("dve", lambda e: e.tensor_tensor(out=bexp[:], in0=bexp[:], in1=cmp[:, 0:NB], op=ALU.add), r=["ebexp", "ecmp"], w=["ebexp"])
            P.op("dve", lambda e: e.tensor_scalar(out=bexp[:], in0=bexp[:], scalar1=float(N_EXP - 1), scalar2=float(NU * 128), op0=ALU.min, op1=ALU.mult),
                 r=["ebexp"], w=["ebexp"])
            P.op("dve", lambda e: e.tensor_tensor(out=WIf[:], in0=bexp[:].unsqueeze(2).broadcast_to([128, NB, NU]),
                                                  in1=iop[:].unsqueeze(1).broadcast_to([128, NB, NU]), op=ALU.add), r=["ebexp", "eiop"], w=["eWIf"])
            P.op("dve", lambda e: e.tensor_copy(out=WI[:], in_=WIf[:]), r=["eWIf"], w=["eWI"])
            P.op("dve", lambda e: e.tensor_tensor(out=ptmp[:], in0=POS[:], in1=pstart[:].unsqueeze(1).broadcast_to([128, NCH, 8]), op=ALU.add),
                 r=["ePOS", "epstart"], w=["eptmp"])
            for (SA, Df, Du, nm_) in ((S1A, D1f, D1, "1"), (S2A, D2f, D2, "2")):
                P.op("dve", lambda e, SA=SA: e.tensor_tensor(out=SA[:], in0=SA[:], in1=ptmp[:], op=ALU.mult), r=["eS1A", "eS2A", "eptmp"], w=["eS%sA" % nm_])
                P.op("dve", lambda e, SA=SA, Df=Df: e.tensor_reduce(out=Df[:], in_=SA[:], axis=AX.X, op=ALU.add), r=["eS%sA" % nm_], w=["eD%sf" % nm_])
                P.op("dve", lambda e, Df=Df: e.tensor_scalar(out=Df[:], in0=Df[:], scalar1=float(NR - 1), scalar2=None, op0=ALU.min),
                     r=["eD%sf" % nm_], w=["eD%sf" % nm_])
                P.op("dve", lambda e, Df=Df, Du=Du: e.tensor_copy(out=Du[:], in_=Df[:]), r=["eD%sf" % nm_], w=["eD%s" % nm_])
            hrow4 = hrow + [sb(nc, l2, "ehrow%d" % i, [128, 1024], BF16) for i in (2, 3)]
            hcb = [sb(nc, l2, "ehc%d" % i, [128, 8, 128], BF16) for i in range(3)]
            ps_tS = [ps_tR, ps_x[:].bitcast(BF16)]
            ps_tSk = ["epst", "ers_ps"]
            for c in range(NCH if "noS" not in DBG else 0):
                hb = c % 4
                h3 = c % 3
                tb = c % 2
                P.op("sp", lambda e, h3=h3, c=c: e.dma_start(out=hcb[h3][:], in_=hts_v[:, :, c * 128:(c + 1) * 128]), r=["HTs"], w=["ehc%d" % h3], dma="e_hi%d" % h3)
                for k in range(8):
                    P.op("pe", lambda e, k=k, h3=h3, tb=tb: e.transpose(out=ps_tS[tb][:, k * 128:(k + 1) * 128], in_=hcb[h3][:, k, :], identity=C.ident_bf[:]),
                         r=["ehc%d" % h3, "ident_bf"], w=[ps_tSk[tb]])
                P.op("act", lambda e, hb=hb, tb=tb: e.activation(out=hrow4[hb][:], in_=ps_tS[tb][:, 0:1024], func=AF.Copy), r=[ps_tSk[tb]], w=["ehrow%d" % hb])
                for (Du, nm_) in ((D1, "1"), (D2, "2")):
                    P.op("pool", lambda e, hb=hb, c=c, Du=Du: e.indirect_dma_start(
                        out=d["Xs"][:, :], out_offset=bass.IndirectOffsetOnAxis(ap=Du[:, c:c + 1], axis=0), in_=hrow4[hb][:], in_offset=None),
                        r=["ehrow%d" % hb, "eD%s" % nm_, "Xz"], w=[("Xsc", 2 * c + int(nm_))], dma="e_sc%s%d" % (nm_, hb))
        P.barrier()
        with ExitStack() as l2:
            g2T = sb(nc, l2, "eg2T", [8, 128], F32)
            pg = [l2.enter_context(nc.psum_tensor("epg%d" % i, [128, 512], F32)) for i in range(2)]
            P.op("pe", lambda e: e.transpose(out=pg[0][0:8, 0:128], in_=mod[:, g0:g0 + 8], identity=C.ident_f[:]), r=["ident_f"], w=["epg0"])
            P.op("act", lambda e: e.activation(out=g2T[:], in_=pg[0][0:8, 0:128], func=AF.Copy), r=["epg0"], w=["eg2T"])
            for k in range(8):
                P.op("pe", lambda e, k=k: e.matmul(pg[k // 4][:, (k % 4) * 128:(k % 4 + 1) * 128], lhsT=sel[:, k, :], rhs=g2T[:], start=True, stop=True),
                     r=["esel", "eg2T"], w=["epg%d" % (k // 4)])
            for hf in range(2):
                P.op("act", lambda e, hf=hf: e.activation(out=g2b[:, hf * 512:(hf + 1) * 512], in_=pg[hf][:], func=AF.Copy), r=["epg%d" % hf], w=["eg2b"])
            P.op("dve", lambda e: e.tensor_copy(out=g2T[:], in_=g2T[:]), r=["eg2b", "eg2T"], w=["eg2T"])
        P.barrier()
        with ExitStack() as l2:
            xrow = [sb(nc, l2, "exrow%d" % i, [128, 4, 1024], BF16) for i in range(2)]
            XT = sb(nc, l2, "eXT", [128, 8, 512], BF16)
            acc = sb(nc, l2, "eacc", [128, 8, 512], F32)
            yrow = sb(nc, l2, "eyrow", [128, 4, 1024], F32)
            wgb = [sb(nc, l2, "ewg%d" % i, [128, 8, HG * 128], BF16) for i in range(2)]
            wub = [sb(nc, l2, "ewu%d" % i, [128, 8, HG * 128], BF16) for i in range(2)]
            wdb = [sb(nc, l2, "ewd%d" % i, [128, HG, 1024], BF16) for i in range(2)]
            aT = [sb(nc, l2, "eaT%d" % i, [128, HG, 512], BF16) for i in range(2)]
            sg = [sb(nc, l2, "esg%d" % i, [128, 512], F32) for i in range(2)]
            ps_g = [l2.enter_context(nc.psum_tensor("epsg%d" % i, [128, 512], F32)) for i in range(2)]
            ps_u = [l2.enter_context(nc.psum_tensor("epsu%d" % i, [128, 512], F32)) for i in range(2)]
            ps_d = [l2.enter_context(nc.psum_tensor("epsd%d" % i, [128, 512], F32)) for i in range(2)]
            ps_t = [l2.enter_context(nc.psum_tensor("epstt%d" % i, [128, 512], F32)) for i in range(2)]
            ps_tb = [p_[:].bitcast(BF16) for p_ in ps_t]
            xs_v = d["Xs"].rearrange("(n p) f -> p n f", p=128)
            ys_v = d["Ys"].rearrange("(n p) f -> p n f", p=128)
            wgv = [d["moe_wg_r0"], d["moe_wg_r1"]]
            wuv = [d["moe_wu_r0"], d["moe_wu_r1"]]
            wdv = [d["moe_wd_r0"], d["moe_wd_r1"]]
            ug = 0
            tcnt = 0
            pend_dn = [None]
            for i in range(NB if "noE" not in DBG else 0):
                xb_ = i % 2
                P.op("sp", lambda e, xb_=xb_, i=i: e.dma_start(out=xrow[xb_][:], in_=xs_v[:, i * 4:(i + 1) * 4, :]), r=["Xs"], w=["exrow%d" % xb_], dma="e_xr%d" % xb_)
                for k in range(8):
                    tb = tcnt % 2
                    tcnt += 1
                    for n in range(4):
                        P.op("pe", lambda e, tb=tb, n=n, k=k, xb_=xb_: e.transpose(out=ps_tb[tb][:, n * 128:(n + 1) * 128], in_=xrow[xb_][:, n, k * 128:(k + 1) * 128],
                                                                                identity=C.ident_bf[:]), r=["exrow%d" % xb_, "ident_bf"], w=["epstt%d" % tb])
                    P.op("act" if k % 2 else "dve", (lambda e, tb=tb, k=k: e.activation(out=XT[:, k, :], in_=ps_tb[tb][:, 0:512], func=AF.Copy)) if k % 2 else
                         (lambda e, tb=tb, k=k: e.tensor_copy(out=XT[:, k, :], in_=ps_tb[tb][:, 0:512])), r=["epstt%d" % tb], w=[("eXT", k)])
                for uu in range(NU):
                    b = ug % 2
                    ug += 1
                    for hf in range(2):
                        P.op("pool", lambda e, b=b, i=i, uu=uu, hf=hf: e.indirect_dma_start(
                            out=wgb[b][:, hf * 4:(hf + 1) * 4, :].rearrange("p k f -> p (k f)"), out_offset=None, in_=wgv[hf][:, :],
                            in_offset=bass.IndirectOffsetOnAxis(ap=WI[:, i, uu:uu + 1], axis=0)),
                            r=["eWI"], w=[("ewg%d" % b, hf)], dma="e_wg%d%d" % (b, hf))
                        P.op("pool", lambda e, b=b, i=i, uu=uu, hf=hf: e.indirect_dma_start(
                            out=wub[b][:, hf * 4:(hf + 1) * 4, :].rearrange("p k f -> p (k f)"), out_offset=None, in_=wuv[hf][:, :],
                            in_offset=bass.IndirectOffsetOnAxis(ap=WI[:, i, uu:uu + 1], axis=0)),
                            r=["eWI"], w=[("ewu%d" % b, hf)], dma="e_wu%d%d" % (b, hf))
                        P.op("pool", lambda e, b=b, i=i, uu=uu, hf=hf: e.indirect_dma_start(
                            out=wdb[b][:, hf * 2:(hf + 1) * 2, :].rearrange("p k f -> p (k f)"), out_offset=None, in_=wdv[hf][:, :],
                            in_offset=bass.IndirectOffsetOnAxis(ap=WI[:, i, uu:uu + 1], axis=0)),
                            r=["eWI"], w=[("ewd%d" % b, hf)], dma="e_wd%d%d" % (b, hf))
                    ab = ug % 2
                    for j in range(HG):
                        pb = j % 2
                        for k in range(8):
                            P.op("pe", lambda e, b=b, pb=pb, j=j, k=k: e.matmul(ps_g[pb][:], lhsT=wgb[b][:, k, j * 128:(j + 1) * 128], rhs=XT[:, k, :],
                                                                                start=(k == 0), stop=(k == 7)), r=["ewg%d" % b, "eXT"], w=["epsg%d" % pb])
                        for k in range(8):
                            P.op("pe", lambda e, b=b, pb=pb, j=j, k=k: e.matmul(ps_u[pb][:], lhsT=wub[b][:, k, j * 128:(j + 1) * 128], rhs=XT[:, k, :],
                                                                                start=(k == 0), stop=(k == 7)), r=["ewu%d" % b, "eXT"], w=["epsu%d" % pb])
                        P.op("act", lambda e, pb=pb: e.activation(out=sg[pb][:], in_=ps_g[pb][:], func=AF.Silu), r=["epsg%d" % pb], w=["esg%d" % pb])
                        P.op("dve", lambda e, pb=pb, ab=ab, j=j: e.tensor_tensor(out=aT[ab][:, j, :], in0=sg[pb][:], in1=ps_u[pb][:], op=ALU.mult),
                             r=["esg%d" % pb, "epsu%d" % pb], w=[("eaT%d" % ab, j)])
                    def _down(b=b, ab=ab, uu=uu):
                        for f in range(8):
                            db = f % 2
                            for j in range(HG):
                                P.op("pe", lambda e, b=b, db=db, j=j, f=f, ab=ab: e.matmul(ps_d[db][:], lhsT=wdb[b][:, j, f * 128:(f + 1) * 128], rhs=aT[ab][:, j, :],
                                                                                           start=(j == 0), stop=(j == HG - 1)), r=["ewd%d" % b, ("eaT%d" % ab, j)], w=["epsd%d" % db])
                            if uu == 0:
                                P.op("act", lambda e, db=db, f=f: e.activation(out=acc[:, f, :], in_=ps_d[db][:], func=AF.Copy), r=["epsd%d" % db], w=[("eacc", f)])
                            else:
                                P.op("dve", lambda e, db=db, f=f: e.tensor_tensor(out=acc[:, f, :], in0=ps_d[db][:], in1=acc[:, f, :], op=ALU.add),
                                     r=["epsd%d" % db, ("eacc", f)], w=[("eacc", f)])
                    _down()
                for n in range(4):
                    for hf in range(2):
                        tb = tcnt % 2
                        tcnt += 1
                        for kk in range(4):
                            k = hf * 4 + kk
                            P.op("pe", lambda e, tb=tb, kk=kk, k=k, n=n: e.transpose(out=ps_t[tb][:, kk * 128:(kk + 1) * 128], in_=acc[:, k, n * 128:(n + 1) * 128],
                                                                                     identity=C.ident_f[:]), r=[("eacc", k), "ident_f"], w=["epstt%d" % tb])
                        P.op("act" if hf else "dve", (lambda e, tb=tb, n=n, hf=hf: e.activation(out=yrow[:, n, hf * 512:(hf + 1) * 512], in_=ps_t[tb][:], func=AF.Copy)) if hf else
                             (lambda e, tb=tb, n=n, hf=hf: e.tensor_copy(out=yrow[:, n, hf * 512:(hf + 1) * 512], in_=ps_t[tb][:])), r=["epstt%d" % tb], w=[("eyrow", n)])
                P.op("sp", lambda e, i=i: e.dma_start(out=ys_v[:, i * 4:(i + 1) * 4, :], in_=yrow[:]), r=["eyrow"], w=["Ys"], dma="e_yo")
        P.barrier()
        with ExitStack() as l2:
            xc = [sb(nc, l2, "exc%d" % i, [128, 8, 128], F32) for i in range(2)]
            xr = [sb(nc, l2, "exr%d" % i, [128, 1024], F32) for i in range(2)]
            y1 = [sb(nc, l2, "ey1%d" % i, [128, 1024], F32) for i in range(2)]
            y2 = [sb(nc, l2, "ey2%d" % i, [128, 1024], F32) for i in range(2)]
            junk = sb(nc, l2, "ejunk", [128, 1024], F32)
            ss = sb(nc, l2, "ess", [128, 2], F32)
            pc = [l2.enter_context(nc.psum_tensor("epc%d" % i, [128, 512], F32)) for i in range(4)]
            out_v = d["out_tok"].rearrange("(n p) f -> p n f", p=128)
            for c in range(NCH if "noC" not in DBG else 0):
                b = c % 2
                P.op("sp", lambda e, b=b, c=c: e.dma_start(out=xc[b][:], in_=xin_v[:, :, c * 128:(c + 1) * 128]), w=["exc%d" % b], dma="e_xc%d" % b)
                P.op("pool", lambda e, b=b, c=c: e.indirect_dma_start(out=y1[b][:], out_offset=None, in_=d["Ys"][:, :],
                                                                      in_offset=bass.IndirectOffsetOnAxis(ap=D1[:, c:c + 1], axis=0)),
                     r=["Ys", "eD1"], w=["ey1%d" % b], dma="e_g1%d" % b)
                P.op("pool", lambda e, b=b, c=c: e.indirect_dma_start(out=y2[b][:], out_offset=None, in_=d["Ys"][:, :],
                                                                      in_offset=bass.IndirectOffsetOnAxis(ap=D2[:, c:c + 1], axis=0)),
                     r=["Ys", "eD2"], w=["ey2%d" % b], dma="e_g2%d" % b)
                for hf in range(2):
                    pcb = pc[(c % 2) * 2 + hf]
                    pk = "epc%d" % ((c % 2) * 2 + hf)
                    for kk in range(4):
                        k = hf * 4 + kk
                        P.op("pe", lambda e, pcb=pcb, kk=kk, k=k, b=b: e.transpose(out=pcb[:, kk * 128:(kk + 1) * 128], in_=xc[b][:, k, :], identity=C.ident_f[:]),
                             r=["exc%d" % b, "ident_f"], w=[pk])
                    P.op("act", lambda e, pcb=pcb, hf=hf, b=b: e.activation(out=xr[b][:, hf * 512:(hf + 1) * 512], in_=pcb[:], func=AF.Copy), r=[pk], w=[("exr%d" % b, hf)])
                P.op("dve", lambda e, b=b, c=c: e.tensor_scalar(out=y1[b][:], in0=y1[b][:], scalar1=GW[:, c, 0:1], scalar2=None, op0=ALU.mult),
                     r=["ey1%d" % b, "eGW"], w=["ey1%d" % b])
                P.op("dve", lambda e, b=b, c=c: e.scalar_tensor_tensor(out=y1[b][:], in0=y2[b][:], scalar=GW[:, c, 1:2], in1=y1[b][:], op0=ALU.mult, op1=ALU.add),
                     r=["ey1%d" % b, "ey2%d" % b, "eGW"], w=["ey1%d" % b])
                P.op("pool", lambda e, b=b: e.tensor_tensor(out=y1[b][:], in0=y1[b][:], in1=g2b[:], op=ALU.mult), r=["ey1%d" % b, "eg2b"], w=["ey1%d" % b])
                P.op("dve", lambda e, b=b: e.tensor_tensor(out=xr[b][:], in0=xr[b][:], in1=y1[b][:], op=ALU.add), r=["exr%d" % b, "ey1%d" % b], w=["exr%d" % b])
                P.op("act", lambda e, b=b: e.activation(out=junk[:], in_=xr[b][:], func=AF.Square, accum_out=ss[:, 0:1]), r=["exr%d" % b], w=["ejunk", "ess"])
                P.op("act", lambda e: e.activation(out=ss[:, 1:2], in_=ss[:, 0:1], func=AF.Sqrt, bias=C.eps_t[:, 0:1], scale=1.0 / D), r=["ess"], w=["ess"])
                P.op("dve", lambda e: e.reciprocal(out=ss[:, 1:2], in_=ss[:, 1:2]), r=["ess"], w=["ess"])
                P.op("dve", lambda e, b=b: e.scalar_tensor_tensor(out=xr[b][:], in0=xr[b][:], scalar=ss[:, 1:2], in1=fgB[:], op0=ALU.mult, op1=ALU.mult),
                     r=["exr%d" % b, "ess", "efgB"], w=["exr%d" % b])
                P.op("sp", lambda e, b=b, c=c: e.dma_start(out=out_v[:, c, :], in_=xr[b][:]), r=["exr%d" % b], w=["out_tok"], dma="e_oo%d" % b)
        P.barrier()
```
